# Optimizing a Trainium2 kernel written in Bass

```python
import math
import jax, jax.numpy as jnp
from jax import lax
import numpy as np

D_MODEL = 1024
BATCH = 4
SEQ = 4096
DEPTH = 2

GRID_W = 64
CTX_LEN = 256
EPS = 1e-6
N_BRANCH = 4
BRANCH_W = D_MODEL // 2

SSD_HEADS = 8
SSD_HEAD_DIM = BRANCH_W // SSD_HEADS
SSD_INNER = SSD_HEADS * SSD_HEAD_DIM
SSD_GROUPS = 2
SSD_STATE = 128
SSD_CHUNK = 128
SSD_XBC = SSD_INNER + 2 * SSD_GROUPS * SSD_STATE
CONV_W = 4

GLA_HEADS = 4
GLA_DK = BRANCH_W // GLA_HEADS
GLA_DV = GLA_DK
GLA_RANK = 16
GLA_GATE_NORM = 16.0
GLA_CHUNK = 64

LRU_W = BRANCH_W
LRU_BLOCKS = 8
LRU_BLOCK = LRU_W // LRU_BLOCKS
LRU_C = 8.0

ATT_HEADS = 4
ATT_KV_HEADS = 2
ATT_HEAD_DIM = BRANCH_W // ATT_HEADS
ATT_BLOCK = 128
ROPE_THETA = 10000.0

MOE_GROUPS = 4
MOE_PER_GROUP = 4
MOE_EXPERTS = MOE_GROUPS * MOE_PER_GROUP
MOE_TOPK = 2
MOE_FF = D_MODEL // 2

IN_SIZES = (
    SSD_INNER,
    SSD_XBC,
    2 * SSD_HEADS,
    GLA_HEADS * GLA_DK,
    GLA_HEADS * GLA_DK,
    GLA_HEADS * GLA_DV,
    2 * GLA_RANK,
    GLA_HEADS * GLA_DV,
    LRU_W,
    LRU_W,
    ATT_HEADS * ATT_HEAD_DIM,
    ATT_KV_HEADS * ATT_HEAD_DIM,
    ATT_KV_HEADS * ATT_HEAD_DIM,
    N_BRANCH * D_MODEL,
)
IN_WIDTH = sum(IN_SIZES)

kernel_name = 'hybrid_ssd_gla_rglru_gqa_hmoe_diffusion_block'

F32 = jnp.float32


def rms_norm(x, g):
    xf = x.astype(F32)
    y = xf * lax.rsqrt(jnp.mean(xf * xf, axis=-1, keepdims=True) + EPS)
    return (y * g.astype(F32)).astype(x.dtype)


def split_in(p):
    out, o = [], 0
    for s in IN_SIZES:
        out.append(p[..., o:o + s])
        o += s
    return out


def dw_conv(x, w, b):
    k = w.shape[0]
    y = lax.conv_general_dilated(x, w[:, None, :].astype(x.dtype), window_strides=(1,),
                                 padding=[(k // 2, k - 1 - k // 2)],
                                 dimension_numbers=('NWC', 'WIO', 'NWC'),
                                 feature_group_count=x.shape[-1])
    return y + b.astype(x.dtype)


def ssd_scan(xh, dt, a, bm, cm, h0, with_y):
    bsz, L, H, P = xh.shape
    G, N = bm.shape[2], bm.shape[3]
    E = H // G
    Q = min(SSD_CHUNK, L)
    nc = L // Q
    dtc = dt.astype(F32).reshape(bsz, nc, Q, G, E)
    dtx = xh.astype(F32).reshape(bsz, nc, Q, G, E, P) * dtc[..., None]
    b = bm.astype(F32).reshape(bsz, nc, Q, G, N)
    acum = jnp.cumsum(dtc * a.astype(F32).reshape(G, E), axis=2)
    a_last = acum[:, :, -1]
    states = jnp.einsum('bclgn,bclge,bclgep->bcgepn', b, jnp.exp(a_last[:, :, None] - acum), dtx)

    def step(h, inp):
        s, al = inp
        return jnp.exp(al)[..., None, None] * h + s, h

    h_fin, h_starts = lax.scan(step, h0.astype(F32).reshape(bsz, G, E, P, N),
                               (jnp.moveaxis(states, 1, 0), jnp.moveaxis(a_last, 1, 0)))
    h_fin = h_fin.reshape(bsz, H, P, N)
    if not with_y:
        return None, h_fin
    h_starts = jnp.moveaxis(h_starts, 0, 1)
    c = cm.astype(F32).reshape(bsz, nc, Q, G, N)
    seg = acum[:, :, :, None] - acum[:, :, None]
    causal = jnp.tril(jnp.ones((Q, Q), bool))[:, :, None, None]
    lmat = jnp.exp(jnp.where(causal, seg, -jnp.inf))
    cb = jnp.einsum('bcign,bcjgn->bcijg', c, b)
    y_diag = jnp.einsum('bcijge,bcjgep->bcigep', cb[..., None] * lmat, dtx)
    y_off = jnp.einsum('bcign,bcige,bcgepn->bcigep', c, jnp.exp(acum), h_starts)
    return (y_diag + y_off).reshape(bsz, L, H, P), h_fin


def ssd_seq(z, xbc, dt_raw, lp, h0_f, h0_b, with_y):
    bsz, L, _ = xbc.shape
    xbc = jax.nn.silu(dw_conv(xbc, lp['ssd_conv_w'], lp['ssd_conv_b']))
    xs = xbc[..., :SSD_INNER]
    bm = xbc[..., SSD_INNER:SSD_INNER + SSD_GROUPS * SSD_STATE].reshape(bsz, L, SSD_GROUPS, SSD_STATE)
    cm = xbc[..., SSD_INNER + SSD_GROUPS * SSD_STATE:].reshape(bsz, L, SSD_GROUPS, SSD_STATE)
    xh = xs.reshape(bsz, L, SSD_HEADS, SSD_HEAD_DIM)
    dt = jax.nn.softplus(dt_raw.astype(F32).reshape(bsz, L, 2, SSD_HEADS) + lp['ssd_dt_bias'].astype(F32))
    a = -jnp.exp(lp['ssd_a_log'].astype(F32))
    yf, hf = ssd_scan(xh, dt[:, :, 0], a[0], bm, cm, h0_f, with_y)
    yb, hb = ssd_scan(xh[:, ::-1], dt[:, ::-1, 1], a[1], bm[:, ::-1], cm[:, ::-1], h0_b, with_y)
    if not with_y:
        return None, hf, hb
    y = yf + yb[:, ::-1] + lp['ssd_d'].astype(F32)[:, None] * xh.astype(F32)
    y = y.reshape(bsz, L, SSD_INNER) * jax.nn.silu(z.astype(F32))
    return rms_norm(y, lp['ssd_norm']).astype(z.dtype), hf, hb


def gla_scan(q, k, v, g, s0, with_y):
    bsz, L, H, K = k.shape
    V = v.shape[-1]
    Q = min(GLA_CHUNK, L)
    nc = L // Q
    k = k.reshape(bsz, nc, Q, H, K)
    v = v.reshape(bsz, nc, Q, H, V)
    gc = jnp.cumsum(g.reshape(bsz, nc, Q, H, K), axis=2)
    g_last = gc[:, :, -1]
    chunk_kv = jnp.einsum('bclhk,bclhv->bchkv', k * jnp.exp(g_last[:, :, None] - gc), v)

    def step(s, inp):
        kv, gl = inp
        return jnp.exp(gl)[..., None] * s + kv, s

    s_fin, s_starts = lax.scan(step, s0.astype(F32),
                               (jnp.moveaxis(chunk_kv, 1, 0), jnp.moveaxis(g_last, 1, 0)))
    if not with_y:
        return None, s_fin
    q = q.reshape(bsz, nc, Q, H, K)
    s_starts = jnp.moveaxis(s_starts, 0, 1)
    o_inter = jnp.einsum('bclhk,bchkv->bclhv', q * jnp.exp(gc), s_starts)
    g_ref = gc[:, :, Q // 2:Q // 2 + 1]
    att = jnp.einsum('bcihk,bcjhk->bchij', q * jnp.exp(gc - g_ref), k * jnp.exp(g_ref - gc))
    att = jnp.where(jnp.tril(jnp.ones((Q, Q), bool)), att, 0.0)
    o_intra = jnp.einsum('bchij,bcjhv->bcihv', att, v)
    return (o_inter + o_intra).reshape(bsz, L, H, V), s_fin


def gla_seq(q, k, v, g1, r, lp, s0_f, s0_b, with_y):
    bsz, L, _ = k.shape
    q = q.astype(F32).reshape(bsz, L, GLA_HEADS, GLA_DK) * (GLA_DK ** -0.5)
    k = k.astype(F32).reshape(bsz, L, GLA_HEADS, GLA_DK)
    v = v.astype(F32).reshape(bsz, L, GLA_HEADS, GLA_DV)
    logit = jnp.einsum('bldr,drk->bldk', g1.astype(F32).reshape(bsz, L, 2, GLA_RANK),
                       lp['gla_g2'].astype(F32)) + lp['gla_gb'].astype(F32)
    g = (jax.nn.log_sigmoid(logit) / GLA_GATE_NORM).reshape(bsz, L, 2, GLA_HEADS, GLA_DK)
    of, sf = gla_scan(q, k, v, g[:, :, 0], s0_f, with_y)
    ob, sb = gla_scan(q[:, ::-1], k[:, ::-1], v[:, ::-1], g[:, ::-1, 1], s0_b, with_y)
    if not with_y:
        return None, sf, sb
    o = rms_norm(of + ob[:, ::-1], lp['gla_norm'].reshape(GLA_HEADS, GLA_DV))
    y = o.reshape(bsz, L, GLA_HEADS * GLA_DV) * jax.nn.silu(r.astype(F32))
    return y.astype(r.dtype), sf, sb


def lru_gates(u, lp, d):
    bsz, L, W = u.shape
    ub = u.reshape(bsz, L, LRU_BLOCKS, LRU_BLOCK)
    r = jax.nn.sigmoid(jnp.einsum('blnc,ncd->blnd', ub, lp['lru_wa'][d].astype(F32)).reshape(bsz, L, W)
                       + lp['lru_ba'][d].astype(F32))
    i = jax.nn.sigmoid(jnp.einsum('blnc,ncd->blnd', ub, lp['lru_wx'][d].astype(F32)).reshape(bsz, L, W)
                       + lp['lru_bx'][d].astype(F32))
    log_a = -LRU_C * jax.nn.softplus(-lp['lru_lambda'][d].astype(F32)) * r
    return log_a, u * i * jnp.sqrt(-jnp.expm1(2.0 * log_a))


def lru_scan(v, log_a, h0):
    def comb(left, right):
        al, bl = left
        ar, br = right
        return al * ar, ar * bl + br
    a_cum, h = lax.associative_scan(comb, (jnp.exp(log_a), v), axis=1)
    return h + a_cum * h0[:, None]


def lru_seq(xb, gb, lp, h0_f, h0_b, with_y):
    u = dw_conv(xb, lp['lru_conv_w'], lp['lru_conv_b']).astype(F32)
    la_f, v_f = lru_gates(u, lp, 0)
    la_b, v_b = lru_gates(u[:, ::-1], lp, 1)
    hf = lru_scan(v_f, la_f, h0_f)
    hb = lru_scan(v_b, la_b, h0_b)
    if not with_y:
        return None, hf[:, -1], hb[:, -1]
    y = (hf + hb[:, ::-1]) * jax.nn.gelu(gb.astype(F32))
    return y.astype(xb.dtype), hf[:, -1], hb[:, -1]


def rope_1d(x, pos):
    f = x.shape[-1] // 2
    inv = ROPE_THETA ** (-jnp.arange(f, dtype=F32) / f)
    ang = pos.astype(F32)[:, None] * inv
    cos, sin = jnp.cos(ang)[None, :, None], jnp.sin(ang)[None, :, None]
    x1, x2 = x[..., :f].astype(F32), x[..., f:].astype(F32)
    return jnp.concatenate([x1 * cos - x2 * sin, x1 * sin + x2 * cos], axis=-1)


def rope_2d(x, row, col):
    h = x.shape[-1] // 2
    return jnp.concatenate([rope_1d(x[..., :h], row), rope_1d(x[..., h:], col)], axis=-1)


def attn_softmax(q, k, v):
    s = jnp.einsum('bqgrd,bkgd->bgrqk', q, k) * (ATT_HEAD_DIM ** -0.5)
    p = jax.nn.softmax(s, axis=-1)
    return jnp.einsum('bgrqk,bkgd->bqgrd', p, v)


def attn_mixer(qc, kc, vc, ql, kl, vl, lp, row, col, with_ctx):
    bsz, L, _ = ql.shape
    C = kc.shape[1]
    rep = ATT_HEADS // ATT_KV_HEADS
    heads = lambda t, n: t.reshape(t.shape[0], t.shape[1], n, ATT_HEAD_DIM)
    kc_h = rms_norm(heads(kc, ATT_KV_HEADS), lp['att_knorm']).astype(F32)
    vc_h = heads(vc, ATT_KV_HEADS).astype(F32)
    ql_h = rope_2d(rms_norm(heads(ql, ATT_HEADS), lp['att_qnorm']), row, col)
    kl_h = rope_2d(rms_norm(heads(kl, ATT_KV_HEADS), lp['att_knorm']), row, col)
    k_all = jnp.concatenate([kc_h, kl_h], axis=1)
    v_all = jnp.concatenate([vc_h, heads(vl, ATT_KV_HEADS).astype(F32)], axis=1)
    nb = L // ATT_BLOCK
    q_blocks = jnp.moveaxis(ql_h.reshape(bsz, nb, ATT_BLOCK, ATT_KV_HEADS, rep, ATT_HEAD_DIM), 1, 0)
    o = lax.map(lambda qb: attn_softmax(qb, k_all, v_all), q_blocks)
    y_lat = jnp.moveaxis(o, 0, 1).reshape(bsz, L, ATT_HEADS * ATT_HEAD_DIM).astype(ql.dtype)
    if not with_ctx:
        return None, y_lat
    qc_h = rms_norm(heads(qc, ATT_HEADS), lp['att_qnorm']).astype(F32)
    oc = attn_softmax(qc_h.reshape(bsz, C, ATT_KV_HEADS, rep, ATT_HEAD_DIM), kc_h, vc_h)
    return oc.reshape(bsz, C, ATT_HEADS * ATT_HEAD_DIM).astype(qc.dtype), y_lat


def merge_branches(ys, gate_raw, lp):
    gates = jax.nn.sigmoid(gate_raw)
    acc = sum(gates[..., n * D_MODEL:(n + 1) * D_MODEL] * (y @ lp['w_branch'][n]) for n, y in enumerate(ys))
    return acc @ lp['w_out']


def hier_moe(h, lp):
    T = h.shape[0]
    hf = h.astype(F32)
    lg = hf @ lp['router_wg'].astype(F32) + lp['router_bg'].astype(F32)
    pg = jax.nn.softmax(lg, axis=-1)
    _, gi = lax.top_k(lg, 1)
    gsel = jax.nn.one_hot(gi[:, 0], MOE_GROUPS, dtype=F32)
    le = (hf @ lp['router_we'].astype(F32) + lp['router_be'].astype(F32)).reshape(T, MOE_GROUPS, MOE_PER_GROUP)
    pe = jax.nn.softmax(jnp.einsum('tg,tge->te', gsel, le), axis=-1)
    tv, ti = lax.top_k(pe, MOE_TOPK)
    tv = tv / jnp.sum(tv, axis=-1, keepdims=True)
    w_in_grp = jnp.einsum('tk,tke->te', tv, jax.nn.one_hot(ti, MOE_PER_GROUP, dtype=F32))
    w = ((gsel * pg)[:, :, None] * w_in_grp[:, None, :]).reshape(T, MOE_EXPERTS).astype(h.dtype)
    out = jnp.zeros_like(h)
    for e in range(MOE_EXPERTS):
        a = jax.nn.silu(h @ lp['exp_w1'][e]) * (h @ lp['exp_w3'][e])
        out = out + w[:, e:e + 1] * (a @ lp['exp_w2'][e])
    return out


def hybrid_layer(xl, xc, mod_l, mod_c, lp, row, col, with_ctx):
    bsz, _, D = xl.shape
    C = xc.shape[1]
    sh1, sc1, g1, sh2, sc2, g2 = jnp.split(mod_l[:, None, :], 6, axis=-1)
    csh1, csc1, cg1, csh2, csc2, cg2 = jnp.split(mod_c, 6, axis=-1)
    hl = rms_norm(xl, lp['norm1']) * (1 + sc1) + sh1
    hc = rms_norm(xc, lp['norm1']) * (1 + csc1) + csh1
    pl = split_in(hl @ lp['w_in'])
    pc = split_in(hc @ lp['w_in'])
    z_ssd = jnp.zeros((bsz, SSD_HEADS, SSD_HEAD_DIM, SSD_STATE), F32)
    z_gla = jnp.zeros((bsz, GLA_HEADS, GLA_DK, GLA_DV), F32)
    z_lru = jnp.zeros((bsz, LRU_W), F32)
    yc_a, sf, sb = ssd_seq(pc[0], pc[1], pc[2], lp, z_ssd, z_ssd, with_ctx)
    yl_a, _, _ = ssd_seq(pl[0], pl[1], pl[2], lp, sf, sb, True)
    yc_b, sf, sb = gla_seq(pc[3], pc[4], pc[5], pc[6], pc[7], lp, z_gla, z_gla, with_ctx)
    yl_b, _, _ = gla_seq(pl[3], pl[4], pl[5], pl[6], pl[7], lp, sf, sb, True)
    yc_c, sf, sb = lru_seq(pc[8], pc[9], lp, z_lru, z_lru, with_ctx)
    yl_c, _, _ = lru_seq(pl[8], pl[9], lp, sf, sb, True)
    yc_d, yl_d = attn_mixer(pc[10], pc[11], pc[12], pl[10], pl[11], pl[12], lp, row, col, with_ctx)
    xl = xl + g1 * merge_branches((yl_a, yl_b, yl_c, yl_d), pl[13], lp)
    hl2 = None
    if with_ctx:
        xc = xc + cg1 * merge_branches((yc_a, yc_b, yc_c, yc_d), pc[13], lp)
        hl2 = rms_norm(xl, lp['norm2']) * (1 + sc2) + sh2
        hc2 = rms_norm(xc, lp['norm2']) * (1 + csc2) + csh2
        f = hier_moe(jnp.concatenate([hc2.reshape(-1, D), hl2.reshape(-1, D)], axis=0), lp)
        xc = xc + cg2 * f[:bsz * C].reshape(xc.shape)
        xl = xl + g2 * f[bsz * C:].reshape(xl.shape)
        return xl, xc
    hl2 = rms_norm(xl, lp['norm2']) * (1 + sc2) + sh2
    xl = xl + g2 * hier_moe(hl2.reshape(-1, D), lp).reshape(xl.shape)
    return xl, None


def setup_inputs(seed: int = 0) -> dict:
    key = jax.random.key(seed)
    ks = jax.random.split(key, 40)
    D, Ld = D_MODEL, DEPTH
    nrm = lambda i, shape, s: jax.random.normal(ks[i], shape, jnp.float32) * s
    uni = lambda i, shape, lo, hi: jax.random.uniform(ks[i], shape, jnp.float32, lo, hi)
    dt0 = jnp.exp(uni(11, (Ld, 2, SSD_HEADS), math.log(1e-3), math.log(1e-1)))
    a_c = uni(24, (Ld, 2, LRU_W), 0.9, 0.999) ** (1.0 / LRU_C)
    return {
        'x': nrm(0, (BATCH, SEQ, D), 1.0),
        'c': nrm(1, (BATCH, D), 1.0),
        'ctx': nrm(2, (BATCH, CTX_LEN, D), 1.0),
        'c_ctx': nrm(3, (D,), 1.0),
        'ada_w': nrm(4, (Ld, D, 6 * D), 0.5 * D ** -0.5),
        'ada_b': nrm(5, (Ld, 6 * D), 0.02),
        'norm1': 1.0 + nrm(6, (Ld, D), 0.02),
        'norm2': 1.0 + nrm(7, (Ld, D), 0.02),
        'w_in': nrm(8, (Ld, D, IN_WIDTH), D ** -0.5),
        'ssd_conv_w': nrm(9, (Ld, CONV_W, SSD_XBC), CONV_W ** -0.5),
        'ssd_conv_b': nrm(10, (Ld, SSD_XBC), 0.02),
        'ssd_dt_bias': dt0 + jnp.log(-jnp.expm1(-dt0)),
        'ssd_a_log': jnp.log(uni(12, (Ld, 2, SSD_HEADS), 1.0, 16.0)),
        'ssd_d': 1.0 + nrm(13, (Ld, SSD_HEADS), 0.1),
        'ssd_norm': 1.0 + nrm(14, (Ld, SSD_INNER), 0.02),
        'gla_g2': nrm(15, (Ld, 2, GLA_RANK, GLA_HEADS * GLA_DK), GLA_RANK ** -0.5),
        'gla_gb': nrm(16, (Ld, 2, GLA_HEADS * GLA_DK), 0.1),
        'gla_norm': 1.0 + nrm(17, (Ld, GLA_HEADS * GLA_DV), 0.02),
        'lru_conv_w': nrm(18, (Ld, CONV_W, LRU_W), CONV_W ** -0.5),
        'lru_conv_b': nrm(19, (Ld, LRU_W), 0.02),
        'lru_wa': nrm(20, (Ld, 2, LRU_BLOCKS, LRU_BLOCK, LRU_BLOCK), LRU_BLOCK ** -0.5),
        'lru_ba': nrm(21, (Ld, 2, LRU_W), 0.02),
        'lru_wx': nrm(22, (Ld, 2, LRU_BLOCKS, LRU_BLOCK, LRU_BLOCK), LRU_BLOCK ** -0.5),
        'lru_bx': nrm(23, (Ld, 2, LRU_W), 0.02),
        'lru_lambda': jnp.log(a_c) - jnp.log1p(-a_c),
        'att_qnorm': 1.0 + nrm(25, (Ld, ATT_HEAD_DIM), 0.02),
        'att_knorm': 1.0 + nrm(26, (Ld, ATT_HEAD_DIM), 0.02),
        'w_branch': nrm(27, (Ld, N_BRANCH, BRANCH_W, D), BRANCH_W ** -0.5),
        'w_out': nrm(28, (Ld, D, D), D ** -0.5),
        'router_wg': nrm(29, (Ld, D, MOE_GROUPS), D ** -0.5),
        'router_bg': nrm(30, (Ld, MOE_GROUPS), 0.01),
        'router_we': nrm(31, (Ld, D, MOE_EXPERTS), D ** -0.5),
        'router_be': nrm(32, (Ld, MOE_EXPERTS), 0.01),
        'exp_w1': nrm(33, (Ld, MOE_EXPERTS, D, MOE_FF), D ** -0.5),
        'exp_w3': nrm(34, (Ld, MOE_EXPERTS, D, MOE_FF), D ** -0.5),
        'exp_w2': nrm(35, (Ld, MOE_EXPERTS, MOE_FF, D), MOE_FF ** -0.5),
    }


def reference(x, c, ctx, c_ctx, ada_w, ada_b, norm1, norm2, w_in, ssd_conv_w, ssd_conv_b, ssd_dt_bias,
              ssd_a_log, ssd_d, ssd_norm, gla_g2, gla_gb, gla_norm, lru_conv_w, lru_conv_b, lru_wa, lru_ba,
              lru_wx, lru_bx, lru_lambda, att_qnorm, att_knorm, w_branch, w_out, router_wg, router_bg,
              router_we, router_be, exp_w1, exp_w3, exp_w2):
    L = x.shape[1]
    rows = L // GRID_W
    row = jnp.repeat(jnp.arange(rows, dtype=jnp.int32), GRID_W)
    col = jnp.tile(jnp.arange(GRID_W, dtype=jnp.int32), rows)
    xl, xc = x, ctx
    for l in range(DEPTH):
        lp = dict(norm1=norm1[l], norm2=norm2[l], w_in=w_in[l],
                  ssd_conv_w=ssd_conv_w[l], ssd_conv_b=ssd_conv_b[l], ssd_dt_bias=ssd_dt_bias[l],
                  ssd_a_log=ssd_a_log[l], ssd_d=ssd_d[l], ssd_norm=ssd_norm[l],
                  gla_g2=gla_g2[l], gla_gb=gla_gb[l], gla_norm=gla_norm[l],
                  lru_conv_w=lru_conv_w[l], lru_conv_b=lru_conv_b[l], lru_wa=lru_wa[l], lru_ba=lru_ba[l],
                  lru_wx=lru_wx[l], lru_bx=lru_bx[l], lru_lambda=lru_lambda[l],
                  att_qnorm=att_qnorm[l], att_knorm=att_knorm[l], w_branch=w_branch[l], w_out=w_out[l],
                  router_wg=router_wg[l], router_bg=router_bg[l], router_we=router_we[l],
                  router_be=router_be[l], exp_w1=exp_w1[l], exp_w3=exp_w3[l], exp_w2=exp_w2[l])
        mod_l = jax.nn.silu(c) @ ada_w[l] + ada_b[l]
        mod_c = jax.nn.silu(c_ctx) @ ada_w[l] + ada_b[l]
        xl, xc = hybrid_layer(xl, xc, mod_l, mod_c, lp, row, col, l < DEPTH - 1)
    return xl
```

```python
import numpy as np
import concourse.bass as bass
import concourse.mybir as mybir
from concourse.bass_utils import run_bass_kernel_spmd
from contextlib import ExitStack

F32 = mybir.dt.float32
BF16 = mybir.dt.bfloat16
AF = mybir.ActivationFunctionType
ALU = mybir.AluOpType
AX = mybir.AxisListType


class Reg:
    __slots__ = ("name", "w", "r")

    def __init__(self, name=""):
        self.name = name
        self.w = None
        self.r = []


class Ins:
    __slots__ = ("eng", "fn", "deps", "sig", "idx", "isdma", "slot", "target", "n")

    def __init__(self):
        self.slot = None


class Prog:
    ENG = ["tensor", "vector", "scalar", "gpsimd", "sync"]
    RING = 12

    def __init__(self, nc):
        self.nc = nc
        self.q = {e: [] for e in self.ENG}
        self.n = 0

    def op(self, eng, fn, reads=(), writes=(), dma=False):
        I = Ins()
        I.eng, I.fn, I.isdma, I.sig, I.idx = eng, fn, dma, False, None
        I.n = self.n
        self.n += 1
        deps = {}
        for r in reads:
            if r.w is not None:
                deps.setdefault(id(r.w), [r.w, set()])[1].add("raw")
        for w in writes:
            if w.w is not None:
                deps.setdefault(id(w.w), [w.w, set()])[1].add("waw")
            for x in w.r:
                deps.setdefault(id(x), [x, set()])[1].add("war")
        final = []
        for J, kinds in deps.values():
            if J is I:
                continue
            if J.eng == eng and not J.isdma and not dma:
                if "raw" not in kinds or eng == "tensor":
                    continue
            final.append(J)
            J.sig = True
        I.deps = final
        for r in reads:
            r.r.append(I)
        for w in writes:
            w.w = I
            w.r = []
        self.q[eng].append(I)
        return I

    def fence(self):
        self.nf = getattr(self, "nf", 0) + 1
        for e in self.ENG:
            last = None
            for I in reversed(self.q[e]):
                if I.fn == "FENCE":
                    break
                if not I.isdma and I.fn is not None:
                    last = I
                    break
            if last is not None:
                last.sig = True
            I = Ins()
            I.eng, I.fn, I.isdma, I.sig, I.idx, I.deps = e, "FENCE", False, False, None, [last] if last is not None else []
            I.n = self.nf
            self.q[e].append(I)

    def dma(self, eng, out, in_, reads=(), writes=(), **kw):
        return self.op(eng, lambda e: e.dma_start(out=out, in_=in_, **kw), reads, writes, dma=True)

    def cc(self, fn, reads=(), writes=()):
        I = self.op("gpsimd", fn, reads, writes, dma=True)
        I.slot = "cc"
        self.ccs = getattr(self, "ccs", []) + [I]
        return I

    def emit(self, final_regs=()):
        nc = self.nc
        self.op("sync", None, reads=list(final_regs))
        sems = {}
        stack = []
        for e in self.ENG:
            cm = nc.semaphore("S_" + e)
            sems[e] = cm.__enter__()
            stack.append(cm)
        cmf = nc.semaphore("S_fence")
        fsem = cmf.__enter__()
        stack.append(cmf)
        rings = {}
        for e in ("sync", "gpsimd", "scalar"):
            rings[e] = []
            for k in range(self.RING):
                cm = nc.semaphore("D_%s_%d" % (e, k))
                rings[e].append(cm.__enter__())
                stack.append(cm)
        ccsem = {}
        cctgt = {}
        if getattr(self, "ccs", []):
            cm = nc.semaphore("C_all")
            csem = cm.__enter__()
            stack.append(cm)
            for k, I in enumerate(self.ccs):
                ccsem[id(I)] = csem
                cctgt[id(I)] = k + 1
        for e in self.ENG:
            cnt = 0
            dcnt = 0
            for I in self.q[e]:
                if I.isdma and getattr(I, "slot", None) == "cc":
                    I.target = 1
                elif I.isdma:
                    I.slot = dcnt % self.RING
                    I.target = 16 * (dcnt // self.RING + 1)
                    dcnt += 1
                elif I.sig:
                    cnt += 1
                    I.idx = cnt
        self.counts = {e: len(self.q[e]) for e in self.ENG}

        def run(e, eng):
            seen = {}
            prev = {}

            def wait(key, sem, val):
                if seen.get(key, 0) < val:
                    eng.wait_ge(sem, val)
                    seen[key] = val

            for I in self.q[e]:
                mx = {}
                for J in I.deps:
                    if J.isdma and J.slot == "cc":
                        wait(("cc",), ccsem[id(J)], cctgt[id(J)])
                    elif J.isdma:
                        wait((J.eng, J.slot), rings[J.eng][J.slot], J.target)
                    else:
                        mx[J.eng] = max(mx.get(J.eng, 0), J.idx)
                for f, v in mx.items():
                    wait(f, sems[f], v)
                if I.isdma and I.slot == "cc":
                    inst = I.fn(eng)
                    inst.then_inc(ccsem[id(I)], 1)
                    wait(("cc",), ccsem[id(I)], cctgt[id(I)])
                    continue
                if I.isdma:
                    p = prev.get(I.slot)
                    if p is not None:
                        wait((e, I.slot), rings[e][I.slot], p.target)
                    prev[I.slot] = I
                if I.fn is None:
                    continue
                if I.fn == "FENCE":
                    for s, pq in prev.items():
                        wait((e, s), rings[e][s], pq.target)
                    eng.sem_inc(fsem, 1)
                    eng.wait_ge(fsem, len(self.ENG) * I.n)
                    continue
                inst = I.fn(eng)
                if I.isdma:
                    inst.then_inc(rings[e][I.slot], 16)
                elif I.sig:
                    inst.then_inc(sems[e], 1)
            if e in rings:
                for s, p in prev.items():
                    wait((e, s), rings[e][s], p.target)

        with nc.Block() as block:
            @block.sync
            def _(eng):
                run("sync", eng)

            @block.tensor
            def _(eng):
                run("tensor", eng)

            @block.vector
            def _(eng):
                run("vector", eng)

            @block.scalar
            def _(eng):
                run("scalar", eng)

            @block.gpsimd
            def _(eng):
                run("gpsimd", eng)
        for cm in reversed(stack):
            cm.__exit__(None, None, None)


EPS = 1e-6
THETA = 10000.0
NCTX, LAT = 256, 4096
T = NCTX + LAT
HT = T // 2
NCC_MIX = 23
NCC_S1 = 39
ARENA_F32 = 50176


class Tile:
    __slots__ = ("t", "r")

    def __init__(self, t, name):
        self.t = t
        self.r = Reg(name)

    def __getitem__(self, k):
        return self.t[k]


class KB:
    def __init__(self):
        self.nc = bass.Bass("TRN2", target_bir_lowering=False)
        self.es = ExitStack()
        self.p = Prog(self.nc)
        self.es.enter_context(self.nc.allow_low_precision("bf16 matmul operands, fp32 accumulation"))
        self.arena = self.es.enter_context(self.nc.sbuf_tensor("arena", [128, ARENA_F32], F32))
        self.banks = [Tile(self.es.enter_context(self.nc.psum_tensor("bank%d" % i, [128, 512], F32)), "bank%d" % i) for i in range(8)]
        self.off = 0
        self.nscr = 0

    def din(self, name, shape, dt=F32):
        return self.nc.dram_tensor(name, list(shape), dt, kind="ExternalInput").ap()

    def dout(self, name, shape, dt=F32):
        return self.nc.dram_tensor(name, list(shape), dt, kind="ExternalOutput").ap()

    def scratch(self, name, shape, dt=F32, debug=False):
        if debug:
            return self.dout(name, shape, dt)
        return self.nc.dram_tensor(name, list(shape), dt).ap()

    def sb(self, name, shape, dt=F32):
        esize = 4 if dt == F32 else 2
        nel = 1
        for d in shape[1:]:
            nel *= d
        nbytes = (nel * esize + 31) // 32 * 32
        assert self.off + nbytes <= ARENA_F32 * 4, ("SBUF arena overflow", name, self.off, nbytes)
        ap = self.arena[0:shape[0], self.off // 4:(self.off + nbytes) // 4]
        if dt != F32:
            ap = ap.bitcast(dt)
        ap = ap[:, 0:nel]
        if len(shape) > 2:
            names = ["d%d" % k for k in range(len(shape) - 1)]
            ap = ap.rearrange("p (%s) -> p %s" % (" ".join(names), " ".join(names)), **{n: shape[k + 1] for k, n in enumerate(names[:-1])})
        self.off += nbytes
        return Tile(ap, name)

    def sub(self, tile, off_el, shape, dt=F32, name="v"):
        esize = 4 if dt == F32 else 2
        nel = 1
        for d in shape[1:]:
            nel *= d
        ap = tile.t[0:shape[0], off_el:off_el + nel * esize // 4]
        if dt != F32:
            ap = ap.bitcast(dt)
        if len(shape) > 2:
            names = ["d%d" % k for k in range(len(shape) - 1)]
            ap = ap.rearrange("p (%s) -> p %s" % (" ".join(names), " ".join(names)), **{n: shape[k + 1] for k, n in enumerate(names[:-1])})
        return Tile(ap, name)

    def psbf(self, bank):
        t = Tile(bank.t[:, 0:512].bitcast(BF16), "bf")
        t.r = bank.r
        return t

    def reset(self):
        self.p.fence()
        self.off = 0

    def finish(self, final_regs):
        self.p.emit(final_regs)
        self.es.close()
        return self.nc


def fm(v, n):
    return np.ascontiguousarray(np.asarray(v).reshape(n, 128).T)


def tok_ranges(t0, n, nctx):
    out = []
    if t0 < nctx:
        m = min(n, nctx - t0)
        out.append((t0, m, 1))
        if n > m:
            out.append((t0 + m, n - m, 0))
    else:
        out.append((t0, n, 0))
    return out


def ld(kb, name, shape, src, dt=F32):
    t = kb.sb(name, shape, dt)
    kb.p.dma("sync", t[:], src, writes=[t.r])
    return t


def consts(kb, ident_d, want_bf=True):
    p = kb.p
    idf = ld(kb, "idf", [128, 128], ident_d)
    ones = kb.sb("ones", [128, 128])
    p.op("vector", lambda e: e.memset(ones[:], 1.0), [], [ones.r])
    idb = None
    if want_bf:
        idb = kb.sb("idb", [128, 128], BF16)
        p.op("vector", lambda e: e.tensor_copy(out=idb[:], in_=idf[:]), [idf.r], [idb.r])
    return idf, idb, ones


def emit_mods(kb, adaw, adab, ncc, cT, aw, psm, name="m"):
    p = kb.p
    ct = kb.sb(name + "ct", [128, 8, 2])
    st = kb.sb(name + "st", [128, 8, 2])
    p.dma("sync", ct[:], cT, writes=[ct.r])
    p.op("scalar", lambda e: e.activation(out=st[:], in_=ct[:], func=AF.Silu), [ct.r], [st.r])
    ab = kb.sb(name + "ab", [128, ncc])
    p.dma("sync", ab[:], adab, writes=[ab.r])
    mod = kb.sb(name + "mod", [128, ncc, 2])
    for g in range(ncc // 4):
        awr = [Reg("aw%d" % k) for k in range(8)]
        for kc in range(8):
            p.dma("sync", aw[:, kc, :], adaw[kc * 128:(kc + 1) * 128, g * 512:(g + 1) * 512], reads=[], writes=[awr[kc], aw.r])

        def mm_mod(e, g=g):
            for cc in range(4):
                for kc in range(8):
                    last = e.matmul(psm[:, 2 * (g * 4 + cc):2 * (g * 4 + cc) + 2], lhsT=aw[:, kc, cc * 128:(cc + 1) * 128], rhs=st[:, kc, :],
                                    start=(kc == 0), stop=(kc == 7))
            return last
        p.op("tensor", mm_mod, [st.r, aw.r] + awr, [psm.r])
    p.op("vector", lambda e: e.tensor_tensor(out=mod[:], in0=psm[:, 0:2 * ncc].rearrange("p (c j) -> p c j", j=2),
                                             in1=ab[:].unsqueeze(2).to_broadcast([128, ncc, 2]), op=ALU.add), [psm.r, ab.r], [mod.r])
    return mod


def nat_pieces(t0, n):
    out = []
    bounds = [(0, 128, 0, 0), (128, 256, 1, 0), (256, 256 + 2048, 0, 128), (256 + 2048, T, 1, 128)]
    for (a, b, h, loc) in bounds:
        lo, hi = max(a, t0), min(b, t0 + n)
        if lo < hi:
            out.append((lo, hi - lo, h, loc + lo - a))
    return out


def xs_write(p, xs, row0, tile, reads, outs, eng="gpsimd"):
    for (a, n, h, loc) in nat_pieces(0, T):
        o = Reg("o")
        outs.append(o)
        p.dma(eng, xs[h * 3072 + row0:h * 3072 + row0 + 128, loc:loc + n], tile[:, a:a + n], reads=reads, writes=[o])


def emit_s1(kb, xsrc, cT, adaw, adab, n1, win, pT, xs, xs_regs, pT_regs):
    p = kb.p
    banks = kb.banks
    aw = kb.sb("aw", [128, 8, 512])
    mod = emit_mods(kb, adaw, adab, 16, cT, aw, banks[0])
    n1t = ld(kb, "n1t", [128, 8], n1)
    asc = kb.sb("asc", [128, 8, 2])
    p.op("vector", lambda e: e.tensor_scalar(out=asc[:], in0=mod[:, 8:16, :], scalar1=1.0, scalar2=None, op0=ALU.add), [mod.r], [asc.r])
    p.op("vector", lambda e: e.tensor_tensor(out=asc[:], in0=asc[:], in1=n1t[:].unsqueeze(2).to_broadcast([128, 8, 2]), op=ALU.mult), [asc.r, n1t.r], [asc.r])
    ones = kb.sb("ones", [128, 128])
    p.op("vector", lambda e: e.memset(ones[:], 1.0), [], [ones.r])
    nch = (T + 511) // 512
    chunks = [(c * 512, min(512, T - c * 512)) for c in range(nch)]
    hT = kb.sb("hT", [128, 8, T], BF16)
    hr = [Reg("h%d" % c) for c in range(nch)]
    xq = [kb.sb("xq%d" % i, [128, 8, 512]) for i in range(2)]
    sq = [kb.sb("sq%d" % i, [128, 512]) for i in range(2)]
    rstd = kb.sb("rstd", [128, 512])
    tmp = [kb.sb("tmp%d" % i, [128, 512]) for i in range(2)]
    pss = banks[1]
    for c, (t0, n) in enumerate(chunks):
        xc = xq[c % 2]
        for kc in range(8):
            for (off, ln, src) in xsrc(kc, t0, n):
                p.dma("sync", xc[:, kc, off:off + ln], src, writes=[xc.r])
        for kc in range(8):
            s_ = sq[kc % 2]
            p.op("scalar", lambda e, s_=s_, xc=xc, kc=kc, n=n: e.activation(out=s_[:, 0:n], in_=xc[:, kc, 0:n], func=AF.Square), [xc.r], [s_.r])
            p.op("tensor", lambda e, s_=s_, kc=kc, n=n: e.matmul(pss[:, 0:n], lhsT=ones[:], rhs=s_[:, 0:n], start=(kc == 0), stop=(kc == 7)), [ones.r, s_.r], [pss.r])
        p.op("vector", lambda e, n=n: e.tensor_scalar(out=rstd[:, 0:n], in0=pss[:, 0:n], scalar1=1.0 / 1024, scalar2=EPS, op0=ALU.mult, op1=ALU.add), [pss.r], [rstd.r])
        p.op("vector", lambda e, n=n: e.reciprocal(out=rstd[:, 0:n], in_=rstd[:, 0:n]), [rstd.r], [rstd.r])
        p.op("scalar", lambda e, n=n: e.activation(out=rstd[:, 0:n], in_=rstd[:, 0:n], func=AF.Sqrt), [rstd.r], [rstd.r])
        for kc in range(8):
            tm = tmp[kc % 2]
            p.op("vector", lambda e, tm=tm, xc=xc, kc=kc, n=n: e.tensor_tensor(out=tm[:, 0:n], in0=xc[:, kc, 0:n], in1=rstd[:, 0:n], op=ALU.mult), [xc.r, rstd.r], [tm.r])

            def modf(e, tm=tm, kc=kc, t0=t0, n=n):
                for (s, m, j) in tok_ranges(t0, n, NCTX):
                    last = e.activation(out=hT[:, kc, s:s + m], in_=tm[:, s - t0:s - t0 + m], func=AF.Identity, scale=asc[:, kc, j:j + 1], bias=mod[:, kc, j:j + 1])
                return last
            p.op("scalar", modf, [tm.r, asc.r, mod.r], [hr[c]])
    wfs = [kb.sb("wf%d" % i, [128, 8, 128]) for i in range(2)]
    wbs = [kb.sb("wb%d" % i, [128, 8, 128], BF16) for i in range(2)]
    stg = [kb.sb("stg%d" % i, [128, T]) for i in range(2)]
    sgr = [[Reg("sg%d_%d" % (i, c)) for c in range(nch)] for i in range(2)]
    pps = banks[2:6]
    cnt = 0
    for cc in range(NCC_S1):
        wf, wb, sg = wfs[cc % 2], wbs[cc % 2], stg[cc % 2]
        p.dma("sync", wf[:], win[:, cc * 128:(cc + 1) * 128].rearrange("(kc p) c -> p kc c", p=128), writes=[wf.r])
        p.op("gpsimd", lambda e, wf=wf, wb=wb: e.tensor_copy(out=wb[:], in_=wf[:]), [wf.r], [wb.r])
        for tcn, (t0, n) in enumerate(chunks):
            pp = pps[cnt % 4]

            def mm(e, pp=pp, wb=wb, t0=t0, n=n):
                for kc in range(8):
                    last = e.matmul(pp[:, 0:n], lhsT=wb[:, kc, :], rhs=hT[:, kc, t0:t0 + n], start=(kc == 0), stop=(kc == 7))
                return last
            p.op("tensor", mm, [wb.r, hr[tcn]], [pp.r])
            if cnt % 2 == 0:
                p.op("vector", lambda e, pp=pp, sg=sg, t0=t0, n=n: e.tensor_copy(out=sg[:, t0:t0 + n], in_=pp[:, 0:n]), [pp.r], [sgr[cc % 2][tcn]])
            else:
                p.op("scalar", lambda e, pp=pp, sg=sg, t0=t0, n=n: e.copy(out=sg[:, t0:t0 + n], in_=pp[:, 0:n]), [pp.r], [sgr[cc % 2][tcn]])
            cnt += 1
        if cc < NCC_MIX:
            p.dma("gpsimd", pT[cc * 128:(cc + 1) * 128, :], sg[:], reads=sgr[cc % 2], writes=[pT_regs[cc]])
        else:
            xs_write(p, xs, 1024 + (cc - NCC_MIX) * 128, sg, sgr[cc % 2], xs_regs)


def rope_tables(L=4096, W=64):
    t = np.arange(L)
    pos = np.stack([t // W, t % W], 0).astype(np.float32)
    d = np.arange(128)
    half = d // 64
    j = d % 32
    inv = (THETA ** (-(j.astype(np.float32)) / 32.0)).astype(np.float32)
    ang = pos[half] * inv[:, None]
    cos = np.cos(ang).astype(np.float32)
    sin = np.sin(ang).astype(np.float32)
    sgn = np.where((d % 64) < 32, -1.0, 1.0).astype(np.float32)
    Rm = np.zeros((128, 128), np.float32)
    partner = np.where((d % 64) < 32, d + 32, d - 32)
    Rm[partner, d] = 1.0
    return cos, (sin * sgn[:, None]).astype(np.float32), Rm


def emit_att(kb, qT_d, kT_d, vT_d, gq_d, gk_d, cos_d, sin_d, rm_d, ident_d, xs, row0, in_regs, outs):
    p = kb.p
    banks = kb.banks
    L = LAT
    NKT = T // 128
    idf, idb, ones = consts(kb, ident_d)
    onesb = kb.sb("onesb", [128, 128], BF16)
    p.op("vector", lambda e: e.tensor_copy(out=onesb[:], in_=ones[:]), [ones.r], [onesb.r])
    rm = ld(kb, "rm", [128, 128], rm_d)
    gq = ld(kb, "gq", [128, 1], gq_d)
    gk = ld(kb, "gk", [128, 1], gk_d)
    cos = ld(kb, "cos", [128, L], cos_d)
    sin = ld(kb, "sin", [128, L], sin_d)
    xst = [kb.sb("xst%d" % i, [128, T]) for i in range(2)]
    vb = kb.sb("vb", [128, NKT, 128], BF16)
    vtb = kb.sb("vtb", [128, T], BF16)
    p.dma("sync", xst[1][:], vT_d, reads=in_regs, writes=[xst[1].r])
    p.op("gpsimd", lambda e: e.tensor_copy(out=vtb[:], in_=xst[1][:]), [xst[1].r], [vtb.r])
    ptr = kb.psbf(banks[1])
    for g in range((NKT + 7) // 8):
        k0, k1 = g * 8, min(NKT, g * 8 + 8)

        def trv(e, k0=k0, k1=k1):
            for kt in range(k0, k1):
                last = e.transpose(out=ptr.t[:, (kt - k0) * 128:(kt - k0 + 1) * 128], in_=vtb[:, kt * 128:(kt + 1) * 128], identity=idb[:])
            return last
        p.op("tensor", trv, [vtb.r, idb.r], [ptr.r])
        p.op("scalar", lambda e, k0=k0, k1=k1: e.copy(out=vb[:, k0:k1, :], in_=ptr.t[:, 0:(k1 - k0) * 128].rearrange("p (a b) -> p a b", b=128)), [ptr.r], [vb.r])

    chunks = [(0, NCTX, False)] + [(NCTX + c * 512, 512, True) for c in range(L // 512)]
    knT = kb.sb("knT", [128, T], BF16)
    qnT = [kb.sb("qnT%d" % i, [128, T], BF16) for i in range(2)]
    sqt = kb.sb("sqt", [128, 512])
    rstd = kb.sb("rstd", [128, 512])
    xg = kb.sb("xg", [128, 512])
    t1 = kb.sb("t1", [128, 512])
    t2 = kb.sb("t2", [128, 512])
    pss, prot = banks[0], banks[1]
    srcs = [(kT_d, gk, knT), (qT_d[0:128, :], gq, qnT[0]), (qT_d[128:256, :], gq, qnT[1])]
    dregs = []
    for si, (src, g, dst) in enumerate(srcs):
        xs_ = xst[si % 2]
        p.dma("sync", xs_[:], src, reads=in_regs, writes=[xs_.r])
        dr = [Reg("d%d_%d" % (si, c)) for c in range(len(chunks))]
        dregs.append(dr)
        for c, (t0, n, lat) in enumerate(chunks):
            p.op("scalar", lambda e, xs_=xs_, t0=t0, n=n: e.activation(out=sqt[:, 0:n], in_=xs_[:, t0:t0 + n], func=AF.Square), [xs_.r], [sqt.r])
            p.op("tensor", lambda e, n=n: e.matmul(pss[:, 0:n], lhsT=ones[:], rhs=sqt[:, 0:n], start=True, stop=True), [ones.r, sqt.r], [pss.r])
            p.op("vector", lambda e, n=n: e.tensor_scalar(out=rstd[:, 0:n], in0=pss[:, 0:n], scalar1=1.0 / 128, scalar2=EPS, op0=ALU.mult, op1=ALU.add), [pss.r], [rstd.r])
            p.op("vector", lambda e, n=n: e.reciprocal(out=rstd[:, 0:n], in_=rstd[:, 0:n]), [rstd.r], [rstd.r])
            p.op("scalar", lambda e, n=n: e.activation(out=rstd[:, 0:n], in_=rstd[:, 0:n], func=AF.Sqrt), [rstd.r], [rstd.r])
            p.op("vector", lambda e, xs_=xs_, g=g, t0=t0, n=n: e.tensor_scalar(out=xg[:, 0:n], in0=xs_[:, t0:t0 + n], scalar1=g[:, 0:1], scalar2=None, op0=ALU.mult), [xs_.r, g.r], [xg.r])
            if lat:
                l0 = t0 - NCTX
                p.op("tensor", lambda e, n=n: e.matmul(prot[:, 0:n], lhsT=rm[:], rhs=xg[:, 0:n], start=True, stop=True), [rm.r, xg.r], [prot.r])
                p.op("gpsimd", lambda e, l0=l0, n=n: e.tensor_tensor(out=t1[:, 0:n], in0=xg[:, 0:n], in1=cos[:, l0:l0 + n], op=ALU.mult), [xg.r, cos.r], [t1.r])
                p.op("vector", lambda e, l0=l0, n=n: e.tensor_tensor(out=t2[:, 0:n], in0=prot[:, 0:n], in1=sin[:, l0:l0 + n], op=ALU.mult), [prot.r, sin.r], [t2.r])
                p.op("gpsimd", lambda e, n=n: e.tensor_tensor(out=t1[:, 0:n], in0=t1[:, 0:n], in1=t2[:, 0:n], op=ALU.add), [t1.r, t2.r], [t1.r])
                p.op("vector", lambda e, dst=dst, t0=t0, n=n: e.tensor_tensor(out=dst[:, t0:t0 + n], in0=t1[:, 0:n], in1=rstd[:, 0:n], op=ALU.mult), [t1.r, rstd.r], [dr[c]])
            else:
                p.op("vector", lambda e, dst=dst, t0=t0, n=n: e.tensor_tensor(out=dst[:, t0:t0 + n], in0=xg[:, 0:n], in1=rstd[:, 0:n], op=ALU.mult), [xg.r, rstd.r], [dr[c]])
    kr, qr = dregs[0], dregs[1:]
    pS = banks[2:5]
    pO = banks[5:7]
    pD = [banks[7], banks[0]]
    pts = [kb.sb("pt%d" % i, [128, 512], BF16) for i in range(3)]
    rden = [kb.sb("rden%d" % i, [128, 512]) for i in range(2)]
    yo = [kb.sb("yo%d" % i, [128, 512]) for i in range(2)]
    scale = 128.0 ** -0.5
    sc = 0
    blk = 0
    for h in range(2):
        for c, (t0, n, lat) in enumerate(chunks):
            nkt = NKT if lat else NCTX // 128
            po, pd = pO[blk % 2], pD[blk % 2]
            for kt in range(nkt):
                ps_, pt = pS[sc % 3], pts[sc % 3]
                sc += 1
                kc = 0 if kt < NCTX // 128 else 1 + (kt * 128 - NCTX) // 512
                p.op("tensor", lambda e, ps_=ps_, kt=kt, h=h, t0=t0, n=n: e.matmul(ps_[:, 0:n], lhsT=knT[:, kt * 128:(kt + 1) * 128], rhs=qnT[h][:, t0:t0 + n], start=True, stop=True),
                     [kr[kc], qr[h][c]], [ps_.r])
                p.op("scalar", lambda e, ps_=ps_, pt=pt, n=n: e.activation(out=pt[:, 0:n], in_=ps_[:, 0:n], func=AF.Exp, scale=scale), [ps_.r], [pt.r])

                def pv(e, po=po, pd=pd, pt=pt, kt=kt, n=n, nkt=nkt):
                    e.matmul(po[:, 0:n], lhsT=vb[:, kt, :], rhs=pt[:, 0:n], start=(kt == 0), stop=(kt == nkt - 1))
                    return e.matmul(pd[:, 0:n], lhsT=onesb[:], rhs=pt[:, 0:n], start=(kt == 0), stop=(kt == nkt - 1))
                p.op("tensor", pv, [vb.r, onesb.r, pt.r], [po.r, pd.r])
            rd, y = rden[blk % 2], yo[blk % 2]
            p.op("vector", lambda e, rd=rd, pd=pd, n=n: e.reciprocal(out=rd[:, 0:n], in_=pd[:, 0:n]), [pd.r], [rd.r])
            p.op("vector", lambda e, rd=rd, po=po, y=y, n=n: e.tensor_tensor(out=y[:, 0:n], in0=po[:, 0:n], in1=rd[:, 0:n], op=ALU.mult), [po.r, rd.r], [y.r])
            for (a, m, hh, loc) in nat_pieces(t0, n):
                o = Reg("o")
                outs.append(o)
                p.dma("gpsimd", xs[hh * 3072 + row0 + h * 128:hh * 3072 + row0 + (h + 1) * 128, loc:loc + m], y[:, a - t0:a - t0 + m], reads=[y.r], writes=[o])
            blk += 1


def emit_conv(kb, p, x, u, w, b, segs, cc, eng_extra="vector"):
    p.op("scalar", lambda e: e.activation(out=u[:], in_=x[:], func=AF.Identity, scale=w[:, cc, 2:3], bias=b[:, cc:cc + 1]), [x.r, w.r, b.r], [u.r])

    def taps(e):
        for (s0, n) in segs:
            for k, off in ((0, -2), (1, -1), (3, 1)):
                lo = max(0, -off)
                hi = n - max(0, off)
                last = e.scalar_tensor_tensor(out=u[:, s0 + lo:s0 + hi], in0=x[:, s0 + lo + off:s0 + hi + off], scalar=w[:, cc, k:k + 1],
                                              in1=u[:, s0 + lo:s0 + hi], op0=ALU.mult, op1=ALU.add)
        return last
    p.op(eng_extra, taps, [x.r, u.r, w.r], [u.r])


def emit_lru(kb, xT_d, gT_d, cw_d, cb_d, wbd_d, bias_d, lam_d, xs, row0, in_regs, outs):
    p = kb.p
    banks = kb.banks
    L = LAT
    cw = ld(kb, "cw", [128, 2, 4], cw_d)
    cb = ld(kb, "cb", [128, 2], cb_d)
    wbd = ld(kb, "wbd", [128, 8, 128], wbd_d)
    bias = ld(kb, "bias", [128, 8], bias_d)
    lam = ld(kb, "lam", [128, 4], lam_d)
    cl = kb.sb("cl", [128, 4])
    p.op("scalar", lambda e: e.activation(out=cl[:], in_=lam[:], func=AF.Exp, scale=-1.0), [lam.r], [cl.r])
    p.op("scalar", lambda e: e.activation(out=cl[:], in_=cl[:], func=AF.Ln, bias=1.0, scale=1.0), [cl.r], [cl.r])
    p.op("vector", lambda e: e.tensor_scalar(out=cl[:], in0=cl[:], scalar1=-8.0, scalar2=None, op0=ALU.mult), [cl.r], [cl.r])
    x = kb.sb("x", [128, T]); u = kb.sb("u", [128, T]); g = kb.sb("g", [128, T])
    av = [[kb.sb("a%d" % d, [128, T]), kb.sb("v%d" % d, [128, T])] for d in range(2)]
    hh = [kb.sb("h%d" % d, [128, T]) for d in range(2)]
    rt = [kb.sb("rt%d" % i, [128, 512]) for i in range(2)]
    it_ = [kb.sb("it%d" % i, [128, 512]) for i in range(2)]
    s2 = [kb.sb("s2%d" % i, [128, 512]) for i in range(2)]
    segs = [(0, NCTX), (NCTX, L)]
    chunks = [(0, NCTX)] + [(NCTX + c * 512, 512) for c in range(L // 512)]
    bc = 0
    for cc in range(2):
        p.dma("sync", x[:], xT_d[cc * 128:(cc + 1) * 128, :], reads=in_regs, writes=[x.r])
        p.dma("sync", g[:], gT_d[cc * 128:(cc + 1) * 128, :], reads=in_regs, writes=[g.r])
        emit_conv(kb, p, x, u, cw, cb, segs, cc)
        for d in range(2):
            a, v = av[d]
            for c, (t0, n) in enumerate(chunks):
                pr, pi = banks[bc % 8], banks[(bc + 1) % 8]
                bc += 2
                r_, i_, s_ = rt[c % 2], it_[c % 2], s2[c % 2]
                ia, ix = 0 * 4 + d * 2 + cc, 1 * 4 + d * 2 + cc
                p.op("tensor", lambda e, pr=pr, ia=ia, t0=t0, n=n: e.matmul(pr[:, 0:n], lhsT=wbd[:, ia, :], rhs=u[:, t0:t0 + n], start=True, stop=True), [wbd.r, u.r], [pr.r])
                p.op("tensor", lambda e, pi=pi, ix=ix, t0=t0, n=n: e.matmul(pi[:, 0:n], lhsT=wbd[:, ix, :], rhs=u[:, t0:t0 + n], start=True, stop=True), [wbd.r, u.r], [pi.r])
                p.op("scalar", lambda e, pr=pr, r_=r_, ia=ia, n=n: e.activation(out=r_[:, 0:n], in_=pr[:, 0:n], func=AF.Sigmoid, bias=bias[:, ia:ia + 1], scale=1.0), [pr.r, bias.r], [r_.r])
                p.op("scalar", lambda e, pi=pi, i_=i_, ix=ix, n=n: e.activation(out=i_[:, 0:n], in_=pi[:, 0:n], func=AF.Sigmoid, bias=bias[:, ix:ix + 1], scale=1.0), [pi.r, bias.r], [i_.r])
                p.op("scalar", lambda e, a=a, r_=r_, d=d, cc=cc, t0=t0, n=n: e.activation(out=a[:, t0:t0 + n], in_=r_[:, 0:n], func=AF.Exp, scale=cl[:, d * 2 + cc:d * 2 + cc + 1]), [r_.r, cl.r], [a.r])
                p.op("gpsimd", lambda e, a=a, s_=s_, t0=t0, n=n: e.tensor_tensor(out=s_[:, 0:n], in0=a[:, t0:t0 + n], in1=a[:, t0:t0 + n], op=ALU.mult), [a.r], [s_.r])
                p.op("scalar", lambda e, s_=s_, n=n: e.activation(out=s_[:, 0:n], in_=s_[:, 0:n], func=AF.Sqrt, scale=-1.0, bias=1.0), [s_.r], [s_.r])
                p.op("vector", lambda e, i_=i_, t0=t0, n=n: e.tensor_tensor(out=i_[:, 0:n], in0=i_[:, 0:n], in1=u[:, t0:t0 + n], op=ALU.mult), [i_.r, u.r], [i_.r])
                p.op("vector", lambda e, v=v, i_=i_, s_=s_, t0=t0, n=n: e.tensor_tensor(out=v[:, t0:t0 + n], in0=i_[:, 0:n], in1=s_[:, 0:n], op=ALU.mult), [i_.r, s_.r], [v.r])
            h = hh[d]
            if d == 0:
                p.op("vector", lambda e, a=a, v=v, h=h: e.tensor_tensor_scan(out=h[:, 0:NCTX], data0=a[:, 0:NCTX], data1=v[:, 0:NCTX], initial=0.0, op0=ALU.mult, op1=ALU.add), [a.r, v.r], [h.r])
                p.op("vector", lambda e, a=a, v=v, h=h: e.tensor_tensor_scan(out=h[:, NCTX:T], data0=a[:, NCTX:T], data1=v[:, NCTX:T], initial=h[:, NCTX - 1:NCTX], op0=ALU.mult, op1=ALU.add), [a.r, v.r, h.r], [h.r])
            else:
                p.op("vector", lambda e, a=a, v=v, h=h: e.tensor_tensor_scan(out=h[:, 0:NCTX][:, ::-1], data0=a[:, 0:NCTX][:, ::-1], data1=v[:, 0:NCTX][:, ::-1], initial=0.0, op0=ALU.mult, op1=ALU.add), [a.r, v.r], [h.r])
                p.op("vector", lambda e, a=a, v=v, h=h: e.tensor_tensor_scan(out=h[:, NCTX:T][:, ::-1], data0=a[:, NCTX:T][:, ::-1], data1=v[:, NCTX:T][:, ::-1], initial=h[:, 0:1], op0=ALU.mult, op1=ALU.add), [a.r, v.r, h.r], [h.r])
        z = av[0][0]
        p.op("gpsimd", lambda e: e.tensor_tensor(out=z[:], in0=g[:], in1=g[:], op=ALU.mult), [g.r], [z.r])
        p.op("vector", lambda e: e.tensor_scalar(out=z[:], in0=z[:], scalar1=0.044715, scalar2=1.0, op0=ALU.mult, op1=ALU.add), [z.r], [z.r])
        p.op("gpsimd", lambda e: e.tensor_tensor(out=z[:], in0=z[:], in1=g[:], op=ALU.mult), [z.r, g.r], [z.r])
        p.op("scalar", lambda e: e.activation(out=z[:], in_=z[:], func=AF.Sigmoid, scale=1.5957691216057308), [z.r], [z.r])
        p.op("gpsimd", lambda e: e.tensor_tensor(out=z[:], in0=z[:], in1=g[:], op=ALU.mult), [z.r, g.r], [z.r])
        p.op("vector", lambda e: e.tensor_tensor(out=hh[0][:], in0=hh[0][:], in1=hh[1][:], op=ALU.add), [hh[0].r, hh[1].r], [hh[0].r])
        p.op("vector", lambda e: e.tensor_tensor(out=hh[0][:], in0=hh[0][:], in1=z[:], op=ALU.mult), [hh[0].r, z.r], [hh[0].r])
        xs_write(p, xs, row0 + cc * 128, hh[0], [hh[0].r], outs)


def rev_segments(e, out_t, in_t, segs):
    for (s0, n) in segs:
        last = e.tensor_copy(out=out_t[:, s0:s0 + n], in_=in_t[:, s0:s0 + n][:, ::-1])
    return last


def scan_nat_lo(t0, n):
    if t0 < NCTX:
        return NCTX - (t0 + n)
    return NCTX + LAT - (t0 - NCTX + n)


def emit_gla(kb, qT_d, kT_d, vT_d, g1_d, rT_d, g2_d, gb_d, gn_d, cm_d, m2_d, hm_d, ident_d, xs, row0, in_regs, outs):
    p = kb.p
    banks = kb.banks
    L = LAT
    NP = T // 128
    ptr = kb.psbf(banks[7])
    plog, patt, pO, pkv = banks[0], banks[1:3], banks[3:5], banks[5:7]
    segs = [(0, NCTX), (NCTX, L)]
    g2 = ld(kb, "g2", [16, 2, 256], g2_d)
    ngb = ld(kb, "ngb", [128, 4], gb_d)
    p.op("vector", lambda e: e.tensor_scalar(out=ngb[:], in0=ngb[:], scalar1=-1.0, scalar2=None, op0=ALU.mult), [ngb.r], [ngb.r])
    gn = ld(kb, "gn", [128, 2], gn_d)
    cm = ld(kb, "cm", [128, 512], cm_d)
    m2 = ld(kb, "m2", [128, 128], m2_d)
    hm = ld(kb, "hm", [128, 2], hm_d)
    idf, idb, ones = consts(kb, ident_d)
    g1b = kb.sb("g1b", [16, 512])
    g1c = kb.sb("g1c", [16, 512])
    osb = [kb.sb("osb%d" % i, [128, T]) for i in range(4)]
    qT = kb.sb("qT", [128, T])
    kT = kb.sb("kT", [128, T])
    tmpT = kb.sb("tmpT", [128, T])
    vtb = kb.sb("vtb", [128, T], BF16)
    vb = kb.sb("vb", [128, NP, 128], BF16)
    F = lambda n: kb.sb(n, [128, 512])
    sp, gcs, dref, dlast, Aex, Bex, Dex, Eex = F("sp"), F("gcs"), F("dref"), F("dlast"), F("Aex"), F("Bex"), F("Dex"), F("Eex")
    dec = kb.sb("dec", [128, 8])
    Bq = lambda n: kb.sb(n, [128, 512], BF16)
    qt, kt, kd, qe = Bq("qt"), Bq("kt"), Bq("kd"), Bq("qe")
    kdT = [kb.sb("kdT%d" % i, [128, 2, 128], BF16) for i in range(2)]
    attm = [kb.sb("attm%d" % i, [128, 128], BF16) for i in range(2)]
    S = kb.sb("S", [128, 128])
    NSB = 4
    Sb = [kb.sb("Sb%d" % i, [128, 128], BF16) for i in range(NSB)]
    scale = 128.0 ** -0.5
    blocks = [(0, NCTX)] + [(NCTX + c * 512, 512) for c in range(L // 512)]
    pc = 0
    for hd in range(4):
        h, d = hd // 2, hd % 2
        for (dst, src) in ((qT, qT_d), (kT, kT_d)):
            if d == 0:
                p.dma("sync", dst[:], src[h * 128:(h + 1) * 128, :], reads=in_regs, writes=[dst.r])
            else:
                p.dma("sync", tmpT[:], src[h * 128:(h + 1) * 128, :], reads=in_regs, writes=[tmpT.r])
                p.op("gpsimd", lambda e, dst=dst: rev_segments(e, dst, tmpT, segs), [tmpT.r], [dst.r])
        p.dma("sync", tmpT[:], vT_d[h * 128:(h + 1) * 128, :], reads=in_regs, writes=[tmpT.r])
        if d == 0:
            p.op("gpsimd", lambda e: e.tensor_copy(out=vtb[:], in_=tmpT[:]), [tmpT.r], [vtb.r])
        else:
            p.op("gpsimd", lambda e: rev_segments(e, vtb, tmpT, segs), [tmpT.r], [vtb.r])
        for g in range((NP + 7) // 8):
            k0, k1 = g * 8, min(NP, g * 8 + 8)

            def trv(e, k0=k0, k1=k1):
                for kt_ in range(k0, k1):
                    last = e.transpose(out=ptr.t[:, (kt_ - k0) * 128:(kt_ - k0 + 1) * 128], in_=vtb[:, kt_ * 128:(kt_ + 1) * 128], identity=idb[:])
                return last
            p.op("tensor", trv, [vtb.r, idb.r], [ptr.r])
            p.op("scalar", lambda e, k0=k0, k1=k1: e.copy(out=vb[:, k0:k1, :], in_=ptr.t[:, 0:(k1 - k0) * 128].rearrange("p (a b) -> p a b", b=128)), [ptr.r], [vb.r])
        p.op("vector", lambda e: e.memset(S[:], 0.0), [], [S.r])
        sbi = 0
        p.op("gpsimd", lambda e, sb_=Sb[0]: e.memset(sb_[:], 0.0), [], [Sb[0].r])
        for (t0, n) in blocks:
            nc_ = n // 64
            v3 = lambda t, n=n: t[:, 0:n].rearrange("p (c l) -> p c l", l=64)
            if d == 0:
                p.dma("sync", g1b[:, 0:n], g1_d[0:16, t0:t0 + n], reads=in_regs, writes=[g1b.r])
                gsrc = g1b
            else:
                lo = scan_nat_lo(t0, n)
                p.dma("sync", g1c[:, 0:n], g1_d[16:32, lo:lo + n], reads=in_regs, writes=[g1c.r])
                p.op("vector", lambda e, n=n: e.tensor_copy(out=g1b[:, 0:n], in_=g1c[:, 0:n][:, ::-1]), [g1c.r], [g1b.r])
                gsrc = g1b
            p.op("tensor", lambda e, d=d, h=h, n=n: e.matmul(plog[:, 0:n], lhsT=g2[:, d, h * 128:(h + 1) * 128], rhs=g1b[:, 0:n], start=True, stop=True), [g2.r, g1b.r], [plog.r])
            p.op("scalar", lambda e, hd=hd, n=n: e.activation(out=sp[:, 0:n], in_=plog[:, 0:n], func=AF.Exp, scale=-1.0, bias=ngb[:, hd:hd + 1]), [plog.r, ngb.r], [sp.r])
            p.op("scalar", lambda e, n=n: e.activation(out=sp[:, 0:n], in_=sp[:, 0:n], func=AF.Ln, scale=1.0, bias=1.0), [sp.r], [sp.r])
            p.op("vector", lambda e, n=n: e.tensor_tensor_scan(out=gcs[:, 0:n], data0=cm[:, 0:n], data1=sp[:, 0:n], initial=0.0, op0=ALU.mult, op1=ALU.add), [cm.r, sp.r], [gcs.r])
            p.op("vector", lambda e, n=n, nc_=nc_, v3=v3: e.tensor_tensor(out=v3(dref), in0=v3(gcs), in1=v3(gcs)[:, :, 32:33].to_broadcast([128, nc_, 64]), op=ALU.subtract), [gcs.r], [dref.r])
            p.op("gpsimd", lambda e, n=n, nc_=nc_, v3=v3: e.tensor_tensor(out=v3(dlast), in0=v3(gcs), in1=v3(gcs)[:, :, 63:64].to_broadcast([128, nc_, 64]), op=ALU.subtract), [gcs.r], [dlast.r])
            A = lambda fn, rd, wr: p.op("scalar", fn, rd, wr)
            A(lambda e, n=n: e.activation(out=Aex[:, 0:n], in_=dref[:, 0:n], func=AF.Exp, scale=-1.0 / 16), [dref.r], [Aex.r])
            A(lambda e, n=n: e.activation(out=Bex[:, 0:n], in_=dref[:, 0:n], func=AF.Exp, scale=1.0 / 16), [dref.r], [Bex.r])
            A(lambda e, n=n: e.activation(out=Dex[:, 0:n], in_=dlast[:, 0:n], func=AF.Exp, scale=1.0 / 16), [dlast.r], [Dex.r])
            A(lambda e, n=n: e.activation(out=Eex[:, 0:n], in_=gcs[:, 0:n], func=AF.Exp, scale=-1.0 / 16), [gcs.r], [Eex.r])
            A(lambda e, nc_=nc_, v3=v3: e.activation(out=dec[:, 0:nc_], in_=v3(gcs)[:, :, 63], func=AF.Exp, scale=-1.0 / 16), [gcs.r], [dec.r])
            p.op("vector", lambda e, t0=t0, n=n: e.scalar_tensor_tensor(out=qt[:, 0:n], in0=qT[:, t0:t0 + n], scalar=scale, in1=Aex[:, 0:n], op0=ALU.mult, op1=ALU.mult), [qT.r, Aex.r], [qt.r])
            p.op("gpsimd", lambda e, t0=t0, n=n: e.tensor_tensor(out=kt[:, 0:n], in0=kT[:, t0:t0 + n], in1=Bex[:, 0:n], op=ALU.mult), [kT.r, Bex.r], [kt.r])
            p.op("vector", lambda e, t0=t0, n=n: e.tensor_tensor(out=kd[:, 0:n], in0=kT[:, t0:t0 + n], in1=Dex[:, 0:n], op=ALU.mult), [kT.r, Dex.r], [kd.r])
            p.op("vector", lambda e, t0=t0, n=n: e.scalar_tensor_tensor(out=qe[:, 0:n], in0=qT[:, t0:t0 + n], scalar=scale, in1=Eex[:, 0:n], op0=ALU.mult, op1=ALU.mult), [qT.r, Eex.r], [qe.r])
            for pp in range(n // 128):
                s = pp * 128
                gp = (t0 + s) // 128
                kT_, am, pa, po, pk = kdT[pc % 2], attm[pc % 2], patt[pc % 2], pO[pc % 2], pkv[pc % 2]
                tr = ptr.t[:, (pc % 2) * 128:(pc % 2) * 128 + 128]
                pc += 1
                p.op("tensor", lambda e, tr=tr, s=s: e.transpose(out=tr, in_=kd[:, s:s + 128], identity=idb[:]), [kd.r, idb.r], [ptr.r])

                def cpk(e, tr=tr, kT_=kT_):
                    e.activation(out=kT_[:, 0, :], in_=tr, func=AF.Identity, scale=hm[:, 0:1])
                    return e.activation(out=kT_[:, 1, :], in_=tr, func=AF.Identity, scale=hm[:, 1:2])
                p.op("scalar", cpk, [ptr.r, hm.r], [kT_.r])
                p.op("tensor", lambda e, pa=pa, s=s: e.matmul(pa[:, 0:128], lhsT=kt[:, s:s + 128], rhs=qt[:, s:s + 128], start=True, stop=True), [kt.r, qt.r], [pa.r])
                p.op("vector", lambda e, pa=pa, am=am: e.tensor_tensor(out=am[:], in0=pa[:, 0:128], in1=m2[:], op=ALU.mult), [pa.r, m2.r], [am.r])

                def mkv(e, pk=pk, kT_=kT_, gp=gp):
                    e.matmul(pk[:, 0:128], lhsT=kT_[:, 0, :], rhs=vb[:, gp, :], start=True, stop=True)
                    return e.matmul(pk[:, 128:256], lhsT=kT_[:, 1, :], rhs=vb[:, gp, :], start=True, stop=True)
                p.op("tensor", mkv, [kT_.r, vb.r], [pk.r])
                sb0 = Sb[sbi % NSB]
                sb1 = Sb[(sbi + 1) % NSB]
                sb2 = Sb[(sbi + 2) % NSB]
                sbi += 2
                c0 = s // 64
                p.op("vector", lambda e, pk=pk, c0=c0: e.scalar_tensor_tensor(out=S[:], in0=S[:], scalar=dec[:, c0:c0 + 1], in1=pk[:, 0:128], op0=ALU.mult, op1=ALU.add), [S.r, dec.r, pk.r], [S.r])
                p.op("scalar", lambda e, sb1=sb1: e.copy(out=sb1[:], in_=S[:]), [S.r], [sb1.r])
                p.op("vector", lambda e, pk=pk, c0=c0: e.scalar_tensor_tensor(out=S[:], in0=S[:], scalar=dec[:, c0 + 1:c0 + 2], in1=pk[:, 128:256], op0=ALU.mult, op1=ALU.add), [S.r, dec.r, pk.r], [S.r])
                p.op("scalar", lambda e, sb2=sb2: e.copy(out=sb2[:], in_=S[:]), [S.r], [sb2.r])

                def mo(e, po=po, am=am, gp=gp, sb0=sb0, sb1=sb1, s=s):
                    e.matmul(po[:, 0:128], lhsT=vb[:, gp, :], rhs=am[:], start=True, stop=False)
                    e.matmul(po[:, 0:64], lhsT=sb0[:], rhs=qe[:, s:s + 64], start=False, stop=False)
                    return e.matmul(po[:, 64:128], lhsT=sb1[:], rhs=qe[:, s + 64:s + 128], start=False, stop=True)
                p.op("tensor", mo, [vb.r, am.r, sb0.r, sb1.r, qe.r], [po.r])
                p.op("scalar", lambda e, po=po, hd=hd, a=t0 + s: e.copy(out=osb[hd][:, a:a + 128], in_=po[:, 0:128]), [po.r], [osb[hd].r])
    rt = kb.sb("rt", [128, 512])
    yy = [kb.sb("yy%d" % i, [128, 512]) for i in range(2)]
    pss = banks[0]
    bi = 0
    for h in range(2):
        of, ob = osb[2 * h], osb[2 * h + 1]

        def comb(e, of=of, ob=ob):
            for (s0, n) in segs:
                last = e.tensor_tensor(out=of[:, s0:s0 + n], in0=of[:, s0:s0 + n], in1=ob[:, s0:s0 + n][:, ::-1], op=ALU.add)
            return last
        p.op("vector", comb, [of.r, ob.r], [of.r])
        for (t0, n) in blocks:
            y_ = yy[bi % 2]
            bi += 1
            p.op("scalar", lambda e, of=of, t0=t0, n=n: e.activation(out=sp[:, 0:n], in_=of[:, t0:t0 + n], func=AF.Square), [of.r], [sp.r])
            p.op("tensor", lambda e, n=n: e.matmul(pss[:, 0:n], lhsT=ones[:], rhs=sp[:, 0:n], start=True, stop=True), [ones.r, sp.r], [pss.r])
            p.op("vector", lambda e, n=n: e.tensor_scalar(out=gcs[:, 0:n], in0=pss[:, 0:n], scalar1=1.0 / 128, scalar2=EPS, op0=ALU.mult, op1=ALU.add), [pss.r], [gcs.r])
            p.op("vector", lambda e, n=n: e.reciprocal(out=gcs[:, 0:n], in_=gcs[:, 0:n]), [gcs.r], [gcs.r])
            p.op("scalar", lambda e, n=n: e.activation(out=gcs[:, 0:n], in_=gcs[:, 0:n], func=AF.Sqrt), [gcs.r], [gcs.r])
            p.dma("sync", rt[:, 0:n], rT_d[h * 128:(h + 1) * 128, t0:t0 + n], reads=in_regs, writes=[rt.r])
            p.op("scalar", lambda e, n=n: e.activation(out=rt[:, 0:n], in_=rt[:, 0:n], func=AF.Silu), [rt.r], [rt.r])
            p.op("vector", lambda e, of=of, y_=y_, t0=t0, n=n: e.tensor_tensor(out=y_[:, 0:n], in0=of[:, t0:t0 + n], in1=gcs[:, 0:n], op=ALU.mult), [of.r, gcs.r], [y_.r])
            p.op("vector", lambda e, h=h, y_=y_, n=n: e.scalar_tensor_tensor(out=y_[:, 0:n], in0=y_[:, 0:n], scalar=gn[:, h:h + 1], in1=rt[:, 0:n], op0=ALU.mult, op1=ALU.mult), [y_.r, gn.r, rt.r], [y_.r])
            for (a, m, hh_, loc) in nat_pieces(t0, n):
                o = Reg("o")
                outs.append(o)
                p.dma("gpsimd", xs[hh_ * 3072 + row0 + h * 128:hh_ * 3072 + row0 + (h + 1) * 128, loc:loc + m], y_[:, a - t0:a - t0 + m], reads=[y_.r], writes=[o])


def emit_ssd(kb, xbc_d, z_d, dt_d, cw_d, cb_d, dtb_d, alog_d, Dp_d, U_d, ident_d, xs, row0, in_regs, outs):
    p = kb.p
    banks = kb.banks
    L = LAT
    NCH = T // 128
    ptr = kb.psbf(banks[7])
    psm, pCB, pbc, pdiag, poff, pst = banks[0], banks[1], banks[2:4], banks[4], banks[5], banks[6]
    cw = ld(kb, "cw", [128, 4, 4], cw_d)
    cb = ld(kb, "cb", [128, 4], cb_d)
    dtb = ld(kb, "dtb", [4, 2], dtb_d)
    aneg = ld(kb, "aneg", [4, 2], alog_d)
    p.op("scalar", lambda e: e.activation(out=aneg[:], in_=aneg[:], func=AF.Exp), [aneg.r], [aneg.r])
    p.op("vector", lambda e: e.tensor_scalar(out=aneg[:], in0=aneg[:], scalar1=-1.0, scalar2=None, op0=ALU.mult), [aneg.r], [aneg.r])
    Dp = ld(kb, "Dp", [128, 2], Dp_d)
    U = ld(kb, "U", [128, 128], U_d)
    idf, idb, ones = consts(kb, ident_d)
    segs = [(0, NCTX), (NCTX, L)]
    arr = [[kb.sb("arr%d_%d" % (d, k), [128, T], BF16) for k in range(4)] for d in range(2)]
    xin = [kb.sb("xin%d" % i, [128, T]) for i in range(2)]
    u = kb.sb("u", [128, T])
    for k in range(4):
        xi = xin[k % 2]
        p.dma("sync", xi[:], xbc_d[k * 128:(k + 1) * 128, :], reads=in_regs, writes=[xi.r])
        emit_conv(kb, p, xi, u, cw, cb, segs, k)
        p.op("scalar", lambda e, k=k: e.activation(out=arr[0][k][:], in_=u[:], func=AF.Silu), [u.r], [arr[0][k].r])
        p.op("gpsimd", lambda e, k=k: rev_segments(e, arr[1][k], arr[0][k], segs), [arr[0][k].r], [arr[1][k].r])
    yacc = [xin[0], xin[1]]
    dtp = kb.sb("dtp", [4, 2, T])
    F = lambda n, s, dt=F32: kb.sb(n, s, dt)
    xBt = [F("xBt%d" % i, [128, 384], BF16) for i in range(2)]
    dtk = [F("dtk%d" % i, [128, 8]) for i in range(2)]
    acl = [F("acl%d" % i, [128, 8]) for i in range(2)]
    nac = [F("nac%d" % i, [128, 4]) for i in range(2)]
    eac = [F("eac%d" % i, [128, 4]) for i in range(2)]
    wdt = [F("wdt%d" % i, [128, 4]) for i in range(2)]
    dcy = [F("dcy%d" % i, [128, 4]) for i in range(2)]
    dtx = [F("dtx%d" % i, [128, 256], BF16) for i in range(2)]
    xw = [F("xw%d" % i, [128, 256], BF16) for i in range(2)]
    rU = [F("rU%d" % i, [128, 128]) for i in range(2)]
    sg = [F("sg%d" % i, [128, 128]) for i in range(2)]
    Lt = [F("Lt%d" % i, [128, 128]) for i in range(2)]
    Mb = [F("Mb%d" % i, [128, 128], BF16) for i in range(4)]
    CBs = [F("CBs%d" % i, [128, 128]) for i in range(2)]
    yt1 = [F("yt1_%d" % i, [128, 256]) for i in range(2)]
    yt2 = [F("yt2_%d" % i, [128, 256]) for i in range(2)]
    H = F("H", [128, 256])
    Hb = [F("Hb%d" % i, [128, 256], BF16) for i in range(2)]
    it = 0
    hc = 0
    for d in range(2):
        slot = 0 if d == 0 else 1
        p.dma("sync", dtp[:, slot, :], dt_d[d * 4:(d + 1) * 4, :], reads=in_regs, writes=[dtp.r])
        p.op("scalar", lambda e, slot=slot, d=d: e.activation(out=dtp[:, slot, :], in_=dtp[:, slot, :], func=AF.Exp, bias=dtb[:, d:d + 1], scale=1.0), [dtp.r, dtb.r], [dtp.r])
        p.op("scalar", lambda e, slot=slot: e.activation(out=dtp[:, slot, :], in_=dtp[:, slot, :], func=AF.Ln, bias=1.0, scale=1.0), [dtp.r], [dtp.r])
        if d == 1:
            def revdt(e):
                for (s0, n) in segs:
                    last = e.tensor_copy(out=dtp[:, 0, s0:s0 + n], in_=dtp[:, 1, s0:s0 + n][:, ::-1])
                return last
            p.op("vector", revdt, [dtp.r], [dtp.r])
        p.op("vector", lambda e, d=d: e.tensor_scalar(out=dtp[:, 1, :], in0=dtp[:, 0, :], scalar1=aneg[:, d:d + 1], scalar2=None, op0=ALU.mult), [dtp.r, aneg.r], [dtp.r])
        p.op("vector", lambda e: e.memset(H[:], 0.0), [], [H.r])
        p.op("gpsimd", lambda e, hb=Hb[hc % 2]: e.memset(hb[:], 0.0), [], [Hb[hc % 2].r])
        A = arr[d]
        for c in range(NCH):
            s = c * 128
            i2 = it % 2
            it += 1
            xb_, dk, ac, na, ea, wd, dc, dx, xw_ = xBt[i2], dtk[i2], acl[i2], nac[i2], eac[i2], wdt[i2], dcy[i2], dtx[i2], xw[i2]
            y1, y2, cbs = yt1[i2], yt2[i2], CBs[i2]
            hb_cur = Hb[hc % 2]
            hb_nxt = Hb[(hc + 1) % 2]
            hc += 1

            def trx(e, s=s, A=A):
                e.transpose(out=ptr.t[:, 0:128], in_=A[0][:, s:s + 128], identity=idb[:])
                e.transpose(out=ptr.t[:, 128:256], in_=A[1][:, s:s + 128], identity=idb[:])
                return e.transpose(out=ptr.t[:, 256:384], in_=A[2][:, s:s + 128], identity=idb[:])
            p.op("tensor", trx, [A[0].r, A[1].r, A[2].r, idb.r], [ptr.r])
            p.op("scalar", lambda e, xb_=xb_: e.copy(out=xb_[:], in_=ptr.t[:, 0:384]), [ptr.r], [xb_.r])

            def trd(e, s=s):
                e.transpose(out=psm[:, 0:4], in_=dtp[:, 0, s:s + 128], identity=idf[0:4, 0:4])
                return e.transpose(out=psm[:, 4:8], in_=dtp[:, 1, s:s + 128], identity=idf[0:4, 0:4])
            p.op("tensor", trd, [dtp.r, idf.r], [psm.r])
            p.op("vector", lambda e, dk=dk: e.tensor_copy(out=dk[:], in_=psm[:, 0:8]), [psm.r], [dk.r])

            def mac(e, dk=dk):
                e.matmul(psm[:, 8:12], lhsT=U[:], rhs=dk[:, 4:8], start=True, stop=True)
                return e.matmul(psm[:, 12:16], lhsT=ones[:], rhs=dk[:, 4:8], start=True, stop=True)
            p.op("tensor", mac, [U.r, ones.r, dk.r], [psm.r])
            p.op("vector", lambda e, ac=ac: e.tensor_copy(out=ac[:], in_=psm[:, 8:16]), [psm.r], [ac.r])
            p.op("vector", lambda e, ac=ac, na=na: e.tensor_scalar(out=na[:], in0=ac[:, 0:4], scalar1=-1.0, scalar2=None, op0=ALU.mult), [ac.r], [na.r])
            p.op("scalar", lambda e, ac=ac, ea=ea: e.activation(out=ea[:], in_=ac[:, 0:4], func=AF.Exp), [ac.r], [ea.r])
            p.op("scalar", lambda e, ac=ac, dc=dc: e.activation(out=dc[:], in_=ac[:, 4:8], func=AF.Exp), [ac.r], [dc.r])
            p.op("vector", lambda e, ac=ac, wd=wd: e.tensor_tensor(out=wd[:], in0=ac[:, 4:8], in1=ac[:, 0:4], op=ALU.subtract), [ac.r], [wd.r])
            p.op("scalar", lambda e, wd=wd: e.activation(out=wd[:], in_=wd[:], func=AF.Exp), [wd.r], [wd.r])
            p.op("vector", lambda e, wd=wd, dk=dk: e.tensor_tensor(out=wd[:], in0=wd[:], in1=dk[:, 0:4], op=ALU.mult), [wd.r, dk.r], [wd.r])
            x3 = lambda t: t[:, 0:256].rearrange("p (h q) -> p h q", q=64)
            p.op("vector", lambda e, dx=dx, xb_=xb_, dk=dk, x3=x3: e.tensor_tensor(out=x3(dx), in0=x3(xb_), in1=dk[:, 0:4].unsqueeze(2).to_broadcast([128, 4, 64]), op=ALU.mult), [xb_.r, dk.r], [dx.r])
            p.op("vector", lambda e, xw_=xw_, xb_=xb_, wd=wd, x3=x3: e.tensor_tensor(out=x3(xw_), in0=x3(xb_), in1=wd[:].unsqueeze(2).to_broadcast([128, 4, 64]), op=ALU.mult), [xb_.r, wd.r], [xw_.r])
            p.op("tensor", lambda e, s=s, A=A: e.matmul(pCB[:, 0:128], lhsT=A[2][:, s:s + 128], rhs=A[3][:, s:s + 128], start=True, stop=True), [A[2].r, A[3].r], [pCB.r])
            p.op("scalar", lambda e, cbs=cbs: e.copy(out=cbs[:], in_=pCB[:, 0:128]), [pCB.r], [cbs.r])
            for h in range(4):
                j2 = h % 2
                ru, sg_, lt, pb = rU[j2], sg[j2], Lt[j2], pbc[j2]
                mb = Mb[h]
                p.op("vector", lambda e, ru=ru, dk=dk, h=h: e.tensor_scalar(out=ru[:], in0=U[:], scalar1=dk[:, 4 + h:5 + h], scalar2=None, op0=ALU.mult), [U.r, dk.r], [ru.r])
                p.op("tensor", lambda e, pb=pb, ru=ru: e.matmul(pb[:, 0:128], lhsT=ones[:], rhs=ru[:], start=True, stop=True), [ones.r, ru.r], [pb.r])
                p.op("vector", lambda e, sg_=sg_, pb=pb, na=na, h=h: e.tensor_scalar(out=sg_[:], in0=pb[:, 0:128], scalar1=na[:, h:h + 1], scalar2=0.0, op0=ALU.add, op1=ALU.min), [pb.r, na.r], [sg_.r])
                p.op("scalar", lambda e, lt=lt, sg_=sg_: e.activation(out=lt[:], in_=sg_[:], func=AF.Exp), [sg_.r], [lt.r])
                p.op("gpsimd", lambda e, lt=lt: e.tensor_tensor(out=lt[:], in0=lt[:], in1=U[:], op=ALU.mult), [lt.r, U.r], [lt.r])
                p.op("vector", lambda e, lt=lt, cbs=cbs, mb=mb: e.tensor_tensor(out=mb[:], in0=lt[:], in1=cbs[:], op=ALU.mult), [lt.r, cbs.r], [mb.r])
                p.op("tensor", lambda e, mb=mb, dx=dx, h=h: e.matmul(pdiag[:, h * 64:(h + 1) * 64], lhsT=mb[:], rhs=dx[:, h * 64:(h + 1) * 64], start=True, stop=True), [mb.r, dx.r], [pdiag.r])
            p.op("tensor", lambda e, s=s, A=A, hb_cur=hb_cur: e.matmul(poff[:, 0:256], lhsT=A[3][:, s:s + 128], rhs=hb_cur[:], start=True, stop=True), [A[3].r, hb_cur.r], [poff.r])
            p.op("tensor", lambda e, xb_=xb_, xw_=xw_: e.matmul(pst[:, 0:256], lhsT=xb_[:, 256:384], rhs=xw_[:], start=True, stop=True), [xb_.r, xw_.r], [pst.r])
            H3 = H[:].rearrange("p (h q) -> p h q", q=64)
            p.op("vector", lambda e, dc=dc, H3=H3: e.tensor_tensor(out=H3, in0=H3, in1=dc[:].unsqueeze(2).to_broadcast([128, 4, 64]), op=ALU.mult), [H.r, dc.r], [H.r])
            p.op("vector", lambda e: e.tensor_tensor(out=H[:], in0=H[:], in1=pst[:, 0:256], op=ALU.add), [H.r, pst.r], [H.r])
            p.op("scalar", lambda e, hb_nxt=hb_nxt: e.copy(out=hb_nxt[:], in_=H[:]), [H.r], [hb_nxt.r])
            p.op("scalar", lambda e, y1=y1: e.copy(out=y1[:], in_=pdiag[:, 0:256]), [pdiag.r], [y1.r])
            p.op("vector", lambda e, y2=y2, ea=ea, x3=x3: e.tensor_tensor(out=x3(y2), in0=poff[:, 0:256].rearrange("p (h q) -> p h q", q=64),
                                                                        in1=ea[:].unsqueeze(2).to_broadcast([128, 4, 64]), op=ALU.mult), [poff.r, ea.r], [y2.r])
            p.op("gpsimd", lambda e, y1=y1, y2=y2: e.tensor_tensor(out=y2[:], in0=y2[:], in1=y1[:], op=ALU.add), [y1.r, y2.r], [y2.r])
            for k in range(2):
                pb = pbc[k]
                p.op("tensor", lambda e, pb=pb, y2=y2, k=k: e.transpose(out=pb[:, 128:256], in_=y2[:, k * 128:(k + 1) * 128], identity=idf[:]), [y2.r, idf.r], [pb.r])
                if d == 0:
                    p.op("scalar", lambda e, pb=pb, k=k, s=s: e.copy(out=yacc[k][:, s:s + 128], in_=pb[:, 128:256]), [pb.r], [yacc[k].r])
                else:
                    lo = scan_nat_lo(s, 128)
                    p.op("vector", lambda e, pb=pb, k=k, lo=lo: e.tensor_tensor(out=yacc[k][:, lo:lo + 128][:, ::-1], in0=yacc[k][:, lo:lo + 128][:, ::-1], in1=pb[:, 128:256], op=ALU.add),
                         [pb.r, yacc[k].r], [yacc[k].r])
    zt = u
    for k in range(2):
        p.op("vector", lambda e, k=k: e.scalar_tensor_tensor(out=yacc[k][:], in0=arr[0][k][:], scalar=Dp[:, k:k + 1], in1=yacc[k][:], op0=ALU.mult, op1=ALU.add),
             [arr[0][k].r, Dp.r, yacc[k].r], [yacc[k].r])
        p.dma("sync", zt[:], z_d[k * 128:(k + 1) * 128, :], reads=in_regs, writes=[zt.r])
        p.op("scalar", lambda e: e.activation(out=zt[:], in_=zt[:], func=AF.Silu), [zt.r], [zt.r])
        p.op("vector", lambda e, k=k: e.tensor_tensor(out=yacc[k][:], in0=yacc[k][:], in1=zt[:], op=ALU.mult), [yacc[k].r, zt.r], [yacc[k].r])
        xs_write(p, xs, row0 + k * 128, yacc[k], [yacc[k].r], outs)


def emit_b1(kb, xg, xT_d, cT, adaw, adab, wbr_d, wo_d, sn_d, bm_d, xm_d, in_regs, outs, TOK=HT):
    p = kb.p
    banks = kb.banks
    XR = 3072
    nch = (TOK + 511) // 512
    chunks = [(c * 512, min(512, TOK - c * 512)) for c in range(nch)]
    ya = kb.sb("ystg_a", [128, 8192])
    ystg = kb.sub(ya, 0, [128, 16, 512], F32, "ystg")
    mod = emit_mods(kb, adaw, adab, 8, cT, kb.sub(ya, 0, [128, 8, 512], F32, "awstg"), banks[0])
    bm = ld(kb, "bm", [128, 2], bm_d)
    ssdn = ld(kb, "ssdn", [128, 4], sn_d)
    ones = kb.sb("ones", [128, 128])
    p.op("vector", lambda e: e.memset(ones[:], 1.0), [], [ones.r])
    wbb = kb.sb("wbb", [128, 16, 1024], BF16)
    wob = kb.sb("wob", [128, 8, 1024], BF16)
    wst = [kb.sb("wst%d" % i, [128, 1024]) for i in range(2)]
    wbr_r = [Reg("wbr%d" % k) for k in range(16)]
    wo_r = [Reg("wo%d" % k) for k in range(8)]
    for k in range(24):
        st = wst[k % 2]
        src = wbr_d[k * 128:(k + 1) * 128, :] if k < 16 else wo_d[(k - 16) * 128:(k - 15) * 128, :]
        p.dma("sync", st[:], src, writes=[st.r])
        if k < 16:
            p.op("gpsimd", lambda e, st=st, k=k: e.tensor_copy(out=wbb[:, k, :], in_=st[:]), [st.r], [wbr_r[k]])
        else:
            p.op("gpsimd", lambda e, st=st, k=k: e.tensor_copy(out=wob[:, k - 16, :], in_=st[:]), [st.r], [wo_r[k - 16]])
    y2 = [kb.sb("y2_%d" % i, [128, 4, 512]) for i in range(2)]
    yb = kb.sb("yb", [128, 16, 512], BF16)
    xt = kb.sb("xt", [128, 8, 512])
    gts = [kb.sb("gt%d" % i, [128, 4, 512]) for i in range(2)]
    gt2 = [kb.sb("gu%d" % i, [128, 4, 512]) for i in range(2)]
    accs = [kb.sb("acc%d" % i, [128, 512]) for i in range(2)]
    tmps = [kb.sb("tmp%d" % i, [128, 512]) for i in range(2)]
    aT = kb.sb("aT", [128, 8, 512], BF16)
    pz = banks[0:4]
    po = banks[4:6]
    pss = banks[6]
    zc = 0
    gc = 0
    yc = 0

    def xrow(h, ch, r):
        return ((h * 24 + ch) * 2 + r) * 128

    def yrow(kc, h):
        n_, r_, j_ = kc // 4, (kc // 2) % 2, kc % 2
        return xrow(h, n_ * 2 + j_, r_)

    def grow(nb, oc, h):
        return xrow(h, 8 + (nb % 2) * 8 + oc, nb // 2)

    for c, (t0, n) in enumerate(chunks):
        for g4 in range(4):
            yy = y2[yc % 2]
            yc += 1
            for j in range(4):
                r0, r1 = yrow(g4 * 4 + j, 0), yrow(g4 * 4 + j, 1)
                p.dma("sync", ystg[:, g4 * 4 + j, 0:n], xg[r0:r0 + 128, t0:t0 + n], reads=in_regs, writes=[ystg.r])
                p.dma("sync", yy[:, j, 0:n], xg[r1:r1 + 128, t0:t0 + n], reads=in_regs, writes=[yy.r])
            p.op("vector", lambda e, g4=g4, n=n: e.tensor_scalar(out=ystg[:, g4 * 4:g4 * 4 + 4, 0:n], in0=ystg[:, g4 * 4:g4 * 4 + 4, 0:n], scalar1=bm[:, 0:1], scalar2=None, op0=ALU.mult),
                 [ystg.r, bm.r], [ystg.r])
            p.op("vector", lambda e, g4=g4, yy=yy, n=n: e.scalar_tensor_tensor(out=ystg[:, g4 * 4:g4 * 4 + 4, 0:n], in0=yy[:, :, 0:n], scalar=bm[:, 1:2], in1=ystg[:, g4 * 4:g4 * 4 + 4, 0:n],
                                                                             op0=ALU.mult, op1=ALU.add), [ystg.r, yy.r, bm.r], [ystg.r])
        for kc in range(4):
            sq = tmps[kc % 2]
            p.op("scalar", lambda e, sq=sq, kc=kc, n=n: e.activation(out=sq[:, 0:n], in_=ystg[:, kc, 0:n], func=AF.Square), [ystg.r], [sq.r])
            p.op("tensor", lambda e, sq=sq, kc=kc, n=n: e.matmul(pss[:, 0:n], lhsT=ones[:], rhs=sq[:, 0:n], start=(kc == 0), stop=(kc == 3)), [ones.r, sq.r], [pss.r])
        rs = accs[0]
        p.op("vector", lambda e, rs=rs, n=n: e.tensor_scalar(out=rs[:, 0:n], in0=pss[:, 0:n], scalar1=1.0 / 512, scalar2=EPS, op0=ALU.mult, op1=ALU.add), [pss.r], [rs.r])
        p.op("vector", lambda e, rs=rs, n=n: e.reciprocal(out=rs[:, 0:n], in_=rs[:, 0:n]), [rs.r], [rs.r])
        p.op("scalar", lambda e, rs=rs, n=n: e.activation(out=rs[:, 0:n], in_=rs[:, 0:n], func=AF.Sqrt), [rs.r], [rs.r])

        def nrm(e, rs=rs, n=n):
            for kc in range(4):
                last = e.scalar_tensor_tensor(out=ystg[:, kc, 0:n], in0=ystg[:, kc, 0:n], scalar=ssdn[:, kc:kc + 1], in1=rs[:, 0:n], op0=ALU.mult, op1=ALU.mult)
            return last
        p.op("vector", nrm, [ystg.r, ssdn.r, rs.r], [ystg.r])
        p.op("gpsimd", lambda e, n=n: e.tensor_copy(out=yb[:, :, 0:n], in_=ystg[:, :, 0:n]), [ystg.r], [yb.r])
        p.dma("sync", xt[:, :, 0:n], xT_d[:, t0:t0 + n].rearrange("(kc p) t -> p kc t", p=128), reads=in_regs, writes=[xt.r])
        for oc in range(8):
            gt, gu = gts[gc % 2], gt2[gc % 2]
            acc = accs[gc % 2]
            gc += 1
            for nb in range(4):
                r0, r1 = grow(nb, oc, 0), grow(nb, oc, 1)
                p.dma("sync", gt[:, nb, 0:n], xg[r0:r0 + 128, t0:t0 + n], reads=in_regs, writes=[gt.r])
                p.dma("sync", gu[:, nb, 0:n], xg[r1:r1 + 128, t0:t0 + n], reads=in_regs, writes=[gu.r])
            p.op("gpsimd", lambda e, gt=gt, n=n: e.tensor_scalar(out=gt[:, :, 0:n], in0=gt[:, :, 0:n], scalar1=bm[:, 0:1], scalar2=None, op0=ALU.mult), [gt.r, bm.r], [gt.r])
            p.op("vector", lambda e, gt=gt, gu=gu, n=n: e.scalar_tensor_tensor(out=gt[:, :, 0:n], in0=gu[:, :, 0:n], scalar=bm[:, 1:2], in1=gt[:, :, 0:n], op0=ALU.mult, op1=ALU.add),
                 [gt.r, gu.r, bm.r], [gt.r])
            p.op("scalar", lambda e, gt=gt, n=n: e.activation(out=gt[:, :, 0:n], in_=gt[:, :, 0:n], func=AF.Sigmoid), [gt.r], [gt.r])
            for nb in range(4):
                z = pz[zc % 4]
                zc += 1

                def mmz(e, z=z, nb=nb, oc=oc, n=n):
                    for kc in range(4):
                        last = e.matmul(z[:, 0:n], lhsT=wbb[:, nb * 4 + kc, oc * 128:(oc + 1) * 128], rhs=yb[:, nb * 4 + kc, 0:n], start=(kc == 0), stop=(kc == 3))
                    return last
                p.op("tensor", mmz, wbr_r[nb * 4:nb * 4 + 4] + [yb.r], [z.r])
                if nb == 0:
                    p.op("vector", lambda e, z=z, gt=gt, acc=acc, n=n: e.tensor_tensor(out=acc[:, 0:n], in0=z[:, 0:n], in1=gt[:, 0, 0:n], op=ALU.mult), [z.r, gt.r], [acc.r])
                else:
                    tmp = tmps[nb % 2]
                    p.op("vector", lambda e, z=z, gt=gt, tmp=tmp, nb=nb, n=n: e.tensor_tensor(out=tmp[:, 0:n], in0=z[:, 0:n], in1=gt[:, nb, 0:n], op=ALU.mult), [z.r, gt.r], [tmp.r])
                    if nb < 3:
                        p.op("gpsimd", lambda e, tmp=tmp, acc=acc, n=n: e.tensor_tensor(out=acc[:, 0:n], in0=acc[:, 0:n], in1=tmp[:, 0:n], op=ALU.add), [acc.r, tmp.r], [acc.r])
                    else:
                        p.op("gpsimd", lambda e, tmp=tmp, acc=acc, oc=oc, n=n: e.tensor_tensor(out=aT[:, oc, 0:n], in0=acc[:, 0:n], in1=tmp[:, 0:n], op=ALU.add), [acc.r, tmp.r], [aT.r])
        for oc in range(8):
            pq = po[oc % 2]

            def mmo(e, pq=pq, oc=oc, n=n):
                for kc in range(8):
                    last = e.matmul(pq[:, 0:n], lhsT=wob[:, kc, oc * 128:(oc + 1) * 128], rhs=aT[:, kc, 0:n], start=(kc == 0), stop=(kc == 7))
                return last
            p.op("tensor", mmo, wo_r + [aT.r], [pq.r])

            def res(e, pq=pq, oc=oc, t0=t0, n=n):
                for (s, m, j) in tok_ranges(t0, n, 128):
                    last = e.scalar_tensor_tensor(out=xt[:, oc, s - t0:s - t0 + m], in0=pq[:, s - t0:s - t0 + m], scalar=mod[:, oc, j:j + 1],
                                                  in1=xt[:, oc, s - t0:s - t0 + m], op0=ALU.mult, op1=ALU.add)
                return last
            p.op("vector", res, [pq.r, mod.r, xt.r], [xt.r])
        o = Reg("o%d" % c)
        outs.append(o)
        p.dma("gpsimd", xm_d[:, t0:t0 + n].rearrange("(kc p) t -> p kc t", p=128), xt[:, :, 0:n], reads=[xt.r], writes=[o])


def emit_b2(kb, xT_d, cT, adaw, adab, n2, wr_d, br_d, sel_d, ident_d, w1_d, w3_d, w2_d, out_d, in_regs, outs, TOK=HT, NCX=128, NE=16):
    p = kb.p
    banks = kb.banks
    nch = (TOK + 511) // 512
    chunks = [(c * 512, min(512, TOK - c * 512)) for c in range(nch)]
    idf = ld(kb, "idf", [128, 128], ident_d)
    ones = kb.sb("ones", [128, 128])
    p.op("vector", lambda e: e.memset(ones[:], 1.0), [], [ones.r])
    sel = ld(kb, "sel", [16, 16, 128], sel_d)
    wr = kb.sb("wr", [128, 8, 20])
    p.dma("sync", wr[:], wr_d.rearrange("(kc p) c -> p kc c", p=128), writes=[wr.r])
    br = ld(kb, "br", [128, 20], br_d)
    n2t = ld(kb, "n2t", [128, 8], n2)
    arena = kb.sb("b2arena", [128, 18432])
    KEL = 256

    class PV:
        def __init__(s, bank, ap):
            s.t, s.r = ap, bank.r

        def __getitem__(s, k):
            return s.t[k]
    sq = kb.sub(arena, 0, [128, 8, 512], F32, "sq")
    h2f = kb.sub(arena, 16 * KEL, [128, 8, 512], F32, "h2f")
    mod = emit_mods(kb, adaw, adab, 24, cT, sq, banks[0])
    asc = kb.sb("asc", [128, 8, 2])
    p.op("vector", lambda e: e.tensor_scalar(out=asc[:], in0=mod[:, 8:16, :], scalar1=1.0, scalar2=None, op0=ALU.add), [mod.r], [asc.r])
    p.op("vector", lambda e: e.tensor_tensor(out=asc[:], in0=asc[:], in1=n2t[:].unsqueeze(2).to_broadcast([128, 8, 2]), op=ALU.mult), [asc.r, n2t.r], [asc.r])
    xT = kb.sb("xT", [128, 8, TOK])
    xr = [[Reg("x%d_%d" % (oc, c)) for c in range(nch)] for oc in range(8)]
    for oc in range(8):
        p.dma("sync", xT[:, oc, :], xT_d[oc * 128:(oc + 1) * 128, :], reads=in_regs, writes=xr[oc])
    h2T = kb.sb("h2T", [128, 8, TOK], BF16)
    h2r = [Reg("h2_%d" % c) for c in range(nch)]
    wT = kb.sb("wT", [16, TOK])
    wTr = [Reg("wT%d" % c) for c in range(nch)]
    pss = banks[7]
    rstd = kb.sb("rstd", [128, 512])
    plg = [PV(banks[5], banks[5].t[:, 0:20]), PV(banks[6], banks[6].t[:, 0:20])]
    pwt = PV(banks[4], banks[4].t[0:16, 0:128])
    R = lambda n, s: kb.sb(n, s)
    L = R("rL", [128, 20]); mg = R("rmg", [128, 1]); nmg = R("rnmg", [128, 1]); eg = R("reg", [128, 4]); sg = R("rsg", [128, 1])
    gsel = R("rgsel", [128, 4]); tmp = R("rtmp", [128, 4, 4]); lsel = R("rlsel", [128, 4]); me = R("rme", [128, 1]); nme = R("rnme", [128, 1])
    ee = R("ree", [128, 4]); m1 = R("rm1", [128, 1]); msk = R("rmsk", [128, 4]); e2 = R("re2", [128, 4]); m2 = R("rm2", [128, 1])
    tv = R("rtv", [128, 4]); sv = R("rsv", [128, 1]); wg = R("rwg", [128, 4]); gp = R("rgp", [128, 4]); wf = R("rwf", [128, 4, 4])
    for c, (t0, n) in enumerate(chunks):
        def sqf(e, t0=t0, n=n):
            for kc in range(8):
                last = e.activation(out=sq[:, kc, 0:n], in_=xT[:, kc, t0:t0 + n], func=AF.Square)
            return last
        p.op("scalar", sqf, [xr[oc][c] for oc in range(8)], [sq.r])

        def ssum(e, n=n):
            for kc in range(8):
                last = e.matmul(pss[:, 0:n], lhsT=ones[:], rhs=sq[:, kc, 0:n], start=(kc == 0), stop=(kc == 7))
            return last
        p.op("tensor", ssum, [sq.r, ones.r], [pss.r])
        p.op("vector", lambda e, n=n: e.tensor_scalar(out=rstd[:, 0:n], in0=pss[:, 0:n], scalar1=1.0 / 1024, scalar2=EPS, op0=ALU.mult, op1=ALU.add), [pss.r], [rstd.r])
        p.op("vector", lambda e, n=n: e.reciprocal(out=rstd[:, 0:n], in_=rstd[:, 0:n]), [rstd.r], [rstd.r])
        p.op("scalar", lambda e, n=n: e.activation(out=rstd[:, 0:n], in_=rstd[:, 0:n], func=AF.Sqrt), [rstd.r], [rstd.r])

        def nrm(e, t0=t0, n=n):
            for kc in range(8):
                last = e.tensor_tensor(out=h2f[:, kc, 0:n], in0=xT[:, kc, t0:t0 + n], in1=rstd[:, 0:n], op=ALU.mult)
            return last
        p.op("vector", nrm, [xr[oc][c] for oc in range(8)] + [rstd.r], [h2f.r])

        def modf(e, t0=t0, n=n):
            for (s, m, j) in tok_ranges(t0, n, NCX):
                for kc in range(8):
                    last = e.activation(out=h2f[:, kc, s - t0:s - t0 + m], in_=h2f[:, kc, s - t0:s - t0 + m], func=AF.Identity, scale=asc[:, kc, j:j + 1], bias=mod[:, kc, j:j + 1])
            return last
        p.op("scalar", modf, [h2f.r, asc.r, mod.r], [h2f.r])
        p.op("gpsimd", lambda e, t0=t0, n=n: e.tensor_copy(out=h2T[:, :, t0:t0 + n], in_=h2f[:, :, 0:n]), [h2f.r], [h2r[c]])
        for tt in range(n // 128):
            pl = plg[tt % 2]

            def rmm(e, tt=tt, pl=pl):
                for kc in range(8):
                    last = e.matmul(pl[:], lhsT=h2f[:, kc, tt * 128:(tt + 1) * 128], rhs=wr[:, kc, :], start=(kc == 0), stop=(kc == 7))
                return last
            p.op("tensor", rmm, [h2f.r, wr.r], [pl.r])
            V = lambda fn, rd, wrt: p.op("vector", fn, rd, wrt)
            V(lambda e, pl=pl: e.tensor_tensor(out=L[:], in0=pl[:], in1=br[:], op=ALU.add), [pl.r, br.r], [L.r])
            V(lambda e: e.reduce_max(out=mg[:], in_=L[:, 0:4], axis=AX.X), [L.r], [mg.r])
            V(lambda e: e.tensor_scalar(out=nmg[:], in0=mg[:], scalar1=-1.0, scalar2=None, op0=ALU.mult), [mg.r], [nmg.r])
            p.op("scalar", lambda e: e.activation(out=eg[:], in_=L[:, 0:4], func=AF.Exp, bias=nmg[:, 0:1], scale=1.0, accum_out=sg[:]), [L.r, nmg.r], [eg.r, sg.r])
            V(lambda e: e.reciprocal(out=sg[:], in_=sg[:]), [sg.r], [sg.r])
            V(lambda e: e.tensor_scalar(out=gsel[:], in0=L[:, 0:4], scalar1=mg[:, 0:1], scalar2=None, op0=ALU.is_ge), [L.r, mg.r], [gsel.r])
            V(lambda e: e.tensor_tensor(out=tmp[:], in0=L[:, 4:20].rearrange("p (g e) -> p g e", g=4), in1=gsel[:].unsqueeze(2).to_broadcast([128, 4, 4]), op=ALU.mult), [L.r, gsel.r], [tmp.r])
            V(lambda e: e.tensor_reduce(out=lsel[:], in_=tmp[:].rearrange("p g e -> p e g"), axis=AX.X, op=ALU.add), [tmp.r], [lsel.r])
            V(lambda e: e.reduce_max(out=me[:], in_=lsel[:], axis=AX.X), [lsel.r], [me.r])
            V(lambda e: e.tensor_scalar(out=nme[:], in0=me[:], scalar1=-1.0, scalar2=None, op0=ALU.mult), [me.r], [nme.r])
            p.op("scalar", lambda e: e.activation(out=ee[:], in_=lsel[:], func=AF.Exp, bias=nme[:, 0:1], scale=1.0), [lsel.r, nme.r], [ee.r])
            V(lambda e: e.reduce_max(out=m1[:], in_=ee[:], axis=AX.X), [ee.r], [m1.r])
            V(lambda e: e.tensor_scalar(out=msk[:], in0=ee[:], scalar1=m1[:, 0:1], scalar2=-1e9, op0=ALU.is_ge, op1=ALU.mult), [ee.r, m1.r], [msk.r])
            V(lambda e: e.tensor_tensor(out=e2[:], in0=ee[:], in1=msk[:], op=ALU.add), [ee.r, msk.r], [e2.r])
            V(lambda e: e.reduce_max(out=m2[:], in_=e2[:], axis=AX.X), [e2.r], [m2.r])
            V(lambda e: e.tensor_scalar(out=tv[:], in0=ee[:], scalar1=m2[:, 0:1], scalar2=None, op0=ALU.is_ge), [ee.r, m2.r], [tv.r])
            V(lambda e: e.tensor_tensor(out=tv[:], in0=tv[:], in1=ee[:], op=ALU.mult), [tv.r, ee.r], [tv.r])
            V(lambda e: e.reduce_sum(out=sv[:], in_=tv[:], axis=AX.X), [tv.r], [sv.r])
            V(lambda e: e.reciprocal(out=sv[:], in_=sv[:]), [sv.r], [sv.r])
            V(lambda e: e.tensor_scalar(out=wg[:], in0=tv[:], scalar1=sv[:, 0:1], scalar2=None, op0=ALU.mult), [tv.r, sv.r], [wg.r])
            V(lambda e: e.tensor_scalar(out=gp[:], in0=gsel[:], scalar1=sg[:, 0:1], scalar2=None, op0=ALU.mult), [gsel.r, sg.r], [gp.r])
            V(lambda e: e.tensor_tensor(out=wf[:], in0=gp[:].unsqueeze(2).to_broadcast([128, 4, 4]), in1=wg[:].unsqueeze(1).to_broadcast([128, 4, 4]), op=ALU.mult), [gp.r, wg.r], [wf.r])
            p.op("tensor", lambda e: e.transpose(out=pwt[:], in_=wf[:].rearrange("p g e -> p (g e)"), identity=idf[:]), [wf.r, idf.r], [pwt.r])
            V(lambda e, a=t0 + tt * 128: e.tensor_copy(out=wT[:, a:a + 128], in_=pwt[:]), [pwt.r], [wTr[c]])
    p.fence()
    w13 = [kb.sub(arena, i * 16 * KEL, [128, 2, 8, 512], BF16, "w13_%d" % i) for i in range(2)]
    w2b = [kb.sub(arena, (32 + 8 * i) * KEL, [128, 4, 1024], BF16, "w2b_%d" % i) for i in range(2)]
    stA = [kb.sub(arena, (48 + 4 * i) * KEL, [128, 1024], F32, "stA%d" % i) for i in range(3)]
    s1 = [kb.sub(arena, (60 + 2 * i) * KEL, [128, 512], F32, "s1_%d" % i) for i in range(2)]
    aT = [kb.sub(arena, (64 + 4 * i) * KEL, [128, 4, 512], BF16, "aT%d" % i) for i in range(2)]
    ph = banks[0:4]
    pw = banks[4]
    po = banks[5:7]
    war_b = [[Reg("wa%d_%d" % (i, k)) for k in range(8)] for i in range(2)]
    wbr_b = [[Reg("wb%d_%d" % (i, k)) for k in range(4)] for i in range(2)]
    si = 0
    hc = 0
    ac = 0
    oc_cnt = 0
    for ex in range(NE):
        wa, wb = w13[ex % 2], w2b[ex % 2]
        war, wbr = war_b[ex % 2], wbr_b[ex % 2]
        for kc in range(8):
            sA = stA[si % 3]; si += 1
            p.dma("sync", sA[:, 0:512], w1_d[ex, kc * 128:(kc + 1) * 128, :], writes=[sA.r])
            p.dma("sync", sA[:, 512:1024], w3_d[ex, kc * 128:(kc + 1) * 128, :], writes=[sA.r])
            p.op("gpsimd", lambda e, sA=sA, wa=wa, kc=kc: e.tensor_copy(out=wa[:, :, kc, :], in_=sA[:].rearrange("p (a c) -> p a c", a=2)), [sA.r], [war[kc]])
        for fc in range(4):
            sA = stA[si % 3]; si += 1
            p.dma("sync", sA[:], w2_d[ex, fc * 128:(fc + 1) * 128, :], writes=[sA.r])
            p.op("gpsimd", lambda e, sA=sA, wb=wb, fc=fc: e.tensor_copy(out=wb[:, fc, :], in_=sA[:]), [sA.r], [wbr[fc]])
        for c, (t0, n) in enumerate(chunks):
            p.op("tensor", lambda e, ex=ex, t0=t0, n=n: e.matmul(pw[:, 0:n], lhsT=sel[:, ex, :], rhs=wT[:, t0:t0 + n], start=True, stop=True), [sel.r, wTr[c]], [pw.r])
            a = aT[ac % 2]; ac += 1
            for fc in range(4):
                p1, p3 = ph[hc % 4], ph[(hc + 1) % 4]; hc += 2
                ss = s1[fc % 2]

                def mm13(e, p1=p1, p3=p3, wa=wa, fc=fc, t0=t0, n=n):
                    for kc in range(8):
                        e.matmul(p1[:, 0:n], lhsT=wa[:, 0, kc, fc * 128:(fc + 1) * 128], rhs=h2T[:, kc, t0:t0 + n], start=(kc == 0), stop=(kc == 7))
                    for kc in range(8):
                        last = e.matmul(p3[:, 0:n], lhsT=wa[:, 1, kc, fc * 128:(fc + 1) * 128], rhs=h2T[:, kc, t0:t0 + n], start=(kc == 0), stop=(kc == 7))
                    return last
                p.op("tensor", mm13, war + [h2r[c]], [p1.r, p3.r])
                p.op("scalar", lambda e, ss=ss, p1=p1, n=n: e.activation(out=ss[:, 0:n], in_=p1[:, 0:n], func=AF.Silu), [p1.r], [ss.r])
                p.op("vector", lambda e, ss=ss, p3=p3, n=n: e.tensor_tensor(out=ss[:, 0:n], in0=ss[:, 0:n], in1=p3[:, 0:n], op=ALU.mult), [ss.r, p3.r], [ss.r])
                p.op("vector", lambda e, ss=ss, a=a, fc=fc, n=n: e.tensor_tensor(out=a[:, fc, 0:n], in0=ss[:, 0:n], in1=pw[:, 0:n], op=ALU.mult), [ss.r, pw.r], [a.r])
            for oc in range(8):
                pq = po[oc_cnt % 2]; oc_cnt += 1

                def mm2(e, pq=pq, wb=wb, a=a, oc=oc, n=n):
                    for fc in range(4):
                        last = e.matmul(pq[:, 0:n], lhsT=wb[:, fc, oc * 128:(oc + 1) * 128], rhs=a[:, fc, 0:n], start=(fc == 0), stop=(fc == 3))
                    return last
                p.op("tensor", mm2, wbr + [a.r], [pq.r])

                def acc(e, pq=pq, oc=oc, t0=t0, n=n):
                    for (s, m, j) in tok_ranges(t0, n, NCX):
                        last = e.scalar_tensor_tensor(out=xT[:, oc, s:s + m], in0=pq[:, s - t0:s - t0 + m], scalar=mod[:, 16 + oc, j:j + 1], in1=xT[:, oc, s:s + m], op0=ALU.mult, op1=ALU.add)
                    return last
                p.op("vector", acc, [pq.r, mod.r, xr[oc][c]], [xr[oc][c]])
    for oc in range(8):
        o = Reg("o%d" % oc)
        outs.append(o)
        p.dma("gpsimd", out_d[oc * 128:(oc + 1) * 128, :], xT[:, oc, :], reads=xr[oc], writes=[o])


IN_SIZES_ = (512, 1024, 16, 512, 512, 512, 32, 512, 512, 512, 512, 256, 256, 4096)
OFFS_ = np.cumsum([0] + list(IN_SIZES_))
GROUPS = [[0, 1], [2, 3], [4, 5], [6, 7]]


def s1_cols(hf):
    o = OFFS_
    pieces = [("z", o[0] + hf * 256, 256), ("x", o[1] + hf * 256, 256), ("B", o[1] + 512 + hf * 128, 128), ("C", o[1] + 768 + hf * 128, 128),
              ("gq", o[3] + hf * 256, 256), ("gk", o[4] + hf * 256, 256), ("gv", o[5] + hf * 256, 256), ("gr", o[7] + hf * 256, 256),
              ("lx", o[8] + hf * 256, 256), ("lg", o[9] + hf * 256, 256),
              ("aq", o[10] + hf * 256, 256), ("ak", o[11] + hf * 128, 128), ("av", o[12] + hf * 128, 128)]
    cols, rows, r = [], {}, 0
    for nm, st, n in pieces:
        cols.append(np.arange(st, st + n))
        rows[nm] = slice(r, r + n)
        r += n
    cols.append(np.concatenate([o[2] + d * 8 + hf * 4 + np.arange(4) for d in range(2)]))
    rows["dt"] = slice(r, r + 8)
    r += 8
    cols.append(np.arange(o[6], o[6] + 32))
    rows["g1"] = slice(r, r + 32)
    r += 32
    pad = NCC_MIX * 128 - r
    cols.append(np.full(pad, -1))
    r += pad
    cols.append(np.arange(o[13] + hf * 2048, o[13] + (hf + 1) * 2048))
    rows["gate"] = slice(r, r + 2048)
    return np.concatenate(cols), rows


LAYER_SPECS = [("adaw", [1024, 6144]), ("adab", [128, 48]), ("n1", [128, 8]), ("n2", [128, 8]), ("win", [1024, NCC_S1 * 128]),
               ("gq", [128, 1]), ("gk", [128, 1]),
               ("lcw", [128, 2, 4]), ("lcb", [128, 2]), ("lwbd", [128, 8, 128]), ("lbias", [128, 8]), ("llam", [128, 4]),
               ("g2", [16, 2, 256]), ("gb", [128, 4]), ("gn", [128, 2]),
               ("scw", [128, 4, 4]), ("scb", [128, 4]), ("dtb", [4, 2]), ("alog", [4, 2]), ("Dp", [128, 2]),
               ("wbr", [2048, 1024]), ("wo", [1024, 1024]), ("ssdn", [128, 4]),
               ("wr", [1024, 20]), ("br", [128, 20]), ("w1", [16, 1024, 512]), ("w3", [16, 1024, 512]), ("w2", [16, 512, 1024])]
GLOBAL_SPECS = [("xT0", [1024, T]), ("xTm0", [1024, HT]), ("cT", [128, 8, 2]), ("ident", [128, 128]), ("cos", [128, LAT]), ("sin", [128, LAT]),
                ("rm", [128, 128]), ("cm", [128, 512]), ("m2", [128, 128]), ("hm", [128, 2]), ("U", [128, 128]), ("sel", [16, 16, 128]), ("bm", [128, 2])]


def build_fused(NL=2, debug=False, stages=None):
    kb = KB()
    p = kb.p
    G = {n: kb.din(n, s) for n, s in GLOBAL_SPECS}
    W = [{n: kb.din("%s_%d" % (n, l), s) for n, s in LAYER_SPECS} for l in range(NL)]
    pT = kb.scratch("pT", [NCC_MIX * 128, T], debug=debug)
    xs = kb.scratch("xs", [2 * 3072, HT], debug=debug)
    xg = kb.scratch("xg", [2 * 24 * 2 * 128, HT], debug=debug)
    xm = kb.scratch("xm", [1024, HT], debug=debug)
    xnew = kb.scratch("xnew", [1024, HT], debug=debug)
    xall = kb.scratch("xall", [2048, HT], debug=debug)
    oT = kb.dout("oT", [1024, HT])
    outs = []
    _, rows = s1_cols(0)
    for l in range(NL):
        w = W[l]
        if l == 0:
            xsrc = lambda kc, t0, n: [(0, n, G["xT0"][kc * 128:(kc + 1) * 128, t0:t0 + n])]
            xres = G["xTm0"]
        else:
            xsrc = lambda kc, t0, n: [(a - t0, m, xall[(kc * 2 + h) * 128:(kc * 2 + h + 1) * 128, loc:loc + m]) for (a, m, h, loc) in nat_pieces(t0, n)]
            xres = xnew
        emit_s1(kb, xsrc, G["cT"], w["adaw"][:, 0:2048], w["adab"][:, 0:16], w["n1"], w["win"], pT, xs, outs, [Reg("pT%d" % i) for i in range(NCC_MIX)])
        kb.reset()
        emit_ssd(kb, pT[256:768, :], pT[rows["z"], :], pT[rows["dt"], :], w["scw"], w["scb"], w["dtb"], w["alog"], w["Dp"], G["U"], G["ident"], xs, 0, [], outs)
        kb.reset()
        emit_gla(kb, pT[rows["gq"], :], pT[rows["gk"], :], pT[rows["gv"], :], pT[rows["g1"], :], pT[rows["gr"], :], w["g2"], w["gb"], w["gn"],
                 G["cm"], G["m2"], G["hm"], G["ident"], xs, 256, [], outs)
        kb.reset()
        emit_lru(kb, pT[rows["lx"], :], pT[rows["lg"], :], w["lcw"], w["lcb"], w["lwbd"], w["lbias"], w["llam"], xs, 512, [], outs)
        kb.reset()
        emit_att(kb, pT[rows["aq"], :], pT[rows["ak"], :], pT[rows["av"], :], w["gq"], w["gk"], G["cos"], G["sin"], G["rm"], G["ident"], xs, 768, [], outs)
        kb.reset()
        for h in range(2):
            for ch in range(24):
                src = xs[h * 3072 + ch * 128:h * 3072 + (ch + 1) * 128, :]
                dst = xg[((h * 24 + ch) * 2) * 128:((h * 24 + ch) * 2 + 2) * 128, :]
                p.cc(lambda e, src=src, dst=dst: e.collective_compute("AllGather", ALU.bypass, replica_groups=GROUPS, ins=[src], outs=[dst]))
        kb.reset()
        emit_b1(kb, xg, xres, G["cT"], w["adaw"][:, 2048:3072], w["adab"][:, 16:24], w["wbr"], w["wo"], w["ssdn"], G["bm"], xm, [], outs)
        kb.reset()
        emit_b2(kb, xm, G["cT"], w["adaw"][:, 3072:6144], w["adab"][:, 24:48], w["n2"], w["wr"], w["br"], G["sel"], G["ident"], w["w1"], w["w3"], w["w2"],
                oT if l == NL - 1 else xnew, [], outs)
        kb.reset()
        if l < NL - 1:
            for kc in range(8):
                src = xnew[kc * 128:(kc + 1) * 128, :]
                dst = xall[kc * 256:(kc + 1) * 256, :]
                p.cc(lambda e, src=src, dst=dst: e.collective_compute("AllGather", ALU.bypass, replica_groups=GROUPS, ins=[src], outs=[dst]))
            kb.reset()
    return kb.finish(outs)


def core_inputs(inp, b, hf, NL=2):
    c = np.ascontiguousarray
    f32 = np.float32
    d = {}
    x_all = np.concatenate([inp["ctx"][b], inp["x"][b]], 0)
    d["xT0"] = c(x_all.T)
    d["xTm0"] = c(np.concatenate([inp["ctx"][b][hf * 128:(hf + 1) * 128], inp["x"][b][hf * 2048:(hf + 1) * 2048]], 0).T)
    c2 = np.stack([inp["c"][b], inp["c_ctx"]], -1)
    d["cT"] = c(c2.reshape(8, 128, 2).transpose(1, 0, 2))
    d["ident"] = np.eye(128, dtype=f32)
    cos, sin, Rm = rope_tables()
    d["cos"], d["sin"], d["rm"] = cos, sin, Rm
    t = np.arange(512)
    d["cm"] = np.broadcast_to((t % 64 != 0).astype(f32)[None], (128, 512)).copy()
    j = np.arange(128)[:, None]
    i = np.arange(128)[None, :]
    d["m2"] = ((j // 64 == i // 64) & (j <= i)).astype(f32)
    d["hm"] = np.stack([(np.arange(128) < 64), (np.arange(128) >= 64)], 1).astype(f32)
    d["U"] = (j <= i).astype(f32)
    sel = np.zeros((16, 16, 128), f32)
    for e in range(16):
        sel[e, e, :] = 1.0
    d["sel"] = sel
    bm = np.zeros((128, 2), f32)
    bm[:, hf] = 1.0
    d["bm"] = bm
    cols, rows = s1_cols(hf)
    ch = slice(hf * 256, (hf + 1) * 256)
    hs = slice(hf * 4, hf * 4 + 4)
    for l in range(NL):
        L = {}
        L["adaw"] = c(inp["ada_w"][l])
        L["adab"] = fm(inp["ada_b"][l], 48)
        L["n1"] = fm(inp["norm1"][l], 8)
        L["n2"] = fm(inp["norm2"][l], 8)
        win = np.zeros((1024, NCC_S1 * 128), f32)
        ok = cols >= 0
        win[:, np.nonzero(ok)[0]] = inp["w_in"][l][:, cols[ok]]
        L["win"] = win
        L["gq"] = c(inp["att_qnorm"][l][:, None])
        L["gk"] = c(inp["att_knorm"][l][:, None])
        L["lcw"] = c(inp["lru_conv_w"][l][:, ch].reshape(4, 2, 128).transpose(2, 1, 0))
        L["lcb"] = c(inp["lru_conv_b"][l][ch].reshape(2, 128).T)
        wbd = np.zeros((128, 8, 128), f32)
        bias = np.zeros((128, 8), f32)
        lam = np.zeros((128, 4), f32)
        for gi, (wk, bk) in enumerate((("lru_wa", "lru_ba"), ("lru_wx", "lru_bx"))):
            for dd in range(2):
                for cc in range(2):
                    idx = gi * 4 + dd * 2 + cc
                    for jj in range(2):
                        blk = hf * 4 + cc * 2 + jj
                        wbd[jj * 64:(jj + 1) * 64, idx, jj * 64:(jj + 1) * 64] = inp[wk][l][dd, blk]
                    bias[:, idx] = inp[bk][l][dd, ch][cc * 128:(cc + 1) * 128]
        for dd in range(2):
            for cc in range(2):
                lam[:, dd * 2 + cc] = inp["lru_lambda"][l][dd, ch][cc * 128:(cc + 1) * 128]
        L["lwbd"], L["lbias"], L["llam"] = wbd, bias, lam
        L["g2"] = c(inp["gla_g2"][l][:, :, ch].transpose(1, 0, 2))
        L["gb"] = c(np.stack([inp["gla_gb"][l][dd, ch][h * 128:(h + 1) * 128] for h in range(2) for dd in range(2)], 1))
        L["gn"] = c(inp["gla_norm"][l][ch].reshape(2, 128).T)
        chans = np.concatenate([np.arange(hf * 256, hf * 256 + 256), 512 + hf * 128 + np.arange(128), 768 + hf * 128 + np.arange(128)])
        L["scw"] = c(inp["ssd_conv_w"][l][:, chans].reshape(4, 4, 128).transpose(2, 1, 0))
        L["scb"] = c(inp["ssd_conv_b"][l][chans].reshape(4, 128).T)
        L["dtb"] = c(inp["ssd_dt_bias"][l][:, hs].T)
        L["alog"] = c(inp["ssd_a_log"][l][:, hs].T)
        L["Dp"] = c(np.repeat(inp["ssd_d"][l][hs], 64).reshape(2, 128).T)
        L["wbr"] = c(inp["w_branch"][l].reshape(2048, 1024))
        L["wo"] = c(inp["w_out"][l])
        L["ssdn"] = fm(inp["ssd_norm"][l], 4)
        L["wr"] = c(np.concatenate([inp["router_wg"][l], inp["router_we"][l]], 1))
        L["br"] = c(np.broadcast_to(np.concatenate([inp["router_bg"][l], inp["router_be"][l]])[None], (128, 20)))
        L["w1"], L["w3"], L["w2"] = c(inp["exp_w1"][l]), c(inp["exp_w3"][l]), c(inp["exp_w2"][l])
        for k, v in L.items():
            d["%s_%d" % (k, l)] = np.ascontiguousarray(v, dtype=f32)
    return {k: np.ascontiguousarray(v, dtype=f32) for k, v in d.items()}


_PROG = {}


def kernel(**inp):
    inp = {k: np.asarray(v) for k, v in inp.items()}
    NB = inp["x"].shape[0]
    if "fused" not in _PROG:
        _PROG["fused"] = build_fused()
    cores = [(b, hf) for b in range(NB) for hf in range(2)]
    ims = [core_inputs(inp, b, hf) for (b, hf) in cores]
    res = run_bass_kernel_spmd(_PROG["fused"], ims, core_ids=list(range(8))).results
    out = np.stack([np.concatenate([res[2 * b]["oT"][:, 128:].T, res[2 * b + 1]["oT"][:, 128:].T], 0) for b in range(NB)])
    return np.ascontiguousarray(out.astype(np.float32))
```

```python
import numpy as np
import concourse.bass as bass
import concourse.mybir as mybir
from concourse.bass_utils import run_bass_kernel_spmd
from contextlib import ExitStack

F32 = mybir.dt.float32
BF16 = mybir.dt.bfloat16
AF = mybir.ActivationFunctionType
ALU = mybir.AluOpType
AX = mybir.AxisListType


class Reg:
    __slots__ = ("name", "w", "r")

    def __init__(self, name=""):
        self.name = name
        self.w = None
        self.r = []


class Ins:
    __slots__ = ("eng", "fn", "deps", "sig", "idx", "isdma", "slot", "target", "n")

    def __init__(self):
        self.slot = None


class Prog:
    ENG = ["tensor", "vector", "scalar", "gpsimd", "sync"]
    RING = 12

    def __init__(self, nc):
        self.nc = nc
        self.q = {e: [] for e in self.ENG}
        self.n = 0

    def op(self, eng, fn, reads=(), writes=(), dma=False):
        I = Ins()
        I.eng, I.fn, I.isdma, I.sig, I.idx = eng, fn, dma, False, None
        I.n = self.n
        self.n += 1
        deps = {}
        for r in reads:
            if r.w is not None:
                deps.setdefault(id(r.w), [r.w, set()])[1].add("raw")
        for w in writes:
            if w.w is not None:
                deps.setdefault(id(w.w), [w.w, set()])[1].add("waw")
            for x in w.r:
                deps.setdefault(id(x), [x, set()])[1].add("war")
        final = []
        for J, kinds in deps.values():
            if J is I:
                continue
            if J.eng == eng and not J.isdma and not dma:
                if "raw" not in kinds or eng == "tensor":
                    continue
            final.append(J)
            J.sig = True
        I.deps = final
        for r in reads:
            r.r.append(I)
        for w in writes:
            w.w = I
            w.r = []
        self.q[eng].append(I)
        return I

    def fence(self):
        self.nf = getattr(self, "nf", 0) + 1
        for e in self.ENG:
            last = None
            for I in reversed(self.q[e]):
                if I.fn == "FENCE":
                    break
                if not I.isdma and I.fn is not None:
                    last = I
                    break
            if last is not None:
                last.sig = True
            I = Ins()
            I.eng, I.fn, I.isdma, I.sig, I.idx, I.deps = e, "FENCE", False, False, None, [last] if last is not None else []
            I.n = self.nf
            self.q[e].append(I)

    def dma(self, eng, out, in_, reads=(), writes=(), **kw):
        return self.op(eng, lambda e: e.dma_start(out=out, in_=in_, **kw), reads, writes, dma=True)

    def cc(self, fn, reads=(), writes=(), blocking=True):
        I = self.op("gpsimd", fn, reads, writes, dma=True)
        I.slot = "cc" if blocking else "ccnb"
        self.ccs = getattr(self, "ccs", []) + [I]
        return I

    def cc_wait_all(self):
        I = Ins()
        I.eng, I.fn, I.isdma, I.sig, I.idx, I.deps = "gpsimd", "CCWAIT", False, False, None, []
        I.n = len(getattr(self, "ccs", []))
        self.q["gpsimd"].append(I)

    def emit(self, final_regs=()):
        nc = self.nc
        self.op("sync", None, reads=list(final_regs))
        sems = {}
        stack = []
        for e in self.ENG:
            cm = nc.semaphore("S_" + e)
            sems[e] = cm.__enter__()
            stack.append(cm)
        cmf = nc.semaphore("S_fence")
        fsem = cmf.__enter__()
        stack.append(cmf)
        rings = {}
        for e in ("sync", "gpsimd", "scalar"):
            rings[e] = []
            for k in range(self.RING):
                cm = nc.semaphore("D_%s_%d" % (e, k))
                rings[e].append(cm.__enter__())
                stack.append(cm)
        ccsem = {}
        cctgt = {}
        if getattr(self, "ccs", []):
            cm = nc.semaphore("C_all")
            csem = cm.__enter__()
            stack.append(cm)
            for k, I in enumerate(self.ccs):
                ccsem[id(I)] = csem
                cctgt[id(I)] = k + 1
        for e in self.ENG:
            cnt = 0
            dcnt = 0
            for I in self.q[e]:
                if I.isdma and getattr(I, "slot", None) in ("cc", "ccnb"):
                    I.target = 1
                elif I.isdma:
                    I.slot = dcnt % self.RING
                    I.target = 16 * (dcnt // self.RING + 1)
                    dcnt += 1
                elif I.sig:
                    cnt += 1
                    I.idx = cnt
        self.counts = {e: len(self.q[e]) for e in self.ENG}

        def run(e, eng):
            seen = {}
            prev = {}

            def wait(key, sem, val):
                if seen.get(key, 0) < val:
                    eng.wait_ge(sem, val)
                    seen[key] = val

            for I in self.q[e]:
                mx = {}
                for J in I.deps:
                    if J.isdma and J.slot in ("cc", "ccnb"):
                        wait(("cc",), ccsem[id(J)], cctgt[id(J)])
                    elif J.isdma:
                        wait((J.eng, J.slot), rings[J.eng][J.slot], J.target)
                    else:
                        mx[J.eng] = max(mx.get(J.eng, 0), J.idx)
                for f, v in mx.items():
                    wait(f, sems[f], v)
                if I.isdma and I.slot in ("cc", "ccnb"):
                    inst = I.fn(eng)
                    inst.then_inc(ccsem[id(I)], 1)
                    if I.slot == "cc":
                        wait(("cc",), ccsem[id(I)], cctgt[id(I)])
                    continue
                if I.fn == "CCWAIT":
                    if I.n > 0:
                        wait(("cc",), csem, I.n)
                    continue
                if I.isdma:
                    p = prev.get(I.slot)
                    if p is not None:
                        wait((e, I.slot), rings[e][I.slot], p.target)
                    prev[I.slot] = I
                if I.fn is None:
                    continue
                if I.fn == "FENCE":
                    for s, pq in prev.items():
                        wait((e, s), rings[e][s], pq.target)
                    eng.sem_inc(fsem, 1)
                    eng.wait_ge(fsem, len(self.ENG) * I.n)
                    continue
                inst = I.fn(eng)
                if I.isdma:
                    inst.then_inc(rings[e][I.slot], 16)
                elif I.sig:
                    inst.then_inc(sems[e], 1)
            if e in rings:
                for s, p in prev.items():
                    wait((e, s), rings[e][s], p.target)

        with nc.Block() as block:
            @block.sync
            def _(eng):
                run("sync", eng)

            @block.tensor
            def _(eng):
                run("tensor", eng)

            @block.vector
            def _(eng):
                run("vector", eng)

            @block.scalar
            def _(eng):
                run("scalar", eng)

            @block.gpsimd
            def _(eng):
                run("gpsimd", eng)
        for cm in reversed(stack):
            cm.__exit__(None, None, None)


EPS = 1e-6
THETA = 10000.0
NCTX, LAT = 256, 4096
T = NCTX + LAT
HT = T // 2
NCC_MIX = 23
NCC_S1 = 39
ARENA_F32 = 50176


class Tile:
    __slots__ = ("t", "r")

    def __init__(self, t, name):
        self.t = t
        self.r = Reg(name)

    def __getitem__(self, k):
        return self.t[k]


class KB:
    def __init__(self):
        self.nc = bass.Bass("TRN2", target_bir_lowering=False)
        self.es = ExitStack()
        self.p = Prog(self.nc)
        self.es.enter_context(self.nc.allow_low_precision("bf16 matmul operands, fp32 accumulation"))
        self.arena = self.es.enter_context(self.nc.sbuf_tensor("arena", [128, ARENA_F32], F32))
        self.banks = [Tile(self.es.enter_context(self.nc.psum_tensor("bank%d" % i, [128, 512], F32)), "bank%d" % i) for i in range(8)]
        self.off = 0
        self.nscr = 0

    def din(self, name, shape, dt=F32):
        return self.nc.dram_tensor(name, list(shape), dt, kind="ExternalInput").ap()

    def dout(self, name, shape, dt=F32):
        return self.nc.dram_tensor(name, list(shape), dt, kind="ExternalOutput").ap()

    def scratch(self, name, shape, dt=F32, debug=False):
        if debug:
            return self.dout(name, shape, dt)
        return self.nc.dram_tensor(name, list(shape), dt).ap()

    def sb(self, name, shape, dt=F32):
        esize = 4 if dt == F32 else 2
        nel = 1
        for d in shape[1:]:
            nel *= d
        nbytes = (nel * esize + 31) // 32 * 32
        assert self.off + nbytes <= ARENA_F32 * 4, ("SBUF arena overflow", name, self.off, nbytes)
        ap = self.arena[0:shape[0], self.off // 4:(self.off + nbytes) // 4]
        if dt != F32:
            ap = ap.bitcast(dt)
        ap = ap[:, 0:nel]
        if len(shape) > 2:
            names = ["d%d" % k for k in range(len(shape) - 1)]
            ap = ap.rearrange("p (%s) -> p %s" % (" ".join(names), " ".join(names)), **{n: shape[k + 1] for k, n in enumerate(names[:-1])})
        self.off += nbytes
        return Tile(ap, name)

    def sub(self, tile, off_el, shape, dt=F32, name="v"):
        esize = 4 if dt == F32 else 2
        nel = 1
        for d in shape[1:]:
            nel *= d
        ap = tile.t[0:shape[0], off_el:off_el + nel * esize // 4]
        if dt != F32:
            ap = ap.bitcast(dt)
        if len(shape) > 2:
            names = ["d%d" % k for k in range(len(shape) - 1)]
            ap = ap.rearrange("p (%s) -> p %s" % (" ".join(names), " ".join(names)), **{n: shape[k + 1] for k, n in enumerate(names[:-1])})
        return Tile(ap, name)

    def psbf(self, bank):
        t = Tile(bank.t[:, 0:512].bitcast(BF16), "bf")
        t.r = bank.r
        return t

    def reset(self):
        self.p.fence()
        self.off = 0

    def finish(self, final_regs):
        self.p.emit(final_regs)
        self.es.close()
        return self.nc


def fm(v, n):
    return np.ascontiguousarray(np.asarray(v).reshape(n, 128).T)


def tok_ranges(t0, n, nctx):
    out = []
    if t0 < nctx:
        m = min(n, nctx - t0)
        out.append((t0, m, 1))
        if n > m:
            out.append((t0 + m, n - m, 0))
    else:
        out.append((t0, n, 0))
    return out


def ld(kb, name, shape, src, dt=F32):
    t = kb.sb(name, shape, dt)
    kb.p.dma("sync", t[:], src, writes=[t.r])
    return t


def consts(kb, ident_d, want_bf=True):
    p = kb.p
    idf = ld(kb, "idf", [128, 128], ident_d)
    ones = kb.sb("ones", [128, 128])
    p.op("vector", lambda e: e.memset(ones[:], 1.0), [], [ones.r])
    idb = None
    if want_bf:
        idb = kb.sb("idb", [128, 128], BF16)
        p.op("vector", lambda e: e.tensor_copy(out=idb[:], in_=idf[:]), [idf.r], [idb.r])
    return idf, idb, ones


def emit_mods(kb, adaw, adab, ncc, cT, aw, psm, name="m"):
    p = kb.p
    ct = kb.sb(name + "ct", [128, 8, 2])
    st = kb.sb(name + "st", [128, 8, 2])
    p.dma("sync", ct[:], cT, writes=[ct.r])
    p.op("scalar", lambda e: e.activation(out=st[:], in_=ct[:], func=AF.Silu), [ct.r], [st.r])
    ab = kb.sb(name + "ab", [128, ncc])
    p.dma("sync", ab[:], adab, writes=[ab.r])
    mod = kb.sb(name + "mod", [128, ncc, 2])
    for g in range(ncc // 4):
        awr = [Reg("aw%d" % k) for k in range(8)]
        for kc in range(8):
            p.dma("sync", aw[:, kc, :], adaw[kc * 128:(kc + 1) * 128, g * 512:(g + 1) * 512], reads=[], writes=[awr[kc], aw.r])

        def mm_mod(e, g=g):
            for cc in range(4):
                for kc in range(8):
                    last = e.matmul(psm[:, 2 * (g * 4 + cc):2 * (g * 4 + cc) + 2], lhsT=aw[:, kc, cc * 128:(cc + 1) * 128], rhs=st[:, kc, :],
                                    start=(kc == 0), stop=(kc == 7))
            return last
        p.op("tensor", mm_mod, [st.r, aw.r] + awr, [psm.r])
    p.op("vector", lambda e: e.tensor_tensor(out=mod[:], in0=psm[:, 0:2 * ncc].rearrange("p (c j) -> p c j", j=2),
                                             in1=ab[:].unsqueeze(2).to_broadcast([128, ncc, 2]), op=ALU.add), [psm.r, ab.r], [mod.r])
    return mod


def nat_pieces(t0, n):
    out = []
    bounds = [(0, 128, 0, 0), (128, 256, 1, 0), (256, 256 + 2048, 0, 128), (256 + 2048, T, 1, 128)]
    for (a, b, h, loc) in bounds:
        lo, hi = max(a, t0), min(b, t0 + n)
        if lo < hi:
            out.append((lo, hi - lo, h, loc + lo - a))
    return out


def xs_write(p, xs, row0, tile, reads, outs, eng="gpsimd"):
    for (a, n, h, loc) in nat_pieces(0, T):
        o = Reg("o")
        outs.append(o)
        p.dma(eng, xs[h * 3072 + row0:h * 3072 + row0 + 128, loc:loc + n], tile[:, a:a + n], reads=reads, writes=[o])


def emit_s1(kb, xsrc, cT, adaw, adab, n1, win, pT, xs, xs_regs, pT_regs):
    p = kb.p
    banks = kb.banks
    aw = kb.sb("aw", [128, 8, 512])
    mod = emit_mods(kb, adaw, adab, 16, cT, aw, banks[0])
    n1t = ld(kb, "n1t", [128, 8], n1)
    asc = kb.sb("asc", [128, 8, 2])
    p.op("vector", lambda e: e.tensor_scalar(out=asc[:], in0=mod[:, 8:16, :], scalar1=1.0, scalar2=None, op0=ALU.add), [mod.r], [asc.r])
    p.op("vector", lambda e: e.tensor_tensor(out=asc[:], in0=asc[:], in1=n1t[:].unsqueeze(2).to_broadcast([128, 8, 2]), op=ALU.mult), [asc.r, n1t.r], [asc.r])
    ones = kb.sb("ones", [128, 128])
    p.op("vector", lambda e: e.memset(ones[:], 1.0), [], [ones.r])
    nch = (T + 511) // 512
    chunks = [(c * 512, min(512, T - c * 512)) for c in range(nch)]
    hT = kb.sb("hT", [128, 8, T], BF16)
    hr = [Reg("h%d" % c) for c in range(nch)]
    xq = [kb.sb("xq%d" % i, [128, 8, 512]) for i in range(2)]
    sq = [kb.sb("sq%d" % i, [128, 512]) for i in range(2)]
    rstd = kb.sb("rstd", [128, 512])
    tmp = [kb.sb("tmp%d" % i, [128, 512]) for i in range(2)]
    pss = banks[1]
    for c, (t0, n) in enumerate(chunks):
        xc = xq[c % 2]
        for kc in range(8):
            for (off, ln, src) in xsrc(kc, t0, n):
                p.dma("sync", xc[:, kc, off:off + ln], src, writes=[xc.r])
        for kc in range(8):
            s_ = sq[kc % 2]
            p.op("scalar", lambda e, s_=s_, xc=xc, kc=kc, n=n: e.activation(out=s_[:, 0:n], in_=xc[:, kc, 0:n], func=AF.Square), [xc.r], [s_.r])
            p.op("tensor", lambda e, s_=s_, kc=kc, n=n: e.matmul(pss[:, 0:n], lhsT=ones[:], rhs=s_[:, 0:n], start=(kc == 0), stop=(kc == 7)), [ones.r, s_.r], [pss.r])
        p.op("vector", lambda e, n=n: e.tensor_scalar(out=rstd[:, 0:n], in0=pss[:, 0:n], scalar1=1.0 / 1024, scalar2=EPS, op0=ALU.mult, op1=ALU.add), [pss.r], [rstd.r])
        p.op("vector", lambda e, n=n: e.reciprocal(out=rstd[:, 0:n], in_=rstd[:, 0:n]), [rstd.r], [rstd.r])
        p.op("scalar", lambda e, n=n: e.activation(out=rstd[:, 0:n], in_=rstd[:, 0:n], func=AF.Sqrt), [rstd.r], [rstd.r])
        for kc in range(8):
            tm = tmp[kc % 2]
            p.op("vector", lambda e, tm=tm, xc=xc, kc=kc, n=n: e.tensor_tensor(out=tm[:, 0:n], in0=xc[:, kc, 0:n], in1=rstd[:, 0:n], op=ALU.mult), [xc.r, rstd.r], [tm.r])

            def modf(e, tm=tm, kc=kc, t0=t0, n=n):
                for (s, m, j) in tok_ranges(t0, n, NCTX):
                    last = e.activation(out=hT[:, kc, s:s + m], in_=tm[:, s - t0:s - t0 + m], func=AF.Identity, scale=asc[:, kc, j:j + 1], bias=mod[:, kc, j:j + 1])
                return last
            p.op("scalar", modf, [tm.r, asc.r, mod.r], [hr[c]])
    wfs = [kb.sb("wf%d" % i, [128, 8, 128]) for i in range(2)]
    wbs = [kb.sb("wb%d" % i, [128, 8, 128], BF16) for i in range(2)]
    stg = [kb.sb("stg%d" % i, [128, T]) for i in range(2)]
    sgr = [[Reg("sg%d_%d" % (i, c)) for c in range(nch)] for i in range(2)]
    pps = banks[2:6]
    cnt = 0
    for cc in range(NCC_S1):
        wf, wb, sg = wfs[cc % 2], wbs[cc % 2], stg[cc % 2]
        p.dma("sync", wf[:], win[:, cc * 128:(cc + 1) * 128].rearrange("(kc p) c -> p kc c", p=128), writes=[wf.r])
        p.op("gpsimd", lambda e, wf=wf, wb=wb: e.tensor_copy(out=wb[:], in_=wf[:]), [wf.r], [wb.r])
        for tcn, (t0, n) in enumerate(chunks):
            pp = pps[cnt % 4]

            def mm(e, pp=pp, wb=wb, t0=t0, n=n):
                for kc in range(8):
                    last = e.matmul(pp[:, 0:n], lhsT=wb[:, kc, :], rhs=hT[:, kc, t0:t0 + n], start=(kc == 0), stop=(kc == 7))
                return last
            p.op("tensor", mm, [wb.r, hr[tcn]], [pp.r])
            if cnt % 2 == 0:
                p.op("vector", lambda e, pp=pp, sg=sg, t0=t0, n=n: e.tensor_copy(out=sg[:, t0:t0 + n], in_=pp[:, 0:n]), [pp.r], [sgr[cc % 2][tcn]])
            else:
                p.op("scalar", lambda e, pp=pp, sg=sg, t0=t0, n=n: e.copy(out=sg[:, t0:t0 + n], in_=pp[:, 0:n]), [pp.r], [sgr[cc % 2][tcn]])
            cnt += 1
        if cc < NCC_MIX:
            p.dma("gpsimd", pT[cc * 128:(cc + 1) * 128, :], sg[:], reads=sgr[cc % 2], writes=[pT_regs[cc]])
        else:
            xs_write(p, xs, 1024 + (cc - NCC_MIX) * 128, sg, sgr[cc % 2], xs_regs)


def rope_tables(L=4096, W=64):
    t = np.arange(L)
    pos = np.stack([t // W, t % W], 0).astype(np.float32)
    d = np.arange(128)
    half = d // 64
    j = d % 32
    inv = (THETA ** (-(j.astype(np.float32)) / 32.0)).astype(np.float32)
    ang = pos[half] * inv[:, None]
    cos = np.cos(ang).astype(np.float32)
    sin = np.sin(ang).astype(np.float32)
    sgn = np.where((d % 64) < 32, -1.0, 1.0).astype(np.float32)
    Rm = np.zeros((128, 128), np.float32)
    partner = np.where((d % 64) < 32, d + 32, d - 32)
    Rm[partner, d] = 1.0
    return cos, (sin * sgn[:, None]).astype(np.float32), Rm


def emit_att(kb, qT_d, kT_d, vT_d, gq_d, gk_d, cos_d, sin_d, rm_d, ident_d, xs, row0, in_regs, outs):
    p = kb.p
    banks = kb.banks
    L = LAT
    NKT = T // 128
    idf, idb, ones = consts(kb, ident_d)
    onesb = kb.sb("onesb", [128, 128], BF16)
    p.op("vector", lambda e: e.tensor_copy(out=onesb[:], in_=ones[:]), [ones.r], [onesb.r])
    rm = ld(kb, "rm", [128, 128], rm_d)
    gq = ld(kb, "gq", [128, 1], gq_d)
    gk = ld(kb, "gk", [128, 1], gk_d)
    cos = ld(kb, "cos", [128, L], cos_d)
    sin = ld(kb, "sin", [128, L], sin_d)
    xst = [kb.sb("xst%d" % i, [128, T]) for i in range(2)]
    vb = kb.sb("vb", [128, NKT, 128], BF16)
    vtb = kb.sb("vtb", [128, T], BF16)
    p.dma("sync", xst[1][:], vT_d, reads=in_regs, writes=[xst[1].r])
    p.op("gpsimd", lambda e: e.tensor_copy(out=vtb[:], in_=xst[1][:]), [xst[1].r], [vtb.r])
    ptr = kb.psbf(banks[1])
    for g in range((NKT + 7) // 8):
        k0, k1 = g * 8, min(NKT, g * 8 + 8)

        def trv(e, k0=k0, k1=k1):
            for kt in range(k0, k1):
                last = e.transpose(out=ptr.t[:, (kt - k0) * 128:(kt - k0 + 1) * 128], in_=vtb[:, kt * 128:(kt + 1) * 128], identity=idb[:])
            return last
        p.op("tensor", trv, [vtb.r, idb.r], [ptr.r])
        p.op("scalar", lambda e, k0=k0, k1=k1: e.copy(out=vb[:, k0:k1, :], in_=ptr.t[:, 0:(k1 - k0) * 128].rearrange("p (a b) -> p a b", b=128)), [ptr.r], [vb.r])

    chunks = [(0, NCTX, False)] + [(NCTX + c * 512, 512, True) for c in range(L // 512)]
    knT = kb.sb("knT", [128, T], BF16)
    qnT = [kb.sb("qnT%d" % i, [128, T], BF16) for i in range(2)]
    sqt = kb.sb("sqt", [128, 512])
    rstd = kb.sb("rstd", [128, 512])
    xg = kb.sb("xg", [128, 512])
    t1 = kb.sb("t1", [128, 512])
    t2 = kb.sb("t2", [128, 512])
    pss, prot = banks[0], banks[1]
    srcs = [(kT_d, gk, knT), (qT_d[0:128, :], gq, qnT[0]), (qT_d[128:256, :], gq, qnT[1])]
    dregs = []
    for si, (src, g, dst) in enumerate(srcs):
        xs_ = xst[si % 2]
        p.dma("sync", xs_[:], src, reads=in_regs, writes=[xs_.r])
        dr = [Reg("d%d_%d" % (si, c)) for c in range(len(chunks))]
        dregs.append(dr)
        for c, (t0, n, lat) in enumerate(chunks):
            p.op("scalar", lambda e, xs_=xs_, t0=t0, n=n: e.activation(out=sqt[:, 0:n], in_=xs_[:, t0:t0 + n], func=AF.Square), [xs_.r], [sqt.r])
            p.op("tensor", lambda e, n=n: e.matmul(pss[:, 0:n], lhsT=ones[:], rhs=sqt[:, 0:n], start=True, stop=True), [ones.r, sqt.r], [pss.r])
            p.op("vector", lambda e, n=n: e.tensor_scalar(out=rstd[:, 0:n], in0=pss[:, 0:n], scalar1=1.0 / 128, scalar2=EPS, op0=ALU.mult, op1=ALU.add), [pss.r], [rstd.r])
            p.op("vector", lambda e, n=n: e.reciprocal(out=rstd[:, 0:n], in_=rstd[:, 0:n]), [rstd.r], [rstd.r])
            p.op("scalar", lambda e, n=n: e.activation(out=rstd[:, 0:n], in_=rstd[:, 0:n], func=AF.Sqrt), [rstd.r], [rstd.r])
            p.op("vector", lambda e, xs_=xs_, g=g, t0=t0, n=n: e.tensor_scalar(out=xg[:, 0:n], in0=xs_[:, t0:t0 + n], scalar1=g[:, 0:1], scalar2=None, op0=ALU.mult), [xs_.r, g.r], [xg.r])
            if lat:
                l0 = t0 - NCTX
                p.op("tensor", lambda e, n=n: e.matmul(prot[:, 0:n], lhsT=rm[:], rhs=xg[:, 0:n], start=True, stop=True), [rm.r, xg.r], [prot.r])
                p.op("gpsimd", lambda e, l0=l0, n=n: e.tensor_tensor(out=t1[:, 0:n], in0=xg[:, 0:n], in1=cos[:, l0:l0 + n], op=ALU.mult), [xg.r, cos.r], [t1.r])
                p.op("vector", lambda e, l0=l0, n=n: e.tensor_tensor(out=t2[:, 0:n], in0=prot[:, 0:n], in1=sin[:, l0:l0 + n], op=ALU.mult), [prot.r, sin.r], [t2.r])
                p.op("gpsimd", lambda e, n=n: e.tensor_tensor(out=t1[:, 0:n], in0=t1[:, 0:n], in1=t2[:, 0:n], op=ALU.add), [t1.r, t2.r], [t1.r])
                p.op("vector", lambda e, dst=dst, t0=t0, n=n: e.tensor_tensor(out=dst[:, t0:t0 + n], in0=t1[:, 0:n], in1=rstd[:, 0:n], op=ALU.mult), [t1.r, rstd.r], [dr[c]])
            else:
                p.op("vector", lambda e, dst=dst, t0=t0, n=n: e.tensor_tensor(out=dst[:, t0:t0 + n], in0=xg[:, 0:n], in1=rstd[:, 0:n], op=ALU.mult), [xg.r, rstd.r], [dr[c]])
    kr, qr = dregs[0], dregs[1:]
    pS = banks[2:5]
    pO = banks[5:7]
    pD = [banks[7], banks[0]]
    pts = [kb.sb("pt%d" % i, [128, 512], BF16) for i in range(3)]
    rden = [kb.sb("rden%d" % i, [128, 512]) for i in range(2)]
    yo = [kb.sb("yo%d" % i, [128, 512]) for i in range(2)]
    scale = 128.0 ** -0.5
    sc = 0
    blk = 0
    for h in range(2):
        for c, (t0, n, lat) in enumerate(chunks):
            nkt = NKT if lat else NCTX // 128
            po, pd = pO[blk % 2], pD[blk % 2]
            def s_exp(kt, sc_):
                ps_, pt = pS[sc_ % 3], pts[sc_ % 3]
                kc = 0 if kt < NCTX // 128 else 1 + (kt * 128 - NCTX) // 512
                p.op("tensor", lambda e, ps_=ps_, kt=kt, h=h, t0=t0, n=n: e.matmul(ps_[:, 0:n], lhsT=knT[:, kt * 128:(kt + 1) * 128], rhs=qnT[h][:, t0:t0 + n], start=True, stop=True),
                     [kr[kc], qr[h][c]], [ps_.r])
                p.op("scalar", lambda e, ps_=ps_, pt=pt, n=n: e.activation(out=pt[:, 0:n], in_=ps_[:, 0:n], func=AF.Exp, scale=scale), [ps_.r], [pt.r])
            s_exp(0, sc)
            for kt in range(nkt):
                pt = pts[sc % 3]
                if kt + 1 < nkt:
                    s_exp(kt + 1, sc + 1)
                sc += 1

                def pv(e, po=po, pd=pd, pt=pt, kt=kt, n=n, nkt=nkt):
                    e.matmul(po[:, 0:n], lhsT=vb[:, kt, :], rhs=pt[:, 0:n], start=(kt == 0), stop=(kt == nkt - 1))
                    return e.matmul(pd[:, 0:n], lhsT=onesb[:], rhs=pt[:, 0:n], start=(kt == 0), stop=(kt == nkt - 1))
                p.op("tensor", pv, [vb.r, onesb.r, pt.r], [po.r, pd.r])
            rd, y = rden[blk % 2], yo[blk % 2]
            p.op("vector", lambda e, rd=rd, pd=pd, n=n: e.reciprocal(out=rd[:, 0:n], in_=pd[:, 0:n]), [pd.r], [rd.r])
            p.op("vector", lambda e, rd=rd, po=po, y=y, n=n: e.tensor_tensor(out=y[:, 0:n], in0=po[:, 0:n], in1=rd[:, 0:n], op=ALU.mult), [po.r, rd.r], [y.r])
            for (a, m, hh, loc) in nat_pieces(t0, n):
                o = Reg("o")
                outs.append(o)
                p.dma("gpsimd", xs[hh * 3072 + row0 + h * 128:hh * 3072 + row0 + (h + 1) * 128, loc:loc + m], y[:, a - t0:a - t0 + m], reads=[y.r], writes=[o])
            blk += 1


def emit_conv(kb, p, x, u, w, b, segs, cc, eng_extra="vector"):
    p.op("scalar", lambda e: e.activation(out=u[:], in_=x[:], func=AF.Identity, scale=w[:, cc, 2:3], bias=b[:, cc:cc + 1]), [x.r, w.r, b.r], [u.r])

    def taps(e):
        for (s0, n) in segs:
            for k, off in ((0, -2), (1, -1), (3, 1)):
                lo = max(0, -off)
                hi = n - max(0, off)
                last = e.scalar_tensor_tensor(out=u[:, s0 + lo:s0 + hi], in0=x[:, s0 + lo + off:s0 + hi + off], scalar=w[:, cc, k:k + 1],
                                              in1=u[:, s0 + lo:s0 + hi], op0=ALU.mult, op1=ALU.add)
        return last
    p.op(eng_extra, taps, [x.r, u.r, w.r], [u.r])


def emit_lru(kb, xT_d, gT_d, cw_d, cb_d, wbd_d, bias_d, lam_d, xs, row0, in_regs, outs):
    p = kb.p
    banks = kb.banks
    L = LAT
    cw = ld(kb, "cw", [128, 2, 4], cw_d)
    cb = ld(kb, "cb", [128, 2], cb_d)
    wbd = ld(kb, "wbd", [128, 8, 128], wbd_d)
    bias = ld(kb, "bias", [128, 8], bias_d)
    lam = ld(kb, "lam", [128, 4], lam_d)
    cl = kb.sb("cl", [128, 4])
    p.op("scalar", lambda e: e.activation(out=cl[:], in_=lam[:], func=AF.Exp, scale=-1.0), [lam.r], [cl.r])
    p.op("scalar", lambda e: e.activation(out=cl[:], in_=cl[:], func=AF.Ln, bias=1.0, scale=1.0), [cl.r], [cl.r])
    p.op("vector", lambda e: e.tensor_scalar(out=cl[:], in0=cl[:], scalar1=-8.0, scalar2=None, op0=ALU.mult), [cl.r], [cl.r])
    x = kb.sb("x", [128, T]); u = kb.sb("u", [128, T]); g = kb.sb("g", [128, T])
    av = [[kb.sb("a%d" % d, [128, T]), kb.sb("v%d" % d, [128, T])] for d in range(2)]
    hh = [kb.sb("h%d" % d, [128, T]) for d in range(2)]
    rt = [kb.sb("rt%d" % i, [128, 512]) for i in range(2)]
    it_ = [kb.sb("it%d" % i, [128, 512]) for i in range(2)]
    s2 = [kb.sb("s2%d" % i, [128, 512]) for i in range(2)]
    segs = [(0, NCTX), (NCTX, L)]
    chunks = [(0, NCTX)] + [(NCTX + c * 512, 512) for c in range(L // 512)]
    bc = 0
    for cc in range(2):
        p.dma("sync", x[:], xT_d[cc * 128:(cc + 1) * 128, :], reads=in_regs, writes=[x.r])
        p.dma("sync", g[:], gT_d[cc * 128:(cc + 1) * 128, :], reads=in_regs, writes=[g.r])
        emit_conv(kb, p, x, u, cw, cb, segs, cc)
        for d in range(2):
            a, v = av[d]
            for c, (t0, n) in enumerate(chunks):
                pr, pi = banks[bc % 8], banks[(bc + 1) % 8]
                bc += 2
                r_, i_, s_ = rt[c % 2], it_[c % 2], s2[c % 2]
                ia, ix = 0 * 4 + d * 2 + cc, 1 * 4 + d * 2 + cc
                p.op("tensor", lambda e, pr=pr, ia=ia, t0=t0, n=n: e.matmul(pr[:, 0:n], lhsT=wbd[:, ia, :], rhs=u[:, t0:t0 + n], start=True, stop=True), [wbd.r, u.r], [pr.r])
                p.op("tensor", lambda e, pi=pi, ix=ix, t0=t0, n=n: e.matmul(pi[:, 0:n], lhsT=wbd[:, ix, :], rhs=u[:, t0:t0 + n], start=True, stop=True), [wbd.r, u.r], [pi.r])
                p.op("scalar", lambda e, pr=pr, r_=r_, ia=ia, n=n: e.activation(out=r_[:, 0:n], in_=pr[:, 0:n], func=AF.Sigmoid, bias=bias[:, ia:ia + 1], scale=1.0), [pr.r, bias.r], [r_.r])
                p.op("scalar", lambda e, pi=pi, i_=i_, ix=ix, n=n: e.activation(out=i_[:, 0:n], in_=pi[:, 0:n], func=AF.Sigmoid, bias=bias[:, ix:ix + 1], scale=1.0), [pi.r, bias.r], [i_.r])
                p.op("scalar", lambda e, a=a, r_=r_, d=d, cc=cc, t0=t0, n=n: e.activation(out=a[:, t0:t0 + n], in_=r_[:, 0:n], func=AF.Exp, scale=cl[:, d * 2 + cc:d * 2 + cc + 1]), [r_.r, cl.r], [a.r])
                p.op("gpsimd", lambda e, a=a, s_=s_, t0=t0, n=n: e.tensor_tensor(out=s_[:, 0:n], in0=a[:, t0:t0 + n], in1=a[:, t0:t0 + n], op=ALU.mult), [a.r], [s_.r])
                p.op("scalar", lambda e, s_=s_, n=n: e.activation(out=s_[:, 0:n], in_=s_[:, 0:n], func=AF.Sqrt, scale=-1.0, bias=1.0), [s_.r], [s_.r])
                p.op("vector", lambda e, i_=i_, t0=t0, n=n: e.tensor_tensor(out=i_[:, 0:n], in0=i_[:, 0:n], in1=u[:, t0:t0 + n], op=ALU.mult), [i_.r, u.r], [i_.r])
                p.op("vector", lambda e, v=v, i_=i_, s_=s_, t0=t0, n=n: e.tensor_tensor(out=v[:, t0:t0 + n], in0=i_[:, 0:n], in1=s_[:, 0:n], op=ALU.mult), [i_.r, s_.r], [v.r])
            h = hh[d]
            if d == 0:
                p.op("vector", lambda e, a=a, v=v, h=h: e.tensor_tensor_scan(out=h[:, 0:NCTX], data0=a[:, 0:NCTX], data1=v[:, 0:NCTX], initial=0.0, op0=ALU.mult, op1=ALU.add), [a.r, v.r], [h.r])
                p.op("vector", lambda e, a=a, v=v, h=h: e.tensor_tensor_scan(out=h[:, NCTX:T], data0=a[:, NCTX:T], data1=v[:, NCTX:T], initial=h[:, NCTX - 1:NCTX], op0=ALU.mult, op1=ALU.add), [a.r, v.r, h.r], [h.r])
            else:
                p.op("vector", lambda e, a=a, v=v, h=h: e.tensor_tensor_scan(out=h[:, 0:NCTX][:, ::-1], data0=a[:, 0:NCTX][:, ::-1], data1=v[:, 0:NCTX][:, ::-1], initial=0.0, op0=ALU.mult, op1=ALU.add), [a.r, v.r], [h.r])
                p.op("vector", lambda e, a=a, v=v, h=h: e.tensor_tensor_scan(out=h[:, NCTX:T][:, ::-1], data0=a[:, NCTX:T][:, ::-1], data1=v[:, NCTX:T][:, ::-1], initial=h[:, 0:1], op0=ALU.mult, op1=ALU.add), [a.r, v.r, h.r], [h.r])
        z = av[0][0]
        p.op("gpsimd", lambda e: e.tensor_tensor(out=z[:], in0=g[:], in1=g[:], op=ALU.mult), [g.r], [z.r])
        p.op("vector", lambda e: e.tensor_scalar(out=z[:], in0=z[:], scalar1=0.044715, scalar2=1.0, op0=ALU.mult, op1=ALU.add), [z.r], [z.r])
        p.op("gpsimd", lambda e: e.tensor_tensor(out=z[:], in0=z[:], in1=g[:], op=ALU.mult), [z.r, g.r], [z.r])
        p.op("scalar", lambda e: e.activation(out=z[:], in_=z[:], func=AF.Sigmoid, scale=1.5957691216057308), [z.r], [z.r])
        p.op("gpsimd", lambda e: e.tensor_tensor(out=z[:], in0=z[:], in1=g[:], op=ALU.mult), [z.r, g.r], [z.r])
        p.op("vector", lambda e: e.tensor_tensor(out=hh[0][:], in0=hh[0][:], in1=hh[1][:], op=ALU.add), [hh[0].r, hh[1].r], [hh[0].r])
        p.op("vector", lambda e: e.tensor_tensor(out=hh[0][:], in0=hh[0][:], in1=z[:], op=ALU.mult), [hh[0].r, z.r], [hh[0].r])
        xs_write(p, xs, row0 + cc * 128, hh[0], [hh[0].r], outs)


def rev_segments(e, out_t, in_t, segs):
    for (s0, n) in segs:
        last = e.tensor_copy(out=out_t[:, s0:s0 + n], in_=in_t[:, s0:s0 + n][:, ::-1])
    return last


def scan_nat_lo(t0, n):
    if t0 < NCTX:
        return NCTX - (t0 + n)
    return NCTX + LAT - (t0 - NCTX + n)


def emit_gla(kb, qT_d, kT_d, vT_d, g1_d, rT_d, g2_d, gb_d, gn_d, cm_d, m2_d, hm_d, ident_d, xs, row0, in_regs, outs):
    p = kb.p
    banks = kb.banks
    L = LAT
    NP = T // 128
    ptr = kb.psbf(banks[7])
    plog, patt, pO, pkv = banks[0], banks[1:3], banks[3:5], banks[5:7]
    segs = [(0, NCTX), (NCTX, L)]
    g2 = ld(kb, "g2", [16, 2, 256], g2_d)
    ngb = ld(kb, "ngb", [128, 4], gb_d)
    p.op("vector", lambda e: e.tensor_scalar(out=ngb[:], in0=ngb[:], scalar1=-1.0, scalar2=None, op0=ALU.mult), [ngb.r], [ngb.r])
    gn = ld(kb, "gn", [128, 2], gn_d)
    cm = ld(kb, "cm", [128, 512], cm_d)
    m2 = ld(kb, "m2", [128, 128], m2_d)
    hm = ld(kb, "hm", [128, 2], hm_d)
    idf, idb, ones = consts(kb, ident_d)
    g1b = kb.sb("g1b", [16, 512])
    g1c = kb.sb("g1c", [16, 512])
    osb = [kb.sb("osb%d" % i, [128, T]) for i in range(4)]
    qT = kb.sb("qT", [128, T])
    kT = kb.sb("kT", [128, T])
    tmpT = kb.sb("tmpT", [128, T])
    vtb = kb.sb("vtb", [128, T], BF16)
    vb = kb.sb("vb", [128, NP, 128], BF16)
    F = lambda n: kb.sb(n, [128, 512])
    sp, gcs, dref, dlast, Aex, Bex, Dex, Eex = F("sp"), F("gcs"), F("dref"), F("dlast"), F("Aex"), F("Bex"), F("Dex"), F("Eex")
    dec = kb.sb("dec", [128, 8])
    Bq = lambda n: kb.sb(n, [128, 512], BF16)
    qt, kt, kd, qe = Bq("qt"), Bq("kt"), Bq("kd"), Bq("qe")
    kdT = [kb.sb("kdT%d" % i, [128, 2, 128], BF16) for i in range(2)]
    attm = [kb.sb("attm%d" % i, [128, 128], BF16) for i in range(2)]
    S = kb.sb("S", [128, 128])
    NSB = 4
    Sb = [kb.sb("Sb%d" % i, [128, 128], BF16) for i in range(NSB)]
    scale = 128.0 ** -0.5
    blocks = [(0, NCTX)] + [(NCTX + c * 512, 512) for c in range(L // 512)]
    pc = 0
    for hd in range(4):
        h, d = hd // 2, hd % 2
        for (dst, src) in ((qT, qT_d), (kT, kT_d)):
            if d == 0:
                p.dma("sync", dst[:], src[h * 128:(h + 1) * 128, :], reads=in_regs, writes=[dst.r])
            else:
                p.dma("sync", tmpT[:], src[h * 128:(h + 1) * 128, :], reads=in_regs, writes=[tmpT.r])
                p.op("gpsimd", lambda e, dst=dst: rev_segments(e, dst, tmpT, segs), [tmpT.r], [dst.r])
        p.dma("sync", tmpT[:], vT_d[h * 128:(h + 1) * 128, :], reads=in_regs, writes=[tmpT.r])
        if d == 0:
            p.op("gpsimd", lambda e: e.tensor_copy(out=vtb[:], in_=tmpT[:]), [tmpT.r], [vtb.r])
        else:
            p.op("gpsimd", lambda e: rev_segments(e, vtb, tmpT, segs), [tmpT.r], [vtb.r])
        for g in range((NP + 7) // 8):
            k0, k1 = g * 8, min(NP, g * 8 + 8)

            def trv(e, k0=k0, k1=k1):
                for kt_ in range(k0, k1):
                    last = e.transpose(out=ptr.t[:, (kt_ - k0) * 128:(kt_ - k0 + 1) * 128], in_=vtb[:, kt_ * 128:(kt_ + 1) * 128], identity=idb[:])
                return last
            p.op("tensor", trv, [vtb.r, idb.r], [ptr.r])
            p.op("scalar", lambda e, k0=k0, k1=k1: e.copy(out=vb[:, k0:k1, :], in_=ptr.t[:, 0:(k1 - k0) * 128].rearrange("p (a b) -> p a b", b=128)), [ptr.r], [vb.r])
        p.op("vector", lambda e: e.memset(S[:], 0.0), [], [S.r])
        sbi = 0
        p.op("gpsimd", lambda e, sb_=Sb[0]: e.memset(sb_[:], 0.0), [], [Sb[0].r])
        for (t0, n) in blocks:
            nc_ = n // 64
            v3 = lambda t, n=n: t[:, 0:n].rearrange("p (c l) -> p c l", l=64)
            if d == 0:
                p.dma("sync", g1b[:, 0:n], g1_d[0:16, t0:t0 + n], reads=in_regs, writes=[g1b.r])
                gsrc = g1b
            else:
                lo = scan_nat_lo(t0, n)
                p.dma("sync", g1c[:, 0:n], g1_d[16:32, lo:lo + n], reads=in_regs, writes=[g1c.r])
                p.op("vector", lambda e, n=n: e.tensor_copy(out=g1b[:, 0:n], in_=g1c[:, 0:n][:, ::-1]), [g1c.r], [g1b.r])
                gsrc = g1b
            p.op("tensor", lambda e, d=d, h=h, n=n: e.matmul(plog[:, 0:n], lhsT=g2[:, d, h * 128:(h + 1) * 128], rhs=g1b[:, 0:n], start=True, stop=True), [g2.r, g1b.r], [plog.r])
            p.op("scalar", lambda e, hd=hd, n=n: e.activation(out=sp[:, 0:n], in_=plog[:, 0:n], func=AF.Exp, scale=-1.0, bias=ngb[:, hd:hd + 1]), [plog.r, ngb.r], [sp.r])
            p.op("scalar", lambda e, n=n: e.activation(out=sp[:, 0:n], in_=sp[:, 0:n], func=AF.Ln, scale=1.0, bias=1.0), [sp.r], [sp.r])
            p.op("vector", lambda e, n=n: e.tensor_tensor_scan(out=gcs[:, 0:n], data0=cm[:, 0:n], data1=sp[:, 0:n], initial=0.0, op0=ALU.mult, op1=ALU.add), [cm.r, sp.r], [gcs.r])
            p.op("vector", lambda e, n=n, nc_=nc_, v3=v3: e.tensor_tensor(out=v3(dref), in0=v3(gcs), in1=v3(gcs)[:, :, 32:33].to_broadcast([128, nc_, 64]), op=ALU.subtract), [gcs.r], [dref.r])
            p.op("gpsimd", lambda e, n=n, nc_=nc_, v3=v3: e.tensor_tensor(out=v3(dlast), in0=v3(gcs), in1=v3(gcs)[:, :, 63:64].to_broadcast([128, nc_, 64]), op=ALU.subtract), [gcs.r], [dlast.r])
            A = lambda fn, rd, wr: p.op("scalar", fn, rd, wr)
            A(lambda e, n=n: e.activation(out=Aex[:, 0:n], in_=dref[:, 0:n], func=AF.Exp, scale=-1.0 / 16), [dref.r], [Aex.r])
            A(lambda e, n=n: e.activation(out=Bex[:, 0:n], in_=dref[:, 0:n], func=AF.Exp, scale=1.0 / 16), [dref.r], [Bex.r])
            A(lambda e, n=n: e.activation(out=Dex[:, 0:n], in_=dlast[:, 0:n], func=AF.Exp, scale=1.0 / 16), [dlast.r], [Dex.r])
            A(lambda e, n=n: e.activation(out=Eex[:, 0:n], in_=gcs[:, 0:n], func=AF.Exp, scale=-1.0 / 16), [gcs.r], [Eex.r])
            A(lambda e, nc_=nc_, v3=v3: e.activation(out=dec[:, 0:nc_], in_=v3(gcs)[:, :, 63], func=AF.Exp, scale=-1.0 / 16), [gcs.r], [dec.r])
            p.op("vector", lambda e, t0=t0, n=n: e.scalar_tensor_tensor(out=qt[:, 0:n], in0=qT[:, t0:t0 + n], scalar=scale, in1=Aex[:, 0:n], op0=ALU.mult, op1=ALU.mult), [qT.r, Aex.r], [qt.r])
            p.op("gpsimd", lambda e, t0=t0, n=n: e.tensor_tensor(out=kt[:, 0:n], in0=kT[:, t0:t0 + n], in1=Bex[:, 0:n], op=ALU.mult), [kT.r, Bex.r], [kt.r])
            p.op("vector", lambda e, t0=t0, n=n: e.tensor_tensor(out=kd[:, 0:n], in0=kT[:, t0:t0 + n], in1=Dex[:, 0:n], op=ALU.mult), [kT.r, Dex.r], [kd.r])
            p.op("vector", lambda e, t0=t0, n=n: e.scalar_tensor_tensor(out=qe[:, 0:n], in0=qT[:, t0:t0 + n], scalar=scale, in1=Eex[:, 0:n], op0=ALU.mult, op1=ALU.mult), [qT.r, Eex.r], [qe.r])
            for pp in range(n // 128):
                s = pp * 128
                gp = (t0 + s) // 128
                kT_, am, pa, po, pk = kdT[pc % 2], attm[pc % 2], patt[pc % 2], pO[pc % 2], pkv[pc % 2]
                tr = ptr.t[:, (pc % 2) * 128:(pc % 2) * 128 + 128]
                pc += 1
                p.op("tensor", lambda e, tr=tr, s=s: e.transpose(out=tr, in_=kd[:, s:s + 128], identity=idb[:]), [kd.r, idb.r], [ptr.r])

                def cpk(e, tr=tr, kT_=kT_):
                    e.activation(out=kT_[:, 0, :], in_=tr, func=AF.Identity, scale=hm[:, 0:1])
                    return e.activation(out=kT_[:, 1, :], in_=tr, func=AF.Identity, scale=hm[:, 1:2])
                p.op("scalar", cpk, [ptr.r, hm.r], [kT_.r])
                p.op("tensor", lambda e, pa=pa, s=s: e.matmul(pa[:, 0:128], lhsT=kt[:, s:s + 128], rhs=qt[:, s:s + 128], start=True, stop=True), [kt.r, qt.r], [pa.r])
                p.op("vector", lambda e, pa=pa, am=am: e.tensor_tensor(out=am[:], in0=pa[:, 0:128], in1=m2[:], op=ALU.mult), [pa.r, m2.r], [am.r])

                def mkv(e, pk=pk, kT_=kT_, gp=gp):
                    e.matmul(pk[:, 0:128], lhsT=kT_[:, 0, :], rhs=vb[:, gp, :], start=True, stop=True)
                    return e.matmul(pk[:, 128:256], lhsT=kT_[:, 1, :], rhs=vb[:, gp, :], start=True, stop=True)
                p.op("tensor", mkv, [kT_.r, vb.r], [pk.r])
                sb0 = Sb[sbi % NSB]
                sb1 = Sb[(sbi + 1) % NSB]
                sb2 = Sb[(sbi + 2) % NSB]
                sbi += 2
                c0 = s // 64
                p.op("vector", lambda e, pk=pk, c0=c0: e.scalar_tensor_tensor(out=S[:], in0=S[:], scalar=dec[:, c0:c0 + 1], in1=pk[:, 0:128], op0=ALU.mult, op1=ALU.add), [S.r, dec.r, pk.r], [S.r])
                p.op("scalar", lambda e, sb1=sb1: e.copy(out=sb1[:], in_=S[:]), [S.r], [sb1.r])
                p.op("vector", lambda e, pk=pk, c0=c0: e.scalar_tensor_tensor(out=S[:], in0=S[:], scalar=dec[:, c0 + 1:c0 + 2], in1=pk[:, 128:256], op0=ALU.mult, op1=ALU.add), [S.r, dec.r, pk.r], [S.r])
                p.op("scalar", lambda e, sb2=sb2: e.copy(out=sb2[:], in_=S[:]), [S.r], [sb2.r])

                def mo(e, po=po, am=am, gp=gp, sb0=sb0, sb1=sb1, s=s):
                    e.matmul(po[:, 0:128], lhsT=vb[:, gp, :], rhs=am[:], start=True, stop=False)
                    e.matmul(po[:, 0:64], lhsT=sb0[:], rhs=qe[:, s:s + 64], start=False, stop=False)
                    return e.matmul(po[:, 64:128], lhsT=sb1[:], rhs=qe[:, s + 64:s + 128], start=False, stop=True)
                p.op("tensor", mo, [vb.r, am.r, sb0.r, sb1.r, qe.r], [po.r])
                p.op("scalar", lambda e, po=po, hd=hd, a=t0 + s: e.copy(out=osb[hd][:, a:a + 128], in_=po[:, 0:128]), [po.r], [osb[hd].r])
    rt = kb.sb("rt", [128, 512])
    yy = [kb.sb("yy%d" % i, [128, 512]) for i in range(2)]
    pss = banks[0]
    bi = 0
    for h in range(2):
        of, ob = osb[2 * h], osb[2 * h + 1]

        def comb(e, of=of, ob=ob):
            for (s0, n) in segs:
                last = e.tensor_tensor(out=of[:, s0:s0 + n], in0=of[:, s0:s0 + n], in1=ob[:, s0:s0 + n][:, ::-1], op=ALU.add)
            return last
        p.op("vector", comb, [of.r, ob.r], [of.r])
        for (t0, n) in blocks:
            y_ = yy[bi % 2]
            bi += 1
            p.op("scalar", lambda e, of=of, t0=t0, n=n: e.activation(out=sp[:, 0:n], in_=of[:, t0:t0 + n], func=AF.Square), [of.r], [sp.r])
            p.op("tensor", lambda e, n=n: e.matmul(pss[:, 0:n], lhsT=ones[:], rhs=sp[:, 0:n], start=True, stop=True), [ones.r, sp.r], [pss.r])
            p.op("vector", lambda e, n=n: e.tensor_scalar(out=gcs[:, 0:n], in0=pss[:, 0:n], scalar1=1.0 / 128, scalar2=EPS, op0=ALU.mult, op1=ALU.add), [pss.r], [gcs.r])
            p.op("vector", lambda e, n=n: e.reciprocal(out=gcs[:, 0:n], in_=gcs[:, 0:n]), [gcs.r], [gcs.r])
            p.op("scalar", lambda e, n=n: e.activation(out=gcs[:, 0:n], in_=gcs[:, 0:n], func=AF.Sqrt), [gcs.r], [gcs.r])
            p.dma("sync", rt[:, 0:n], rT_d[h * 128:(h + 1) * 128, t0:t0 + n], reads=in_regs, writes=[rt.r])
            p.op("scalar", lambda e, n=n: e.activation(out=rt[:, 0:n], in_=rt[:, 0:n], func=AF.Silu), [rt.r], [rt.r])
            p.op("vector", lambda e, of=of, y_=y_, t0=t0, n=n: e.tensor_tensor(out=y_[:, 0:n], in0=of[:, t0:t0 + n], in1=gcs[:, 0:n], op=ALU.mult), [of.r, gcs.r], [y_.r])
            p.op("vector", lambda e, h=h, y_=y_, n=n: e.scalar_tensor_tensor(out=y_[:, 0:n], in0=y_[:, 0:n], scalar=gn[:, h:h + 1], in1=rt[:, 0:n], op0=ALU.mult, op1=ALU.mult), [y_.r, gn.r, rt.r], [y_.r])
            for (a, m, hh_, loc) in nat_pieces(t0, n):
                o = Reg("o")
                outs.append(o)
                p.dma("gpsimd", xs[hh_ * 3072 + row0 + h * 128:hh_ * 3072 + row0 + (h + 1) * 128, loc:loc + m], y_[:, a - t0:a - t0 + m], reads=[y_.r], writes=[o])


def emit_ssd(kb, xbc_d, z_d, dt_d, cw_d, cb_d, dtb_d, alog_d, Dp_d, U_d, ident_d, xs, row0, in_regs, outs):
    p = kb.p
    banks = kb.banks
    L = LAT
    NCH = T // 128
    ptr = kb.psbf(banks[7])
    psm, pCB, pbc, pdiag, poff, pst = banks[0], banks[1], banks[2:4], banks[4], banks[5], banks[6]
    cw = ld(kb, "cw", [128, 4, 4], cw_d)
    cb = ld(kb, "cb", [128, 4], cb_d)
    dtb = ld(kb, "dtb", [4, 2], dtb_d)
    aneg = ld(kb, "aneg", [4, 2], alog_d)
    p.op("scalar", lambda e: e.activation(out=aneg[:], in_=aneg[:], func=AF.Exp), [aneg.r], [aneg.r])
    p.op("vector", lambda e: e.tensor_scalar(out=aneg[:], in0=aneg[:], scalar1=-1.0, scalar2=None, op0=ALU.mult), [aneg.r], [aneg.r])
    Dp = ld(kb, "Dp", [128, 2], Dp_d)
    U = ld(kb, "U", [128, 128], U_d)
    idf, idb, ones = consts(kb, ident_d)
    segs = [(0, NCTX), (NCTX, L)]
    arr = [[kb.sb("arr%d_%d" % (d, k), [128, T], BF16) for k in range(4)] for d in range(2)]
    xin = [kb.sb("xin%d" % i, [128, T]) for i in range(2)]
    u = kb.sb("u", [128, T])
    for k in range(4):
        xi = xin[k % 2]
        p.dma("sync", xi[:], xbc_d[k * 128:(k + 1) * 128, :], reads=in_regs, writes=[xi.r])
        emit_conv(kb, p, xi, u, cw, cb, segs, k)
        p.op("scalar", lambda e, k=k: e.activation(out=arr[0][k][:], in_=u[:], func=AF.Silu), [u.r], [arr[0][k].r])
        p.op("gpsimd", lambda e, k=k: rev_segments(e, arr[1][k], arr[0][k], segs), [arr[0][k].r], [arr[1][k].r])
    yacc = [xin[0], xin[1]]
    dtp = kb.sb("dtp", [4, 2, T])
    F = lambda n, s, dt=F32: kb.sb(n, s, dt)
    xBt = [F("xBt%d" % i, [128, 384], BF16) for i in range(2)]
    dtk = [F("dtk%d" % i, [128, 8]) for i in range(2)]
    acl = [F("acl%d" % i, [128, 8]) for i in range(2)]
    nac = [F("nac%d" % i, [128, 4]) for i in range(2)]
    eac = [F("eac%d" % i, [128, 4]) for i in range(2)]
    wdt = [F("wdt%d" % i, [128, 4]) for i in range(2)]
    dcy = [F("dcy%d" % i, [128, 4]) for i in range(2)]
    dtx = [F("dtx%d" % i, [128, 256], BF16) for i in range(2)]
    xw = [F("xw%d" % i, [128, 256], BF16) for i in range(2)]
    rU = [F("rU%d" % i, [128, 128]) for i in range(2)]
    sg = [F("sg%d" % i, [128, 128]) for i in range(2)]
    Lt = [F("Lt%d" % i, [128, 128]) for i in range(2)]
    Mb = [F("Mb%d" % i, [128, 128], BF16) for i in range(4)]
    CBs = [F("CBs%d" % i, [128, 128]) for i in range(2)]
    yt1 = [F("yt1_%d" % i, [128, 256]) for i in range(2)]
    yt2 = [F("yt2_%d" % i, [128, 256]) for i in range(2)]
    H = F("H", [128, 256])
    Hb = [F("Hb%d" % i, [128, 256], BF16) for i in range(2)]
    it = 0
    hc = 0
    for d in range(2):
        slot = 0 if d == 0 else 1
        p.dma("sync", dtp[:, slot, :], dt_d[d * 4:(d + 1) * 4, :], reads=in_regs, writes=[dtp.r])
        p.op("scalar", lambda e, slot=slot, d=d: e.activation(out=dtp[:, slot, :], in_=dtp[:, slot, :], func=AF.Exp, bias=dtb[:, d:d + 1], scale=1.0), [dtp.r, dtb.r], [dtp.r])
        p.op("scalar", lambda e, slot=slot: e.activation(out=dtp[:, slot, :], in_=dtp[:, slot, :], func=AF.Ln, bias=1.0, scale=1.0), [dtp.r], [dtp.r])
        if d == 1:
            def revdt(e):
                for (s0, n) in segs:
                    last = e.tensor_copy(out=dtp[:, 0, s0:s0 + n], in_=dtp[:, 1, s0:s0 + n][:, ::-1])
                return last
            p.op("vector", revdt, [dtp.r], [dtp.r])
        p.op("vector", lambda e, d=d: e.tensor_scalar(out=dtp[:, 1, :], in0=dtp[:, 0, :], scalar1=aneg[:, d:d + 1], scalar2=None, op0=ALU.mult), [dtp.r, aneg.r], [dtp.r])
        p.op("vector", lambda e: e.memset(H[:], 0.0), [], [H.r])
        p.op("gpsimd", lambda e, hb=Hb[hc % 2]: e.memset(hb[:], 0.0), [], [Hb[hc % 2].r])
        A = arr[d]
        for c in range(NCH):
            s = c * 128
            i2 = it % 2
            it += 1
            xb_, dk, ac, na, ea, wd, dc, dx, xw_ = xBt[i2], dtk[i2], acl[i2], nac[i2], eac[i2], wdt[i2], dcy[i2], dtx[i2], xw[i2]
            y1, y2, cbs = yt1[i2], yt2[i2], CBs[i2]
            hb_cur = Hb[hc % 2]
            hb_nxt = Hb[(hc + 1) % 2]
            hc += 1

            def trx(e, s=s, A=A):
                e.transpose(out=ptr.t[:, 0:128], in_=A[0][:, s:s + 128], identity=idb[:])
                e.transpose(out=ptr.t[:, 128:256], in_=A[1][:, s:s + 128], identity=idb[:])
                return e.transpose(out=ptr.t[:, 256:384], in_=A[2][:, s:s + 128], identity=idb[:])
            p.op("tensor", trx, [A[0].r, A[1].r, A[2].r, idb.r], [ptr.r])
            p.op("scalar", lambda e, xb_=xb_: e.copy(out=xb_[:], in_=ptr.t[:, 0:384]), [ptr.r], [xb_.r])

            def trd(e, s=s):
                e.transpose(out=psm[:, 0:4], in_=dtp[:, 0, s:s + 128], identity=idf[0:4, 0:4])
                return e.transpose(out=psm[:, 4:8], in_=dtp[:, 1, s:s + 128], identity=idf[0:4, 0:4])
            p.op("tensor", trd, [dtp.r, idf.r], [psm.r])
            p.op("vector", lambda e, dk=dk: e.tensor_copy(out=dk[:], in_=psm[:, 0:8]), [psm.r], [dk.r])

            def mac(e, dk=dk):
                e.matmul(psm[:, 8:12], lhsT=U[:], rhs=dk[:, 4:8], start=True, stop=True)
                return e.matmul(psm[:, 12:16], lhsT=ones[:], rhs=dk[:, 4:8], start=True, stop=True)
            p.op("tensor", mac, [U.r, ones.r, dk.r], [psm.r])
            p.op("vector", lambda e, ac=ac: e.tensor_copy(out=ac[:], in_=psm[:, 8:16]), [psm.r], [ac.r])
            p.op("vector", lambda e, ac=ac, na=na: e.tensor_scalar(out=na[:], in0=ac[:, 0:4], scalar1=-1.0, scalar2=None, op0=ALU.mult), [ac.r], [na.r])
            p.op("scalar", lambda e, ac=ac, ea=ea: e.activation(out=ea[:], in_=ac[:, 0:4], func=AF.Exp), [ac.r], [ea.r])
            p.op("scalar", lambda e, ac=ac, dc=dc: e.activation(out=dc[:], in_=ac[:, 4:8], func=AF.Exp), [ac.r], [dc.r])
            p.op("vector", lambda e, ac=ac, wd=wd: e.tensor_tensor(out=wd[:], in0=ac[:, 4:8], in1=ac[:, 0:4], op=ALU.subtract), [ac.r], [wd.r])
            p.op("scalar", lambda e, wd=wd: e.activation(out=wd[:], in_=wd[:], func=AF.Exp), [wd.r], [wd.r])
            p.op("vector", lambda e, wd=wd, dk=dk: e.tensor_tensor(out=wd[:], in0=wd[:], in1=dk[:, 0:4], op=ALU.mult), [wd.r, dk.r], [wd.r])
            x3 = lambda t: t[:, 0:256].rearrange("p (h q) -> p h q", q=64)
            p.op("vector", lambda e, dx=dx, xb_=xb_, dk=dk, x3=x3: e.tensor_tensor(out=x3(dx), in0=x3(xb_), in1=dk[:, 0:4].unsqueeze(2).to_broadcast([128, 4, 64]), op=ALU.mult), [xb_.r, dk.r], [dx.r])
            p.op("vector", lambda e, xw_=xw_, xb_=xb_, wd=wd, x3=x3: e.tensor_tensor(out=x3(xw_), in0=x3(xb_), in1=wd[:].unsqueeze(2).to_broadcast([128, 4, 64]), op=ALU.mult), [xb_.r, wd.r], [xw_.r])
            p.op("tensor", lambda e, s=s, A=A: e.matmul(pCB[:, 0:128], lhsT=A[2][:, s:s + 128], rhs=A[3][:, s:s + 128], start=True, stop=True), [A[2].r, A[3].r], [pCB.r])
            p.op("scalar", lambda e, cbs=cbs: e.copy(out=cbs[:], in_=pCB[:, 0:128]), [pCB.r], [cbs.r])
            for h in range(4):
                j2 = h % 2
                ru, sg_, lt, pb = rU[j2], sg[j2], Lt[j2], pbc[j2]
                mb = Mb[h]
                p.op("vector", lambda e, ru=ru, dk=dk, h=h: e.tensor_scalar(out=ru[:], in0=U[:], scalar1=dk[:, 4 + h:5 + h], scalar2=None, op0=ALU.mult), [U.r, dk.r], [ru.r])
                p.op("tensor", lambda e, pb=pb, ru=ru: e.matmul(pb[:, 0:128], lhsT=ones[:], rhs=ru[:], start=True, stop=True), [ones.r, ru.r], [pb.r])
                p.op("vector", lambda e, sg_=sg_, pb=pb, na=na, h=h: e.tensor_scalar(out=sg_[:], in0=pb[:, 0:128], scalar1=na[:, h:h + 1], scalar2=0.0, op0=ALU.add, op1=ALU.min), [pb.r, na.r], [sg_.r])
                p.op("scalar", lambda e, lt=lt, sg_=sg_: e.activation(out=lt[:], in_=sg_[:], func=AF.Exp), [sg_.r], [lt.r])
                p.op("gpsimd", lambda e, lt=lt: e.tensor_tensor(out=lt[:], in0=lt[:], in1=U[:], op=ALU.mult), [lt.r, U.r], [lt.r])
                p.op("vector", lambda e, lt=lt, cbs=cbs, mb=mb: e.tensor_tensor(out=mb[:], in0=lt[:], in1=cbs[:], op=ALU.mult), [lt.r, cbs.r], [mb.r])
                p.op("tensor", lambda e, mb=mb, dx=dx, h=h: e.matmul(pdiag[:, h * 64:(h + 1) * 64], lhsT=mb[:], rhs=dx[:, h * 64:(h + 1) * 64], start=True, stop=True), [mb.r, dx.r], [pdiag.r])
            p.op("tensor", lambda e, s=s, A=A, hb_cur=hb_cur: e.matmul(poff[:, 0:256], lhsT=A[3][:, s:s + 128], rhs=hb_cur[:], start=True, stop=True), [A[3].r, hb_cur.r], [poff.r])
            p.op("tensor", lambda e, xb_=xb_, xw_=xw_: e.matmul(pst[:, 0:256], lhsT=xb_[:, 256:384], rhs=xw_[:], start=True, stop=True), [xb_.r, xw_.r], [pst.r])
            H3 = H[:].rearrange("p (h q) -> p h q", q=64)
            p.op("vector", lambda e, dc=dc, H3=H3: e.tensor_tensor(out=H3, in0=H3, in1=dc[:].unsqueeze(2).to_broadcast([128, 4, 64]), op=ALU.mult), [H.r, dc.r], [H.r])
            p.op("vector", lambda e: e.tensor_tensor(out=H[:], in0=H[:], in1=pst[:, 0:256], op=ALU.add), [H.r, pst.r], [H.r])
            p.op("scalar", lambda e, hb_nxt=hb_nxt: e.copy(out=hb_nxt[:], in_=H[:]), [H.r], [hb_nxt.r])
            p.op("scalar", lambda e, y1=y1: e.copy(out=y1[:], in_=pdiag[:, 0:256]), [pdiag.r], [y1.r])
            p.op("vector", lambda e, y2=y2, ea=ea, x3=x3: e.tensor_tensor(out=x3(y2), in0=poff[:, 0:256].rearrange("p (h q) -> p h q", q=64),
                                                                        in1=ea[:].unsqueeze(2).to_broadcast([128, 4, 64]), op=ALU.mult), [poff.r, ea.r], [y2.r])
            p.op("gpsimd", lambda e, y1=y1, y2=y2: e.tensor_tensor(out=y2[:], in0=y2[:], in1=y1[:], op=ALU.add), [y1.r, y2.r], [y2.r])
            for k in range(2):
                pb = pbc[k]
                p.op("tensor", lambda e, pb=pb, y2=y2, k=k: e.transpose(out=pb[:, 128:256], in_=y2[:, k * 128:(k + 1) * 128], identity=idf[:]), [y2.r, idf.r], [pb.r])
                if d == 0:
                    p.op("scalar", lambda e, pb=pb, k=k, s=s: e.copy(out=yacc[k][:, s:s + 128], in_=pb[:, 128:256]), [pb.r], [yacc[k].r])
                else:
                    lo = scan_nat_lo(s, 128)
                    p.op("vector", lambda e, pb=pb, k=k, lo=lo: e.tensor_tensor(out=yacc[k][:, lo:lo + 128][:, ::-1], in0=yacc[k][:, lo:lo + 128][:, ::-1], in1=pb[:, 128:256], op=ALU.add),
                         [pb.r, yacc[k].r], [yacc[k].r])
    zt = u
    for k in range(2):
        p.op("vector", lambda e, k=k: e.scalar_tensor_tensor(out=yacc[k][:], in0=arr[0][k][:], scalar=Dp[:, k:k + 1], in1=yacc[k][:], op0=ALU.mult, op1=ALU.add),
             [arr[0][k].r, Dp.r, yacc[k].r], [yacc[k].r])
        p.dma("sync", zt[:], z_d[k * 128:(k + 1) * 128, :], reads=in_regs, writes=[zt.r])
        p.op("scalar", lambda e: e.activation(out=zt[:], in_=zt[:], func=AF.Silu), [zt.r], [zt.r])
        p.op("vector", lambda e, k=k: e.tensor_tensor(out=yacc[k][:], in0=yacc[k][:], in1=zt[:], op=ALU.mult), [yacc[k].r, zt.r], [yacc[k].r])
        xs_write(p, xs, row0 + k * 128, yacc[k], [yacc[k].r], outs)


def emit_b1(kb, xg, xT_d, cT, adaw, adab, wbr_d, wo_d, sn_d, bm_d, xm_d, in_regs, outs, TOK=HT):
    p = kb.p
    banks = kb.banks
    XR = 3072
    nch = (TOK + 511) // 512
    chunks = [(c * 512, min(512, TOK - c * 512)) for c in range(nch)]
    ya = kb.sb("ystg_a", [128, 8192])
    ystg = kb.sub(ya, 0, [128, 16, 512], F32, "ystg")
    mod = emit_mods(kb, adaw, adab, 8, cT, kb.sub(ya, 0, [128, 8, 512], F32, "awstg"), banks[0])
    bm = ld(kb, "bm", [128, 2], bm_d)
    ssdn = ld(kb, "ssdn", [128, 4], sn_d)
    ones = kb.sb("ones", [128, 128])
    p.op("vector", lambda e: e.memset(ones[:], 1.0), [], [ones.r])
    wbb = kb.sb("wbb", [128, 16, 1024], BF16)
    wob = kb.sb("wob", [128, 8, 1024], BF16)
    wst = [kb.sb("wst%d" % i, [128, 1024]) for i in range(2)]
    wbr_r = [Reg("wbr%d" % k) for k in range(16)]
    wo_r = [Reg("wo%d" % k) for k in range(8)]
    for k in range(24):
        st = wst[k % 2]
        src = wbr_d[k * 128:(k + 1) * 128, :] if k < 16 else wo_d[(k - 16) * 128:(k - 15) * 128, :]
        p.dma("sync", st[:], src, writes=[st.r])
        if k < 16:
            p.op("gpsimd", lambda e, st=st, k=k: e.tensor_copy(out=wbb[:, k, :], in_=st[:]), [st.r], [wbr_r[k]])
        else:
            p.op("gpsimd", lambda e, st=st, k=k: e.tensor_copy(out=wob[:, k - 16, :], in_=st[:]), [st.r], [wo_r[k - 16]])
    y2 = [kb.sb("y2_%d" % i, [128, 4, 512]) for i in range(2)]
    yb = kb.sb("yb", [128, 16, 512], BF16)
    xt = kb.sb("xt", [128, 8, 512])
    gts = [kb.sb("gt%d" % i, [128, 4, 512]) for i in range(2)]
    gt2 = [kb.sb("gu%d" % i, [128, 4, 512]) for i in range(2)]
    accs = [kb.sb("acc%d" % i, [128, 512]) for i in range(2)]
    tmps = [kb.sb("tmp%d" % i, [128, 512]) for i in range(2)]
    aT = kb.sb("aT", [128, 8, 512], BF16)
    pz = banks[0:4]
    po = banks[4:6]
    pss = banks[6]
    zc = 0
    gc = 0
    yc = 0

    def xrow(h, ch, r):
        return ((h * 24 + ch) * 2 + r) * 128

    def yrow(kc, h):
        n_, r_, j_ = kc // 4, (kc // 2) % 2, kc % 2
        return xrow(h, n_ * 2 + j_, r_)

    def grow(nb, oc, h):
        return xrow(h, 8 + (nb % 2) * 8 + oc, nb // 2)

    for c, (t0, n) in enumerate(chunks):
        for g4 in range(4):
            yy = y2[yc % 2]
            yc += 1
            for j in range(4):
                r0, r1 = yrow(g4 * 4 + j, 0), yrow(g4 * 4 + j, 1)
                p.dma("sync", ystg[:, g4 * 4 + j, 0:n], xg[r0:r0 + 128, t0:t0 + n], reads=in_regs, writes=[ystg.r])
                p.dma("sync", yy[:, j, 0:n], xg[r1:r1 + 128, t0:t0 + n], reads=in_regs, writes=[yy.r])
            p.op("vector", lambda e, g4=g4, n=n: e.tensor_scalar(out=ystg[:, g4 * 4:g4 * 4 + 4, 0:n], in0=ystg[:, g4 * 4:g4 * 4 + 4, 0:n], scalar1=bm[:, 0:1], scalar2=None, op0=ALU.mult),
                 [ystg.r, bm.r], [ystg.r])
            p.op("vector", lambda e, g4=g4, yy=yy, n=n: e.scalar_tensor_tensor(out=ystg[:, g4 * 4:g4 * 4 + 4, 0:n], in0=yy[:, :, 0:n], scalar=bm[:, 1:2], in1=ystg[:, g4 * 4:g4 * 4 + 4, 0:n],
                                                                             op0=ALU.mult, op1=ALU.add), [ystg.r, yy.r, bm.r], [ystg.r])
        for kc in range(4):
            sq = tmps[kc % 2]
            p.op("scalar", lambda e, sq=sq, kc=kc, n=n: e.activation(out=sq[:, 0:n], in_=ystg[:, kc, 0:n], func=AF.Square), [ystg.r], [sq.r])
            p.op("tensor", lambda e, sq=sq, kc=kc, n=n: e.matmul(pss[:, 0:n], lhsT=ones[:], rhs=sq[:, 0:n], start=(kc == 0), stop=(kc == 3)), [ones.r, sq.r], [pss.r])
        rs = accs[0]
        p.op("vector", lambda e, rs=rs, n=n: e.tensor_scalar(out=rs[:, 0:n], in0=pss[:, 0:n], scalar1=1.0 / 512, scalar2=EPS, op0=ALU.mult, op1=ALU.add), [pss.r], [rs.r])
        p.op("vector", lambda e, rs=rs, n=n: e.reciprocal(out=rs[:, 0:n], in_=rs[:, 0:n]), [rs.r], [rs.r])
        p.op("scalar", lambda e, rs=rs, n=n: e.activation(out=rs[:, 0:n], in_=rs[:, 0:n], func=AF.Sqrt), [rs.r], [rs.r])

        def nrm(e, rs=rs, n=n):
            for kc in range(4):
                last = e.scalar_tensor_tensor(out=ystg[:, kc, 0:n], in0=ystg[:, kc, 0:n], scalar=ssdn[:, kc:kc + 1], in1=rs[:, 0:n], op0=ALU.mult, op1=ALU.mult)
            return last
        p.op("vector", nrm, [ystg.r, ssdn.r, rs.r], [ystg.r])
        p.op("gpsimd", lambda e, n=n: e.tensor_copy(out=yb[:, :, 0:n], in_=ystg[:, :, 0:n]), [ystg.r], [yb.r])
        p.dma("sync", xt[:, :, 0:n], xT_d[:, t0:t0 + n].rearrange("(kc p) t -> p kc t", p=128), reads=in_regs, writes=[xt.r])
        for oc in range(8):
            gt, gu = gts[gc % 2], gt2[gc % 2]
            acc = accs[gc % 2]
            gc += 1
            for nb in range(4):
                r0, r1 = grow(nb, oc, 0), grow(nb, oc, 1)
                p.dma("sync", gt[:, nb, 0:n], xg[r0:r0 + 128, t0:t0 + n], reads=in_regs, writes=[gt.r])
                p.dma("sync", gu[:, nb, 0:n], xg[r1:r1 + 128, t0:t0 + n], reads=in_regs, writes=[gu.r])
            p.op("gpsimd", lambda e, gt=gt, n=n: e.tensor_scalar(out=gt[:, :, 0:n], in0=gt[:, :, 0:n], scalar1=bm[:, 0:1], scalar2=None, op0=ALU.mult), [gt.r, bm.r], [gt.r])
            p.op("vector", lambda e, gt=gt, gu=gu, n=n: e.scalar_tensor_tensor(out=gt[:, :, 0:n], in0=gu[:, :, 0:n], scalar=bm[:, 1:2], in1=gt[:, :, 0:n], op0=ALU.mult, op1=ALU.add),
                 [gt.r, gu.r, bm.r], [gt.r])
            p.op("scalar", lambda e, gt=gt, n=n: e.activation(out=gt[:, :, 0:n], in_=gt[:, :, 0:n], func=AF.Sigmoid), [gt.r], [gt.r])
            for nb in range(4):
                z = pz[zc % 4]
                zc += 1

                def mmz(e, z=z, nb=nb, oc=oc, n=n):
                    for kc in range(4):
                        last = e.matmul(z[:, 0:n], lhsT=wbb[:, nb * 4 + kc, oc * 128:(oc + 1) * 128], rhs=yb[:, nb * 4 + kc, 0:n], start=(kc == 0), stop=(kc == 3))
                    return last
                p.op("tensor", mmz, wbr_r[nb * 4:nb * 4 + 4] + [yb.r], [z.r])
                if nb == 0:
                    p.op("vector", lambda e, z=z, gt=gt, acc=acc, n=n: e.tensor_tensor(out=acc[:, 0:n], in0=z[:, 0:n], in1=gt[:, 0, 0:n], op=ALU.mult), [z.r, gt.r], [acc.r])
                else:
                    tmp = tmps[nb % 2]
                    p.op("vector", lambda e, z=z, gt=gt, tmp=tmp, nb=nb, n=n: e.tensor_tensor(out=tmp[:, 0:n], in0=z[:, 0:n], in1=gt[:, nb, 0:n], op=ALU.mult), [z.r, gt.r], [tmp.r])
                    if nb < 3:
                        p.op("gpsimd", lambda e, tmp=tmp, acc=acc, n=n: e.tensor_tensor(out=acc[:, 0:n], in0=acc[:, 0:n], in1=tmp[:, 0:n], op=ALU.add), [acc.r, tmp.r], [acc.r])
                    else:
                        p.op("gpsimd", lambda e, tmp=tmp, acc=acc, oc=oc, n=n: e.tensor_tensor(out=aT[:, oc, 0:n], in0=acc[:, 0:n], in1=tmp[:, 0:n], op=ALU.add), [acc.r, tmp.r], [aT.r])
        for oc in range(8):
            pq = po[oc % 2]

            def mmo(e, pq=pq, oc=oc, n=n):
                for kc in range(8):
                    last = e.matmul(pq[:, 0:n], lhsT=wob[:, kc, oc * 128:(oc + 1) * 128], rhs=aT[:, kc, 0:n], start=(kc == 0), stop=(kc == 7))
                return last
            p.op("tensor", mmo, wo_r + [aT.r], [pq.r])

            def res(e, pq=pq, oc=oc, t0=t0, n=n):
                for (s, m, j) in tok_ranges(t0, n, 128):
                    last = e.scalar_tensor_tensor(out=xt[:, oc, s - t0:s - t0 + m], in0=pq[:, s - t0:s - t0 + m], scalar=mod[:, oc, j:j + 1],
                                                  in1=xt[:, oc, s - t0:s - t0 + m], op0=ALU.mult, op1=ALU.add)
                return last
            p.op("vector", res, [pq.r, mod.r, xt.r], [xt.r])
        o = Reg("o%d" % c)
        outs.append(o)
        p.dma("gpsimd", xm_d[:, t0:t0 + n].rearrange("(kc p) t -> p kc t", p=128), xt[:, :, 0:n], reads=[xt.r], writes=[o])


def emit_b2(kb, xT_d, cT, adaw, adab, n2, wr_d, br_d, sel_d, ident_d, w1_d, w3_d, w2_d, out_d, in_regs, outs, TOK=HT, NCX=128, NE=16):
    p = kb.p
    banks = kb.banks
    nch = (TOK + 511) // 512
    chunks = [(c * 512, min(512, TOK - c * 512)) for c in range(nch)]
    idf = ld(kb, "idf", [128, 128], ident_d)
    ones = kb.sb("ones", [128, 128])
    p.op("vector", lambda e: e.memset(ones[:], 1.0), [], [ones.r])
    sel = ld(kb, "sel", [16, 16, 128], sel_d)
    wr = kb.sb("wr", [128, 8, 20])
    p.dma("sync", wr[:], wr_d.rearrange("(kc p) c -> p kc c", p=128), writes=[wr.r])
    br = ld(kb, "br", [128, 20], br_d)
    n2t = ld(kb, "n2t", [128, 8], n2)
    arena = kb.sb("b2arena", [128, 18432])
    KEL = 256

    class PV:
        def __init__(s, bank, ap):
            s.t, s.r = ap, bank.r

        def __getitem__(s, k):
            return s.t[k]
    sq = kb.sub(arena, 0, [128, 8, 512], F32, "sq")
    h2f = kb.sub(arena, 16 * KEL, [128, 8, 512], F32, "h2f")
    mod = emit_mods(kb, adaw, adab, 24, cT, sq, banks[0])
    asc = kb.sb("asc", [128, 8, 2])
    p.op("vector", lambda e: e.tensor_scalar(out=asc[:], in0=mod[:, 8:16, :], scalar1=1.0, scalar2=None, op0=ALU.add), [mod.r], [asc.r])
    p.op("vector", lambda e: e.tensor_tensor(out=asc[:], in0=asc[:], in1=n2t[:].unsqueeze(2).to_broadcast([128, 8, 2]), op=ALU.mult), [asc.r, n2t.r], [asc.r])
    xT = kb.sb("xT", [128, 8, TOK])
    xr = [[Reg("x%d_%d" % (oc, c)) for c in range(nch)] for oc in range(8)]
    for oc in range(8):
        p.dma("sync", xT[:, oc, :], xT_d[oc * 128:(oc + 1) * 128, :], reads=in_regs, writes=xr[oc])
    h2T = kb.sb("h2T", [128, 8, TOK], BF16)
    h2r = [Reg("h2_%d" % c) for c in range(nch)]
    wT = kb.sb("wT", [16, TOK])
    wTr = [Reg("wT%d" % c) for c in range(nch)]
    pss = banks[7]
    rstd = kb.sb("rstd", [128, 512])
    plg = [PV(banks[5], banks[5].t[:, 0:20]), PV(banks[6], banks[6].t[:, 0:20])]
    pwt = PV(banks[4], banks[4].t[0:16, 0:128])
    R = lambda n, s: kb.sb(n, s)
    L = R("rL", [128, 20]); mg = R("rmg", [128, 1]); nmg = R("rnmg", [128, 1]); eg = R("reg", [128, 4]); sg = R("rsg", [128, 1])
    gsel = R("rgsel", [128, 4]); tmp = R("rtmp", [128, 4, 4]); lsel = R("rlsel", [128, 4]); me = R("rme", [128, 1]); nme = R("rnme", [128, 1])
    ee = R("ree", [128, 4]); m1 = R("rm1", [128, 1]); msk = R("rmsk", [128, 4]); e2 = R("re2", [128, 4]); m2 = R("rm2", [128, 1])
    tv = R("rtv", [128, 4]); sv = R("rsv", [128, 1]); wg = R("rwg", [128, 4]); gp = R("rgp", [128, 4]); wf = R("rwf", [128, 4, 4])
    for c, (t0, n) in enumerate(chunks):
        def sqf(e, t0=t0, n=n):
            for kc in range(8):
                last = e.activation(out=sq[:, kc, 0:n], in_=xT[:, kc, t0:t0 + n], func=AF.Square)
            return last
        p.op("scalar", sqf, [xr[oc][c] for oc in range(8)], [sq.r])

        def ssum(e, n=n):
            for kc in range(8):
                last = e.matmul(pss[:, 0:n], lhsT=ones[:], rhs=sq[:, kc, 0:n], start=(kc == 0), stop=(kc == 7))
            return last
        p.op("tensor", ssum, [sq.r, ones.r], [pss.r])
        p.op("vector", lambda e, n=n: e.tensor_scalar(out=rstd[:, 0:n], in0=pss[:, 0:n], scalar1=1.0 / 1024, scalar2=EPS, op0=ALU.mult, op1=ALU.add), [pss.r], [rstd.r])
        p.op("vector", lambda e, n=n: e.reciprocal(out=rstd[:, 0:n], in_=rstd[:, 0:n]), [rstd.r], [rstd.r])
        p.op("scalar", lambda e, n=n: e.activation(out=rstd[:, 0:n], in_=rstd[:, 0:n], func=AF.Sqrt), [rstd.r], [rstd.r])

        def nrm(e, t0=t0, n=n):
            for kc in range(8):
                last = e.tensor_tensor(out=h2f[:, kc, 0:n], in0=xT[:, kc, t0:t0 + n], in1=rstd[:, 0:n], op=ALU.mult)
            return last
        p.op("vector", nrm, [xr[oc][c] for oc in range(8)] + [rstd.r], [h2f.r])

        def modf(e, t0=t0, n=n):
            for (s, m, j) in tok_ranges(t0, n, NCX):
                for kc in range(8):
                    last = e.activation(out=h2f[:, kc, s - t0:s - t0 + m], in_=h2f[:, kc, s - t0:s - t0 + m], func=AF.Identity, scale=asc[:, kc, j:j + 1], bias=mod[:, kc, j:j + 1])
            return last
        p.op("scalar", modf, [h2f.r, asc.r, mod.r], [h2f.r])
        p.op("gpsimd", lambda e, t0=t0, n=n: e.tensor_copy(out=h2T[:, :, t0:t0 + n], in_=h2f[:, :, 0:n]), [h2f.r], [h2r[c]])
        for tt in range(n // 128):
            pl = plg[tt % 2]

            def rmm(e, tt=tt, pl=pl):
                for kc in range(8):
                    last = e.matmul(pl[:], lhsT=h2f[:, kc, tt * 128:(tt + 1) * 128], rhs=wr[:, kc, :], start=(kc == 0), stop=(kc == 7))
                return last
            p.op("tensor", rmm, [h2f.r, wr.r], [pl.r])
            V = lambda fn, rd, wrt: p.op("vector", fn, rd, wrt)
            V(lambda e, pl=pl: e.tensor_tensor(out=L[:], in0=pl[:], in1=br[:], op=ALU.add), [pl.r, br.r], [L.r])
            V(lambda e: e.reduce_max(out=mg[:], in_=L[:, 0:4], axis=AX.X), [L.r], [mg.r])
            V(lambda e: e.tensor_scalar(out=nmg[:], in0=mg[:], scalar1=-1.0, scalar2=None, op0=ALU.mult), [mg.r], [nmg.r])
            p.op("scalar", lambda e: e.activation(out=eg[:], in_=L[:, 0:4], func=AF.Exp, bias=nmg[:, 0:1], scale=1.0, accum_out=sg[:]), [L.r, nmg.r], [eg.r, sg.r])
            V(lambda e: e.reciprocal(out=sg[:], in_=sg[:]), [sg.r], [sg.r])
            V(lambda e: e.tensor_scalar(out=gsel[:], in0=L[:, 0:4], scalar1=mg[:, 0:1], scalar2=None, op0=ALU.is_ge), [L.r, mg.r], [gsel.r])
            V(lambda e: e.tensor_tensor(out=tmp[:], in0=L[:, 4:20].rearrange("p (g e) -> p g e", g=4), in1=gsel[:].unsqueeze(2).to_broadcast([128, 4, 4]), op=ALU.mult), [L.r, gsel.r], [tmp.r])
            V(lambda e: e.tensor_reduce(out=lsel[:], in_=tmp[:].rearrange("p g e -> p e g"), axis=AX.X, op=ALU.add), [tmp.r], [lsel.r])
            V(lambda e: e.reduce_max(out=me[:], in_=lsel[:], axis=AX.X), [lsel.r], [me.r])
            V(lambda e: e.tensor_scalar(out=nme[:], in0=me[:], scalar1=-1.0, scalar2=None, op0=ALU.mult), [me.r], [nme.r])
            p.op("scalar", lambda e: e.activation(out=ee[:], in_=lsel[:], func=AF.Exp, bias=nme[:, 0:1], scale=1.0), [lsel.r, nme.r], [ee.r])
            V(lambda e: e.reduce_max(out=m1[:], in_=ee[:], axis=AX.X), [ee.r], [m1.r])
            V(lambda e: e.tensor_scalar(out=msk[:], in0=ee[:], scalar1=m1[:, 0:1], scalar2=-1e9, op0=ALU.is_ge, op1=ALU.mult), [ee.r, m1.r], [msk.r])
            V(lambda e: e.tensor_tensor(out=e2[:], in0=ee[:], in1=msk[:], op=ALU.add), [ee.r, msk.r], [e2.r])
            V(lambda e: e.reduce_max(out=m2[:], in_=e2[:], axis=AX.X), [e2.r], [m2.r])
            V(lambda e: e.tensor_scalar(out=tv[:], in0=ee[:], scalar1=m2[:, 0:1], scalar2=None, op0=ALU.is_ge), [ee.r, m2.r], [tv.r])
            V(lambda e: e.tensor_tensor(out=tv[:], in0=tv[:], in1=ee[:], op=ALU.mult), [tv.r, ee.r], [tv.r])
            V(lambda e: e.reduce_sum(out=sv[:], in_=tv[:], axis=AX.X), [tv.r], [sv.r])
            V(lambda e: e.reciprocal(out=sv[:], in_=sv[:]), [sv.r], [sv.r])
            V(lambda e: e.tensor_scalar(out=wg[:], in0=tv[:], scalar1=sv[:, 0:1], scalar2=None, op0=ALU.mult), [tv.r, sv.r], [wg.r])
            V(lambda e: e.tensor_scalar(out=gp[:], in0=gsel[:], scalar1=sg[:, 0:1], scalar2=None, op0=ALU.mult), [gsel.r, sg.r], [gp.r])
            V(lambda e: e.tensor_tensor(out=wf[:], in0=gp[:].unsqueeze(2).to_broadcast([128, 4, 4]), in1=wg[:].unsqueeze(1).to_broadcast([128, 4, 4]), op=ALU.mult), [gp.r, wg.r], [wf.r])
            p.op("tensor", lambda e: e.transpose(out=pwt[:], in_=wf[:].rearrange("p g e -> p (g e)"), identity=idf[:]), [wf.r, idf.r], [pwt.r])
            V(lambda e, a=t0 + tt * 128: e.tensor_copy(out=wT[:, a:a + 128], in_=pwt[:]), [pwt.r], [wTr[c]])
    p.fence()
    w13 = [kb.sub(arena, i * 16 * KEL, [128, 2, 8, 512], BF16, "w13_%d" % i) for i in range(2)]
    w2b = [kb.sub(arena, (32 + 8 * i) * KEL, [128, 4, 1024], BF16, "w2b_%d" % i) for i in range(2)]
    stA = [kb.sub(arena, (48 + 4 * i) * KEL, [128, 1024], F32, "stA%d" % i) for i in range(3)]
    s1 = [kb.sub(arena, (60 + 2 * i) * KEL, [128, 512], F32, "s1_%d" % i) for i in range(2)]
    aT = [kb.sub(arena, (64 + 4 * i) * KEL, [128, 4, 512], BF16, "aT%d" % i) for i in range(2)]
    ph = banks[0:4]
    pw = banks[4]
    po = banks[5:7]
    war_b = [[Reg("wa%d_%d" % (i, k)) for k in range(8)] for i in range(2)]
    wbr_b = [[Reg("wb%d_%d" % (i, k)) for k in range(4)] for i in range(2)]
    si = 0
    hc = 0
    ac = 0
    oc_cnt = 0
    for ex in range(NE):
        wa, wb = w13[ex % 2], w2b[ex % 2]
        war, wbr = war_b[ex % 2], wbr_b[ex % 2]
        for kc in range(8):
            sA = stA[si % 3]; si += 1
            p.dma("sync", sA[:, 0:512], w1_d[ex, kc * 128:(kc + 1) * 128, :], writes=[sA.r])
            p.dma("sync", sA[:, 512:1024], w3_d[ex, kc * 128:(kc + 1) * 128, :], writes=[sA.r])
            p.op("gpsimd", lambda e, sA=sA, wa=wa, kc=kc: e.tensor_copy(out=wa[:, :, kc, :], in_=sA[:].rearrange("p (a c) -> p a c", a=2)), [sA.r], [war[kc]])
        for fc in range(4):
            sA = stA[si % 3]; si += 1
            p.dma("sync", sA[:], w2_d[ex, fc * 128:(fc + 1) * 128, :], writes=[sA.r])
            p.op("gpsimd", lambda e, sA=sA, wb=wb, fc=fc: e.tensor_copy(out=wb[:, fc, :], in_=sA[:]), [sA.r], [wbr[fc]])
        for c, (t0, n) in enumerate(chunks):
            p.op("tensor", lambda e, ex=ex, t0=t0, n=n: e.matmul(pw[:, 0:n], lhsT=sel[:, ex, :], rhs=wT[:, t0:t0 + n], start=True, stop=True), [sel.r, wTr[c]], [pw.r])
            a = aT[ac % 2]; ac += 1
            for fc in range(4):
                p1, p3 = ph[hc % 4], ph[(hc + 1) % 4]; hc += 2
                ss = s1[fc % 2]

                def mm13(e, p1=p1, p3=p3, wa=wa, fc=fc, t0=t0, n=n):
                    for kc in range(8):
                        e.matmul(p1[:, 0:n], lhsT=wa[:, 0, kc, fc * 128:(fc + 1) * 128], rhs=h2T[:, kc, t0:t0 + n], start=(kc == 0), stop=(kc == 7))
                    for kc in range(8):
                        last = e.matmul(p3[:, 0:n], lhsT=wa[:, 1, kc, fc * 128:(fc + 1) * 128], rhs=h2T[:, kc, t0:t0 + n], start=(kc == 0), stop=(kc == 7))
                    return last
                p.op("tensor", mm13, war + [h2r[c]], [p1.r, p3.r])
                p.op("scalar", lambda e, ss=ss, p1=p1, n=n: e.activation(out=ss[:, 0:n], in_=p1[:, 0:n], func=AF.Silu), [p1.r], [ss.r])
                p.op("vector", lambda e, ss=ss, p3=p3, n=n: e.tensor_tensor(out=ss[:, 0:n], in0=ss[:, 0:n], in1=p3[:, 0:n], op=ALU.mult), [ss.r, p3.r], [ss.r])
                p.op("vector", lambda e, ss=ss, a=a, fc=fc, n=n: e.tensor_tensor(out=a[:, fc, 0:n], in0=ss[:, 0:n], in1=pw[:, 0:n], op=ALU.mult), [ss.r, pw.r], [a.r])
            for oc in range(8):
                pq = po[oc_cnt % 2]; oc_cnt += 1

                def mm2(e, pq=pq, wb=wb, a=a, oc=oc, n=n):
                    for fc in range(4):
                        last = e.matmul(pq[:, 0:n], lhsT=wb[:, fc, oc * 128:(oc + 1) * 128], rhs=a[:, fc, 0:n], start=(fc == 0), stop=(fc == 3))
                    return last
                p.op("tensor", mm2, wbr + [a.r], [pq.r])

                def acc(e, pq=pq, oc=oc, t0=t0, n=n):
                    for (s, m, j) in tok_ranges(t0, n, NCX):
                        last = e.scalar_tensor_tensor(out=xT[:, oc, s:s + m], in0=pq[:, s - t0:s - t0 + m], scalar=mod[:, 16 + oc, j:j + 1], in1=xT[:, oc, s:s + m], op0=ALU.mult, op1=ALU.add)
                    return last
                p.op("vector", acc, [pq.r, mod.r, xr[oc][c]], [xr[oc][c]])
    for oc in range(8):
        o = Reg("o%d" % oc)
        outs.append(o)
        p.dma("gpsimd", out_d[oc * 128:(oc + 1) * 128, :], xT[:, oc, :], reads=xr[oc], writes=[o])


IN_SIZES_ = (512, 1024, 16, 512, 512, 512, 32, 512, 512, 512, 512, 256, 256, 4096)
OFFS_ = np.cumsum([0] + list(IN_SIZES_))
GROUPS = [[0, 1], [2, 3], [4, 5], [6, 7]]


def s1_cols(hf):
    o = OFFS_
    pieces = [("z", o[0] + hf * 256, 256), ("x", o[1] + hf * 256, 256), ("B", o[1] + 512 + hf * 128, 128), ("C", o[1] + 768 + hf * 128, 128),
              ("gq", o[3] + hf * 256, 256), ("gk", o[4] + hf * 256, 256), ("gv", o[5] + hf * 256, 256), ("gr", o[7] + hf * 256, 256),
              ("lx", o[8] + hf * 256, 256), ("lg", o[9] + hf * 256, 256),
              ("aq", o[10] + hf * 256, 256), ("ak", o[11] + hf * 128, 128), ("av", o[12] + hf * 128, 128)]
    cols, rows, r = [], {}, 0
    for nm, st, n in pieces:
        cols.append(np.arange(st, st + n))
        rows[nm] = slice(r, r + n)
        r += n
    cols.append(np.concatenate([o[2] + d * 8 + hf * 4 + np.arange(4) for d in range(2)]))
    rows["dt"] = slice(r, r + 8)
    r += 8
    cols.append(np.arange(o[6], o[6] + 32))
    rows["g1"] = slice(r, r + 32)
    r += 32
    pad = NCC_MIX * 128 - r
    cols.append(np.full(pad, -1))
    r += pad
    cols.append(np.arange(o[13] + hf * 2048, o[13] + (hf + 1) * 2048))
    rows["gate"] = slice(r, r + 2048)
    return np.concatenate(cols), rows


LAYER_SPECS = [("adaw", [1024, 6144]), ("adab", [128, 48]), ("n1", [128, 8]), ("n2", [128, 8]), ("win", [1024, NCC_S1 * 128]),
               ("gq", [128, 1]), ("gk", [128, 1]),
               ("lcw", [128, 2, 4]), ("lcb", [128, 2]), ("lwbd", [128, 8, 128]), ("lbias", [128, 8]), ("llam", [128, 4]),
               ("g2", [16, 2, 256]), ("gb", [128, 4]), ("gn", [128, 2]),
               ("scw", [128, 4, 4]), ("scb", [128, 4]), ("dtb", [4, 2]), ("alog", [4, 2]), ("Dp", [128, 2]),
               ("wbr", [2048, 1024]), ("wo", [1024, 1024]), ("ssdn", [128, 4]),
               ("wr", [1024, 20]), ("br", [128, 20]), ("w1", [16, 1024, 512]), ("w3", [16, 1024, 512]), ("w2", [16, 512, 1024])]
GLOBAL_SPECS = [("xT0", [1024, T]), ("xTm0", [1024, HT]), ("cT", [128, 8, 2]), ("ident", [128, 128]), ("cos", [128, LAT]), ("sin", [128, LAT]),
                ("rm", [128, 128]), ("cm", [128, 512]), ("m2", [128, 128]), ("hm", [128, 2]), ("U", [128, 128]), ("sel", [16, 16, 128]), ("bm", [128, 2])]


def build_fused(NL=2, debug=False, stages=None):
    kb = KB()
    p = kb.p
    G = {n: kb.din(n, s) for n, s in GLOBAL_SPECS}
    W = [{n: kb.din("%s_%d" % (n, l), s) for n, s in LAYER_SPECS} for l in range(NL)]
    pT = kb.scratch("pT", [NCC_MIX * 128, T], debug=debug)
    xs = kb.scratch("xs", [2 * 3072, HT], debug=debug)
    xg = kb.scratch("xg", [2 * 24 * 2 * 128, HT], debug=debug)
    xm = kb.scratch("xm", [1024, HT], debug=debug)
    xnew = kb.scratch("xnew", [1024, HT], debug=debug)
    xall = kb.scratch("xall", [2048, HT], debug=debug)
    oT = kb.dout("oT", [1024, HT])
    outs = []
    _, rows = s1_cols(0)
    for l in range(NL):
        w = W[l]
        if l == 0:
            xsrc = lambda kc, t0, n: [(0, n, G["xT0"][kc * 128:(kc + 1) * 128, t0:t0 + n])]
            xres = G["xTm0"]
        else:
            xsrc = lambda kc, t0, n: [(a - t0, m, xall[(kc * 2 + h) * 128:(kc * 2 + h + 1) * 128, loc:loc + m]) for (a, m, h, loc) in nat_pieces(t0, n)]
            xres = xnew
        def gather_chunks(chs):
            for h in range(2):
                for ch in chs:
                    src = xs[h * 3072 + ch * 128:h * 3072 + (ch + 1) * 128, :]
                    dst = xg[((h * 24 + ch) * 2) * 128:((h * 24 + ch) * 2 + 2) * 128, :]
                    p.cc(lambda e, src=src, dst=dst: e.collective_compute("AllGather", ALU.bypass, replica_groups=GROUPS, ins=[src], outs=[dst]), blocking=True)
        emit_s1(kb, xsrc, G["cT"], w["adaw"][:, 0:2048], w["adab"][:, 0:16], w["n1"], w["win"], pT, xs, outs, [Reg("pT%d" % i) for i in range(NCC_MIX)])
        kb.reset()
        emit_ssd(kb, pT[256:768, :], pT[rows["z"], :], pT[rows["dt"], :], w["scw"], w["scb"], w["dtb"], w["alog"], w["Dp"], G["U"], G["ident"], xs, 0, [], outs)
        kb.reset()
        emit_gla(kb, pT[rows["gq"], :], pT[rows["gk"], :], pT[rows["gv"], :], pT[rows["g1"], :], pT[rows["gr"], :], w["g2"], w["gb"], w["gn"],
                 G["cm"], G["m2"], G["hm"], G["ident"], xs, 256, [], outs)
        kb.reset()
        emit_lru(kb, pT[rows["lx"], :], pT[rows["lg"], :], w["lcw"], w["lcb"], w["lwbd"], w["lbias"], w["llam"], xs, 512, [], outs)
        kb.reset()
        emit_att(kb, pT[rows["aq"], :], pT[rows["ak"], :], pT[rows["av"], :], w["gq"], w["gk"], G["cos"], G["sin"], G["rm"], G["ident"], xs, 768, [], outs)
        kb.reset()
        gather_chunks(range(24))
        kb.reset()
        emit_b1(kb, xg, xres, G["cT"], w["adaw"][:, 2048:3072], w["adab"][:, 16:24], w["wbr"], w["wo"], w["ssdn"], G["bm"], xm, [], outs)
        kb.reset()
        emit_b2(kb, xm, G["cT"], w["adaw"][:, 3072:6144], w["adab"][:, 24:48], w["n2"], w["wr"], w["br"], G["sel"], G["ident"], w["w1"], w["w3"], w["w2"],
                oT if l == NL - 1 else xnew, [], outs)
        kb.reset()
        if l < NL - 1:
            for kc in range(8):
                src = xnew[kc * 128:(kc + 1) * 128, :]
                dst = xall[kc * 256:(kc + 1) * 256, :]
                p.cc(lambda e, src=src, dst=dst: e.collective_compute("AllGather", ALU.bypass, replica_groups=GROUPS, ins=[src], outs=[dst]))
            kb.reset()
    return kb.finish(outs)


def core_inputs(inp, b, hf, NL=2):
    c = np.ascontiguousarray
    f32 = np.float32
    d = {}
    x_all = np.concatenate([inp["ctx"][b], inp["x"][b]], 0)
    d["xT0"] = c(x_all.T)
    d["xTm0"] = c(np.concatenate([inp["ctx"][b][hf * 128:(hf + 1) * 128], inp["x"][b][hf * 2048:(hf + 1) * 2048]], 0).T)
    c2 = np.stack([inp["c"][b], inp["c_ctx"]], -1)
    d["cT"] = c(c2.reshape(8, 128, 2).transpose(1, 0, 2))
    d["ident"] = np.eye(128, dtype=f32)
    cos, sin, Rm = rope_tables()
    d["cos"], d["sin"], d["rm"] = cos, sin, Rm
    t = np.arange(512)
    d["cm"] = np.broadcast_to((t % 64 != 0).astype(f32)[None], (128, 512)).copy()
    j = np.arange(128)[:, None]
    i = np.arange(128)[None, :]
    d["m2"] = ((j // 64 == i // 64) & (j <= i)).astype(f32)
    d["hm"] = np.stack([(np.arange(128) < 64), (np.arange(128) >= 64)], 1).astype(f32)
    d["U"] = (j <= i).astype(f32)
    sel = np.zeros((16, 16, 128), f32)
    for e in range(16):
        sel[e, e, :] = 1.0
    d["sel"] = sel
    bm = np.zeros((128, 2), f32)
    bm[:, hf] = 1.0
    d["bm"] = bm
    cols, rows = s1_cols(hf)
    ch = slice(hf * 256, (hf + 1) * 256)
    hs = slice(hf * 4, hf * 4 + 4)
    for l in range(NL):
        L = {}
        L["adaw"] = c(inp["ada_w"][l])
        L["adab"] = fm(inp["ada_b"][l], 48)
        L["n1"] = fm(inp["norm1"][l], 8)
        L["n2"] = fm(inp["norm2"][l], 8)
        win = np.zeros((1024, NCC_S1 * 128), f32)
        ok = cols >= 0
        win[:, np.nonzero(ok)[0]] = inp["w_in"][l][:, cols[ok]]
        L["win"] = win
        L["gq"] = c(inp["att_qnorm"][l][:, None])
        L["gk"] = c(inp["att_knorm"][l][:, None])
        L["lcw"] = c(inp["lru_conv_w"][l][:, ch].reshape(4, 2, 128).transpose(2, 1, 0))
        L["lcb"] = c(inp["lru_conv_b"][l][ch].reshape(2, 128).T)
        wbd = np.zeros((128, 8, 128), f32)
        bias = np.zeros((128, 8), f32)
        lam = np.zeros((128, 4), f32)
        for gi, (wk, bk) in enumerate((("lru_wa", "lru_ba"), ("lru_wx", "lru_bx"))):
            for dd in range(2):
                for cc in range(2):
                    idx = gi * 4 + dd * 2 + cc
                    for jj in range(2):
                        blk = hf * 4 + cc * 2 + jj
                        wbd[jj * 64:(jj + 1) * 64, idx, jj * 64:(jj + 1) * 64] = inp[wk][l][dd, blk]
                    bias[:, idx] = inp[bk][l][dd, ch][cc * 128:(cc + 1) * 128]
        for dd in range(2):
            for cc in range(2):
                lam[:, dd * 2 + cc] = inp["lru_lambda"][l][dd, ch][cc * 128:(cc + 1) * 128]
        L["lwbd"], L["lbias"], L["llam"] = wbd, bias, lam
        L["g2"] = c(inp["gla_g2"][l][:, :, ch].transpose(1, 0, 2))
        L["gb"] = c(np.stack([inp["gla_gb"][l][dd, ch][h * 128:(h + 1) * 128] for h in range(2) for dd in range(2)], 1))
        L["gn"] = c(inp["gla_norm"][l][ch].reshape(2, 128).T)
        chans = np.concatenate([np.arange(hf * 256, hf * 256 + 256), 512 + hf * 128 + np.arange(128), 768 + hf * 128 + np.arange(128)])
        L["scw"] = c(inp["ssd_conv_w"][l][:, chans].reshape(4, 4, 128).transpose(2, 1, 0))
        L["scb"] = c(inp["ssd_conv_b"][l][chans].reshape(4, 128).T)
        L["dtb"] = c(inp["ssd_dt_bias"][l][:, hs].T)
        L["alog"] = c(inp["ssd_a_log"][l][:, hs].T)
        L["Dp"] = c(np.repeat(inp["ssd_d"][l][hs], 64).reshape(2, 128).T)
        L["wbr"] = c(inp["w_branch"][l].reshape(2048, 1024))
        L["wo"] = c(inp["w_out"][l])
        L["ssdn"] = fm(inp["ssd_norm"][l], 4)
        L["wr"] = c(np.concatenate([inp["router_wg"][l], inp["router_we"][l]], 1))
        L["br"] = c(np.broadcast_to(np.concatenate([inp["router_bg"][l], inp["router_be"][l]])[None], (128, 20)))
        L["w1"], L["w3"], L["w2"] = c(inp["exp_w1"][l]), c(inp["exp_w3"][l]), c(inp["exp_w2"][l])
        for k, v in L.items():
            d["%s_%d" % (k, l)] = np.ascontiguousarray(v, dtype=f32)
    return {k: np.ascontiguousarray(v, dtype=f32) for k, v in d.items()}


_PROG = {}


def kernel(**inp):
    inp = {k: np.asarray(v) for k, v in inp.items()}
    NB = inp["x"].shape[0]
    if "fused" not in _PROG:
        _PROG["fused"] = build_fused()
    cores = [(b, hf) for b in range(NB) for hf in range(2)]
    ims = [core_inputs(inp, b, hf) for (b, hf) in cores]
    res = run_bass_kernel_spmd(_PROG["fused"], ims, core_ids=list(range(8))).results
    out = np.stack([np.concatenate([res[2 * b]["oT"][:, 128:].T, res[2 * b + 1]["oT"][:, 128:].T], 0) for b in range(NB)])
    return np.ascontiguousarray(out.astype(np.float32))
```

```python
import numpy as np
import concourse.bass as bass
import concourse.mybir as mybir
from concourse.bass_utils import run_bass_kernel_spmd
from contextlib import ExitStack

F32 = mybir.dt.float32
BF16 = mybir.dt.bfloat16
AF = mybir.ActivationFunctionType
ALU = mybir.AluOpType
AX = mybir.AxisListType


class Reg:
    __slots__ = ("name", "w", "r")

    def __init__(self, name=""):
        self.name = name
        self.w = None
        self.r = []


class Ins:
    __slots__ = ("eng", "fn", "deps", "sig", "idx", "isdma", "slot", "target", "n")

    def __init__(self):
        self.slot = None


class Prog:
    ENG = ["tensor", "vector", "scalar", "gpsimd", "sync"]
    RING = 12

    def __init__(self, nc):
        self.nc = nc
        self.q = {e: [] for e in self.ENG}
        self.n = 0

    def op(self, eng, fn, reads=(), writes=(), dma=False):
        I = Ins()
        I.eng, I.fn, I.isdma, I.sig, I.idx = eng, fn, dma, False, None
        I.n = self.n
        self.n += 1
        deps = {}
        for r in reads:
            if r.w is not None:
                deps.setdefault(id(r.w), [r.w, set()])[1].add("raw")
        for w in writes:
            if w.w is not None:
                deps.setdefault(id(w.w), [w.w, set()])[1].add("waw")
            for x in w.r:
                deps.setdefault(id(x), [x, set()])[1].add("war")
        final = []
        for J, kinds in deps.values():
            if J is I:
                continue
            if J.eng == eng and not J.isdma and not dma:
                if "raw" not in kinds or eng == "tensor":
                    continue
            final.append(J)
            J.sig = True
        I.deps = final
        for r in reads:
            r.r.append(I)
        for w in writes:
            w.w = I
            w.r = []
        self.q[eng].append(I)
        return I

    def fence(self):
        self.nf = getattr(self, "nf", 0) + 1
        for e in self.ENG:
            last = None
            for I in reversed(self.q[e]):
                if I.fn == "FENCE":
                    break
                if not I.isdma and I.fn is not None:
                    last = I
                    break
            if last is not None:
                last.sig = True
            I = Ins()
            I.eng, I.fn, I.isdma, I.sig, I.idx, I.deps = e, "FENCE", False, False, None, [last] if last is not None else []
            I.n = self.nf
            self.q[e].append(I)

    def dma(self, eng, out, in_, reads=(), writes=(), **kw):
        return self.op(eng, lambda e: e.dma_start(out=out, in_=in_, **kw), reads, writes, dma=True)

    def cc(self, fn, reads=(), writes=(), blocking=True):
        I = self.op("gpsimd", fn, reads, writes, dma=True)
        I.slot = "cc" if blocking else "ccnb"
        self.ccs = getattr(self, "ccs", []) + [I]
        return I

    def cc_wait_all(self):
        I = Ins()
        I.eng, I.fn, I.isdma, I.sig, I.idx, I.deps = "gpsimd", "CCWAIT", False, False, None, []
        I.n = len(getattr(self, "ccs", []))
        self.q["gpsimd"].append(I)

    def emit(self, final_regs=()):
        nc = self.nc
        self.op("sync", None, reads=list(final_regs))
        sems = {}
        stack = []
        for e in self.ENG:
            cm = nc.semaphore("S_" + e)
            sems[e] = cm.__enter__()
            stack.append(cm)
        cmf = nc.semaphore("S_fence")
        fsem = cmf.__enter__()
        stack.append(cmf)
        rings = {}
        for e in ("sync", "gpsimd", "scalar"):
            rings[e] = []
            for k in range(self.RING):
                cm = nc.semaphore("D_%s_%d" % (e, k))
                rings[e].append(cm.__enter__())
                stack.append(cm)
        ccsem = {}
        cctgt = {}
        if getattr(self, "ccs", []):
            cm = nc.semaphore("C_all")
            csem = cm.__enter__()
            stack.append(cm)
            for k, I in enumerate(self.ccs):
                ccsem[id(I)] = csem
                cctgt[id(I)] = k + 1
        for e in self.ENG:
            cnt = 0
            dcnt = 0
            for I in self.q[e]:
                if I.isdma and getattr(I, "slot", None) in ("cc", "ccnb"):
                    I.target = 1
                elif I.isdma:
                    I.slot = dcnt % self.RING
                    I.target = 16 * (dcnt // self.RING + 1)
                    dcnt += 1
                elif I.sig:
                    cnt += 1
                    I.idx = cnt
        self.counts = {e: len(self.q[e]) for e in self.ENG}

        def run(e, eng):
            seen = {}
            prev = {}

            def wait(key, sem, val):
                if seen.get(key, 0) < val:
                    eng.wait_ge(sem, val)
                    seen[key] = val

            for I in self.q[e]:
                mx = {}
                for J in I.deps:
                    if J.isdma and J.slot in ("cc", "ccnb"):
                        wait(("cc",), ccsem[id(J)], cctgt[id(J)])
                    elif J.isdma:
                        wait((J.eng, J.slot), rings[J.eng][J.slot], J.target)
                    else:
                        mx[J.eng] = max(mx.get(J.eng, 0), J.idx)
                for f, v in mx.items():
                    wait(f, sems[f], v)
                if I.isdma and I.slot in ("cc", "ccnb"):
                    inst = I.fn(eng)
                    inst.then_inc(ccsem[id(I)], 1)
                    if I.slot == "cc":
                        wait(("cc",), ccsem[id(I)], cctgt[id(I)])
                    continue
                if I.fn == "CCWAIT":
                    if I.n > 0:
                        wait(("cc",), csem, I.n)
                    continue
                if I.isdma:
                    p = prev.get(I.slot)
                    if p is not None:
                        wait((e, I.slot), rings[e][I.slot], p.target)
                    prev[I.slot] = I
                if I.fn is None:
                    continue
                if I.fn == "FENCE":
                    for s, pq in prev.items():
                        wait((e, s), rings[e][s], pq.target)
                    eng.sem_inc(fsem, 1)
                    eng.wait_ge(fsem, len(self.ENG) * I.n)
                    continue
                inst = I.fn(eng)
                if I.isdma:
                    inst.then_inc(rings[e][I.slot], 16)
                elif I.sig:
                    inst.then_inc(sems[e], 1)
            if e in rings:
                for s, p in prev.items():
                    wait((e, s), rings[e][s], p.target)

        with nc.Block() as block:
            @block.sync
            def _(eng):
                run("sync", eng)

            @block.tensor
            def _(eng):
                run("tensor", eng)

            @block.vector
            def _(eng):
                run("vector", eng)

            @block.scalar
            def _(eng):
                run("scalar", eng)

            @block.gpsimd
            def _(eng):
                run("gpsimd", eng)
        for cm in reversed(stack):
            cm.__exit__(None, None, None)


EPS = 1e-6
THETA = 10000.0
NCTX, LAT = 256, 4096
T = NCTX + LAT
HT = T // 2
NCC_MIX = 23
NCC_S1 = 39
ARENA_F32 = 50176


class Tile:
    __slots__ = ("t", "r")

    def __init__(self, t, name):
        self.t = t
        self.r = Reg(name)

    def __getitem__(self, k):
        return self.t[k]


class KB:
    def __init__(self):
        self.nc = bass.Bass("TRN2", target_bir_lowering=False)
        self.es = ExitStack()
        self.p = Prog(self.nc)
        self.es.enter_context(self.nc.allow_low_precision("bf16 matmul operands, fp32 accumulation"))
        self.arena = self.es.enter_context(self.nc.sbuf_tensor("arena", [128, ARENA_F32], F32))
        self.banks = [Tile(self.es.enter_context(self.nc.psum_tensor("bank%d" % i, [128, 512], F32)), "bank%d" % i) for i in range(8)]
        self.off = 0
        self.nscr = 0

    def din(self, name, shape, dt=F32):
        return self.nc.dram_tensor(name, list(shape), dt, kind="ExternalInput").ap()

    def dout(self, name, shape, dt=F32):
        return self.nc.dram_tensor(name, list(shape), dt, kind="ExternalOutput").ap()

    def scratch(self, name, shape, dt=F32, debug=False):
        if debug:
            return self.dout(name, shape, dt)
        return self.nc.dram_tensor(name, list(shape), dt).ap()

    def sb(self, name, shape, dt=F32):
        esize = 4 if dt == F32 else 2
        nel = 1
        for d in shape[1:]:
            nel *= d
        nbytes = (nel * esize + 31) // 32 * 32
        assert self.off + nbytes <= ARENA_F32 * 4, ("SBUF arena overflow", name, self.off, nbytes)
        ap = self.arena[0:shape[0], self.off // 4:(self.off + nbytes) // 4]
        if dt != F32:
            ap = ap.bitcast(dt)
        ap = ap[:, 0:nel]
        if len(shape) > 2:
            names = ["d%d" % k for k in range(len(shape) - 1)]
            ap = ap.rearrange("p (%s) -> p %s" % (" ".join(names), " ".join(names)), **{n: shape[k + 1] for k, n in enumerate(names[:-1])})
        self.off += nbytes
        return Tile(ap, name)

    def sub(self, tile, off_el, shape, dt=F32, name="v"):
        esize = 4 if dt == F32 else 2
        nel = 1
        for d in shape[1:]:
            nel *= d
        ap = tile.t[0:shape[0], off_el:off_el + nel * esize // 4]
        if dt != F32:
            ap = ap.bitcast(dt)
        if len(shape) > 2:
            names = ["d%d" % k for k in range(len(shape) - 1)]
            ap = ap.rearrange("p (%s) -> p %s" % (" ".join(names), " ".join(names)), **{n: shape[k + 1] for k, n in enumerate(names[:-1])})
        return Tile(ap, name)

    def psbf(self, bank):
        t = Tile(bank.t[:, 0:512].bitcast(BF16), "bf")
        t.r = bank.r
        return t

    def reset(self):
        self.p.fence()
        self.off = 0

    def finish(self, final_regs):
        self.p.emit(final_regs)
        self.es.close()
        return self.nc


def fm(v, n):
    return np.ascontiguousarray(np.asarray(v).reshape(n, 128).T)


def tok_ranges(t0, n, nctx):
    out = []
    if t0 < nctx:
        m = min(n, nctx - t0)
        out.append((t0, m, 1))
        if n > m:
            out.append((t0 + m, n - m, 0))
    else:
        out.append((t0, n, 0))
    return out


def ld(kb, name, shape, src, dt=F32):
    t = kb.sb(name, shape, dt)
    kb.p.dma("sync", t[:], src, writes=[t.r])
    return t


def consts(kb, ident_d, want_bf=True):
    p = kb.p
    idf = ld(kb, "idf", [128, 128], ident_d)
    ones = kb.sb("ones", [128, 128])
    p.op("vector", lambda e: e.memset(ones[:], 1.0), [], [ones.r])
    idb = None
    if want_bf:
        idb = kb.sb("idb", [128, 128], BF16)
        p.op("vector", lambda e: e.tensor_copy(out=idb[:], in_=idf[:]), [idf.r], [idb.r])
    return idf, idb, ones


def emit_mods(kb, adaw, adab, ncc, cT, aw, psm, name="m"):
    p = kb.p
    ct = kb.sb(name + "ct", [128, 8, 2])
    st = kb.sb(name + "st", [128, 8, 2])
    p.dma("sync", ct[:], cT, writes=[ct.r])
    p.op("scalar", lambda e: e.activation(out=st[:], in_=ct[:], func=AF.Silu), [ct.r], [st.r])
    ab = kb.sb(name + "ab", [128, ncc])
    p.dma("sync", ab[:], adab, writes=[ab.r])
    mod = kb.sb(name + "mod", [128, ncc, 2])
    for g in range(ncc // 4):
        awr = [Reg("aw%d" % k) for k in range(8)]
        for kc in range(8):
            p.dma("sync", aw[:, kc, :], adaw[kc * 128:(kc + 1) * 128, g * 512:(g + 1) * 512], reads=[], writes=[awr[kc], aw.r])

        def mm_mod(e, g=g):
            for cc in range(4):
                for kc in range(8):
                    last = e.matmul(psm[:, 2 * (g * 4 + cc):2 * (g * 4 + cc) + 2], lhsT=aw[:, kc, cc * 128:(cc + 1) * 128], rhs=st[:, kc, :],
                                    start=(kc == 0), stop=(kc == 7))
            return last
        p.op("tensor", mm_mod, [st.r, aw.r] + awr, [psm.r])
    p.op("vector", lambda e: e.tensor_tensor(out=mod[:], in0=psm[:, 0:2 * ncc].rearrange("p (c j) -> p c j", j=2),
                                             in1=ab[:].unsqueeze(2).to_broadcast([128, ncc, 2]), op=ALU.add), [psm.r, ab.r], [mod.r])
    return mod


def nat_pieces(t0, n):
    out = []
    bounds = [(0, 128, 0, 0), (128, 256, 1, 0), (256, 256 + 2048, 0, 128), (256 + 2048, T, 1, 128)]
    for (a, b, h, loc) in bounds:
        lo, hi = max(a, t0), min(b, t0 + n)
        if lo < hi:
            out.append((lo, hi - lo, h, loc + lo - a))
    return out


PIECE = 240


def gath_parts(total, f0, r, base0=0, n=128):
    parts = []
    f = f0
    while f < f0 + n:
        i = f // PIECE
        Ri = min(PIECE, total - PIECE * i)
        end = min(f0 + n, PIECE * (i + 1))
        parts.append((base0 + 2 * PIECE * i + r * Ri + (f - PIECE * i), end - f, f - f0))
        f = end
    return parts


def xs_write(p, xs, row0, tile, reads, outs, eng="gpsimd"):
    for (a, n, h, loc) in nat_pieces(0, T):
        o = Reg("o")
        outs.append(o)
        p.dma(eng, xs[h * 3072 + row0:h * 3072 + row0 + 128, loc:loc + n], tile[:, a:a + n], reads=reads, writes=[o])


def emit_s1(kb, xsrc, cT, adaw, adab, n1, win, pT, xs, xs_regs, pT_regs, xall_ap=None):
    p = kb.p
    banks = kb.banks
    aw = kb.sb("aw", [128, 8, 512])
    mod = emit_mods(kb, adaw, adab, 16, cT, aw, banks[0])
    n1t = ld(kb, "n1t", [128, 8], n1)
    asc = kb.sb("asc", [128, 8, 2])
    p.op("vector", lambda e: e.tensor_scalar(out=asc[:], in0=mod[:, 8:16, :], scalar1=1.0, scalar2=None, op0=ALU.add), [mod.r], [asc.r])
    p.op("vector", lambda e: e.tensor_tensor(out=asc[:], in0=asc[:], in1=n1t[:].unsqueeze(2).to_broadcast([128, 8, 2]), op=ALU.mult), [asc.r, n1t.r], [asc.r])
    ones = kb.sb("ones", [128, 128])
    p.op("vector", lambda e: e.memset(ones[:], 1.0), [], [ones.r])
    nch = (T + 511) // 512
    chunks = [(c * 512, min(512, T - c * 512)) for c in range(nch)]
    hT = kb.sb("hT", [128, 8, T], BF16)
    hr = [Reg("h%d" % c) for c in range(nch)]
    xq = [kb.sb("xq%d" % i, [128, 8, 512]) for i in range(2)]
    sq = [kb.sb("sq%d" % i, [128, 512]) for i in range(2)]
    rstd = kb.sb("rstd", [128, 512])
    tmp = [kb.sb("tmp%d" % i, [128, 512]) for i in range(2)]
    pss = banks[1]
    for c, (t0, n) in enumerate(chunks):
        xc = xq[c % 2]
        for kc in range(8):
            for piece in xsrc(kc, t0, n):
                if len(piece) == 3:
                    off, ln, src = piece
                    p.dma("sync", xc[:, kc, off:off + ln], src, writes=[xc.r])
                else:
                    off, ln, h_, loc = piece
                    for (row, cnt, po_) in gath_parts(1024, kc * 128, h_):
                        p.dma("sync", xc.t[po_:po_ + cnt, kc, off:off + ln], xall_ap[row:row + cnt, loc:loc + ln], writes=[xc.r])
        for kc in range(8):
            s_ = sq[kc % 2]
            p.op("scalar", lambda e, s_=s_, xc=xc, kc=kc, n=n: e.activation(out=s_[:, 0:n], in_=xc[:, kc, 0:n], func=AF.Square), [xc.r], [s_.r])
            p.op("tensor", lambda e, s_=s_, kc=kc, n=n: e.matmul(pss[:, 0:n], lhsT=ones[:], rhs=s_[:, 0:n], start=(kc == 0), stop=(kc == 7)), [ones.r, s_.r], [pss.r])
        p.op("vector", lambda e, n=n: e.tensor_scalar(out=rstd[:, 0:n], in0=pss[:, 0:n], scalar1=1.0 / 1024, scalar2=EPS, op0=ALU.mult, op1=ALU.add), [pss.r], [rstd.r])
        p.op("vector", lambda e, n=n: e.reciprocal(out=rstd[:, 0:n], in_=rstd[:, 0:n]), [rstd.r], [rstd.r])
        p.op("scalar", lambda e, n=n: e.activation(out=rstd[:, 0:n], in_=rstd[:, 0:n], func=AF.Sqrt), [rstd.r], [rstd.r])
        for kc in range(8):
            tm = tmp[kc % 2]
            p.op("vector", lambda e, tm=tm, xc=xc, kc=kc, n=n: e.tensor_tensor(out=tm[:, 0:n], in0=xc[:, kc, 0:n], in1=rstd[:, 0:n], op=ALU.mult), [xc.r, rstd.r], [tm.r])

            def modf(e, tm=tm, kc=kc, t0=t0, n=n):
                for (s, m, j) in tok_ranges(t0, n, NCTX):
                    last = e.activation(out=hT[:, kc, s:s + m], in_=tm[:, s - t0:s - t0 + m], func=AF.Identity, scale=asc[:, kc, j:j + 1], bias=mod[:, kc, j:j + 1])
                return last
            p.op("scalar", modf, [tm.r, asc.r, mod.r], [hr[c]])
    wfs = [kb.sb("wf%d" % i, [128, 8, 128]) for i in range(2)]
    wbs = [kb.sb("wb%d" % i, [128, 8, 128], BF16) for i in range(2)]
    stg = [kb.sb("stg%d" % i, [128, T]) for i in range(2)]
    sgr = [[Reg("sg%d_%d" % (i, c)) for c in range(nch)] for i in range(2)]
    pps = banks[2:6]
    cnt = 0
    for cc in range(NCC_S1):
        wf, wb, sg = wfs[cc % 2], wbs[cc % 2], stg[cc % 2]
        p.dma("sync", wf[:], win[:, cc * 128:(cc + 1) * 128].rearrange("(kc p) c -> p kc c", p=128), writes=[wf.r])
        p.op("gpsimd", lambda e, wf=wf, wb=wb: e.tensor_copy(out=wb[:], in_=wf[:]), [wf.r], [wb.r])
        for tcn, (t0, n) in enumerate(chunks):
            pp = pps[cnt % 4]

            def mm(e, pp=pp, wb=wb, t0=t0, n=n):
                for kc in range(8):
                    last = e.matmul(pp[:, 0:n], lhsT=wb[:, kc, :], rhs=hT[:, kc, t0:t0 + n], start=(kc == 0), stop=(kc == 7))
                return last
            p.op("tensor", mm, [wb.r, hr[tcn]], [pp.r])
            if cnt % 2 == 0:
                p.op("vector", lambda e, pp=pp, sg=sg, t0=t0, n=n: e.tensor_copy(out=sg[:, t0:t0 + n], in_=pp[:, 0:n]), [pp.r], [sgr[cc % 2][tcn]])
            else:
                p.op("scalar", lambda e, pp=pp, sg=sg, t0=t0, n=n: e.copy(out=sg[:, t0:t0 + n], in_=pp[:, 0:n]), [pp.r], [sgr[cc % 2][tcn]])
            cnt += 1
        if cc < NCC_MIX:
            p.dma("gpsimd", pT[cc * 128:(cc + 1) * 128, :], sg[:], reads=sgr[cc % 2], writes=[pT_regs[cc]])
        else:
            xs_write(p, xs, 1024 + (cc - NCC_MIX) * 128, sg, sgr[cc % 2], xs_regs)


def rope_tables(L=4096, W=64):
    t = np.arange(L)
    pos = np.stack([t // W, t % W], 0).astype(np.float32)
    d = np.arange(128)
    half = d // 64
    j = d % 32
    inv = (THETA ** (-(j.astype(np.float32)) / 32.0)).astype(np.float32)
    ang = pos[half] * inv[:, None]
    cos = np.cos(ang).astype(np.float32)
    sin = np.sin(ang).astype(np.float32)
    sgn = np.where((d % 64) < 32, -1.0, 1.0).astype(np.float32)
    Rm = np.zeros((128, 128), np.float32)
    partner = np.where((d % 64) < 32, d + 32, d - 32)
    Rm[partner, d] = 1.0
    return cos, (sin * sgn[:, None]).astype(np.float32), Rm


def emit_att(kb, qT_d, kT_d, vT_d, gq_d, gk_d, cos_d, sin_d, rm_d, ident_d, xs, row0, in_regs, outs):
    p = kb.p
    banks = kb.banks
    L = LAT
    NKT = T // 128
    idf, idb, ones = consts(kb, ident_d)
    onesb = kb.sb("onesb", [128, 128], BF16)
    p.op("vector", lambda e: e.tensor_copy(out=onesb[:], in_=ones[:]), [ones.r], [onesb.r])
    rm = ld(kb, "rm", [128, 128], rm_d)
    gq = ld(kb, "gq", [128, 1], gq_d)
    gk = ld(kb, "gk", [128, 1], gk_d)
    cos = ld(kb, "cos", [128, L], cos_d)
    sin = ld(kb, "sin", [128, L], sin_d)
    xst = [kb.sb("xst%d" % i, [128, T]) for i in range(2)]
    vb = kb.sb("vb", [128, NKT, 128], BF16)
    vtb = kb.sb("vtb", [128, T], BF16)
    p.dma("sync", xst[1][:], vT_d, reads=in_regs, writes=[xst[1].r])
    p.op("gpsimd", lambda e: e.tensor_copy(out=vtb[:], in_=xst[1][:]), [xst[1].r], [vtb.r])
    ptr = kb.psbf(banks[1])
    for g in range((NKT + 7) // 8):
        k0, k1 = g * 8, min(NKT, g * 8 + 8)

        def trv(e, k0=k0, k1=k1):
            for kt in range(k0, k1):
                last = e.transpose(out=ptr.t[:, (kt - k0) * 128:(kt - k0 + 1) * 128], in_=vtb[:, kt * 128:(kt + 1) * 128], identity=idb[:])
            return last
        p.op("tensor", trv, [vtb.r, idb.r], [ptr.r])
        p.op("scalar", lambda e, k0=k0, k1=k1: e.copy(out=vb[:, k0:k1, :], in_=ptr.t[:, 0:(k1 - k0) * 128].rearrange("p (a b) -> p a b", b=128)), [ptr.r], [vb.r])

    chunks = [(0, NCTX, False)] + [(NCTX + c * 512, 512, True) for c in range(L // 512)]
    knT = kb.sb("knT", [128, T], BF16)
    qnT = [kb.sb("qnT%d" % i, [128, T], BF16) for i in range(2)]
    sqt = kb.sb("sqt", [128, 512])
    rstd = kb.sb("rstd", [128, 512])
    xg = kb.sb("xg", [128, 512])
    t1 = kb.sb("t1", [128, 512])
    t2 = kb.sb("t2", [128, 512])
    pss, prot = banks[0], banks[1]
    srcs = [(kT_d, gk, knT), (qT_d[0:128, :], gq, qnT[0]), (qT_d[128:256, :], gq, qnT[1])]
    dregs = []
    for si, (src, g, dst) in enumerate(srcs):
        xs_ = xst[si % 2]
        p.dma("sync", xs_[:], src, reads=in_regs, writes=[xs_.r])
        dr = [Reg("d%d_%d" % (si, c)) for c in range(len(chunks))]
        dregs.append(dr)
        for c, (t0, n, lat) in enumerate(chunks):
            p.op("scalar", lambda e, xs_=xs_, t0=t0, n=n: e.activation(out=sqt[:, 0:n], in_=xs_[:, t0:t0 + n], func=AF.Square), [xs_.r], [sqt.r])
            p.op("tensor", lambda e, n=n: e.matmul(pss[:, 0:n], lhsT=ones[:], rhs=sqt[:, 0:n], start=True, stop=True), [ones.r, sqt.r], [pss.r])
            p.op("vector", lambda e, n=n: e.tensor_scalar(out=rstd[:, 0:n], in0=pss[:, 0:n], scalar1=1.0 / 128, scalar2=EPS, op0=ALU.mult, op1=ALU.add), [pss.r], [rstd.r])
            p.op("vector", lambda e, n=n: e.reciprocal(out=rstd[:, 0:n], in_=rstd[:, 0:n]), [rstd.r], [rstd.r])
            p.op("scalar", lambda e, n=n: e.activation(out=rstd[:, 0:n], in_=rstd[:, 0:n], func=AF.Sqrt), [rstd.r], [rstd.r])
            p.op("vector", lambda e, xs_=xs_, g=g, t0=t0, n=n: e.tensor_scalar(out=xg[:, 0:n], in0=xs_[:, t0:t0 + n], scalar1=g[:, 0:1], scalar2=None, op0=ALU.mult), [xs_.r, g.r], [xg.r])
            if lat:
                l0 = t0 - NCTX
                p.op("tensor", lambda e, n=n: e.matmul(prot[:, 0:n], lhsT=rm[:], rhs=xg[:, 0:n], start=True, stop=True), [rm.r, xg.r], [prot.r])
                p.op("gpsimd", lambda e, l0=l0, n=n: e.tensor_tensor(out=t1[:, 0:n], in0=xg[:, 0:n], in1=cos[:, l0:l0 + n], op=ALU.mult), [xg.r, cos.r], [t1.r])
                p.op("vector", lambda e, l0=l0, n=n: e.tensor_tensor(out=t2[:, 0:n], in0=prot[:, 0:n], in1=sin[:, l0:l0 + n], op=ALU.mult), [prot.r, sin.r], [t2.r])
                p.op("gpsimd", lambda e, n=n: e.tensor_tensor(out=t1[:, 0:n], in0=t1[:, 0:n], in1=t2[:, 0:n], op=ALU.add), [t1.r, t2.r], [t1.r])
                p.op("vector", lambda e, dst=dst, t0=t0, n=n: e.tensor_tensor(out=dst[:, t0:t0 + n], in0=t1[:, 0:n], in1=rstd[:, 0:n], op=ALU.mult), [t1.r, rstd.r], [dr[c]])
            else:
                p.op("vector", lambda e, dst=dst, t0=t0, n=n: e.tensor_tensor(out=dst[:, t0:t0 + n], in0=xg[:, 0:n], in1=rstd[:, 0:n], op=ALU.mult), [xg.r, rstd.r], [dr[c]])
    kr, qr = dregs[0], dregs[1:]
    pS = banks[2:5]
    pO = banks[5:7]
    pD = [banks[7], banks[0]]
    pts = [kb.sb("pt%d" % i, [128, 512], BF16) for i in range(3)]
    rden = [kb.sb("rden%d" % i, [128, 512]) for i in range(2)]
    yo = [kb.sb("yo%d" % i, [128, 512]) for i in range(2)]
    scale = 128.0 ** -0.5
    sc = 0
    blk = 0
    for h in range(2):
        for c, (t0, n, lat) in enumerate(chunks):
            nkt = NKT if lat else NCTX // 128
            po, pd = pO[blk % 2], pD[blk % 2]
            def s_exp(kt, sc_):
                ps_, pt = pS[sc_ % 3], pts[sc_ % 3]
                kc = 0 if kt < NCTX // 128 else 1 + (kt * 128 - NCTX) // 512
                p.op("tensor", lambda e, ps_=ps_, kt=kt, h=h, t0=t0, n=n: e.matmul(ps_[:, 0:n], lhsT=knT[:, kt * 128:(kt + 1) * 128], rhs=qnT[h][:, t0:t0 + n], start=True, stop=True),
                     [kr[kc], qr[h][c]], [ps_.r])
                p.op("scalar", lambda e, ps_=ps_, pt=pt, n=n: e.activation(out=pt[:, 0:n], in_=ps_[:, 0:n], func=AF.Exp, scale=scale), [ps_.r], [pt.r])
            s_exp(0, sc)
            for kt in range(nkt):
                pt = pts[sc % 3]
                if kt + 1 < nkt:
                    s_exp(kt + 1, sc + 1)
                sc += 1

                def pv(e, po=po, pd=pd, pt=pt, kt=kt, n=n, nkt=nkt):
                    e.matmul(po[:, 0:n], lhsT=vb[:, kt, :], rhs=pt[:, 0:n], start=(kt == 0), stop=(kt == nkt - 1))
                    return e.matmul(pd[:, 0:n], lhsT=onesb[:], rhs=pt[:, 0:n], start=(kt == 0), stop=(kt == nkt - 1))
                p.op("tensor", pv, [vb.r, onesb.r, pt.r], [po.r, pd.r])
            rd, y = rden[blk % 2], yo[blk % 2]
            p.op("vector", lambda e, rd=rd, pd=pd, n=n: e.reciprocal(out=rd[:, 0:n], in_=pd[:, 0:n]), [pd.r], [rd.r])
            p.op("vector", lambda e, rd=rd, po=po, y=y, n=n: e.tensor_tensor(out=y[:, 0:n], in0=po[:, 0:n], in1=rd[:, 0:n], op=ALU.mult), [po.r, rd.r], [y.r])
            for (a, m, hh, loc) in nat_pieces(t0, n):
                o = Reg("o")
                outs.append(o)
                p.dma("gpsimd", xs[hh * 3072 + row0 + h * 128:hh * 3072 + row0 + (h + 1) * 128, loc:loc + m], y[:, a - t0:a - t0 + m], reads=[y.r], writes=[o])
            blk += 1


def emit_conv(kb, p, x, u, w, b, segs, cc, eng_extra="vector"):
    p.op("scalar", lambda e: e.activation(out=u[:], in_=x[:], func=AF.Identity, scale=w[:, cc, 2:3], bias=b[:, cc:cc + 1]), [x.r, w.r, b.r], [u.r])

    def taps(e):
        for (s0, n) in segs:
            for k, off in ((0, -2), (1, -1), (3, 1)):
                lo = max(0, -off)
                hi = n - max(0, off)
                last = e.scalar_tensor_tensor(out=u[:, s0 + lo:s0 + hi], in0=x[:, s0 + lo + off:s0 + hi + off], scalar=w[:, cc, k:k + 1],
                                              in1=u[:, s0 + lo:s0 + hi], op0=ALU.mult, op1=ALU.add)
        return last
    p.op(eng_extra, taps, [x.r, u.r, w.r], [u.r])


def emit_lru(kb, xT_d, gT_d, cw_d, cb_d, wbd_d, bias_d, lam_d, xs, row0, in_regs, outs):
    p = kb.p
    banks = kb.banks
    L = LAT
    cw = ld(kb, "cw", [128, 2, 4], cw_d)
    cb = ld(kb, "cb", [128, 2], cb_d)
    wbd = ld(kb, "wbd", [128, 8, 128], wbd_d)
    bias = ld(kb, "bias", [128, 8], bias_d)
    lam = ld(kb, "lam", [128, 4], lam_d)
    cl = kb.sb("cl", [128, 4])
    p.op("scalar", lambda e: e.activation(out=cl[:], in_=lam[:], func=AF.Exp, scale=-1.0), [lam.r], [cl.r])
    p.op("scalar", lambda e: e.activation(out=cl[:], in_=cl[:], func=AF.Ln, bias=1.0, scale=1.0), [cl.r], [cl.r])
    p.op("vector", lambda e: e.tensor_scalar(out=cl[:], in0=cl[:], scalar1=-8.0, scalar2=None, op0=ALU.mult), [cl.r], [cl.r])
    x = kb.sb("x", [128, T]); u = kb.sb("u", [128, T]); g = kb.sb("g", [128, T])
    av = [[kb.sb("a%d" % d, [128, T]), kb.sb("v%d" % d, [128, T])] for d in range(2)]
    hh = [kb.sb("h%d" % d, [128, T]) for d in range(2)]
    rt = [kb.sb("rt%d" % i, [128, 512]) for i in range(2)]
    it_ = [kb.sb("it%d" % i, [128, 512]) for i in range(2)]
    s2 = [kb.sb("s2%d" % i, [128, 512]) for i in range(2)]
    segs = [(0, NCTX), (NCTX, L)]
    chunks = [(0, NCTX)] + [(NCTX + c * 512, 512) for c in range(L // 512)]
    bc = 0
    for cc in range(2):
        p.dma("sync", x[:], xT_d[cc * 128:(cc + 1) * 128, :], reads=in_regs, writes=[x.r])
        p.dma("sync", g[:], gT_d[cc * 128:(cc + 1) * 128, :], reads=in_regs, writes=[g.r])
        emit_conv(kb, p, x, u, cw, cb, segs, cc)
        for d in range(2):
            a, v = av[d]
            for c, (t0, n) in enumerate(chunks):
                pr, pi = banks[bc % 8], banks[(bc + 1) % 8]
                bc += 2
                r_, i_, s_ = rt[c % 2], it_[c % 2], s2[c % 2]
                ia, ix = 0 * 4 + d * 2 + cc, 1 * 4 + d * 2 + cc
                p.op("tensor", lambda e, pr=pr, ia=ia, t0=t0, n=n: e.matmul(pr[:, 0:n], lhsT=wbd[:, ia, :], rhs=u[:, t0:t0 + n], start=True, stop=True), [wbd.r, u.r], [pr.r])
                p.op("tensor", lambda e, pi=pi, ix=ix, t0=t0, n=n: e.matmul(pi[:, 0:n], lhsT=wbd[:, ix, :], rhs=u[:, t0:t0 + n], start=True, stop=True), [wbd.r, u.r], [pi.r])
                p.op("scalar", lambda e, pr=pr, r_=r_, ia=ia, n=n: e.activation(out=r_[:, 0:n], in_=pr[:, 0:n], func=AF.Sigmoid, bias=bias[:, ia:ia + 1], scale=1.0), [pr.r, bias.r], [r_.r])
                p.op("scalar", lambda e, pi=pi, i_=i_, ix=ix, n=n: e.activation(out=i_[:, 0:n], in_=pi[:, 0:n], func=AF.Sigmoid, bias=bias[:, ix:ix + 1], scale=1.0), [pi.r, bias.r], [i_.r])
                p.op("scalar", lambda e, a=a, r_=r_, d=d, cc=cc, t0=t0, n=n: e.activation(out=a[:, t0:t0 + n], in_=r_[:, 0:n], func=AF.Exp, scale=cl[:, d * 2 + cc:d * 2 + cc + 1]), [r_.r, cl.r], [a.r])
                p.op("gpsimd", lambda e, a=a, s_=s_, t0=t0, n=n: e.tensor_tensor(out=s_[:, 0:n], in0=a[:, t0:t0 + n], in1=a[:, t0:t0 + n], op=ALU.mult), [a.r], [s_.r])
                p.op("scalar", lambda e, s_=s_, n=n: e.activation(out=s_[:, 0:n], in_=s_[:, 0:n], func=AF.Sqrt, scale=-1.0, bias=1.0), [s_.r], [s_.r])
                p.op("vector", lambda e, i_=i_, t0=t0, n=n: e.tensor_tensor(out=i_[:, 0:n], in0=i_[:, 0:n], in1=u[:, t0:t0 + n], op=ALU.mult), [i_.r, u.r], [i_.r])
                p.op("vector", lambda e, v=v, i_=i_, s_=s_, t0=t0, n=n: e.tensor_tensor(out=v[:, t0:t0 + n], in0=i_[:, 0:n], in1=s_[:, 0:n], op=ALU.mult), [i_.r, s_.r], [v.r])
            h = hh[d]
            if d == 0:
                p.op("vector", lambda e, a=a, v=v, h=h: e.tensor_tensor_scan(out=h[:, 0:NCTX], data0=a[:, 0:NCTX], data1=v[:, 0:NCTX], initial=0.0, op0=ALU.mult, op1=ALU.add), [a.r, v.r], [h.r])
                p.op("vector", lambda e, a=a, v=v, h=h: e.tensor_tensor_scan(out=h[:, NCTX:T], data0=a[:, NCTX:T], data1=v[:, NCTX:T], initial=h[:, NCTX - 1:NCTX], op0=ALU.mult, op1=ALU.add), [a.r, v.r, h.r], [h.r])
            else:
                p.op("vector", lambda e, a=a, v=v, h=h: e.tensor_tensor_scan(out=h[:, 0:NCTX][:, ::-1], data0=a[:, 0:NCTX][:, ::-1], data1=v[:, 0:NCTX][:, ::-1], initial=0.0, op0=ALU.mult, op1=ALU.add), [a.r, v.r], [h.r])
                p.op("vector", lambda e, a=a, v=v, h=h: e.tensor_tensor_scan(out=h[:, NCTX:T][:, ::-1], data0=a[:, NCTX:T][:, ::-1], data1=v[:, NCTX:T][:, ::-1], initial=h[:, 0:1], op0=ALU.mult, op1=ALU.add), [a.r, v.r, h.r], [h.r])
        z = av[0][0]
        p.op("gpsimd", lambda e: e.tensor_tensor(out=z[:], in0=g[:], in1=g[:], op=ALU.mult), [g.r], [z.r])
        p.op("vector", lambda e: e.tensor_scalar(out=z[:], in0=z[:], scalar1=0.044715, scalar2=1.0, op0=ALU.mult, op1=ALU.add), [z.r], [z.r])
        p.op("gpsimd", lambda e: e.tensor_tensor(out=z[:], in0=z[:], in1=g[:], op=ALU.mult), [z.r, g.r], [z.r])
        p.op("scalar", lambda e: e.activation(out=z[:], in_=z[:], func=AF.Sigmoid, scale=1.5957691216057308), [z.r], [z.r])
        p.op("gpsimd", lambda e: e.tensor_tensor(out=z[:], in0=z[:], in1=g[:], op=ALU.mult), [z.r, g.r], [z.r])
        p.op("vector", lambda e: e.tensor_tensor(out=hh[0][:], in0=hh[0][:], in1=hh[1][:], op=ALU.add), [hh[0].r, hh[1].r], [hh[0].r])
        p.op("vector", lambda e: e.tensor_tensor(out=hh[0][:], in0=hh[0][:], in1=z[:], op=ALU.mult), [hh[0].r, z.r], [hh[0].r])
        xs_write(p, xs, row0 + cc * 128, hh[0], [hh[0].r], outs)


def rev_segments(e, out_t, in_t, segs):
    for (s0, n) in segs:
        last = e.tensor_copy(out=out_t[:, s0:s0 + n], in_=in_t[:, s0:s0 + n][:, ::-1])
    return last


def scan_nat_lo(t0, n):
    if t0 < NCTX:
        return NCTX - (t0 + n)
    return NCTX + LAT - (t0 - NCTX + n)


def emit_gla(kb, qT_d, kT_d, vT_d, g1_d, rT_d, g2_d, gb_d, gn_d, cm_d, m2_d, hm_d, ident_d, xs, row0, in_regs, outs):
    p = kb.p
    banks = kb.banks
    L = LAT
    NP = T // 128
    ptr = kb.psbf(banks[7])
    plog, patt, pO, pkv = banks[0], banks[1:3], banks[3:5], banks[5:7]
    segs = [(0, NCTX), (NCTX, L)]
    g2 = ld(kb, "g2", [16, 2, 256], g2_d)
    ngb = ld(kb, "ngb", [128, 4], gb_d)
    p.op("vector", lambda e: e.tensor_scalar(out=ngb[:], in0=ngb[:], scalar1=-1.0, scalar2=None, op0=ALU.mult), [ngb.r], [ngb.r])
    gn = ld(kb, "gn", [128, 2], gn_d)
    cm = ld(kb, "cm", [128, 512], cm_d)
    m2 = ld(kb, "m2", [128, 128], m2_d)
    hm = ld(kb, "hm", [128, 2], hm_d)
    idf, idb, ones = consts(kb, ident_d)
    g1b = kb.sb("g1b", [16, 512])
    g1c = kb.sb("g1c", [16, 512])
    osb = [kb.sb("osb%d" % i, [128, T]) for i in range(4)]
    qT = kb.sb("qT", [128, T])
    kT = kb.sb("kT", [128, T])
    tmpT = kb.sb("tmpT", [128, T])
    vtb = kb.sb("vtb", [128, T], BF16)
    vb = kb.sb("vb", [128, NP, 128], BF16)
    F = lambda n: kb.sb(n, [128, 512])
    sp, gcs, dref, dlast, Aex, Bex, Dex, Eex = F("sp"), F("gcs"), F("dref"), F("dlast"), F("Aex"), F("Bex"), F("Dex"), F("Eex")
    dec = kb.sb("dec", [128, 8])
    Bq = lambda n: kb.sb(n, [128, 512], BF16)
    qt, kt, kd, qe = Bq("qt"), Bq("kt"), Bq("kd"), Bq("qe")
    kdT = [kb.sb("kdT%d" % i, [128, 2, 128], BF16) for i in range(2)]
    attm = [kb.sb("attm%d" % i, [128, 128], BF16) for i in range(2)]
    S = kb.sb("S", [128, 128])
    NSB = 4
    Sb = [kb.sb("Sb%d" % i, [128, 128], BF16) for i in range(NSB)]
    scale = 128.0 ** -0.5
    blocks = [(0, NCTX)] + [(NCTX + c * 512, 512) for c in range(L // 512)]
    pc = 0
    for hd in range(4):
        h, d = hd // 2, hd % 2
        for (dst, src) in ((qT, qT_d), (kT, kT_d)):
            if d == 0:
                p.dma("sync", dst[:], src[h * 128:(h + 1) * 128, :], reads=in_regs, writes=[dst.r])
            else:
                p.dma("sync", tmpT[:], src[h * 128:(h + 1) * 128, :], reads=in_regs, writes=[tmpT.r])
                p.op("gpsimd", lambda e, dst=dst: rev_segments(e, dst, tmpT, segs), [tmpT.r], [dst.r])
        p.dma("sync", tmpT[:], vT_d[h * 128:(h + 1) * 128, :], reads=in_regs, writes=[tmpT.r])
        if d == 0:
            p.op("gpsimd", lambda e: e.tensor_copy(out=vtb[:], in_=tmpT[:]), [tmpT.r], [vtb.r])
        else:
            p.op("gpsimd", lambda e: rev_segments(e, vtb, tmpT, segs), [tmpT.r], [vtb.r])
        for g in range((NP + 7) // 8):
            k0, k1 = g * 8, min(NP, g * 8 + 8)

            def trv(e, k0=k0, k1=k1):
                for kt_ in range(k0, k1):
                    last = e.transpose(out=ptr.t[:, (kt_ - k0) * 128:(kt_ - k0 + 1) * 128], in_=vtb[:, kt_ * 128:(kt_ + 1) * 128], identity=idb[:])
                return last
            p.op("tensor", trv, [vtb.r, idb.r], [ptr.r])
            p.op("scalar", lambda e, k0=k0, k1=k1: e.copy(out=vb[:, k0:k1, :], in_=ptr.t[:, 0:(k1 - k0) * 128].rearrange("p (a b) -> p a b", b=128)), [ptr.r], [vb.r])
        p.op("vector", lambda e: e.memset(S[:], 0.0), [], [S.r])
        sbi = 0
        p.op("gpsimd", lambda e, sb_=Sb[0]: e.memset(sb_[:], 0.0), [], [Sb[0].r])
        for (t0, n) in blocks:
            nc_ = n // 64
            v3 = lambda t, n=n: t[:, 0:n].rearrange("p (c l) -> p c l", l=64)
            if d == 0:
                p.dma("sync", g1b[:, 0:n], g1_d[0:16, t0:t0 + n], reads=in_regs, writes=[g1b.r])
                gsrc = g1b
            else:
                lo = scan_nat_lo(t0, n)
                p.dma("sync", g1c[:, 0:n], g1_d[16:32, lo:lo + n], reads=in_regs, writes=[g1c.r])
                p.op("vector", lambda e, n=n: e.tensor_copy(out=g1b[:, 0:n], in_=g1c[:, 0:n][:, ::-1]), [g1c.r], [g1b.r])
                gsrc = g1b
            p.op("tensor", lambda e, d=d, h=h, n=n: e.matmul(plog[:, 0:n], lhsT=g2[:, d, h * 128:(h + 1) * 128], rhs=g1b[:, 0:n], start=True, stop=True), [g2.r, g1b.r], [plog.r])
            p.op("scalar", lambda e, hd=hd, n=n: e.activation(out=sp[:, 0:n], in_=plog[:, 0:n], func=AF.Exp, scale=-1.0, bias=ngb[:, hd:hd + 1]), [plog.r, ngb.r], [sp.r])
            p.op("scalar", lambda e, n=n: e.activation(out=sp[:, 0:n], in_=sp[:, 0:n], func=AF.Ln, scale=1.0, bias=1.0), [sp.r], [sp.r])
            p.op("vector", lambda e, n=n: e.tensor_tensor_scan(out=gcs[:, 0:n], data0=cm[:, 0:n], data1=sp[:, 0:n], initial=0.0, op0=ALU.mult, op1=ALU.add), [cm.r, sp.r], [gcs.r])
            p.op("vector", lambda e, n=n, nc_=nc_, v3=v3: e.tensor_tensor(out=v3(dref), in0=v3(gcs), in1=v3(gcs)[:, :, 32:33].to_broadcast([128, nc_, 64]), op=ALU.subtract), [gcs.r], [dref.r])
            p.op("gpsimd", lambda e, n=n, nc_=nc_, v3=v3: e.tensor_tensor(out=v3(dlast), in0=v3(gcs), in1=v3(gcs)[:, :, 63:64].to_broadcast([128, nc_, 64]), op=ALU.subtract), [gcs.r], [dlast.r])
            A = lambda fn, rd, wr: p.op("scalar", fn, rd, wr)
            A(lambda e, n=n: e.activation(out=Aex[:, 0:n], in_=dref[:, 0:n], func=AF.Exp, scale=-1.0 / 16), [dref.r], [Aex.r])
            A(lambda e, n=n: e.activation(out=Bex[:, 0:n], in_=dref[:, 0:n], func=AF.Exp, scale=1.0 / 16), [dref.r], [Bex.r])
            A(lambda e, n=n: e.activation(out=Dex[:, 0:n], in_=dlast[:, 0:n], func=AF.Exp, scale=1.0 / 16), [dlast.r], [Dex.r])
            A(lambda e, n=n: e.activation(out=Eex[:, 0:n], in_=gcs[:, 0:n], func=AF.Exp, scale=-1.0 / 16), [gcs.r], [Eex.r])
            A(lambda e, nc_=nc_, v3=v3: e.activation(out=dec[:, 0:nc_], in_=v3(gcs)[:, :, 63], func=AF.Exp, scale=-1.0 / 16), [gcs.r], [dec.r])
            p.op("vector", lambda e, t0=t0, n=n: e.scalar_tensor_tensor(out=qt[:, 0:n], in0=qT[:, t0:t0 + n], scalar=scale, in1=Aex[:, 0:n], op0=ALU.mult, op1=ALU.mult), [qT.r, Aex.r], [qt.r])
            p.op("gpsimd", lambda e, t0=t0, n=n: e.tensor_tensor(out=kt[:, 0:n], in0=kT[:, t0:t0 + n], in1=Bex[:, 0:n], op=ALU.mult), [kT.r, Bex.r], [kt.r])
            p.op("vector", lambda e, t0=t0, n=n: e.tensor_tensor(out=kd[:, 0:n], in0=kT[:, t0:t0 + n], in1=Dex[:, 0:n], op=ALU.mult), [kT.r, Dex.r], [kd.r])
            p.op("vector", lambda e, t0=t0, n=n: e.scalar_tensor_tensor(out=qe[:, 0:n], in0=qT[:, t0:t0 + n], scalar=scale, in1=Eex[:, 0:n], op0=ALU.mult, op1=ALU.mult), [qT.r, Eex.r], [qe.r])
            for pp in range(n // 128):
                s = pp * 128
                gp = (t0 + s) // 128
                kT_, am, pa, po, pk = kdT[pc % 2], attm[pc % 2], patt[pc % 2], pO[pc % 2], pkv[pc % 2]
                tr = ptr.t[:, (pc % 2) * 128:(pc % 2) * 128 + 128]
                pc += 1
                p.op("tensor", lambda e, tr=tr, s=s: e.transpose(out=tr, in_=kd[:, s:s + 128], identity=idb[:]), [kd.r, idb.r], [ptr.r])

                def cpk(e, tr=tr, kT_=kT_):
                    e.activation(out=kT_[:, 0, :], in_=tr, func=AF.Identity, scale=hm[:, 0:1])
                    return e.activation(out=kT_[:, 1, :], in_=tr, func=AF.Identity, scale=hm[:, 1:2])
                p.op("scalar", cpk, [ptr.r, hm.r], [kT_.r])
                p.op("tensor", lambda e, pa=pa, s=s: e.matmul(pa[:, 0:128], lhsT=kt[:, s:s + 128], rhs=qt[:, s:s + 128], start=True, stop=True), [kt.r, qt.r], [pa.r])
                p.op("vector", lambda e, pa=pa, am=am: e.tensor_tensor(out=am[:], in0=pa[:, 0:128], in1=m2[:], op=ALU.mult), [pa.r, m2.r], [am.r])

                def mkv(e, pk=pk, kT_=kT_, gp=gp):
                    e.matmul(pk[:, 0:128], lhsT=kT_[:, 0, :], rhs=vb[:, gp, :], start=True, stop=True)
                    return e.matmul(pk[:, 128:256], lhsT=kT_[:, 1, :], rhs=vb[:, gp, :], start=True, stop=True)
                p.op("tensor", mkv, [kT_.r, vb.r], [pk.r])
                sb0 = Sb[sbi % NSB]
                sb1 = Sb[(sbi + 1) % NSB]
                sb2 = Sb[(sbi + 2) % NSB]
                sbi += 2
                c0 = s // 64
                p.op("vector", lambda e, pk=pk, c0=c0: e.scalar_tensor_tensor(out=S[:], in0=S[:], scalar=dec[:, c0:c0 + 1], in1=pk[:, 0:128], op0=ALU.mult, op1=ALU.add), [S.r, dec.r, pk.r], [S.r])
                p.op("scalar", lambda e, sb1=sb1: e.copy(out=sb1[:], in_=S[:]), [S.r], [sb1.r])
                p.op("vector", lambda e, pk=pk, c0=c0: e.scalar_tensor_tensor(out=S[:], in0=S[:], scalar=dec[:, c0 + 1:c0 + 2], in1=pk[:, 128:256], op0=ALU.mult, op1=ALU.add), [S.r, dec.r, pk.r], [S.r])
                p.op("scalar", lambda e, sb2=sb2: e.copy(out=sb2[:], in_=S[:]), [S.r], [sb2.r])

                def mo(e, po=po, am=am, gp=gp, sb0=sb0, sb1=sb1, s=s):
                    e.matmul(po[:, 0:128], lhsT=vb[:, gp, :], rhs=am[:], start=True, stop=False)
                    e.matmul(po[:, 0:64], lhsT=sb0[:], rhs=qe[:, s:s + 64], start=False, stop=False)
                    return e.matmul(po[:, 64:128], lhsT=sb1[:], rhs=qe[:, s + 64:s + 128], start=False, stop=True)
                p.op("tensor", mo, [vb.r, am.r, sb0.r, sb1.r, qe.r], [po.r])
                p.op("scalar", lambda e, po=po, hd=hd, a=t0 + s: e.copy(out=osb[hd][:, a:a + 128], in_=po[:, 0:128]), [po.r], [osb[hd].r])
    rt = kb.sb("rt", [128, 512])
    yy = [kb.sb("yy%d" % i, [128, 512]) for i in range(2)]
    pss = banks[0]
    bi = 0
    for h in range(2):
        of, ob = osb[2 * h], osb[2 * h + 1]

        def comb(e, of=of, ob=ob):
            for (s0, n) in segs:
                last = e.tensor_tensor(out=of[:, s0:s0 + n], in0=of[:, s0:s0 + n], in1=ob[:, s0:s0 + n][:, ::-1], op=ALU.add)
            return last
        p.op("vector", comb, [of.r, ob.r], [of.r])
        for (t0, n) in blocks:
            y_ = yy[bi % 2]
            bi += 1
            p.op("scalar", lambda e, of=of, t0=t0, n=n: e.activation(out=sp[:, 0:n], in_=of[:, t0:t0 + n], func=AF.Square), [of.r], [sp.r])
            p.op("tensor", lambda e, n=n: e.matmul(pss[:, 0:n], lhsT=ones[:], rhs=sp[:, 0:n], start=True, stop=True), [ones.r, sp.r], [pss.r])
            p.op("vector", lambda e, n=n: e.tensor_scalar(out=gcs[:, 0:n], in0=pss[:, 0:n], scalar1=1.0 / 128, scalar2=EPS, op0=ALU.mult, op1=ALU.add), [pss.r], [gcs.r])
            p.op("vector", lambda e, n=n: e.reciprocal(out=gcs[:, 0:n], in_=gcs[:, 0:n]), [gcs.r], [gcs.r])
            p.op("scalar", lambda e, n=n: e.activation(out=gcs[:, 0:n], in_=gcs[:, 0:n], func=AF.Sqrt), [gcs.r], [gcs.r])
            p.dma("sync", rt[:, 0:n], rT_d[h * 128:(h + 1) * 128, t0:t0 + n], reads=in_regs, writes=[rt.r])
            p.op("scalar", lambda e, n=n: e.activation(out=rt[:, 0:n], in_=rt[:, 0:n], func=AF.Silu), [rt.r], [rt.r])
            p.op("vector", lambda e, of=of, y_=y_, t0=t0, n=n: e.tensor_tensor(out=y_[:, 0:n], in0=of[:, t0:t0 + n], in1=gcs[:, 0:n], op=ALU.mult), [of.r, gcs.r], [y_.r])
            p.op("vector", lambda e, h=h, y_=y_, n=n: e.scalar_tensor_tensor(out=y_[:, 0:n], in0=y_[:, 0:n], scalar=gn[:, h:h + 1], in1=rt[:, 0:n], op0=ALU.mult, op1=ALU.mult), [y_.r, gn.r, rt.r], [y_.r])
            for (a, m, hh_, loc) in nat_pieces(t0, n):
                o = Reg("o")
                outs.append(o)
                p.dma("gpsimd", xs[hh_ * 3072 + row0 + h * 128:hh_ * 3072 + row0 + (h + 1) * 128, loc:loc + m], y_[:, a - t0:a - t0 + m], reads=[y_.r], writes=[o])


def emit_ssd(kb, xbc_d, z_d, dt_d, cw_d, cb_d, dtb_d, alog_d, Dp_d, U_d, ident_d, xs, row0, in_regs, outs):
    p = kb.p
    banks = kb.banks
    L = LAT
    NCH = T // 128
    ptr = kb.psbf(banks[7])
    psm, pCB, pbc, pdiag, poff, pst = banks[0], banks[1], banks[2:4], banks[4], banks[5], banks[6]
    cw = ld(kb, "cw", [128, 4, 4], cw_d)
    cb = ld(kb, "cb", [128, 4], cb_d)
    dtb = ld(kb, "dtb", [4, 2], dtb_d)
    aneg = ld(kb, "aneg", [4, 2], alog_d)
    p.op("scalar", lambda e: e.activation(out=aneg[:], in_=aneg[:], func=AF.Exp), [aneg.r], [aneg.r])
    p.op("vector", lambda e: e.tensor_scalar(out=aneg[:], in0=aneg[:], scalar1=-1.0, scalar2=None, op0=ALU.mult), [aneg.r], [aneg.r])
    Dp = ld(kb, "Dp", [128, 2], Dp_d)
    U = ld(kb, "U", [128, 128], U_d)
    idf, idb, ones = consts(kb, ident_d)
    segs = [(0, NCTX), (NCTX, L)]
    arr = [[kb.sb("arr%d_%d" % (d, k), [128, T], BF16) for k in range(4)] for d in range(2)]
    xin = [kb.sb("xin%d" % i, [128, T]) for i in range(2)]
    u = kb.sb("u", [128, T])
    for k in range(4):
        xi = xin[k % 2]
        p.dma("sync", xi[:], xbc_d[k * 128:(k + 1) * 128, :], reads=in_regs, writes=[xi.r])
        emit_conv(kb, p, xi, u, cw, cb, segs, k)
        p.op("scalar", lambda e, k=k: e.activation(out=arr[0][k][:], in_=u[:], func=AF.Silu), [u.r], [arr[0][k].r])
        p.op("gpsimd", lambda e, k=k: rev_segments(e, arr[1][k], arr[0][k], segs), [arr[0][k].r], [arr[1][k].r])
    yacc = [xin[0], xin[1]]
    dtp = kb.sb("dtp", [4, 2, T])
    F = lambda n, s, dt=F32: kb.sb(n, s, dt)
    xBt = [F("xBt%d" % i, [128, 384], BF16) for i in range(2)]
    dtk = [F("dtk%d" % i, [128, 8]) for i in range(2)]
    acl = [F("acl%d" % i, [128, 8]) for i in range(2)]
    nac = [F("nac%d" % i, [128, 4]) for i in range(2)]
    eac = [F("eac%d" % i, [128, 4]) for i in range(2)]
    wdt = [F("wdt%d" % i, [128, 4]) for i in range(2)]
    dcy = [F("dcy%d" % i, [128, 4]) for i in range(2)]
    dtx = [F("dtx%d" % i, [128, 256], BF16) for i in range(2)]
    xw = [F("xw%d" % i, [128, 256], BF16) for i in range(2)]
    rU = [F("rU%d" % i, [128, 128]) for i in range(2)]
    sg = [F("sg%d" % i, [128, 128]) for i in range(2)]
    Lt = [F("Lt%d" % i, [128, 128]) for i in range(2)]
    Mb = [F("Mb%d" % i, [128, 128], BF16) for i in range(4)]
    CBs = [F("CBs%d" % i, [128, 128]) for i in range(2)]
    yt1 = [F("yt1_%d" % i, [128, 256]) for i in range(2)]
    yt2 = [F("yt2_%d" % i, [128, 256]) for i in range(2)]
    H = F("H", [128, 256])
    Hb = [F("Hb%d" % i, [128, 256], BF16) for i in range(2)]
    it = 0
    hc = 0
    for d in range(2):
        slot = 0 if d == 0 else 1
        p.dma("sync", dtp[:, slot, :], dt_d[d * 4:(d + 1) * 4, :], reads=in_regs, writes=[dtp.r])
        p.op("scalar", lambda e, slot=slot, d=d: e.activation(out=dtp[:, slot, :], in_=dtp[:, slot, :], func=AF.Exp, bias=dtb[:, d:d + 1], scale=1.0), [dtp.r, dtb.r], [dtp.r])
        p.op("scalar", lambda e, slot=slot: e.activation(out=dtp[:, slot, :], in_=dtp[:, slot, :], func=AF.Ln, bias=1.0, scale=1.0), [dtp.r], [dtp.r])
        if d == 1:
            def revdt(e):
                for (s0, n) in segs:
                    last = e.tensor_copy(out=dtp[:, 0, s0:s0 + n], in_=dtp[:, 1, s0:s0 + n][:, ::-1])
                return last
            p.op("vector", revdt, [dtp.r], [dtp.r])
        p.op("vector", lambda e, d=d: e.tensor_scalar(out=dtp[:, 1, :], in0=dtp[:, 0, :], scalar1=aneg[:, d:d + 1], scalar2=None, op0=ALU.mult), [dtp.r, aneg.r], [dtp.r])
        p.op("vector", lambda e: e.memset(H[:], 0.0), [], [H.r])
        p.op("gpsimd", lambda e, hb=Hb[hc % 2]: e.memset(hb[:], 0.0), [], [Hb[hc % 2].r])
        A = arr[d]
        for c in range(NCH):
            s = c * 128
            i2 = it % 2
            it += 1
            xb_, dk, ac, na, ea, wd, dc, dx, xw_ = xBt[i2], dtk[i2], acl[i2], nac[i2], eac[i2], wdt[i2], dcy[i2], dtx[i2], xw[i2]
            y1, y2, cbs = yt1[i2], yt2[i2], CBs[i2]
            hb_cur = Hb[hc % 2]
            hb_nxt = Hb[(hc + 1) % 2]
            hc += 1

            def trx(e, s=s, A=A):
                e.transpose(out=ptr.t[:, 0:128], in_=A[0][:, s:s + 128], identity=idb[:])
                e.transpose(out=ptr.t[:, 128:256], in_=A[1][:, s:s + 128], identity=idb[:])
                return e.transpose(out=ptr.t[:, 256:384], in_=A[2][:, s:s + 128], identity=idb[:])
            p.op("tensor", trx, [A[0].r, A[1].r, A[2].r, idb.r], [ptr.r])
            p.op("scalar", lambda e, xb_=xb_: e.copy(out=xb_[:], in_=ptr.t[:, 0:384]), [ptr.r], [xb_.r])

            def trd(e, s=s):
                e.transpose(out=psm[:, 0:4], in_=dtp[:, 0, s:s + 128], identity=idf[0:4, 0:4])
                return e.transpose(out=psm[:, 4:8], in_=dtp[:, 1, s:s + 128], identity=idf[0:4, 0:4])
            p.op("tensor", trd, [dtp.r, idf.r], [psm.r])
            p.op("vector", lambda e, dk=dk: e.tensor_copy(out=dk[:], in_=psm[:, 0:8]), [psm.r], [dk.r])

            def mac(e, dk=dk):
                e.matmul(psm[:, 8:12], lhsT=U[:], rhs=dk[:, 4:8], start=True, stop=True)
                return e.matmul(psm[:, 12:16], lhsT=ones[:], rhs=dk[:, 4:8], start=True, stop=True)
            p.op("tensor", mac, [U.r, ones.r, dk.r], [psm.r])
            p.op("vector", lambda e, ac=ac: e.tensor_copy(out=ac[:], in_=psm[:, 8:16]), [psm.r], [ac.r])
            p.op("vector", lambda e, ac=ac, na=na: e.tensor_scalar(out=na[:], in0=ac[:, 0:4], scalar1=-1.0, scalar2=None, op0=ALU.mult), [ac.r], [na.r])
            p.op("scalar", lambda e, ac=ac, ea=ea: e.activation(out=ea[:], in_=ac[:, 0:4], func=AF.Exp), [ac.r], [ea.r])
            p.op("scalar", lambda e, ac=ac, dc=dc: e.activation(out=dc[:], in_=ac[:, 4:8], func=AF.Exp), [ac.r], [dc.r])
            p.op("vector", lambda e, ac=ac, wd=wd: e.tensor_tensor(out=wd[:], in0=ac[:, 4:8], in1=ac[:, 0:4], op=ALU.subtract), [ac.r], [wd.r])
            p.op("scalar", lambda e, wd=wd: e.activation(out=wd[:], in_=wd[:], func=AF.Exp), [wd.r], [wd.r])
            p.op("vector", lambda e, wd=wd, dk=dk: e.tensor_tensor(out=wd[:], in0=wd[:], in1=dk[:, 0:4], op=ALU.mult), [wd.r, dk.r], [wd.r])
            x3 = lambda t: t[:, 0:256].rearrange("p (h q) -> p h q", q=64)
            p.op("vector", lambda e, dx=dx, xb_=xb_, dk=dk, x3=x3: e.tensor_tensor(out=x3(dx), in0=x3(xb_), in1=dk[:, 0:4].unsqueeze(2).to_broadcast([128, 4, 64]), op=ALU.mult), [xb_.r, dk.r], [dx.r])
            p.op("vector", lambda e, xw_=xw_, xb_=xb_, wd=wd, x3=x3: e.tensor_tensor(out=x3(xw_), in0=x3(xb_), in1=wd[:].unsqueeze(2).to_broadcast([128, 4, 64]), op=ALU.mult), [xb_.r, wd.r], [xw_.r])
            p.op("tensor", lambda e, s=s, A=A: e.matmul(pCB[:, 0:128], lhsT=A[2][:, s:s + 128], rhs=A[3][:, s:s + 128], start=True, stop=True), [A[2].r, A[3].r], [pCB.r])
            p.op("scalar", lambda e, cbs=cbs: e.copy(out=cbs[:], in_=pCB[:, 0:128]), [pCB.r], [cbs.r])
            for h in range(4):
                j2 = h % 2
                ru, sg_, lt, pb = rU[j2], sg[j2], Lt[j2], pbc[j2]
                mb = Mb[h]
                p.op("vector", lambda e, ru=ru, dk=dk, h=h: e.tensor_scalar(out=ru[:], in0=U[:], scalar1=dk[:, 4 + h:5 + h], scalar2=None, op0=ALU.mult), [U.r, dk.r], [ru.r])
                p.op("tensor", lambda e, pb=pb, ru=ru: e.matmul(pb[:, 0:128], lhsT=ones[:], rhs=ru[:], start=True, stop=True), [ones.r, ru.r], [pb.r])
                p.op("vector", lambda e, sg_=sg_, pb=pb, na=na, h=h: e.tensor_scalar(out=sg_[:], in0=pb[:, 0:128], scalar1=na[:, h:h + 1], scalar2=0.0, op0=ALU.add, op1=ALU.min), [pb.r, na.r], [sg_.r])
                p.op("scalar", lambda e, lt=lt, sg_=sg_: e.activation(out=lt[:], in_=sg_[:], func=AF.Exp), [sg_.r], [lt.r])
                p.op("gpsimd", lambda e, lt=lt: e.tensor_tensor(out=lt[:], in0=lt[:], in1=U[:], op=ALU.mult), [lt.r, U.r], [lt.r])
                p.op("vector", lambda e, lt=lt, cbs=cbs, mb=mb: e.tensor_tensor(out=mb[:], in0=lt[:], in1=cbs[:], op=ALU.mult), [lt.r, cbs.r], [mb.r])
                p.op("tensor", lambda e, mb=mb, dx=dx, h=h: e.matmul(pdiag[:, h * 64:(h + 1) * 64], lhsT=mb[:], rhs=dx[:, h * 64:(h + 1) * 64], start=True, stop=True), [mb.r, dx.r], [pdiag.r])
            p.op("tensor", lambda e, s=s, A=A, hb_cur=hb_cur: e.matmul(poff[:, 0:256], lhsT=A[3][:, s:s + 128], rhs=hb_cur[:], start=True, stop=True), [A[3].r, hb_cur.r], [poff.r])
            p.op("tensor", lambda e, xb_=xb_, xw_=xw_: e.matmul(pst[:, 0:256], lhsT=xb_[:, 256:384], rhs=xw_[:], start=True, stop=True), [xb_.r, xw_.r], [pst.r])
            H3 = H[:].rearrange("p (h q) -> p h q", q=64)
            p.op("vector", lambda e, dc=dc, H3=H3: e.tensor_tensor(out=H3, in0=H3, in1=dc[:].unsqueeze(2).to_broadcast([128, 4, 64]), op=ALU.mult), [H.r, dc.r], [H.r])
            p.op("vector", lambda e: e.tensor_tensor(out=H[:], in0=H[:], in1=pst[:, 0:256], op=ALU.add), [H.r, pst.r], [H.r])
            p.op("scalar", lambda e, hb_nxt=hb_nxt: e.copy(out=hb_nxt[:], in_=H[:]), [H.r], [hb_nxt.r])
            p.op("scalar", lambda e, y1=y1: e.copy(out=y1[:], in_=pdiag[:, 0:256]), [pdiag.r], [y1.r])
            p.op("vector", lambda e, y2=y2, ea=ea, x3=x3: e.tensor_tensor(out=x3(y2), in0=poff[:, 0:256].rearrange("p (h q) -> p h q", q=64),
                                                                        in1=ea[:].unsqueeze(2).to_broadcast([128, 4, 64]), op=ALU.mult), [poff.r, ea.r], [y2.r])
            p.op("gpsimd", lambda e, y1=y1, y2=y2: e.tensor_tensor(out=y2[:], in0=y2[:], in1=y1[:], op=ALU.add), [y1.r, y2.r], [y2.r])
            for k in range(2):
                pb = pbc[k]
                p.op("tensor", lambda e, pb=pb, y2=y2, k=k: e.transpose(out=pb[:, 128:256], in_=y2[:, k * 128:(k + 1) * 128], identity=idf[:]), [y2.r, idf.r], [pb.r])
                if d == 0:
                    p.op("scalar", lambda e, pb=pb, k=k, s=s: e.copy(out=yacc[k][:, s:s + 128], in_=pb[:, 128:256]), [pb.r], [yacc[k].r])
                else:
                    lo = scan_nat_lo(s, 128)
                    p.op("vector", lambda e, pb=pb, k=k, lo=lo: e.tensor_tensor(out=yacc[k][:, lo:lo + 128][:, ::-1], in0=yacc[k][:, lo:lo + 128][:, ::-1], in1=pb[:, 128:256], op=ALU.add),
                         [pb.r, yacc[k].r], [yacc[k].r])
    zt = u
    for k in range(2):
        p.op("vector", lambda e, k=k: e.scalar_tensor_tensor(out=yacc[k][:], in0=arr[0][k][:], scalar=Dp[:, k:k + 1], in1=yacc[k][:], op0=ALU.mult, op1=ALU.add),
             [arr[0][k].r, Dp.r, yacc[k].r], [yacc[k].r])
        p.dma("sync", zt[:], z_d[k * 128:(k + 1) * 128, :], reads=in_regs, writes=[zt.r])
        p.op("scalar", lambda e: e.activation(out=zt[:], in_=zt[:], func=AF.Silu), [zt.r], [zt.r])
        p.op("vector", lambda e, k=k: e.tensor_tensor(out=yacc[k][:], in0=yacc[k][:], in1=zt[:], op=ALU.mult), [yacc[k].r, zt.r], [yacc[k].r])
        xs_write(p, xs, row0 + k * 128, yacc[k], [yacc[k].r], outs)


def emit_b1(kb, xg, xT_d, cT, adaw, adab, wbr_d, wo_d, sn_d, bm_d, xm_d, in_regs, outs, TOK=HT):
    p = kb.p
    banks = kb.banks
    XR = 3072
    nch = (TOK + 511) // 512
    chunks = [(c * 512, min(512, TOK - c * 512)) for c in range(nch)]
    ya = kb.sb("ystg_a", [128, 8192])
    ystg = kb.sub(ya, 0, [128, 16, 512], F32, "ystg")
    mod = emit_mods(kb, adaw, adab, 8, cT, kb.sub(ya, 0, [128, 8, 512], F32, "awstg"), banks[0])
    bm = ld(kb, "bm", [128, 2], bm_d)
    ssdn = ld(kb, "ssdn", [128, 4], sn_d)
    ones = kb.sb("ones", [128, 128])
    p.op("vector", lambda e: e.memset(ones[:], 1.0), [], [ones.r])
    wbb = kb.sb("wbb", [128, 16, 1024], BF16)
    wob = kb.sb("wob", [128, 8, 1024], BF16)
    wst = [kb.sb("wst%d" % i, [128, 1024]) for i in range(2)]
    wbr_r = [Reg("wbr%d" % k) for k in range(16)]
    wo_r = [Reg("wo%d" % k) for k in range(8)]
    for k in range(24):
        st = wst[k % 2]
        src = wbr_d[k * 128:(k + 1) * 128, :] if k < 16 else wo_d[(k - 16) * 128:(k - 15) * 128, :]
        p.dma("sync", st[:], src, writes=[st.r])
        if k < 16:
            p.op("gpsimd", lambda e, st=st, k=k: e.tensor_copy(out=wbb[:, k, :], in_=st[:]), [st.r], [wbr_r[k]])
        else:
            p.op("gpsimd", lambda e, st=st, k=k: e.tensor_copy(out=wob[:, k - 16, :], in_=st[:]), [st.r], [wo_r[k - 16]])
    y2 = [kb.sb("y2_%d" % i, [128, 4, 512]) for i in range(2)]
    yb = kb.sb("yb", [128, 16, 512], BF16)
    xt = kb.sb("xt", [128, 8, 512])
    gts = [kb.sb("gt%d" % i, [128, 4, 512]) for i in range(2)]
    gt2 = [kb.sb("gu%d" % i, [128, 4, 512]) for i in range(2)]
    accs = [kb.sb("acc%d" % i, [128, 512]) for i in range(2)]
    tmps = [kb.sb("tmp%d" % i, [128, 512]) for i in range(2)]
    aT = kb.sb("aT", [128, 8, 512], BF16)
    pz = banks[0:4]
    po = banks[4:6]
    pss = banks[6]
    zc = 0
    gc = 0
    yc = 0

    def yparts(kc, h):
        n_, r_, j_ = kc // 4, (kc // 2) % 2, kc % 2
        return gath_parts(3072, n_ * 256 + j_ * 128, r_, h * 6144)

    def gparts(nb, oc, h):
        return gath_parts(3072, 1024 + (nb % 2) * 1024 + oc * 128, nb // 2, h * 6144)

    for c, (t0, n) in enumerate(chunks):
        for g4 in range(4):
            yy = y2[yc % 2]
            yc += 1
            for j in range(4):
                for (row, cnt, po_) in yparts(g4 * 4 + j, 0):
                    p.dma("sync", ystg.t[po_:po_ + cnt, g4 * 4 + j, 0:n], xg[row:row + cnt, t0:t0 + n], reads=in_regs, writes=[ystg.r])
                for (row, cnt, po_) in yparts(g4 * 4 + j, 1):
                    p.dma("sync", yy.t[po_:po_ + cnt, j, 0:n], xg[row:row + cnt, t0:t0 + n], reads=in_regs, writes=[yy.r])
            p.op("vector", lambda e, g4=g4, n=n: e.tensor_scalar(out=ystg[:, g4 * 4:g4 * 4 + 4, 0:n], in0=ystg[:, g4 * 4:g4 * 4 + 4, 0:n], scalar1=bm[:, 0:1], scalar2=None, op0=ALU.mult),
                 [ystg.r, bm.r], [ystg.r])
            p.op("vector", lambda e, g4=g4, yy=yy, n=n: e.scalar_tensor_tensor(out=ystg[:, g4 * 4:g4 * 4 + 4, 0:n], in0=yy[:, :, 0:n], scalar=bm[:, 1:2], in1=ystg[:, g4 * 4:g4 * 4 + 4, 0:n],
                                                                             op0=ALU.mult, op1=ALU.add), [ystg.r, yy.r, bm.r], [ystg.r])
        for kc in range(4):
            sq = tmps[kc % 2]
            p.op("scalar", lambda e, sq=sq, kc=kc, n=n: e.activation(out=sq[:, 0:n], in_=ystg[:, kc, 0:n], func=AF.Square), [ystg.r], [sq.r])
            p.op("tensor", lambda e, sq=sq, kc=kc, n=n: e.matmul(pss[:, 0:n], lhsT=ones[:], rhs=sq[:, 0:n], start=(kc == 0), stop=(kc == 3)), [ones.r, sq.r], [pss.r])
        rs = accs[0]
        p.op("vector", lambda e, rs=rs, n=n: e.tensor_scalar(out=rs[:, 0:n], in0=pss[:, 0:n], scalar1=1.0 / 512, scalar2=EPS, op0=ALU.mult, op1=ALU.add), [pss.r], [rs.r])
        p.op("vector", lambda e, rs=rs, n=n: e.reciprocal(out=rs[:, 0:n], in_=rs[:, 0:n]), [rs.r], [rs.r])
        p.op("scalar", lambda e, rs=rs, n=n: e.activation(out=rs[:, 0:n], in_=rs[:, 0:n], func=AF.Sqrt), [rs.r], [rs.r])

        def nrm(e, rs=rs, n=n):
            for kc in range(4):
                last = e.scalar_tensor_tensor(out=ystg[:, kc, 0:n], in0=ystg[:, kc, 0:n], scalar=ssdn[:, kc:kc + 1], in1=rs[:, 0:n], op0=ALU.mult, op1=ALU.mult)
            return last
        p.op("vector", nrm, [ystg.r, ssdn.r, rs.r], [ystg.r])
        p.op("gpsimd", lambda e, n=n: e.tensor_copy(out=yb[:, :, 0:n], in_=ystg[:, :, 0:n]), [ystg.r], [yb.r])
        p.dma("sync", xt[:, :, 0:n], xT_d[:, t0:t0 + n].rearrange("(kc p) t -> p kc t", p=128), reads=in_regs, writes=[xt.r])
        for oc in range(8):
            gt, gu = gts[gc % 2], gt2[gc % 2]
            acc = accs[gc % 2]
            gc += 1
            for nb in range(4):
                for (row, cnt, po_) in gparts(nb, oc, 0):
                    p.dma("sync", gt.t[po_:po_ + cnt, nb, 0:n], xg[row:row + cnt, t0:t0 + n], reads=in_regs, writes=[gt.r])
                for (row, cnt, po_) in gparts(nb, oc, 1):
                    p.dma("sync", gu.t[po_:po_ + cnt, nb, 0:n], xg[row:row + cnt, t0:t0 + n], reads=in_regs, writes=[gu.r])
            p.op("gpsimd", lambda e, gt=gt, n=n: e.tensor_scalar(out=gt[:, :, 0:n], in0=gt[:, :, 0:n], scalar1=bm[:, 0:1], scalar2=None, op0=ALU.mult), [gt.r, bm.r], [gt.r])
            p.op("vector", lambda e, gt=gt, gu=gu, n=n: e.scalar_tensor_tensor(out=gt[:, :, 0:n], in0=gu[:, :, 0:n], scalar=bm[:, 1:2], in1=gt[:, :, 0:n], op0=ALU.mult, op1=ALU.add),
                 [gt.r, gu.r, bm.r], [gt.r])
            p.op("scalar", lambda e, gt=gt, n=n: e.activation(out=gt[:, :, 0:n], in_=gt[:, :, 0:n], func=AF.Sigmoid), [gt.r], [gt.r])
            for nb in range(4):
                z = pz[zc % 4]
                zc += 1

                def mmz(e, z=z, nb=nb, oc=oc, n=n):
                    for kc in range(4):
                        last = e.matmul(z[:, 0:n], lhsT=wbb[:, nb * 4 + kc, oc * 128:(oc + 1) * 128], rhs=yb[:, nb * 4 + kc, 0:n], start=(kc == 0), stop=(kc == 3))
                    return last
                p.op("tensor", mmz, wbr_r[nb * 4:nb * 4 + 4] + [yb.r], [z.r])
                if nb == 0:
                    p.op("vector", lambda e, z=z, gt=gt, acc=acc, n=n: e.tensor_tensor(out=acc[:, 0:n], in0=z[:, 0:n], in1=gt[:, 0, 0:n], op=ALU.mult), [z.r, gt.r], [acc.r])
                else:
                    tmp = tmps[nb % 2]
                    p.op("vector", lambda e, z=z, gt=gt, tmp=tmp, nb=nb, n=n: e.tensor_tensor(out=tmp[:, 0:n], in0=z[:, 0:n], in1=gt[:, nb, 0:n], op=ALU.mult), [z.r, gt.r], [tmp.r])
                    if nb < 3:
                        p.op("gpsimd", lambda e, tmp=tmp, acc=acc, n=n: e.tensor_tensor(out=acc[:, 0:n], in0=acc[:, 0:n], in1=tmp[:, 0:n], op=ALU.add), [acc.r, tmp.r], [acc.r])
                    else:
                        p.op("gpsimd", lambda e, tmp=tmp, acc=acc, oc=oc, n=n: e.tensor_tensor(out=aT[:, oc, 0:n], in0=acc[:, 0:n], in1=tmp[:, 0:n], op=ALU.add), [acc.r, tmp.r], [aT.r])
        for oc in range(8):
            pq = po[oc % 2]

            def mmo(e, pq=pq, oc=oc, n=n):
                for kc in range(8):
                    last = e.matmul(pq[:, 0:n], lhsT=wob[:, kc, oc * 128:(oc + 1) * 128], rhs=aT[:, kc, 0:n], start=(kc == 0), stop=(kc == 7))
                return last
            p.op("tensor", mmo, wo_r + [aT.r], [pq.r])

            def res(e, pq=pq, oc=oc, t0=t0, n=n):
                for (s, m, j) in tok_ranges(t0, n, 128):
                    last = e.scalar_tensor_tensor(out=xt[:, oc, s - t0:s - t0 + m], in0=pq[:, s - t0:s - t0 + m], scalar=mod[:, oc, j:j + 1],
                                                  in1=xt[:, oc, s - t0:s - t0 + m], op0=ALU.mult, op1=ALU.add)
                return last
            p.op("vector", res, [pq.r, mod.r, xt.r], [xt.r])
        o = Reg("o%d" % c)
        outs.append(o)
        p.dma("gpsimd", xm_d[:, t0:t0 + n].rearrange("(kc p) t -> p kc t", p=128), xt[:, :, 0:n], reads=[xt.r], writes=[o])


def emit_b2(kb, xT_d, cT, adaw, adab, n2, wr_d, br_d, sel_d, ident_d, w1_d, w3_d, w2_d, out_d, in_regs, outs, TOK=HT, NCX=128, NE=16):
    p = kb.p
    banks = kb.banks
    nch = (TOK + 511) // 512
    chunks = [(c * 512, min(512, TOK - c * 512)) for c in range(nch)]
    idf = ld(kb, "idf", [128, 128], ident_d)
    ones = kb.sb("ones", [128, 128])
    p.op("vector", lambda e: e.memset(ones[:], 1.0), [], [ones.r])
    sel = ld(kb, "sel", [16, 16, 128], sel_d)
    wr = kb.sb("wr", [128, 8, 20])
    p.dma("sync", wr[:], wr_d.rearrange("(kc p) c -> p kc c", p=128), writes=[wr.r])
    br = ld(kb, "br", [128, 20], br_d)
    n2t = ld(kb, "n2t", [128, 8], n2)
    arena = kb.sb("b2arena", [128, 18432])
    KEL = 256

    class PV:
        def __init__(s, bank, ap):
            s.t, s.r = ap, bank.r

        def __getitem__(s, k):
            return s.t[k]
    sq = kb.sub(arena, 0, [128, 8, 512], F32, "sq")
    h2f = kb.sub(arena, 16 * KEL, [128, 8, 512], F32, "h2f")
    mod = emit_mods(kb, adaw, adab, 24, cT, sq, banks[0])
    asc = kb.sb("asc", [128, 8, 2])
    p.op("vector", lambda e: e.tensor_scalar(out=asc[:], in0=mod[:, 8:16, :], scalar1=1.0, scalar2=None, op0=ALU.add), [mod.r], [asc.r])
    p.op("vector", lambda e: e.tensor_tensor(out=asc[:], in0=asc[:], in1=n2t[:].unsqueeze(2).to_broadcast([128, 8, 2]), op=ALU.mult), [asc.r, n2t.r], [asc.r])
    xT = kb.sb("xT", [128, 8, TOK])
    xr = [[Reg("x%d_%d" % (oc, c)) for c in range(nch)] for oc in range(8)]
    for oc in range(8):
        p.dma("sync", xT[:, oc, :], xT_d[oc * 128:(oc + 1) * 128, :], reads=in_regs, writes=xr[oc])
    h2T = kb.sb("h2T", [128, 8, TOK], BF16)
    h2r = [Reg("h2_%d" % c) for c in range(nch)]
    wT = kb.sb("wT", [16, TOK])
    wTr = [Reg("wT%d" % c) for c in range(nch)]
    pss = banks[7]
    rstd = kb.sb("rstd", [128, 512])
    plg = [PV(banks[5], banks[5].t[:, 0:20]), PV(banks[6], banks[6].t[:, 0:20])]
    pwt = PV(banks[4], banks[4].t[0:16, 0:128])
    R = lambda n, s: kb.sb(n, s)
    L = R("rL", [128, 20]); mg = R("rmg", [128, 1]); nmg = R("rnmg", [128, 1]); eg = R("reg", [128, 4]); sg = R("rsg", [128, 1])
    gsel = R("rgsel", [128, 4]); tmp = R("rtmp", [128, 4, 4]); lsel = R("rlsel", [128, 4]); me = R("rme", [128, 1]); nme = R("rnme", [128, 1])
    ee = R("ree", [128, 4]); m1 = R("rm1", [128, 1]); msk = R("rmsk", [128, 4]); e2 = R("re2", [128, 4]); m2 = R("rm2", [128, 1])
    tv = R("rtv", [128, 4]); sv = R("rsv", [128, 1]); wg = R("rwg", [128, 4]); gp = R("rgp", [128, 4]); wf = R("rwf", [128, 4, 4])
    for c, (t0, n) in enumerate(chunks):
        def sqf(e, t0=t0, n=n):
            for kc in range(8):
                last = e.activation(out=sq[:, kc, 0:n], in_=xT[:, kc, t0:t0 + n], func=AF.Square)
            return last
        p.op("scalar", sqf, [xr[oc][c] for oc in range(8)], [sq.r])

        def ssum(e, n=n):
            for kc in range(8):
                last = e.matmul(pss[:, 0:n], lhsT=ones[:], rhs=sq[:, kc, 0:n], start=(kc == 0), stop=(kc == 7))
            return last
        p.op("tensor", ssum, [sq.r, ones.r], [pss.r])
        p.op("vector", lambda e, n=n: e.tensor_scalar(out=rstd[:, 0:n], in0=pss[:, 0:n], scalar1=1.0 / 1024, scalar2=EPS, op0=ALU.mult, op1=ALU.add), [pss.r], [rstd.r])
        p.op("vector", lambda e, n=n: e.reciprocal(out=rstd[:, 0:n], in_=rstd[:, 0:n]), [rstd.r], [rstd.r])
        p.op("scalar", lambda e, n=n: e.activation(out=rstd[:, 0:n], in_=rstd[:, 0:n], func=AF.Sqrt), [rstd.r], [rstd.r])

        def nrm(e, t0=t0, n=n):
            for kc in range(8):
                last = e.tensor_tensor(out=h2f[:, kc, 0:n], in0=xT[:, kc, t0:t0 + n], in1=rstd[:, 0:n], op=ALU.mult)
            return last
        p.op("vector", nrm, [xr[oc][c] for oc in range(8)] + [rstd.r], [h2f.r])

        def modf(e, t0=t0, n=n):
            for (s, m, j) in tok_ranges(t0, n, NCX):
                for kc in range(8):
                    last = e.activation(out=h2f[:, kc, s - t0:s - t0 + m], in_=h2f[:, kc, s - t0:s - t0 + m], func=AF.Identity, scale=asc[:, kc, j:j + 1], bias=mod[:, kc, j:j + 1])
            return last
        p.op("scalar", modf, [h2f.r, asc.r, mod.r], [h2f.r])
        p.op("gpsimd", lambda e, t0=t0, n=n: e.tensor_copy(out=h2T[:, :, t0:t0 + n], in_=h2f[:, :, 0:n]), [h2f.r], [h2r[c]])
        for tt in range(n // 128):
            pl = plg[tt % 2]

            def rmm(e, tt=tt, pl=pl):
                for kc in range(8):
                    last = e.matmul(pl[:], lhsT=h2f[:, kc, tt * 128:(tt + 1) * 128], rhs=wr[:, kc, :], start=(kc == 0), stop=(kc == 7))
                return last
            p.op("tensor", rmm, [h2f.r, wr.r], [pl.r])
            V = lambda fn, rd, wrt: p.op("vector", fn, rd, wrt)
            V(lambda e, pl=pl: e.tensor_tensor(out=L[:], in0=pl[:], in1=br[:], op=ALU.add), [pl.r, br.r], [L.r])
            V(lambda e: e.reduce_max(out=mg[:], in_=L[:, 0:4], axis=AX.X), [L.r], [mg.r])
            V(lambda e: e.tensor_scalar(out=nmg[:], in0=mg[:], scalar1=-1.0, scalar2=None, op0=ALU.mult), [mg.r], [nmg.r])
            p.op("scalar", lambda e: e.activation(out=eg[:], in_=L[:, 0:4], func=AF.Exp, bias=nmg[:, 0:1], scale=1.0, accum_out=sg[:]), [L.r, nmg.r], [eg.r, sg.r])
            V(lambda e: e.reciprocal(out=sg[:], in_=sg[:]), [sg.r], [sg.r])
            V(lambda e: e.tensor_scalar(out=gsel[:], in0=L[:, 0:4], scalar1=mg[:, 0:1], scalar2=None, op0=ALU.is_ge), [L.r, mg.r], [gsel.r])
            V(lambda e: e.tensor_tensor(out=tmp[:], in0=L[:, 4:20].rearrange("p (g e) -> p g e", g=4), in1=gsel[:].unsqueeze(2).to_broadcast([128, 4, 4]), op=ALU.mult), [L.r, gsel.r], [tmp.r])
            V(lambda e: e.tensor_reduce(out=lsel[:], in_=tmp[:].rearrange("p g e -> p e g"), axis=AX.X, op=ALU.add), [tmp.r], [lsel.r])
            V(lambda e: e.reduce_max(out=me[:], in_=lsel[:], axis=AX.X), [lsel.r], [me.r])
            V(lambda e: e.tensor_scalar(out=nme[:], in0=me[:], scalar1=-1.0, scalar2=None, op0=ALU.mult), [me.r], [nme.r])
            p.op("scalar", lambda e: e.activation(out=ee[:], in_=lsel[:], func=AF.Exp, bias=nme[:, 0:1], scale=1.0), [lsel.r, nme.r], [ee.r])
            V(lambda e: e.reduce_max(out=m1[:], in_=ee[:], axis=AX.X), [ee.r], [m1.r])
            V(lambda e: e.tensor_scalar(out=msk[:], in0=ee[:], scalar1=m1[:, 0:1], scalar2=-1e9, op0=ALU.is_ge, op1=ALU.mult), [ee.r, m1.r], [msk.r])
            V(lambda e: e.tensor_tensor(out=e2[:], in0=ee[:], in1=msk[:], op=ALU.add), [ee.r, msk.r], [e2.r])
            V(lambda e: e.reduce_max(out=m2[:], in_=e2[:], axis=AX.X), [e2.r], [m2.r])
            V(lambda e: e.tensor_scalar(out=tv[:], in0=ee[:], scalar1=m2[:, 0:1], scalar2=None, op0=ALU.is_ge), [ee.r, m2.r], [tv.r])
            V(lambda e: e.tensor_tensor(out=tv[:], in0=tv[:], in1=ee[:], op=ALU.mult), [tv.r, ee.r], [tv.r])
            V(lambda e: e.reduce_sum(out=sv[:], in_=tv[:], axis=AX.X), [tv.r], [sv.r])
            V(lambda e: e.reciprocal(out=sv[:], in_=sv[:]), [sv.r], [sv.r])
            V(lambda e: e.tensor_scalar(out=wg[:], in0=tv[:], scalar1=sv[:, 0:1], scalar2=None, op0=ALU.mult), [tv.r, sv.r], [wg.r])
            V(lambda e: e.tensor_scalar(out=gp[:], in0=gsel[:], scalar1=sg[:, 0:1], scalar2=None, op0=ALU.mult), [gsel.r, sg.r], [gp.r])
            V(lambda e: e.tensor_tensor(out=wf[:], in0=gp[:].unsqueeze(2).to_broadcast([128, 4, 4]), in1=wg[:].unsqueeze(1).to_broadcast([128, 4, 4]), op=ALU.mult), [gp.r, wg.r], [wf.r])
            p.op("tensor", lambda e: e.transpose(out=pwt[:], in_=wf[:].rearrange("p g e -> p (g e)"), identity=idf[:]), [wf.r, idf.r], [pwt.r])
            V(lambda e, a=t0 + tt * 128: e.tensor_copy(out=wT[:, a:a + 128], in_=pwt[:]), [pwt.r], [wTr[c]])
    p.fence()
    w13 = [kb.sub(arena, i * 16 * KEL, [128, 2, 8, 512], BF16, "w13_%d" % i) for i in range(2)]
    w2b = [kb.sub(arena, (32 + 8 * i) * KEL, [128, 4, 1024], BF16, "w2b_%d" % i) for i in range(2)]
    stA = [kb.sub(arena, (48 + 4 * i) * KEL, [128, 1024], F32, "stA%d" % i) for i in range(3)]
    s1 = [kb.sub(arena, (60 + 2 * i) * KEL, [128, 512], F32, "s1_%d" % i) for i in range(2)]
    aT = [kb.sub(arena, (64 + 4 * i) * KEL, [128, 4, 512], BF16, "aT%d" % i) for i in range(2)]
    ph = banks[0:4]
    pw = banks[4]
    po = banks[5:7]
    war_b = [[Reg("wa%d_%d" % (i, k)) for k in range(8)] for i in range(2)]
    wbr_b = [[Reg("wb%d_%d" % (i, k)) for k in range(4)] for i in range(2)]
    si = 0
    hc = 0
    ac = 0
    oc_cnt = 0
    for ex in range(NE):
        wa, wb = w13[ex % 2], w2b[ex % 2]
        war, wbr = war_b[ex % 2], wbr_b[ex % 2]
        for kc in range(8):
            sA = stA[si % 3]; si += 1
            p.dma("sync", sA[:, 0:512], w1_d[ex, kc * 128:(kc + 1) * 128, :], writes=[sA.r])
            p.dma("sync", sA[:, 512:1024], w3_d[ex, kc * 128:(kc + 1) * 128, :], writes=[sA.r])
            p.op("gpsimd", lambda e, sA=sA, wa=wa, kc=kc: e.tensor_copy(out=wa[:, :, kc, :], in_=sA[:].rearrange("p (a c) -> p a c", a=2)), [sA.r], [war[kc]])
        for fc in range(4):
            sA = stA[si % 3]; si += 1
            p.dma("sync", sA[:], w2_d[ex, fc * 128:(fc + 1) * 128, :], writes=[sA.r])
            p.op("gpsimd", lambda e, sA=sA, wb=wb, fc=fc: e.tensor_copy(out=wb[:, fc, :], in_=sA[:]), [sA.r], [wbr[fc]])
        for c, (t0, n) in enumerate(chunks):
            p.op("tensor", lambda e, ex=ex, t0=t0, n=n: e.matmul(pw[:, 0:n], lhsT=sel[:, ex, :], rhs=wT[:, t0:t0 + n], start=True, stop=True), [sel.r, wTr[c]], [pw.r])
            a = aT[ac % 2]; ac += 1
            for fc in range(4):
                p1, p3 = ph[hc % 4], ph[(hc + 1) % 4]; hc += 2
                ss = s1[fc % 2]

                def mm13(e, p1=p1, p3=p3, wa=wa, fc=fc, t0=t0, n=n):
                    for kc in range(8):
                        e.matmul(p1[:, 0:n], lhsT=wa[:, 0, kc, fc * 128:(fc + 1) * 128], rhs=h2T[:, kc, t0:t0 + n], start=(kc == 0), stop=(kc == 7))
                    for kc in range(8):
                        last = e.matmul(p3[:, 0:n], lhsT=wa[:, 1, kc, fc * 128:(fc + 1) * 128], rhs=h2T[:, kc, t0:t0 + n], start=(kc == 0), stop=(kc == 7))
                    return last
                p.op("tensor", mm13, war + [h2r[c]], [p1.r, p3.r])
                p.op("scalar", lambda e, ss=ss, p1=p1, n=n: e.activation(out=ss[:, 0:n], in_=p1[:, 0:n], func=AF.Silu), [p1.r], [ss.r])
                p.op("vector", lambda e, ss=ss, p3=p3, n=n: e.tensor_tensor(out=ss[:, 0:n], in0=ss[:, 0:n], in1=p3[:, 0:n], op=ALU.mult), [ss.r, p3.r], [ss.r])
                p.op("vector", lambda e, ss=ss, a=a, fc=fc, n=n: e.tensor_tensor(out=a[:, fc, 0:n], in0=ss[:, 0:n], in1=pw[:, 0:n], op=ALU.mult), [ss.r, pw.r], [a.r])
            for oc in range(8):
                pq = po[oc_cnt % 2]; oc_cnt += 1

                def mm2(e, pq=pq, wb=wb, a=a, oc=oc, n=n):
                    for fc in range(4):
                        last = e.matmul(pq[:, 0:n], lhsT=wb[:, fc, oc * 128:(oc + 1) * 128], rhs=a[:, fc, 0:n], start=(fc == 0), stop=(fc == 3))
                    return last
                p.op("tensor", mm2, wbr + [a.r], [pq.r])

                def acc(e, pq=pq, oc=oc, t0=t0, n=n):
                    for (s, m, j) in tok_ranges(t0, n, NCX):
                        last = e.scalar_tensor_tensor(out=xT[:, oc, s:s + m], in0=pq[:, s - t0:s - t0 + m], scalar=mod[:, 16 + oc, j:j + 1], in1=xT[:, oc, s:s + m], op0=ALU.mult, op1=ALU.add)
                    return last
                p.op("vector", acc, [pq.r, mod.r, xr[oc][c]], [xr[oc][c]])
    for oc in range(8):
        o = Reg("o%d" % oc)
        outs.append(o)
        p.dma("gpsimd", out_d[oc * 128:(oc + 1) * 128, :], xT[:, oc, :], reads=xr[oc], writes=[o])


IN_SIZES_ = (512, 1024, 16, 512, 512, 512, 32, 512, 512, 512, 512, 256, 256, 4096)
OFFS_ = np.cumsum([0] + list(IN_SIZES_))
GROUPS = [[0, 1], [2, 3], [4, 5], [6, 7]]


def s1_cols(hf):
    o = OFFS_
    pieces = [("z", o[0] + hf * 256, 256), ("x", o[1] + hf * 256, 256), ("B", o[1] + 512 + hf * 128, 128), ("C", o[1] + 768 + hf * 128, 128),
              ("gq", o[3] + hf * 256, 256), ("gk", o[4] + hf * 256, 256), ("gv", o[5] + hf * 256, 256), ("gr", o[7] + hf * 256, 256),
              ("lx", o[8] + hf * 256, 256), ("lg", o[9] + hf * 256, 256),
              ("aq", o[10] + hf * 256, 256), ("ak", o[11] + hf * 128, 128), ("av", o[12] + hf * 128, 128)]
    cols, rows, r = [], {}, 0
    for nm, st, n in pieces:
        cols.append(np.arange(st, st + n))
        rows[nm] = slice(r, r + n)
        r += n
    cols.append(np.concatenate([o[2] + d * 8 + hf * 4 + np.arange(4) for d in range(2)]))
    rows["dt"] = slice(r, r + 8)
    r += 8
    cols.append(np.arange(o[6], o[6] + 32))
    rows["g1"] = slice(r, r + 32)
    r += 32
    pad = NCC_MIX * 128 - r
    cols.append(np.full(pad, -1))
    r += pad
    cols.append(np.arange(o[13] + hf * 2048, o[13] + (hf + 1) * 2048))
    rows["gate"] = slice(r, r + 2048)
    return np.concatenate(cols), rows


LAYER_SPECS = [("adaw", [1024, 6144]), ("adab", [128, 48]), ("n1", [128, 8]), ("n2", [128, 8]), ("win", [1024, NCC_S1 * 128]),
               ("gq", [128, 1]), ("gk", [128, 1]),
               ("lcw", [128, 2, 4]), ("lcb", [128, 2]), ("lwbd", [128, 8, 128]), ("lbias", [128, 8]), ("llam", [128, 4]),
               ("g2", [16, 2, 256]), ("gb", [128, 4]), ("gn", [128, 2]),
               ("scw", [128, 4, 4]), ("scb", [128, 4]), ("dtb", [4, 2]), ("alog", [4, 2]), ("Dp", [128, 2]),
               ("wbr", [2048, 1024]), ("wo", [1024, 1024]), ("ssdn", [128, 4]),
               ("wr", [1024, 20]), ("br", [128, 20]), ("w1", [16, 1024, 512]), ("w3", [16, 1024, 512]), ("w2", [16, 512, 1024])]
GLOBAL_SPECS = [("xT0", [1024, T]), ("xTm0", [1024, HT]), ("cT", [128, 8, 2]), ("ident", [128, 128]), ("cos", [128, LAT]), ("sin", [128, LAT]),
                ("rm", [128, 128]), ("cm", [128, 512]), ("m2", [128, 128]), ("hm", [128, 2]), ("U", [128, 128]), ("sel", [16, 16, 128]), ("bm", [128, 2])]


def build_fused(NL=2, debug=False, stages=None):
    kb = KB()
    p = kb.p
    G = {n: kb.din(n, s) for n, s in GLOBAL_SPECS}
    W = [{n: kb.din("%s_%d" % (n, l), s) for n, s in LAYER_SPECS} for l in range(NL)]
    pT = kb.scratch("pT", [NCC_MIX * 128, T], debug=debug)
    xs = kb.scratch("xs", [2 * 3072, HT], debug=debug)
    xg = kb.scratch("xg", [2 * 24 * 2 * 128, HT], debug=debug)
    xm = kb.scratch("xm", [1024, HT], debug=debug)
    xnew = kb.scratch("xnew", [1024, HT], debug=debug)
    xall = kb.scratch("xall", [2048, HT], debug=debug)
    oT = kb.dout("oT", [1024, HT])
    outs = []
    _, rows = s1_cols(0)
    for l in range(NL):
        w = W[l]
        if l == 0:
            xsrc = lambda kc, t0, n: [(0, n, G["xT0"][kc * 128:(kc + 1) * 128, t0:t0 + n])]
            xres = G["xTm0"]
        else:
            xsrc = lambda kc, t0, n: [(a - t0, m, h, loc) for (a, m, h, loc) in nat_pieces(t0, n)]
            xres = xnew
        def gather_chunks(chs):
            for h in range(2):
                for i in range((3072 + PIECE - 1) // PIECE):
                    Ri = min(PIECE, 3072 - PIECE * i)
                    src = xs[h * 3072 + PIECE * i:h * 3072 + PIECE * i + Ri, :]
                    dst = xg[h * 6144 + 2 * PIECE * i:h * 6144 + 2 * PIECE * i + 2 * Ri, :]
                    p.cc(lambda e, src=src, dst=dst: e.collective_compute("AllGather", ALU.bypass, replica_groups=GROUPS, ins=[src], outs=[dst]), blocking=True)
        emit_s1(kb, xsrc, G["cT"], w["adaw"][:, 0:2048], w["adab"][:, 0:16], w["n1"], w["win"], pT, xs, outs, [Reg("pT%d" % i) for i in range(NCC_MIX)], xall_ap=xall)
        kb.reset()
        emit_ssd(kb, pT[256:768, :], pT[rows["z"], :], pT[rows["dt"], :], w["scw"], w["scb"], w["dtb"], w["alog"], w["Dp"], G["U"], G["ident"], xs, 0, [], outs)
        kb.reset()
        emit_gla(kb, pT[rows["gq"], :], pT[rows["gk"], :], pT[rows["gv"], :], pT[rows["g1"], :], pT[rows["gr"], :], w["g2"], w["gb"], w["gn"],
                 G["cm"], G["m2"], G["hm"], G["ident"], xs, 256, [], outs)
        kb.reset()
        emit_lru(kb, pT[rows["lx"], :], pT[rows["lg"], :], w["lcw"], w["lcb"], w["lwbd"], w["lbias"], w["llam"], xs, 512, [], outs)
        kb.reset()
        emit_att(kb, pT[rows["aq"], :], pT[rows["ak"], :], pT[rows["av"], :], w["gq"], w["gk"], G["cos"], G["sin"], G["rm"], G["ident"], xs, 768, [], outs)
        kb.reset()
        gather_chunks(range(24))
        kb.reset()
        emit_b1(kb, xg, xres, G["cT"], w["adaw"][:, 2048:3072], w["adab"][:, 16:24], w["wbr"], w["wo"], w["ssdn"], G["bm"], xm, [], outs)
        kb.reset()
        emit_b2(kb, xm, G["cT"], w["adaw"][:, 3072:6144], w["adab"][:, 24:48], w["n2"], w["wr"], w["br"], G["sel"], G["ident"], w["w1"], w["w3"], w["w2"],
                oT if l == NL - 1 else xnew, [], outs)
        kb.reset()
        if l < NL - 1:
            for i in range((1024 + PIECE - 1) // PIECE):
                Ri = min(PIECE, 1024 - PIECE * i)
                src = xnew[PIECE * i:PIECE * i + Ri, :]
                dst = xall[2 * PIECE * i:2 * PIECE * i + 2 * Ri, :]
                p.cc(lambda e, src=src, dst=dst: e.collective_compute("AllGather", ALU.bypass, replica_groups=GROUPS, ins=[src], outs=[dst]))
            kb.reset()
    return kb.finish(outs)


def core_inputs(inp, b, hf, NL=2):
    c = np.ascontiguousarray
    f32 = np.float32
    d = {}
    x_all = np.concatenate([inp["ctx"][b], inp["x"][b]], 0)
    d["xT0"] = c(x_all.T)
    d["xTm0"] = c(np.concatenate([inp["ctx"][b][hf * 128:(hf + 1) * 128], inp["x"][b][hf * 2048:(hf + 1) * 2048]], 0).T)
    c2 = np.stack([inp["c"][b], inp["c_ctx"]], -1)
    d["cT"] = c(c2.reshape(8, 128, 2).transpose(1, 0, 2))
    d["ident"] = np.eye(128, dtype=f32)
    cos, sin, Rm = rope_tables()
    d["cos"], d["sin"], d["rm"] = cos, sin, Rm
    t = np.arange(512)
    d["cm"] = np.broadcast_to((t % 64 != 0).astype(f32)[None], (128, 512)).copy()
    j = np.arange(128)[:, None]
    i = np.arange(128)[None, :]
    d["m2"] = ((j // 64 == i // 64) & (j <= i)).astype(f32)
    d["hm"] = np.stack([(np.arange(128) < 64), (np.arange(128) >= 64)], 1).astype(f32)
    d["U"] = (j <= i).astype(f32)
    sel = np.zeros((16, 16, 128), f32)
    for e in range(16):
        sel[e, e, :] = 1.0
    d["sel"] = sel
    bm = np.zeros((128, 2), f32)
    bm[:, hf] = 1.0
    d["bm"] = bm
    cols, rows = s1_cols(hf)
    ch = slice(hf * 256, (hf + 1) * 256)
    hs = slice(hf * 4, hf * 4 + 4)
    for l in range(NL):
        L = {}
        L["adaw"] = c(inp["ada_w"][l])
        L["adab"] = fm(inp["ada_b"][l], 48)
        L["n1"] = fm(inp["norm1"][l], 8)
        L["n2"] = fm(inp["norm2"][l], 8)
        win = np.zeros((1024, NCC_S1 * 128), f32)
        ok = cols >= 0
        win[:, np.nonzero(ok)[0]] = inp["w_in"][l][:, cols[ok]]
        L["win"] = win
        L["gq"] = c(inp["att_qnorm"][l][:, None])
        L["gk"] = c(inp["att_knorm"][l][:, None])
        L["lcw"] = c(inp["lru_conv_w"][l][:, ch].reshape(4, 2, 128).transpose(2, 1, 0))
        L["lcb"] = c(inp["lru_conv_b"][l][ch].reshape(2, 128).T)
        wbd = np.zeros((128, 8, 128), f32)
        bias = np.zeros((128, 8), f32)
        lam = np.zeros((128, 4), f32)
        for gi, (wk, bk) in enumerate((("lru_wa", "lru_ba"), ("lru_wx", "lru_bx"))):
            for dd in range(2):
                for cc in range(2):
                    idx = gi * 4 + dd * 2 + cc
                    for jj in range(2):
                        blk = hf * 4 + cc * 2 + jj
                        wbd[jj * 64:(jj + 1) * 64, idx, jj * 64:(jj + 1) * 64] = inp[wk][l][dd, blk]
                    bias[:, idx] = inp[bk][l][dd, ch][cc * 128:(cc + 1) * 128]
        for dd in range(2):
            for cc in range(2):
                lam[:, dd * 2 + cc] = inp["lru_lambda"][l][dd, ch][cc * 128:(cc + 1) * 128]
        L["lwbd"], L["lbias"], L["llam"] = wbd, bias, lam
        L["g2"] = c(inp["gla_g2"][l][:, :, ch].transpose(1, 0, 2))
        L["gb"] = c(np.stack([inp["gla_gb"][l][dd, ch][h * 128:(h + 1) * 128] for h in range(2) for dd in range(2)], 1))
        L["gn"] = c(inp["gla_norm"][l][ch].reshape(2, 128).T)
        chans = np.concatenate([np.arange(hf * 256, hf * 256 + 256), 512 + hf * 128 + np.arange(128), 768 + hf * 128 + np.arange(128)])
        L["scw"] = c(inp["ssd_conv_w"][l][:, chans].reshape(4, 4, 128).transpose(2, 1, 0))
        L["scb"] = c(inp["ssd_conv_b"][l][chans].reshape(4, 128).T)
        L["dtb"] = c(inp["ssd_dt_bias"][l][:, hs].T)
        L["alog"] = c(inp["ssd_a_log"][l][:, hs].T)
        L["Dp"] = c(np.repeat(inp["ssd_d"][l][hs], 64).reshape(2, 128).T)
        L["wbr"] = c(inp["w_branch"][l].reshape(2048, 1024))
        L["wo"] = c(inp["w_out"][l])
        L["ssdn"] = fm(inp["ssd_norm"][l], 4)
        L["wr"] = c(np.concatenate([inp["router_wg"][l], inp["router_we"][l]], 1))
        L["br"] = c(np.broadcast_to(np.concatenate([inp["router_bg"][l], inp["router_be"][l]])[None], (128, 20)))
        L["w1"], L["w3"], L["w2"] = c(inp["exp_w1"][l]), c(inp["exp_w3"][l]), c(inp["exp_w2"][l])
        for k, v in L.items():
            d["%s_%d" % (k, l)] = np.ascontiguousarray(v, dtype=f32)
    return {k: np.ascontiguousarray(v, dtype=f32) for k, v in d.items()}


_PROG = {}


def kernel(**inp):
    inp = {k: np.asarray(v) for k, v in inp.items()}
    NB = inp["x"].shape[0]
    if "fused" not in _PROG:
        _PROG["fused"] = build_fused()
    cores = [(b, hf) for b in range(NB) for hf in range(2)]
    ims = [core_inputs(inp, b, hf) for (b, hf) in cores]
    res = run_bass_kernel_spmd(_PROG["fused"], ims, core_ids=list(range(8))).results
    out = np.stack([np.concatenate([res[2 * b]["oT"][:, 128:].T, res[2 * b + 1]["oT"][:, 128:].T], 0) for b in range(NB)])
    return np.ascontiguousarray(out.astype(np.float32))
```

```python
import numpy as np
import concourse.bass as bass
import concourse.mybir as mybir
from concourse.bass_utils import run_bass_kernel_spmd
from contextlib import ExitStack

F32 = mybir.dt.float32
BF16 = mybir.dt.bfloat16
AF = mybir.ActivationFunctionType
ALU = mybir.AluOpType
AX = mybir.AxisListType


class Reg:
    __slots__ = ("name", "w", "r")

    def __init__(self, name=""):
        self.name = name
        self.w = None
        self.r = []


class Ins:
    __slots__ = ("eng", "fn", "deps", "sig", "idx", "isdma", "slot", "target", "n")

    def __init__(self):
        self.slot = None


class Prog:
    ENG = ["tensor", "vector", "scalar", "gpsimd", "sync"]
    RING = 12

    def __init__(self, nc):
        self.nc = nc
        self.q = {e: [] for e in self.ENG}
        self.n = 0

    def op(self, eng, fn, reads=(), writes=(), dma=False):
        I = Ins()
        I.eng, I.fn, I.isdma, I.sig, I.idx = eng, fn, dma, False, None
        I.n = self.n
        self.n += 1
        deps = {}
        for r in reads:
            if r.w is not None:
                deps.setdefault(id(r.w), [r.w, set()])[1].add("raw")
        for w in writes:
            if w.w is not None:
                deps.setdefault(id(w.w), [w.w, set()])[1].add("waw")
            for x in w.r:
                deps.setdefault(id(x), [x, set()])[1].add("war")
        final = []
        for J, kinds in deps.values():
            if J is I:
                continue
            if J.eng == eng and not J.isdma and not dma:
                if "raw" not in kinds or eng == "tensor":
                    continue
            final.append(J)
            J.sig = True
        I.deps = final
        for r in reads:
            r.r.append(I)
        for w in writes:
            w.w = I
            w.r = []
        self.q[eng].append(I)
        return I

    def fence(self):
        self.nf = getattr(self, "nf", 0) + 1
        for e in self.ENG:
            last = None
            for I in reversed(self.q[e]):
                if I.fn == "FENCE":
                    break
                if not I.isdma and I.fn is not None:
                    last = I
                    break
            if last is not None:
                last.sig = True
            I = Ins()
            I.eng, I.fn, I.isdma, I.sig, I.idx, I.deps = e, "FENCE", False, False, None, [last] if last is not None else []
            I.n = self.nf
            self.q[e].append(I)

    def dma(self, eng, out, in_, reads=(), writes=(), **kw):
        return self.op(eng, lambda e: e.dma_start(out=out, in_=in_, **kw), reads, writes, dma=True)

    def cc(self, fn, reads=(), writes=(), blocking=True):
        I = self.op("gpsimd", fn, reads, writes, dma=True)
        I.slot = "cc" if blocking else "ccnb"
        self.ccs = getattr(self, "ccs", []) + [I]
        return I

    def cc_wait_all(self):
        I = Ins()
        I.eng, I.fn, I.isdma, I.sig, I.idx, I.deps = "gpsimd", "CCWAIT", False, False, None, []
        I.n = len(getattr(self, "ccs", []))
        self.q["gpsimd"].append(I)

    def emit(self, final_regs=()):
        nc = self.nc
        self.op("sync", None, reads=list(final_regs))
        sems = {}
        stack = []
        for e in self.ENG:
            cm = nc.semaphore("S_" + e)
            sems[e] = cm.__enter__()
            stack.append(cm)
        cmf = nc.semaphore("S_fence")
        fsem = cmf.__enter__()
        stack.append(cmf)
        rings = {}
        for e in ("sync", "gpsimd", "scalar"):
            rings[e] = []
            for k in range(self.RING):
                cm = nc.semaphore("D_%s_%d" % (e, k))
                rings[e].append(cm.__enter__())
                stack.append(cm)
        ccsem = {}
        cctgt = {}
        if getattr(self, "ccs", []):
            cm = nc.semaphore("C_all")
            csem = cm.__enter__()
            stack.append(cm)
            for k, I in enumerate(self.ccs):
                ccsem[id(I)] = csem
                cctgt[id(I)] = k + 1
        for e in self.ENG:
            cnt = 0
            dcnt = 0
            for I in self.q[e]:
                if I.isdma and getattr(I, "slot", None) in ("cc", "ccnb"):
                    I.target = 1
                elif I.isdma:
                    I.slot = dcnt % self.RING
                    I.target = 16 * (dcnt // self.RING + 1)
                    dcnt += 1
                elif I.sig:
                    cnt += 1
                    I.idx = cnt
        self.counts = {e: len(self.q[e]) for e in self.ENG}

        def run(e, eng):
            seen = {}
            prev = {}

            def wait(key, sem, val):
                if seen.get(key, 0) < val:
                    eng.wait_ge(sem, val)
                    seen[key] = val

            for I in self.q[e]:
                mx = {}
                for J in I.deps:
                    if J.isdma and J.slot in ("cc", "ccnb"):
                        wait(("cc",), ccsem[id(J)], cctgt[id(J)])
                    elif J.isdma:
                        wait((J.eng, J.slot), rings[J.eng][J.slot], J.target)
                    else:
                        mx[J.eng] = max(mx.get(J.eng, 0), J.idx)
                for f, v in mx.items():
                    wait(f, sems[f], v)
                if I.isdma and I.slot in ("cc", "ccnb"):
                    inst = I.fn(eng)
                    inst.then_inc(ccsem[id(I)], 1)
                    if I.slot == "cc":
                        wait(("cc",), ccsem[id(I)], cctgt[id(I)])
                    continue
                if I.fn == "CCWAIT":
                    if I.n > 0:
                        wait(("cc",), csem, I.n)
                    continue
                if I.isdma:
                    p = prev.get(I.slot)
                    if p is not None:
                        wait((e, I.slot), rings[e][I.slot], p.target)
                    prev[I.slot] = I
                if I.fn is None:
                    continue
                if I.fn == "FENCE":
                    for s, pq in prev.items():
                        wait((e, s), rings[e][s], pq.target)
                    eng.sem_inc(fsem, 1)
                    eng.wait_ge(fsem, len(self.ENG) * I.n)
                    continue
                inst = I.fn(eng)
                if I.isdma:
                    inst.then_inc(rings[e][I.slot], 16)
                elif I.sig:
                    inst.then_inc(sems[e], 1)
            if e in rings:
                for s, p in prev.items():
                    wait((e, s), rings[e][s], p.target)

        with nc.Block() as block:
            @block.sync
            def _(eng):
                run("sync", eng)

            @block.tensor
            def _(eng):
                run("tensor", eng)

            @block.vector
            def _(eng):
                run("vector", eng)

            @block.scalar
            def _(eng):
                run("scalar", eng)

            @block.gpsimd
            def _(eng):
                run("gpsimd", eng)
        for cm in reversed(stack):
            cm.__exit__(None, None, None)


EPS = 1e-6
THETA = 10000.0
NCTX, LAT = 256, 4096
T = NCTX + LAT
HT = T // 2
NCC_MIX = 23
NCC_S1 = 39
ARENA_F32 = 50176


class Tile:
    __slots__ = ("t", "r")

    def __init__(self, t, name):
        self.t = t
        self.r = Reg(name)

    def __getitem__(self, k):
        return self.t[k]


class KB:
    def __init__(self):
        self.nc = bass.Bass("TRN2", target_bir_lowering=False)
        self.es = ExitStack()
        self.p = Prog(self.nc)
        self.es.enter_context(self.nc.allow_low_precision("bf16 matmul operands, fp32 accumulation"))
        self.arena = self.es.enter_context(self.nc.sbuf_tensor("arena", [128, ARENA_F32], F32))
        self.banks = [Tile(self.es.enter_context(self.nc.psum_tensor("bank%d" % i, [128, 512], F32)), "bank%d" % i) for i in range(8)]
        self.off = 0
        self.nscr = 0

    def din(self, name, shape, dt=F32):
        return self.nc.dram_tensor(name, list(shape), dt, kind="ExternalInput").ap()

    def dout(self, name, shape, dt=F32):
        return self.nc.dram_tensor(name, list(shape), dt, kind="ExternalOutput").ap()

    def scratch(self, name, shape, dt=F32, debug=False):
        if debug:
            return self.dout(name, shape, dt)
        return self.nc.dram_tensor(name, list(shape), dt).ap()

    def sb(self, name, shape, dt=F32):
        esize = 4 if dt == F32 else 2
        nel = 1
        for d in shape[1:]:
            nel *= d
        nbytes = (nel * esize + 31) // 32 * 32
        assert self.off + nbytes <= ARENA_F32 * 4, ("SBUF arena overflow", name, self.off, nbytes)
        ap = self.arena[0:shape[0], self.off // 4:(self.off + nbytes) // 4]
        if dt != F32:
            ap = ap.bitcast(dt)
        ap = ap[:, 0:nel]
        if len(shape) > 2:
            names = ["d%d" % k for k in range(len(shape) - 1)]
            ap = ap.rearrange("p (%s) -> p %s" % (" ".join(names), " ".join(names)), **{n: shape[k + 1] for k, n in enumerate(names[:-1])})
        self.off += nbytes
        return Tile(ap, name)

    def sub(self, tile, off_el, shape, dt=F32, name="v"):
        esize = 4 if dt == F32 else 2
        nel = 1
        for d in shape[1:]:
            nel *= d
        ap = tile.t[0:shape[0], off_el:off_el + nel * esize // 4]
        if dt != F32:
            ap = ap.bitcast(dt)
        if len(shape) > 2:
            names = ["d%d" % k for k in range(len(shape) - 1)]
            ap = ap.rearrange("p (%s) -> p %s" % (" ".join(names), " ".join(names)), **{n: shape[k + 1] for k, n in enumerate(names[:-1])})
        return Tile(ap, name)

    def psbf(self, bank):
        t = Tile(bank.t[:, 0:512].bitcast(BF16), "bf")
        t.r = bank.r
        return t

    def reset(self):
        self.p.fence()
        self.off = 0

    def finish(self, final_regs):
        self.p.emit(final_regs)
        self.es.close()
        return self.nc


def fm(v, n):
    return np.ascontiguousarray(np.asarray(v).reshape(n, 128).T)


def tok_ranges(t0, n, nctx):
    out = []
    if t0 < nctx:
        m = min(n, nctx - t0)
        out.append((t0, m, 1))
        if n > m:
            out.append((t0 + m, n - m, 0))
    else:
        out.append((t0, n, 0))
    return out


def ld(kb, name, shape, src, dt=F32):
    t = kb.sb(name, shape, dt)
    kb.p.dma("sync", t[:], src, writes=[t.r])
    return t


def consts(kb, ident_d, want_bf=True):
    p = kb.p
    idf = ld(kb, "idf", [128, 128], ident_d)
    ones = kb.sb("ones", [128, 128])
    p.op("vector", lambda e: e.memset(ones[:], 1.0), [], [ones.r])
    idb = None
    if want_bf:
        idb = kb.sb("idb", [128, 128], BF16)
        p.op("vector", lambda e: e.tensor_copy(out=idb[:], in_=idf[:]), [idf.r], [idb.r])
    return idf, idb, ones


def emit_mods(kb, adaw, adab, ncc, cT, aw, psm, name="m"):
    p = kb.p
    ct = kb.sb(name + "ct", [128, 8, 2])
    st = kb.sb(name + "st", [128, 8, 2])
    p.dma("sync", ct[:], cT, writes=[ct.r])
    p.op("scalar", lambda e: e.activation(out=st[:], in_=ct[:], func=AF.Silu), [ct.r], [st.r])
    ab = kb.sb(name + "ab", [128, ncc])
    p.dma("sync", ab[:], adab, writes=[ab.r])
    mod = kb.sb(name + "mod", [128, ncc, 2])
    for g in range(ncc // 4):
        awr = [Reg("aw%d" % k) for k in range(8)]
        for kc in range(8):
            p.dma("sync", aw[:, kc, :], adaw[kc * 128:(kc + 1) * 128, g * 512:(g + 1) * 512], reads=[], writes=[awr[kc], aw.r])

        def mm_mod(e, g=g):
            for cc in range(4):
                for kc in range(8):
                    last = e.matmul(psm[:, 2 * (g * 4 + cc):2 * (g * 4 + cc) + 2], lhsT=aw[:, kc, cc * 128:(cc + 1) * 128], rhs=st[:, kc, :],
                                    start=(kc == 0), stop=(kc == 7))
            return last
        p.op("tensor", mm_mod, [st.r, aw.r] + awr, [psm.r])
    p.op("vector", lambda e: e.tensor_tensor(out=mod[:], in0=psm[:, 0:2 * ncc].rearrange("p (c j) -> p c j", j=2),
                                             in1=ab[:].unsqueeze(2).to_broadcast([128, ncc, 2]), op=ALU.add), [psm.r, ab.r], [mod.r])
    return mod


def nat_pieces(t0, n):
    out = []
    bounds = [(0, 128, 0, 0), (128, 256, 1, 0), (256, 256 + 2048, 0, 128), (256 + 2048, T, 1, 128)]
    for (a, b, h, loc) in bounds:
        lo, hi = max(a, t0), min(b, t0 + n)
        if lo < hi:
            out.append((lo, hi - lo, h, loc + lo - a))
    return out


PIECE = 240


def gath_parts(total, f0, r, base0=0, n=128):
    parts = []
    f = f0
    while f < f0 + n:
        i = f // PIECE
        Ri = min(PIECE, total - PIECE * i)
        end = min(f0 + n, PIECE * (i + 1))
        parts.append((base0 + 2 * PIECE * i + r * Ri + (f - PIECE * i), end - f, f - f0))
        f = end
    return parts


def xs_write(p, xs, row0, tile, reads, outs, eng="gpsimd"):
    for (a, n, h, loc) in nat_pieces(0, T):
        o = Reg("o")
        outs.append(o)
        p.dma(eng, xs[h * 3072 + row0:h * 3072 + row0 + 128, loc:loc + n], tile[:, a:a + n], reads=reads, writes=[o])


def emit_s1(kb, xsrc, cT, adaw, adab, n1, win, pT, xs, xs_regs, pT_regs, xall_ap=None):
    p = kb.p
    banks = kb.banks
    aw = kb.sb("aw", [128, 8, 512])
    mod = emit_mods(kb, adaw, adab, 16, cT, aw, banks[0])
    n1t = ld(kb, "n1t", [128, 8], n1)
    asc = kb.sb("asc", [128, 8, 2])
    p.op("vector", lambda e: e.tensor_scalar(out=asc[:], in0=mod[:, 8:16, :], scalar1=1.0, scalar2=None, op0=ALU.add), [mod.r], [asc.r])
    p.op("vector", lambda e: e.tensor_tensor(out=asc[:], in0=asc[:], in1=n1t[:].unsqueeze(2).to_broadcast([128, 8, 2]), op=ALU.mult), [asc.r, n1t.r], [asc.r])
    ones = kb.sb("ones", [128, 128])
    p.op("vector", lambda e: e.memset(ones[:], 1.0), [], [ones.r])
    nch = (T + 511) // 512
    chunks = [(c * 512, min(512, T - c * 512)) for c in range(nch)]
    hT = kb.sb("hT", [128, 8, T], BF16)
    hr = [Reg("h%d" % c) for c in range(nch)]
    xq = [kb.sb("xq%d" % i, [128, 8, 512]) for i in range(2)]
    sq = [kb.sb("sq%d" % i, [128, 512]) for i in range(2)]
    rstd = kb.sb("rstd", [128, 512])
    tmp = [kb.sb("tmp%d" % i, [128, 512]) for i in range(2)]
    pss = banks[1]
    for c, (t0, n) in enumerate(chunks):
        xc = xq[c % 2]
        for kc in range(8):
            for piece in xsrc(kc, t0, n):
                if len(piece) == 3:
                    off, ln, src = piece
                    p.dma("sync", xc[:, kc, off:off + ln], src, writes=[xc.r])
                else:
                    off, ln, h_, loc = piece
                    for (row, cnt, po_) in gath_parts(1024, kc * 128, h_):
                        p.dma("sync", xc.t[po_:po_ + cnt, kc, off:off + ln], xall_ap[row:row + cnt, loc:loc + ln], writes=[xc.r])
        for kc in range(8):
            s_ = sq[kc % 2]
            p.op("scalar", lambda e, s_=s_, xc=xc, kc=kc, n=n: e.activation(out=s_[:, 0:n], in_=xc[:, kc, 0:n], func=AF.Square), [xc.r], [s_.r])
            p.op("tensor", lambda e, s_=s_, kc=kc, n=n: e.matmul(pss[:, 0:n], lhsT=ones[:], rhs=s_[:, 0:n], start=(kc == 0), stop=(kc == 7)), [ones.r, s_.r], [pss.r])
        p.op("vector", lambda e, n=n: e.tensor_scalar(out=rstd[:, 0:n], in0=pss[:, 0:n], scalar1=1.0 / 1024, scalar2=EPS, op0=ALU.mult, op1=ALU.add), [pss.r], [rstd.r])
        p.op("vector", lambda e, n=n: e.reciprocal(out=rstd[:, 0:n], in_=rstd[:, 0:n]), [rstd.r], [rstd.r])
        p.op("scalar", lambda e, n=n: e.activation(out=rstd[:, 0:n], in_=rstd[:, 0:n], func=AF.Sqrt), [rstd.r], [rstd.r])
        for kc in range(8):
            tm = tmp[kc % 2]
            p.op("vector", lambda e, tm=tm, xc=xc, kc=kc, n=n: e.tensor_tensor(out=tm[:, 0:n], in0=xc[:, kc, 0:n], in1=rstd[:, 0:n], op=ALU.mult), [xc.r, rstd.r], [tm.r])

            def modf(e, tm=tm, kc=kc, t0=t0, n=n):
                for (s, m, j) in tok_ranges(t0, n, NCTX):
                    last = e.activation(out=hT[:, kc, s:s + m], in_=tm[:, s - t0:s - t0 + m], func=AF.Identity, scale=asc[:, kc, j:j + 1], bias=mod[:, kc, j:j + 1])
                return last
            p.op("scalar", modf, [tm.r, asc.r, mod.r], [hr[c]])
    wfs = [kb.sb("wf%d" % i, [128, 8, 128]) for i in range(2)]
    wbs = [kb.sb("wb%d" % i, [128, 8, 128], BF16) for i in range(2)]
    stg = [kb.sb("stg%d" % i, [128, T]) for i in range(2)]
    sgr = [[Reg("sg%d_%d" % (i, c)) for c in range(nch)] for i in range(2)]
    pps = banks[2:6]
    cnt = 0
    for cc in range(NCC_S1):
        wf, wb, sg = wfs[cc % 2], wbs[cc % 2], stg[cc % 2]
        p.dma("sync", wf[:], win[:, cc * 128:(cc + 1) * 128].rearrange("(kc p) c -> p kc c", p=128), writes=[wf.r])
        p.op("gpsimd", lambda e, wf=wf, wb=wb: e.tensor_copy(out=wb[:], in_=wf[:]), [wf.r], [wb.r])
        for tcn, (t0, n) in enumerate(chunks):
            pp = pps[cnt % 4]

            def mm(e, pp=pp, wb=wb, t0=t0, n=n):
                for kc in range(8):
                    last = e.matmul(pp[:, 0:n], lhsT=wb[:, kc, :], rhs=hT[:, kc, t0:t0 + n], start=(kc == 0), stop=(kc == 7))
                return last
            p.op("tensor", mm, [wb.r, hr[tcn]], [pp.r])
            if cnt % 2 == 0:
                p.op("vector", lambda e, pp=pp, sg=sg, t0=t0, n=n: e.tensor_copy(out=sg[:, t0:t0 + n], in_=pp[:, 0:n]), [pp.r], [sgr[cc % 2][tcn]])
            else:
                p.op("scalar", lambda e, pp=pp, sg=sg, t0=t0, n=n: e.copy(out=sg[:, t0:t0 + n], in_=pp[:, 0:n]), [pp.r], [sgr[cc % 2][tcn]])
            cnt += 1
        if cc < NCC_MIX:
            p.dma("gpsimd", pT[cc * 128:(cc + 1) * 128, :], sg[:], reads=sgr[cc % 2], writes=[pT_regs[cc]])
        else:
            xs_write(p, xs, 1024 + (cc - NCC_MIX) * 128, sg, sgr[cc % 2], xs_regs)


def rope_tables(L=4096, W=64):
    t = np.arange(L)
    pos = np.stack([t // W, t % W], 0).astype(np.float32)
    d = np.arange(128)
    half = d // 64
    j = d % 32
    inv = (THETA ** (-(j.astype(np.float32)) / 32.0)).astype(np.float32)
    ang = pos[half] * inv[:, None]
    cos = np.cos(ang).astype(np.float32)
    sin = np.sin(ang).astype(np.float32)
    sgn = np.where((d % 64) < 32, -1.0, 1.0).astype(np.float32)
    Rm = np.zeros((128, 128), np.float32)
    partner = np.where((d % 64) < 32, d + 32, d - 32)
    Rm[partner, d] = 1.0
    return cos, (sin * sgn[:, None]).astype(np.float32), Rm


def emit_att(kb, qT_d, kT_d, vT_d, gq_d, gk_d, cos_d, sin_d, rm_d, ident_d, xs, row0, in_regs, outs):
    p = kb.p
    banks = kb.banks
    L = LAT
    NKT = T // 128
    idf, idb, ones = consts(kb, ident_d)
    onesb = kb.sb("onesb", [128, 128], BF16)
    p.op("vector", lambda e: e.tensor_copy(out=onesb[:], in_=ones[:]), [ones.r], [onesb.r])
    rm = ld(kb, "rm", [128, 128], rm_d)
    gq = ld(kb, "gq", [128, 1], gq_d)
    gk = ld(kb, "gk", [128, 1], gk_d)
    cos = ld(kb, "cos", [128, L], cos_d)
    sin = ld(kb, "sin", [128, L], sin_d)
    xst = [kb.sb("xst%d" % i, [128, T]) for i in range(2)]
    vb = kb.sb("vb", [128, NKT, 128], BF16)
    vtb = kb.sb("vtb", [128, T], BF16)
    p.dma("sync", xst[1][:], vT_d, reads=in_regs, writes=[xst[1].r])
    p.op("gpsimd", lambda e: e.tensor_copy(out=vtb[:], in_=xst[1][:]), [xst[1].r], [vtb.r])
    ptr = kb.psbf(banks[1])
    for g in range((NKT + 7) // 8):
        k0, k1 = g * 8, min(NKT, g * 8 + 8)

        def trv(e, k0=k0, k1=k1):
            for kt in range(k0, k1):
                last = e.transpose(out=ptr.t[:, (kt - k0) * 128:(kt - k0 + 1) * 128], in_=vtb[:, kt * 128:(kt + 1) * 128], identity=idb[:])
            return last
        p.op("tensor", trv, [vtb.r, idb.r], [ptr.r])
        p.op("scalar", lambda e, k0=k0, k1=k1: e.copy(out=vb[:, k0:k1, :], in_=ptr.t[:, 0:(k1 - k0) * 128].rearrange("p (a b) -> p a b", b=128)), [ptr.r], [vb.r])

    chunks = [(0, NCTX, False)] + [(NCTX + c * 512, 512, True) for c in range(L // 512)]
    knT = kb.sb("knT", [128, T], BF16)
    qnT = [kb.sb("qnT%d" % i, [128, T], BF16) for i in range(2)]
    sqt = kb.sb("sqt", [128, 512])
    rstd = kb.sb("rstd", [128, 512])
    xg = kb.sb("xg", [128, 512])
    t1 = kb.sb("t1", [128, 512])
    t2 = kb.sb("t2", [128, 512])
    pss, prot = banks[0], banks[1]
    srcs = [(kT_d, gk, knT), (qT_d[0:128, :], gq, qnT[0]), (qT_d[128:256, :], gq, qnT[1])]
    dregs = []
    for si, (src, g, dst) in enumerate(srcs):
        xs_ = xst[si % 2]
        p.dma("sync", xs_[:], src, reads=in_regs, writes=[xs_.r])
        dr = [Reg("d%d_%d" % (si, c)) for c in range(len(chunks))]
        dregs.append(dr)
        for c, (t0, n, lat) in enumerate(chunks):
            p.op("scalar", lambda e, xs_=xs_, t0=t0, n=n: e.activation(out=sqt[:, 0:n], in_=xs_[:, t0:t0 + n], func=AF.Square), [xs_.r], [sqt.r])
            p.op("tensor", lambda e, n=n: e.matmul(pss[:, 0:n], lhsT=ones[:], rhs=sqt[:, 0:n], start=True, stop=True), [ones.r, sqt.r], [pss.r])
            p.op("vector", lambda e, n=n: e.tensor_scalar(out=rstd[:, 0:n], in0=pss[:, 0:n], scalar1=1.0 / 128, scalar2=EPS, op0=ALU.mult, op1=ALU.add), [pss.r], [rstd.r])
            p.op("vector", lambda e, n=n: e.reciprocal(out=rstd[:, 0:n], in_=rstd[:, 0:n]), [rstd.r], [rstd.r])
            p.op("scalar", lambda e, n=n: e.activation(out=rstd[:, 0:n], in_=rstd[:, 0:n], func=AF.Sqrt), [rstd.r], [rstd.r])
            p.op("vector", lambda e, xs_=xs_, g=g, t0=t0, n=n: e.tensor_scalar(out=xg[:, 0:n], in0=xs_[:, t0:t0 + n], scalar1=g[:, 0:1], scalar2=None, op0=ALU.mult), [xs_.r, g.r], [xg.r])
            if lat:
                l0 = t0 - NCTX
                p.op("tensor", lambda e, n=n: e.matmul(prot[:, 0:n], lhsT=rm[:], rhs=xg[:, 0:n], start=True, stop=True), [rm.r, xg.r], [prot.r])
                p.op("gpsimd", lambda e, l0=l0, n=n: e.tensor_tensor(out=t1[:, 0:n], in0=xg[:, 0:n], in1=cos[:, l0:l0 + n], op=ALU.mult), [xg.r, cos.r], [t1.r])
                p.op("vector", lambda e, l0=l0, n=n: e.tensor_tensor(out=t2[:, 0:n], in0=prot[:, 0:n], in1=sin[:, l0:l0 + n], op=ALU.mult), [prot.r, sin.r], [t2.r])
                p.op("gpsimd", lambda e, n=n: e.tensor_tensor(out=t1[:, 0:n], in0=t1[:, 0:n], in1=t2[:, 0:n], op=ALU.add), [t1.r, t2.r], [t1.r])
                p.op("vector", lambda e, dst=dst, t0=t0, n=n: e.tensor_tensor(out=dst[:, t0:t0 + n], in0=t1[:, 0:n], in1=rstd[:, 0:n], op=ALU.mult), [t1.r, rstd.r], [dr[c]])
            else:
                p.op("vector", lambda e, dst=dst, t0=t0, n=n: e.tensor_tensor(out=dst[:, t0:t0 + n], in0=xg[:, 0:n], in1=rstd[:, 0:n], op=ALU.mult), [xg.r, rstd.r], [dr[c]])
    kr, qr = dregs[0], dregs[1:]
    pS = banks[2:5]
    pO = banks[5:7]
    pD = [banks[7], banks[0]]
    pts = [kb.sb("pt%d" % i, [128, 512], BF16) for i in range(3)]
    rden = [kb.sb("rden%d" % i, [128, 512]) for i in range(2)]
    yo = [kb.sb("yo%d" % i, [128, 512]) for i in range(2)]
    scale = 128.0 ** -0.5
    sc = 0
    blk = 0
    for h in range(2):
        for c, (t0, n, lat) in enumerate(chunks):
            nkt = NKT if lat else NCTX // 128
            po, pd = pO[blk % 2], pD[blk % 2]
            def s_exp(kt, sc_):
                ps_, pt = pS[sc_ % 3], pts[sc_ % 3]
                kc = 0 if kt < NCTX // 128 else 1 + (kt * 128 - NCTX) // 512
                p.op("tensor", lambda e, ps_=ps_, kt=kt, h=h, t0=t0, n=n: e.matmul(ps_[:, 0:n], lhsT=knT[:, kt * 128:(kt + 1) * 128], rhs=qnT[h][:, t0:t0 + n], start=True, stop=True),
                     [kr[kc], qr[h][c]], [ps_.r])
                p.op("scalar", lambda e, ps_=ps_, pt=pt, n=n: e.activation(out=pt[:, 0:n], in_=ps_[:, 0:n], func=AF.Exp, scale=scale), [ps_.r], [pt.r])
            s_exp(0, sc)
            for kt in range(nkt):
                pt = pts[sc % 3]
                if kt + 1 < nkt:
                    s_exp(kt + 1, sc + 1)
                sc += 1

                def pv(e, po=po, pd=pd, pt=pt, kt=kt, n=n, nkt=nkt):
                    e.matmul(po[:, 0:n], lhsT=vb[:, kt, :], rhs=pt[:, 0:n], start=(kt == 0), stop=(kt == nkt - 1))
                    return e.matmul(pd[:, 0:n], lhsT=onesb[:], rhs=pt[:, 0:n], start=(kt == 0), stop=(kt == nkt - 1))
                p.op("tensor", pv, [vb.r, onesb.r, pt.r], [po.r, pd.r])
            rd, y = rden[blk % 2], yo[blk % 2]
            p.op("vector", lambda e, rd=rd, pd=pd, n=n: e.reciprocal(out=rd[:, 0:n], in_=pd[:, 0:n]), [pd.r], [rd.r])
            p.op("vector", lambda e, rd=rd, po=po, y=y, n=n: e.tensor_tensor(out=y[:, 0:n], in0=po[:, 0:n], in1=rd[:, 0:n], op=ALU.mult), [po.r, rd.r], [y.r])
            for (a, m, hh, loc) in nat_pieces(t0, n):
                o = Reg("o")
                outs.append(o)
                p.dma("gpsimd", xs[hh * 3072 + row0 + h * 128:hh * 3072 + row0 + (h + 1) * 128, loc:loc + m], y[:, a - t0:a - t0 + m], reads=[y.r], writes=[o])
            blk += 1


def emit_conv(kb, p, x, u, w, b, segs, cc, eng_extra="vector"):
    p.op("scalar", lambda e: e.activation(out=u[:], in_=x[:], func=AF.Identity, scale=w[:, cc, 2:3], bias=b[:, cc:cc + 1]), [x.r, w.r, b.r], [u.r])

    def taps(e):
        for (s0, n) in segs:
            for k, off in ((0, -2), (1, -1), (3, 1)):
                lo = max(0, -off)
                hi = n - max(0, off)
                last = e.scalar_tensor_tensor(out=u[:, s0 + lo:s0 + hi], in0=x[:, s0 + lo + off:s0 + hi + off], scalar=w[:, cc, k:k + 1],
                                              in1=u[:, s0 + lo:s0 + hi], op0=ALU.mult, op1=ALU.add)
        return last
    p.op(eng_extra, taps, [x.r, u.r, w.r], [u.r])


def emit_lru(kb, xT_d, gT_d, cw_d, cb_d, wbd_d, bias_d, lam_d, xs, row0, in_regs, outs):
    p = kb.p
    banks = kb.banks
    L = LAT
    cw = ld(kb, "cw", [128, 2, 4], cw_d)
    cb = ld(kb, "cb", [128, 2], cb_d)
    wbd = ld(kb, "wbd", [128, 8, 128], wbd_d)
    bias = ld(kb, "bias", [128, 8], bias_d)
    lam = ld(kb, "lam", [128, 4], lam_d)
    cl = kb.sb("cl", [128, 4])
    p.op("scalar", lambda e: e.activation(out=cl[:], in_=lam[:], func=AF.Exp, scale=-1.0), [lam.r], [cl.r])
    p.op("scalar", lambda e: e.activation(out=cl[:], in_=cl[:], func=AF.Ln, bias=1.0, scale=1.0), [cl.r], [cl.r])
    p.op("vector", lambda e: e.tensor_scalar(out=cl[:], in0=cl[:], scalar1=-8.0, scalar2=None, op0=ALU.mult), [cl.r], [cl.r])
    x = kb.sb("x", [128, T]); u = kb.sb("u", [128, T]); g = kb.sb("g", [128, T])
    av = [[kb.sb("a%d" % d, [128, T]), kb.sb("v%d" % d, [128, T])] for d in range(2)]
    hh = [kb.sb("h%d" % d, [128, T]) for d in range(2)]
    rt = [kb.sb("rt%d" % i, [128, 512]) for i in range(2)]
    it_ = [kb.sb("it%d" % i, [128, 512]) for i in range(2)]
    s2 = [kb.sb("s2%d" % i, [128, 512]) for i in range(2)]
    segs = [(0, NCTX), (NCTX, L)]
    chunks = [(0, NCTX)] + [(NCTX + c * 512, 512) for c in range(L // 512)]
    bc = 0
    for cc in range(2):
        p.dma("sync", x[:], xT_d[cc * 128:(cc + 1) * 128, :], reads=in_regs, writes=[x.r])
        p.dma("sync", g[:], gT_d[cc * 128:(cc + 1) * 128, :], reads=in_regs, writes=[g.r])
        emit_conv(kb, p, x, u, cw, cb, segs, cc)
        for d in range(2):
            a, v = av[d]
            for c, (t0, n) in enumerate(chunks):
                pr, pi = banks[bc % 8], banks[(bc + 1) % 8]
                bc += 2
                r_, i_, s_ = rt[c % 2], it_[c % 2], s2[c % 2]
                ia, ix = 0 * 4 + d * 2 + cc, 1 * 4 + d * 2 + cc
                p.op("tensor", lambda e, pr=pr, ia=ia, t0=t0, n=n: e.matmul(pr[:, 0:n], lhsT=wbd[:, ia, :], rhs=u[:, t0:t0 + n], start=True, stop=True), [wbd.r, u.r], [pr.r])
                p.op("tensor", lambda e, pi=pi, ix=ix, t0=t0, n=n: e.matmul(pi[:, 0:n], lhsT=wbd[:, ix, :], rhs=u[:, t0:t0 + n], start=True, stop=True), [wbd.r, u.r], [pi.r])
                p.op("scalar", lambda e, pr=pr, r_=r_, ia=ia, n=n: e.activation(out=r_[:, 0:n], in_=pr[:, 0:n], func=AF.Sigmoid, bias=bias[:, ia:ia + 1], scale=1.0), [pr.r, bias.r], [r_.r])
                p.op("scalar", lambda e, pi=pi, i_=i_, ix=ix, n=n: e.activation(out=i_[:, 0:n], in_=pi[:, 0:n], func=AF.Sigmoid, bias=bias[:, ix:ix + 1], scale=1.0), [pi.r, bias.r], [i_.r])
                p.op("scalar", lambda e, a=a, r_=r_, d=d, cc=cc, t0=t0, n=n: e.activation(out=a[:, t0:t0 + n], in_=r_[:, 0:n], func=AF.Exp, scale=cl[:, d * 2 + cc:d * 2 + cc + 1]), [r_.r, cl.r], [a.r])
                p.op("gpsimd", lambda e, a=a, s_=s_, t0=t0, n=n: e.tensor_tensor(out=s_[:, 0:n], in0=a[:, t0:t0 + n], in1=a[:, t0:t0 + n], op=ALU.mult), [a.r], [s_.r])
                p.op("scalar", lambda e, s_=s_, n=n: e.activation(out=s_[:, 0:n], in_=s_[:, 0:n], func=AF.Sqrt, scale=-1.0, bias=1.0), [s_.r], [s_.r])
                p.op("vector", lambda e, i_=i_, t0=t0, n=n: e.tensor_tensor(out=i_[:, 0:n], in0=i_[:, 0:n], in1=u[:, t0:t0 + n], op=ALU.mult), [i_.r, u.r], [i_.r])
                p.op("vector", lambda e, v=v, i_=i_, s_=s_, t0=t0, n=n: e.tensor_tensor(out=v[:, t0:t0 + n], in0=i_[:, 0:n], in1=s_[:, 0:n], op=ALU.mult), [i_.r, s_.r], [v.r])
            h = hh[d]
            if d == 0:
                p.op("vector", lambda e, a=a, v=v, h=h: e.tensor_tensor_scan(out=h[:, 0:NCTX], data0=a[:, 0:NCTX], data1=v[:, 0:NCTX], initial=0.0, op0=ALU.mult, op1=ALU.add), [a.r, v.r], [h.r])
                p.op("vector", lambda e, a=a, v=v, h=h: e.tensor_tensor_scan(out=h[:, NCTX:T], data0=a[:, NCTX:T], data1=v[:, NCTX:T], initial=h[:, NCTX - 1:NCTX], op0=ALU.mult, op1=ALU.add), [a.r, v.r, h.r], [h.r])
            else:
                p.op("vector", lambda e, a=a, v=v, h=h: e.tensor_tensor_scan(out=h[:, 0:NCTX][:, ::-1], data0=a[:, 0:NCTX][:, ::-1], data1=v[:, 0:NCTX][:, ::-1], initial=0.0, op0=ALU.mult, op1=ALU.add), [a.r, v.r], [h.r])
                p.op("vector", lambda e, a=a, v=v, h=h: e.tensor_tensor_scan(out=h[:, NCTX:T][:, ::-1], data0=a[:, NCTX:T][:, ::-1], data1=v[:, NCTX:T][:, ::-1], initial=h[:, 0:1], op0=ALU.mult, op1=ALU.add), [a.r, v.r, h.r], [h.r])
        z = av[0][0]
        p.op("gpsimd", lambda e: e.tensor_tensor(out=z[:], in0=g[:], in1=g[:], op=ALU.mult), [g.r], [z.r])
        p.op("vector", lambda e: e.tensor_scalar(out=z[:], in0=z[:], scalar1=0.044715, scalar2=1.0, op0=ALU.mult, op1=ALU.add), [z.r], [z.r])
        p.op("gpsimd", lambda e: e.tensor_tensor(out=z[:], in0=z[:], in1=g[:], op=ALU.mult), [z.r, g.r], [z.r])
        p.op("scalar", lambda e: e.activation(out=z[:], in_=z[:], func=AF.Sigmoid, scale=1.5957691216057308), [z.r], [z.r])
        p.op("gpsimd", lambda e: e.tensor_tensor(out=z[:], in0=z[:], in1=g[:], op=ALU.mult), [z.r, g.r], [z.r])
        p.op("vector", lambda e: e.tensor_tensor(out=hh[0][:], in0=hh[0][:], in1=hh[1][:], op=ALU.add), [hh[0].r, hh[1].r], [hh[0].r])
        p.op("vector", lambda e: e.tensor_tensor(out=hh[0][:], in0=hh[0][:], in1=z[:], op=ALU.mult), [hh[0].r, z.r], [hh[0].r])
        xs_write(p, xs, row0 + cc * 128, hh[0], [hh[0].r], outs)


def rev_segments(e, out_t, in_t, segs):
    for (s0, n) in segs:
        last = e.tensor_copy(out=out_t[:, s0:s0 + n], in_=in_t[:, s0:s0 + n][:, ::-1])
    return last


def scan_nat_lo(t0, n):
    if t0 < NCTX:
        return NCTX - (t0 + n)
    return NCTX + LAT - (t0 - NCTX + n)


def emit_gla(kb, qT_d, kT_d, vT_d, g1_d, rT_d, g2_d, gb_d, gn_d, cm_d, m2_d, hm_d, ident_d, xs, row0, in_regs, outs):
    p = kb.p
    banks = kb.banks
    L = LAT
    NP = T // 128
    ptr = kb.psbf(banks[7])
    plog, patt, pO, pkv = banks[0], banks[1:3], banks[3:5], banks[5:7]
    segs = [(0, NCTX), (NCTX, L)]
    g2 = ld(kb, "g2", [16, 2, 256], g2_d)
    ngb = ld(kb, "ngb", [128, 4], gb_d)
    p.op("vector", lambda e: e.tensor_scalar(out=ngb[:], in0=ngb[:], scalar1=-1.0, scalar2=None, op0=ALU.mult), [ngb.r], [ngb.r])
    gn = ld(kb, "gn", [128, 2], gn_d)
    cm = ld(kb, "cm", [128, 512], cm_d)
    m2 = ld(kb, "m2", [128, 128], m2_d)
    hm = ld(kb, "hm", [128, 2], hm_d)
    idf, idb, ones = consts(kb, ident_d)
    g1b = kb.sb("g1b", [16, 512])
    g1c = kb.sb("g1c", [16, 512])
    osb = [kb.sb("osb%d" % i, [128, T]) for i in range(4)]
    qT = kb.sb("qT", [128, T])
    kT = kb.sb("kT", [128, T])
    tmpT = kb.sb("tmpT", [128, T])
    vtb = kb.sb("vtb", [128, T], BF16)
    vb = kb.sb("vb", [128, NP, 128], BF16)
    F = lambda n: kb.sb(n, [128, 512])
    sp, gcs, dref, dlast, Aex, Bex, Dex, Eex = F("sp"), F("gcs"), F("dref"), F("dlast"), F("Aex"), F("Bex"), F("Dex"), F("Eex")
    dec = kb.sb("dec", [128, 8])
    Bq = lambda n: kb.sb(n, [128, 512], BF16)
    qt, kt, kd, qe = Bq("qt"), Bq("kt"), Bq("kd"), Bq("qe")
    kdT = [kb.sb("kdT%d" % i, [128, 2, 128], BF16) for i in range(2)]
    attm = [kb.sb("attm%d" % i, [128, 128], BF16) for i in range(2)]
    S = kb.sb("S", [128, 128])
    NSB = 4
    Sb = [kb.sb("Sb%d" % i, [128, 128], BF16) for i in range(NSB)]
    scale = 128.0 ** -0.5
    blocks = [(0, NCTX)] + [(NCTX + c * 512, 512) for c in range(L // 512)]
    pc = 0
    for hd in range(4):
        h, d = hd // 2, hd % 2
        for (dst, src) in ((qT, qT_d), (kT, kT_d)):
            if d == 0:
                p.dma("sync", dst[:], src[h * 128:(h + 1) * 128, :], reads=in_regs, writes=[dst.r])
            else:
                p.dma("sync", tmpT[:], src[h * 128:(h + 1) * 128, :], reads=in_regs, writes=[tmpT.r])
                p.op("gpsimd", lambda e, dst=dst: rev_segments(e, dst, tmpT, segs), [tmpT.r], [dst.r])
        p.dma("sync", tmpT[:], vT_d[h * 128:(h + 1) * 128, :], reads=in_regs, writes=[tmpT.r])
        if d == 0:
            p.op("gpsimd", lambda e: e.tensor_copy(out=vtb[:], in_=tmpT[:]), [tmpT.r], [vtb.r])
        else:
            p.op("gpsimd", lambda e: rev_segments(e, vtb, tmpT, segs), [tmpT.r], [vtb.r])
        for g in range((NP + 7) // 8):
            k0, k1 = g * 8, min(NP, g * 8 + 8)

            def trv(e, k0=k0, k1=k1):
                for kt_ in range(k0, k1):
                    last = e.transpose(out=ptr.t[:, (kt_ - k0) * 128:(kt_ - k0 + 1) * 128], in_=vtb[:, kt_ * 128:(kt_ + 1) * 128], identity=idb[:])
                return last
            p.op("tensor", trv, [vtb.r, idb.r], [ptr.r])
            p.op("scalar", lambda e, k0=k0, k1=k1: e.copy(out=vb[:, k0:k1, :], in_=ptr.t[:, 0:(k1 - k0) * 128].rearrange("p (a b) -> p a b", b=128)), [ptr.r], [vb.r])
        p.op("vector", lambda e: e.memset(S[:], 0.0), [], [S.r])
        sbi = 0
        p.op("gpsimd", lambda e, sb_=Sb[0]: e.memset(sb_[:], 0.0), [], [Sb[0].r])
        for (t0, n) in blocks:
            nc_ = n // 64
            v3 = lambda t, n=n: t[:, 0:n].rearrange("p (c l) -> p c l", l=64)
            if d == 0:
                p.dma("sync", g1b[:, 0:n], g1_d[0:16, t0:t0 + n], reads=in_regs, writes=[g1b.r])
                gsrc = g1b
            else:
                lo = scan_nat_lo(t0, n)
                p.dma("sync", g1c[:, 0:n], g1_d[16:32, lo:lo + n], reads=in_regs, writes=[g1c.r])
                p.op("vector", lambda e, n=n: e.tensor_copy(out=g1b[:, 0:n], in_=g1c[:, 0:n][:, ::-1]), [g1c.r], [g1b.r])
                gsrc = g1b
            p.op("tensor", lambda e, d=d, h=h, n=n: e.matmul(plog[:, 0:n], lhsT=g2[:, d, h * 128:(h + 1) * 128], rhs=g1b[:, 0:n], start=True, stop=True), [g2.r, g1b.r], [plog.r])
            p.op("scalar", lambda e, hd=hd, n=n: e.activation(out=sp[:, 0:n], in_=plog[:, 0:n], func=AF.Exp, scale=-1.0, bias=ngb[:, hd:hd + 1]), [plog.r, ngb.r], [sp.r])
            p.op("scalar", lambda e, n=n: e.activation(out=sp[:, 0:n], in_=sp[:, 0:n], func=AF.Ln, scale=1.0, bias=1.0), [sp.r], [sp.r])
            p.op("vector", lambda e, n=n: e.tensor_tensor_scan(out=gcs[:, 0:n], data0=cm[:, 0:n], data1=sp[:, 0:n], initial=0.0, op0=ALU.mult, op1=ALU.add), [cm.r, sp.r], [gcs.r])
            p.op("vector", lambda e, n=n, nc_=nc_, v3=v3: e.tensor_tensor(out=v3(dref), in0=v3(gcs), in1=v3(gcs)[:, :, 32:33].to_broadcast([128, nc_, 64]), op=ALU.subtract), [gcs.r], [dref.r])
            p.op("gpsimd", lambda e, n=n, nc_=nc_, v3=v3: e.tensor_tensor(out=v3(dlast), in0=v3(gcs), in1=v3(gcs)[:, :, 63:64].to_broadcast([128, nc_, 64]), op=ALU.subtract), [gcs.r], [dlast.r])
            A = lambda fn, rd, wr: p.op("scalar", fn, rd, wr)
            A(lambda e, n=n: e.activation(out=Aex[:, 0:n], in_=dref[:, 0:n], func=AF.Exp, scale=-1.0 / 16), [dref.r], [Aex.r])
            A(lambda e, n=n: e.activation(out=Bex[:, 0:n], in_=dref[:, 0:n], func=AF.Exp, scale=1.0 / 16), [dref.r], [Bex.r])
            A(lambda e, n=n: e.activation(out=Dex[:, 0:n], in_=dlast[:, 0:n], func=AF.Exp, scale=1.0 / 16), [dlast.r], [Dex.r])
            A(lambda e, n=n: e.activation(out=Eex[:, 0:n], in_=gcs[:, 0:n], func=AF.Exp, scale=-1.0 / 16), [gcs.r], [Eex.r])
            A(lambda e, nc_=nc_, v3=v3: e.activation(out=dec[:, 0:nc_], in_=v3(gcs)[:, :, 63], func=AF.Exp, scale=-1.0 / 16), [gcs.r], [dec.r])
            p.op("vector", lambda e, t0=t0, n=n: e.scalar_tensor_tensor(out=qt[:, 0:n], in0=qT[:, t0:t0 + n], scalar=scale, in1=Aex[:, 0:n], op0=ALU.mult, op1=ALU.mult), [qT.r, Aex.r], [qt.r])
            p.op("gpsimd", lambda e, t0=t0, n=n: e.tensor_tensor(out=kt[:, 0:n], in0=kT[:, t0:t0 + n], in1=Bex[:, 0:n], op=ALU.mult), [kT.r, Bex.r], [kt.r])
            p.op("vector", lambda e, t0=t0, n=n: e.tensor_tensor(out=kd[:, 0:n], in0=kT[:, t0:t0 + n], in1=Dex[:, 0:n], op=ALU.mult), [kT.r, Dex.r], [kd.r])
            p.op("vector", lambda e, t0=t0, n=n: e.scalar_tensor_tensor(out=qe[:, 0:n], in0=qT[:, t0:t0 + n], scalar=scale, in1=Eex[:, 0:n], op0=ALU.mult, op1=ALU.mult), [qT.r, Eex.r], [qe.r])
            for pp in range(n // 128):
                s = pp * 128
                gp = (t0 + s) // 128
                kT_, am, pa, po, pk = kdT[pc % 2], attm[pc % 2], patt[pc % 2], pO[pc % 2], pkv[pc % 2]
                tr = ptr.t[:, (pc % 2) * 128:(pc % 2) * 128 + 128]
                pc += 1
                p.op("tensor", lambda e, tr=tr, s=s: e.transpose(out=tr, in_=kd[:, s:s + 128], identity=idb[:]), [kd.r, idb.r], [ptr.r])

                def cpk(e, tr=tr, kT_=kT_):
                    e.activation(out=kT_[:, 0, :], in_=tr, func=AF.Identity, scale=hm[:, 0:1])
                    return e.activation(out=kT_[:, 1, :], in_=tr, func=AF.Identity, scale=hm[:, 1:2])
                p.op("scalar", cpk, [ptr.r, hm.r], [kT_.r])
                p.op("tensor", lambda e, pa=pa, s=s: e.matmul(pa[:, 0:128], lhsT=kt[:, s:s + 128], rhs=qt[:, s:s + 128], start=True, stop=True), [kt.r, qt.r], [pa.r])
                p.op("vector", lambda e, pa=pa, am=am: e.tensor_tensor(out=am[:], in0=pa[:, 0:128], in1=m2[:], op=ALU.mult), [pa.r, m2.r], [am.r])

                def mkv(e, pk=pk, kT_=kT_, gp=gp):
                    e.matmul(pk[:, 0:128], lhsT=kT_[:, 0, :], rhs=vb[:, gp, :], start=True, stop=True)
                    return e.matmul(pk[:, 128:256], lhsT=kT_[:, 1, :], rhs=vb[:, gp, :], start=True, stop=True)
                p.op("tensor", mkv, [kT_.r, vb.r], [pk.r])
                sb0 = Sb[sbi % NSB]
                sb1 = Sb[(sbi + 1) % NSB]
                sb2 = Sb[(sbi + 2) % NSB]
                sbi += 2
                c0 = s // 64
                p.op("vector", lambda e, pk=pk, c0=c0: e.scalar_tensor_tensor(out=S[:], in0=S[:], scalar=dec[:, c0:c0 + 1], in1=pk[:, 0:128], op0=ALU.mult, op1=ALU.add), [S.r, dec.r, pk.r], [S.r])
                p.op("scalar", lambda e, sb1=sb1: e.copy(out=sb1[:], in_=S[:]), [S.r], [sb1.r])
                p.op("vector", lambda e, pk=pk, c0=c0: e.scalar_tensor_tensor(out=S[:], in0=S[:], scalar=dec[:, c0 + 1:c0 + 2], in1=pk[:, 128:256], op0=ALU.mult, op1=ALU.add), [S.r, dec.r, pk.r], [S.r])
                p.op("scalar", lambda e, sb2=sb2: e.copy(out=sb2[:], in_=S[:]), [S.r], [sb2.r])

                def mo(e, po=po, am=am, gp=gp, sb0=sb0, sb1=sb1, s=s):
                    e.matmul(po[:, 0:128], lhsT=vb[:, gp, :], rhs=am[:], start=True, stop=False)
                    e.matmul(po[:, 0:64], lhsT=sb0[:], rhs=qe[:, s:s + 64], start=False, stop=False)
                    return e.matmul(po[:, 64:128], lhsT=sb1[:], rhs=qe[:, s + 64:s + 128], start=False, stop=True)
                p.op("tensor", mo, [vb.r, am.r, sb0.r, sb1.r, qe.r], [po.r])
                p.op("scalar", lambda e, po=po, hd=hd, a=t0 + s: e.copy(out=osb[hd][:, a:a + 128], in_=po[:, 0:128]), [po.r], [osb[hd].r])
    rt = kb.sb("rt", [128, 512])
    yy = [kb.sb("yy%d" % i, [128, 512]) for i in range(2)]
    pss = banks[0]
    bi = 0
    for h in range(2):
        of, ob = osb[2 * h], osb[2 * h + 1]

        def comb(e, of=of, ob=ob):
            for (s0, n) in segs:
                last = e.tensor_tensor(out=of[:, s0:s0 + n], in0=of[:, s0:s0 + n], in1=ob[:, s0:s0 + n][:, ::-1], op=ALU.add)
            return last
        p.op("vector", comb, [of.r, ob.r], [of.r])
        for (t0, n) in blocks:
            y_ = yy[bi % 2]
            bi += 1
            p.op("scalar", lambda e, of=of, t0=t0, n=n: e.activation(out=sp[:, 0:n], in_=of[:, t0:t0 + n], func=AF.Square), [of.r], [sp.r])
            p.op("tensor", lambda e, n=n: e.matmul(pss[:, 0:n], lhsT=ones[:], rhs=sp[:, 0:n], start=True, stop=True), [ones.r, sp.r], [pss.r])
            p.op("vector", lambda e, n=n: e.tensor_scalar(out=gcs[:, 0:n], in0=pss[:, 0:n], scalar1=1.0 / 128, scalar2=EPS, op0=ALU.mult, op1=ALU.add), [pss.r], [gcs.r])
            p.op("vector", lambda e, n=n: e.reciprocal(out=gcs[:, 0:n], in_=gcs[:, 0:n]), [gcs.r], [gcs.r])
            p.op("scalar", lambda e, n=n: e.activation(out=gcs[:, 0:n], in_=gcs[:, 0:n], func=AF.Sqrt), [gcs.r], [gcs.r])
            p.dma("sync", rt[:, 0:n], rT_d[h * 128:(h + 1) * 128, t0:t0 + n], reads=in_regs, writes=[rt.r])
            p.op("scalar", lambda e, n=n: e.activation(out=rt[:, 0:n], in_=rt[:, 0:n], func=AF.Silu), [rt.r], [rt.r])
            p.op("vector", lambda e, of=of, y_=y_, t0=t0, n=n: e.tensor_tensor(out=y_[:, 0:n], in0=of[:, t0:t0 + n], in1=gcs[:, 0:n], op=ALU.mult), [of.r, gcs.r], [y_.r])
            p.op("vector", lambda e, h=h, y_=y_, n=n: e.scalar_tensor_tensor(out=y_[:, 0:n], in0=y_[:, 0:n], scalar=gn[:, h:h + 1], in1=rt[:, 0:n], op0=ALU.mult, op1=ALU.mult), [y_.r, gn.r, rt.r], [y_.r])
            for (a, m, hh_, loc) in nat_pieces(t0, n):
                o = Reg("o")
                outs.append(o)
                p.dma("gpsimd", xs[hh_ * 3072 + row0 + h * 128:hh_ * 3072 + row0 + (h + 1) * 128, loc:loc + m], y_[:, a - t0:a - t0 + m], reads=[y_.r], writes=[o])


def emit_ssd(kb, xbc_d, z_d, dt_d, cw_d, cb_d, dtb_d, alog_d, Dp_d, U_d, ident_d, xs, row0, in_regs, outs):
    p = kb.p
    banks = kb.banks
    L = LAT
    NCH = T // 128
    ptr = kb.psbf(banks[7])
    psm, pCB, pbc, pdiag, poff, pst = banks[0], banks[1], banks[2:4], banks[4], banks[5], banks[6]
    cw = ld(kb, "cw", [128, 4, 4], cw_d)
    cb = ld(kb, "cb", [128, 4], cb_d)
    dtb = ld(kb, "dtb", [4, 2], dtb_d)
    aneg = ld(kb, "aneg", [4, 2], alog_d)
    p.op("scalar", lambda e: e.activation(out=aneg[:], in_=aneg[:], func=AF.Exp), [aneg.r], [aneg.r])
    p.op("vector", lambda e: e.tensor_scalar(out=aneg[:], in0=aneg[:], scalar1=-1.0, scalar2=None, op0=ALU.mult), [aneg.r], [aneg.r])
    Dp = ld(kb, "Dp", [128, 2], Dp_d)
    U = ld(kb, "U", [128, 128], U_d)
    idf, idb, ones = consts(kb, ident_d)
    segs = [(0, NCTX), (NCTX, L)]
    arr = [[kb.sb("arr%d_%d" % (d, k), [128, T], BF16) for k in range(4)] for d in range(2)]
    xin = [kb.sb("xin%d" % i, [128, T]) for i in range(2)]
    u = kb.sb("u", [128, T])
    for k in range(4):
        xi = xin[k % 2]
        p.dma("sync", xi[:], xbc_d[k * 128:(k + 1) * 128, :], reads=in_regs, writes=[xi.r])
        emit_conv(kb, p, xi, u, cw, cb, segs, k)
        p.op("scalar", lambda e, k=k: e.activation(out=arr[0][k][:], in_=u[:], func=AF.Silu), [u.r], [arr[0][k].r])
        p.op("gpsimd", lambda e, k=k: rev_segments(e, arr[1][k], arr[0][k], segs), [arr[0][k].r], [arr[1][k].r])
    yacc = [xin[0], xin[1]]
    dtp = kb.sb("dtp", [4, 2, T])
    F = lambda n, s, dt=F32: kb.sb(n, s, dt)
    xBt = [F("xBt%d" % i, [128, 384], BF16) for i in range(2)]
    dtk = [F("dtk%d" % i, [128, 8]) for i in range(2)]
    acl = [F("acl%d" % i, [128, 8]) for i in range(2)]
    nac = [F("nac%d" % i, [128, 4]) for i in range(2)]
    eac = [F("eac%d" % i, [128, 4]) for i in range(2)]
    wdt = [F("wdt%d" % i, [128, 4]) for i in range(2)]
    dcy = [F("dcy%d" % i, [128, 4]) for i in range(2)]
    dtx = [F("dtx%d" % i, [128, 256], BF16) for i in range(2)]
    xw = [F("xw%d" % i, [128, 256], BF16) for i in range(2)]
    rU = [F("rU%d" % i, [128, 128]) for i in range(2)]
    sg = [F("sg%d" % i, [128, 128]) for i in range(2)]
    Lt = [F("Lt%d" % i, [128, 128]) for i in range(2)]
    Mb = [F("Mb%d" % i, [128, 128], BF16) for i in range(8)]
    CBs = [F("CBs%d" % i, [128, 128]) for i in range(2)]
    yt1 = [F("yt1_%d" % i, [128, 256]) for i in range(2)]
    yt2 = [F("yt2_%d" % i, [128, 256]) for i in range(2)]
    H = F("H", [128, 256])
    Hb = [F("Hb%d" % i, [128, 256], BF16) for i in range(2)]
    nonlocal_it = [0]
    nonlocal_hc = [0]
    for d in range(2):
        slot = 0 if d == 0 else 1
        p.dma("sync", dtp[:, slot, :], dt_d[d * 4:(d + 1) * 4, :], reads=in_regs, writes=[dtp.r])
        p.op("scalar", lambda e, slot=slot, d=d: e.activation(out=dtp[:, slot, :], in_=dtp[:, slot, :], func=AF.Exp, bias=dtb[:, d:d + 1], scale=1.0), [dtp.r, dtb.r], [dtp.r])
        p.op("scalar", lambda e, slot=slot: e.activation(out=dtp[:, slot, :], in_=dtp[:, slot, :], func=AF.Ln, bias=1.0, scale=1.0), [dtp.r], [dtp.r])
        if d == 1:
            def revdt(e):
                for (s0, n) in segs:
                    last = e.tensor_copy(out=dtp[:, 0, s0:s0 + n], in_=dtp[:, 1, s0:s0 + n][:, ::-1])
                return last
            p.op("vector", revdt, [dtp.r], [dtp.r])
        p.op("vector", lambda e, d=d: e.tensor_scalar(out=dtp[:, 1, :], in0=dtp[:, 0, :], scalar1=aneg[:, d:d + 1], scalar2=None, op0=ALU.mult), [dtp.r, aneg.r], [dtp.r])
        p.op("vector", lambda e: e.memset(H[:], 0.0), [], [H.r])
        p.op("gpsimd", lambda e, hb=Hb[nonlocal_hc[0] % 2]: e.memset(hb[:], 0.0), [], [Hb[nonlocal_hc[0] % 2].r])
        A = arr[d]
        def prep(c):
                nonlocal_it[0] += 1
                s = c * 128
                i2 = (nonlocal_it[0] - 1) % 2
                xb_, dk, ac, na, ea, wd, dc, dx, xw_ = xBt[i2], dtk[i2], acl[i2], nac[i2], eac[i2], wdt[i2], dcy[i2], dtx[i2], xw[i2]
                y1, y2, cbs = yt1[i2], yt2[i2], CBs[i2]
                hb_cur = Hb[nonlocal_hc[0] % 2]
                hb_nxt = Hb[(nonlocal_hc[0] + 1) % 2]
                nonlocal_hc[0] += 1
                return dict(s=s, i2=i2, xb_=xb_, dk=dk, ac=ac, na=na, ea=ea, wd=wd, dc=dc, dx=dx, xw_=xw_, y1=y1, y2=y2, cbs=cbs, hb_cur=hb_cur, hb_nxt=hb_nxt)

        def stage1(c, v):
            s, i2, xb_, dk, ac, na, ea, wd, dc, dx, xw_, cbs = v['s'], v['i2'], v['xb_'], v['dk'], v['ac'], v['na'], v['ea'], v['wd'], v['dc'], v['dx'], v['xw_'], v['cbs']

            def trx(e, s=s, A=A):
                e.transpose(out=ptr.t[:, 0:128], in_=A[0][:, s:s + 128], identity=idb[:])
                e.transpose(out=ptr.t[:, 128:256], in_=A[1][:, s:s + 128], identity=idb[:])
                return e.transpose(out=ptr.t[:, 256:384], in_=A[2][:, s:s + 128], identity=idb[:])
            p.op("tensor", trx, [A[0].r, A[1].r, A[2].r, idb.r], [ptr.r])
            p.op("scalar", lambda e, xb_=xb_: e.copy(out=xb_[:], in_=ptr.t[:, 0:384]), [ptr.r], [xb_.r])

            def trd(e, s=s):
                e.transpose(out=psm[:, 0:4], in_=dtp[:, 0, s:s + 128], identity=idf[0:4, 0:4])
                return e.transpose(out=psm[:, 4:8], in_=dtp[:, 1, s:s + 128], identity=idf[0:4, 0:4])
            p.op("tensor", trd, [dtp.r, idf.r], [psm.r])
            p.op("vector", lambda e, dk=dk: e.tensor_copy(out=dk[:], in_=psm[:, 0:8]), [psm.r], [dk.r])

            def mac(e, dk=dk):
                e.matmul(psm[:, 8:12], lhsT=U[:], rhs=dk[:, 4:8], start=True, stop=True)
                return e.matmul(psm[:, 12:16], lhsT=ones[:], rhs=dk[:, 4:8], start=True, stop=True)
            p.op("tensor", mac, [U.r, ones.r, dk.r], [psm.r])
            p.op("vector", lambda e, ac=ac: e.tensor_copy(out=ac[:], in_=psm[:, 8:16]), [psm.r], [ac.r])
            p.op("vector", lambda e, ac=ac, na=na: e.tensor_scalar(out=na[:], in0=ac[:, 0:4], scalar1=-1.0, scalar2=None, op0=ALU.mult), [ac.r], [na.r])
            p.op("scalar", lambda e, ac=ac, ea=ea: e.activation(out=ea[:], in_=ac[:, 0:4], func=AF.Exp), [ac.r], [ea.r])
            p.op("scalar", lambda e, ac=ac, dc=dc: e.activation(out=dc[:], in_=ac[:, 4:8], func=AF.Exp), [ac.r], [dc.r])
            p.op("vector", lambda e, ac=ac, wd=wd: e.tensor_tensor(out=wd[:], in0=ac[:, 4:8], in1=ac[:, 0:4], op=ALU.subtract), [ac.r], [wd.r])
            p.op("scalar", lambda e, wd=wd: e.activation(out=wd[:], in_=wd[:], func=AF.Exp), [wd.r], [wd.r])
            p.op("vector", lambda e, wd=wd, dk=dk: e.tensor_tensor(out=wd[:], in0=wd[:], in1=dk[:, 0:4], op=ALU.mult), [wd.r, dk.r], [wd.r])
            x3 = lambda t: t[:, 0:256].rearrange("p (h q) -> p h q", q=64)
            p.op("vector", lambda e, dx=dx, xb_=xb_, dk=dk, x3=x3: e.tensor_tensor(out=x3(dx), in0=x3(xb_), in1=dk[:, 0:4].unsqueeze(2).to_broadcast([128, 4, 64]), op=ALU.mult), [xb_.r, dk.r], [dx.r])
            p.op("vector", lambda e, xw_=xw_, xb_=xb_, wd=wd, x3=x3: e.tensor_tensor(out=x3(xw_), in0=x3(xb_), in1=wd[:].unsqueeze(2).to_broadcast([128, 4, 64]), op=ALU.mult), [xb_.r, wd.r], [xw_.r])
            p.op("tensor", lambda e, s=s, A=A: e.matmul(pCB[:, 0:128], lhsT=A[2][:, s:s + 128], rhs=A[3][:, s:s + 128], start=True, stop=True), [A[2].r, A[3].r], [pCB.r])
            p.op("scalar", lambda e, cbs=cbs: e.copy(out=cbs[:], in_=pCB[:, 0:128]), [pCB.r], [cbs.r])
            for h in range(4):
                j2 = h % 2
                ru, sg_, lt, pb = rU[j2], sg[j2], Lt[j2], pbc[j2]
                mb = Mb[i2 * 4 + h]
                p.op("vector", lambda e, ru=ru, dk=dk, h=h: e.tensor_scalar(out=ru[:], in0=U[:], scalar1=dk[:, 4 + h:5 + h], scalar2=None, op0=ALU.mult), [U.r, dk.r], [ru.r])
                p.op("tensor", lambda e, pb=pb, ru=ru: e.matmul(pb[:, 0:128], lhsT=ones[:], rhs=ru[:], start=True, stop=True), [ones.r, ru.r], [pb.r])
                p.op("vector", lambda e, sg_=sg_, pb=pb, na=na, h=h: e.tensor_scalar(out=sg_[:], in0=pb[:, 0:128], scalar1=na[:, h:h + 1], scalar2=0.0, op0=ALU.add, op1=ALU.min), [pb.r, na.r], [sg_.r])
                p.op("scalar", lambda e, lt=lt, sg_=sg_: e.activation(out=lt[:], in_=sg_[:], func=AF.Exp), [sg_.r], [lt.r])
                p.op("gpsimd", lambda e, lt=lt: e.tensor_tensor(out=lt[:], in0=lt[:], in1=U[:], op=ALU.mult), [lt.r, U.r], [lt.r])
                p.op("vector", lambda e, lt=lt, cbs=cbs, mb=mb: e.tensor_tensor(out=mb[:], in0=lt[:], in1=cbs[:], op=ALU.mult), [lt.r, cbs.r], [mb.r])

        def stage2(c, v):
            s, xb_, xw_, dc, ea, y1, y2, hb_cur, hb_nxt = v['s'], v['xb_'], v['xw_'], v['dc'], v['ea'], v['y1'], v['y2'], v['hb_cur'], v['hb_nxt']
            x3 = lambda t: t[:, 0:256].rearrange("p (h q) -> p h q", q=64)
            dx, i2 = v['dx'], v['i2']
            for h in range(4):
                mb = Mb[i2 * 4 + h]
                p.op("tensor", lambda e, mb=mb, dx=dx, h=h: e.matmul(pdiag[:, h * 64:(h + 1) * 64], lhsT=mb[:], rhs=dx[:, h * 64:(h + 1) * 64], start=True, stop=True), [mb.r, dx.r], [pdiag.r])
            p.op("tensor", lambda e, s=s, A=A, hb_cur=hb_cur: e.matmul(poff[:, 0:256], lhsT=A[3][:, s:s + 128], rhs=hb_cur[:], start=True, stop=True), [A[3].r, hb_cur.r], [poff.r])
            p.op("tensor", lambda e, xb_=xb_, xw_=xw_: e.matmul(pst[:, 0:256], lhsT=xb_[:, 256:384], rhs=xw_[:], start=True, stop=True), [xb_.r, xw_.r], [pst.r])
            H3 = H[:].rearrange("p (h q) -> p h q", q=64)
            p.op("vector", lambda e, dc=dc, H3=H3: e.tensor_tensor(out=H3, in0=H3, in1=dc[:].unsqueeze(2).to_broadcast([128, 4, 64]), op=ALU.mult), [H.r, dc.r], [H.r])
            p.op("vector", lambda e: e.tensor_tensor(out=H[:], in0=H[:], in1=pst[:, 0:256], op=ALU.add), [H.r, pst.r], [H.r])
            p.op("scalar", lambda e, hb_nxt=hb_nxt: e.copy(out=hb_nxt[:], in_=H[:]), [H.r], [hb_nxt.r])
            p.op("scalar", lambda e, y1=y1: e.copy(out=y1[:], in_=pdiag[:, 0:256]), [pdiag.r], [y1.r])
            p.op("vector", lambda e, y2=y2, ea=ea, x3=x3: e.tensor_tensor(out=x3(y2), in0=poff[:, 0:256].rearrange("p (h q) -> p h q", q=64),
                                                                        in1=ea[:].unsqueeze(2).to_broadcast([128, 4, 64]), op=ALU.mult), [poff.r, ea.r], [y2.r])
            p.op("gpsimd", lambda e, y1=y1, y2=y2: e.tensor_tensor(out=y2[:], in0=y2[:], in1=y1[:], op=ALU.add), [y1.r, y2.r], [y2.r])
            for k in range(2):
                pb = pbc[k]
                p.op("tensor", lambda e, pb=pb, y2=y2, k=k: e.transpose(out=pb[:, 128:256], in_=y2[:, k * 128:(k + 1) * 128], identity=idf[:]), [y2.r, idf.r], [pb.r])
                if d == 0:
                    p.op("scalar", lambda e, pb=pb, k=k, s=s: e.copy(out=yacc[k][:, s:s + 128], in_=pb[:, 128:256]), [pb.r], [yacc[k].r])
                else:
                    lo = scan_nat_lo(s, 128)
                    p.op("vector", lambda e, pb=pb, k=k, lo=lo: e.tensor_tensor(out=yacc[k][:, lo:lo + 128][:, ::-1], in0=yacc[k][:, lo:lo + 128][:, ::-1], in1=pb[:, 128:256], op=ALU.add),
                         [pb.r, yacc[k].r], [yacc[k].r])

        vs_ = [prep(c) for c in range(NCH)]
        stage1(0, vs_[0])
        for c in range(NCH):
            if c + 1 < NCH:
                stage1(c + 1, vs_[c + 1])
            stage2(c, vs_[c])
    zt = u
    for k in range(2):
        p.op("vector", lambda e, k=k: e.scalar_tensor_tensor(out=yacc[k][:], in0=arr[0][k][:], scalar=Dp[:, k:k + 1], in1=yacc[k][:], op0=ALU.mult, op1=ALU.add),
             [arr[0][k].r, Dp.r, yacc[k].r], [yacc[k].r])
        p.dma("sync", zt[:], z_d[k * 128:(k + 1) * 128, :], reads=in_regs, writes=[zt.r])
        p.op("scalar", lambda e: e.activation(out=zt[:], in_=zt[:], func=AF.Silu), [zt.r], [zt.r])
        p.op("vector", lambda e, k=k: e.tensor_tensor(out=yacc[k][:], in0=yacc[k][:], in1=zt[:], op=ALU.mult), [yacc[k].r, zt.r], [yacc[k].r])
        xs_write(p, xs, row0 + k * 128, yacc[k], [yacc[k].r], outs)


def emit_b1(kb, xg, xT_d, cT, adaw, adab, wbr_d, wo_d, sn_d, bm_d, xm_d, in_regs, outs, TOK=HT):
    p = kb.p
    banks = kb.banks
    XR = 3072
    nch = (TOK + 511) // 512
    chunks = [(c * 512, min(512, TOK - c * 512)) for c in range(nch)]
    ya = kb.sb("ystg_a", [128, 8192])
    ystg = kb.sub(ya, 0, [128, 16, 512], F32, "ystg")
    mod = emit_mods(kb, adaw, adab, 8, cT, kb.sub(ya, 0, [128, 8, 512], F32, "awstg"), banks[0])
    bm = ld(kb, "bm", [128, 2], bm_d)
    ssdn = ld(kb, "ssdn", [128, 4], sn_d)
    ones = kb.sb("ones", [128, 128])
    p.op("vector", lambda e: e.memset(ones[:], 1.0), [], [ones.r])
    wbb = kb.sb("wbb", [128, 16, 1024], BF16)
    wob = kb.sb("wob", [128, 8, 1024], BF16)
    wst = [kb.sb("wst%d" % i, [128, 1024]) for i in range(2)]
    wbr_r = [Reg("wbr%d" % k) for k in range(16)]
    wo_r = [Reg("wo%d" % k) for k in range(8)]
    for k in range(24):
        st = wst[k % 2]
        src = wbr_d[k * 128:(k + 1) * 128, :] if k < 16 else wo_d[(k - 16) * 128:(k - 15) * 128, :]
        p.dma("sync", st[:], src, writes=[st.r])
        if k < 16:
            p.op("gpsimd", lambda e, st=st, k=k: e.tensor_copy(out=wbb[:, k, :], in_=st[:]), [st.r], [wbr_r[k]])
        else:
            p.op("gpsimd", lambda e, st=st, k=k: e.tensor_copy(out=wob[:, k - 16, :], in_=st[:]), [st.r], [wo_r[k - 16]])
    y2 = [kb.sb("y2_%d" % i, [128, 4, 512]) for i in range(2)]
    yb = kb.sb("yb", [128, 16, 512], BF16)
    xt = kb.sb("xt", [128, 8, 512])
    gts = [kb.sb("gt%d" % i, [128, 4, 512]) for i in range(2)]
    gt2 = [kb.sb("gu%d" % i, [128, 4, 512]) for i in range(2)]
    accs = [kb.sb("acc%d" % i, [128, 512]) for i in range(2)]
    tmps = [kb.sb("tmp%d" % i, [128, 512]) for i in range(2)]
    aT = kb.sb("aT", [128, 8, 512], BF16)
    pz = banks[0:4]
    po = banks[4:6]
    pss = banks[6]
    zc = 0
    gc = 0
    yc = 0

    def yparts(kc, h):
        n_, r_, j_ = kc // 4, (kc // 2) % 2, kc % 2
        return gath_parts(3072, n_ * 256 + j_ * 128, r_, h * 6144)

    def gparts(nb, oc, h):
        return gath_parts(3072, 1024 + (nb % 2) * 1024 + oc * 128, nb // 2, h * 6144)

    for c, (t0, n) in enumerate(chunks):
        for g4 in range(4):
            yy = y2[yc % 2]
            yc += 1
            for j in range(4):
                for (row, cnt, po_) in yparts(g4 * 4 + j, 0):
                    p.dma("sync", ystg.t[po_:po_ + cnt, g4 * 4 + j, 0:n], xg[row:row + cnt, t0:t0 + n], reads=in_regs, writes=[ystg.r])
                for (row, cnt, po_) in yparts(g4 * 4 + j, 1):
                    p.dma("sync", yy.t[po_:po_ + cnt, j, 0:n], xg[row:row + cnt, t0:t0 + n], reads=in_regs, writes=[yy.r])
            p.op("vector", lambda e, g4=g4, n=n: e.tensor_scalar(out=ystg[:, g4 * 4:g4 * 4 + 4, 0:n], in0=ystg[:, g4 * 4:g4 * 4 + 4, 0:n], scalar1=bm[:, 0:1], scalar2=None, op0=ALU.mult),
                 [ystg.r, bm.r], [ystg.r])
            p.op("vector", lambda e, g4=g4, yy=yy, n=n: e.scalar_tensor_tensor(out=ystg[:, g4 * 4:g4 * 4 + 4, 0:n], in0=yy[:, :, 0:n], scalar=bm[:, 1:2], in1=ystg[:, g4 * 4:g4 * 4 + 4, 0:n],
                                                                             op0=ALU.mult, op1=ALU.add), [ystg.r, yy.r, bm.r], [ystg.r])
        for kc in range(4):
            sq = tmps[kc % 2]
            p.op("scalar", lambda e, sq=sq, kc=kc, n=n: e.activation(out=sq[:, 0:n], in_=ystg[:, kc, 0:n], func=AF.Square), [ystg.r], [sq.r])
            p.op("tensor", lambda e, sq=sq, kc=kc, n=n: e.matmul(pss[:, 0:n], lhsT=ones[:], rhs=sq[:, 0:n], start=(kc == 0), stop=(kc == 3)), [ones.r, sq.r], [pss.r])
        rs = accs[0]
        p.op("vector", lambda e, rs=rs, n=n: e.tensor_scalar(out=rs[:, 0:n], in0=pss[:, 0:n], scalar1=1.0 / 512, scalar2=EPS, op0=ALU.mult, op1=ALU.add), [pss.r], [rs.r])
        p.op("vector", lambda e, rs=rs, n=n: e.reciprocal(out=rs[:, 0:n], in_=rs[:, 0:n]), [rs.r], [rs.r])
        p.op("scalar", lambda e, rs=rs, n=n: e.activation(out=rs[:, 0:n], in_=rs[:, 0:n], func=AF.Sqrt), [rs.r], [rs.r])

        def nrm(e, rs=rs, n=n):
            for kc in range(4):
                last = e.scalar_tensor_tensor(out=ystg[:, kc, 0:n], in0=ystg[:, kc, 0:n], scalar=ssdn[:, kc:kc + 1], in1=rs[:, 0:n], op0=ALU.mult, op1=ALU.mult)
            return last
        p.op("vector", nrm, [ystg.r, ssdn.r, rs.r], [ystg.r])
        p.op("gpsimd", lambda e, n=n: e.tensor_copy(out=yb[:, :, 0:n], in_=ystg[:, :, 0:n]), [ystg.r], [yb.r])
        p.dma("sync", xt[:, :, 0:n], xT_d[:, t0:t0 + n].rearrange("(kc p) t -> p kc t", p=128), reads=in_regs, writes=[xt.r])
        for oc in range(8):
            gt, gu = gts[gc % 2], gt2[gc % 2]
            acc = accs[gc % 2]
            gc += 1
            for nb in range(4):
                for (row, cnt, po_) in gparts(nb, oc, 0):
                    p.dma("sync", gt.t[po_:po_ + cnt, nb, 0:n], xg[row:row + cnt, t0:t0 + n], reads=in_regs, writes=[gt.r])
                for (row, cnt, po_) in gparts(nb, oc, 1):
                    p.dma("sync", gu.t[po_:po_ + cnt, nb, 0:n], xg[row:row + cnt, t0:t0 + n], reads=in_regs, writes=[gu.r])
            p.op("gpsimd", lambda e, gt=gt, n=n: e.tensor_scalar(out=gt[:, :, 0:n], in0=gt[:, :, 0:n], scalar1=bm[:, 0:1], scalar2=None, op0=ALU.mult), [gt.r, bm.r], [gt.r])
            p.op("vector", lambda e, gt=gt, gu=gu, n=n: e.scalar_tensor_tensor(out=gt[:, :, 0:n], in0=gu[:, :, 0:n], scalar=bm[:, 1:2], in1=gt[:, :, 0:n], op0=ALU.mult, op1=ALU.add),
                 [gt.r, gu.r, bm.r], [gt.r])
            p.op("scalar", lambda e, gt=gt, n=n: e.activation(out=gt[:, :, 0:n], in_=gt[:, :, 0:n], func=AF.Sigmoid), [gt.r], [gt.r])
            for nb in range(4):
                z = pz[zc % 4]
                zc += 1

                def mmz(e, z=z, nb=nb, oc=oc, n=n):
                    for kc in range(4):
                        last = e.matmul(z[:, 0:n], lhsT=wbb[:, nb * 4 + kc, oc * 128:(oc + 1) * 128], rhs=yb[:, nb * 4 + kc, 0:n], start=(kc == 0), stop=(kc == 3))
                    return last
                p.op("tensor", mmz, wbr_r[nb * 4:nb * 4 + 4] + [yb.r], [z.r])
                if nb == 0:
                    p.op("vector", lambda e, z=z, gt=gt, acc=acc, n=n: e.tensor_tensor(out=acc[:, 0:n], in0=z[:, 0:n], in1=gt[:, 0, 0:n], op=ALU.mult), [z.r, gt.r], [acc.r])
                else:
                    tmp = tmps[nb % 2]
                    p.op("vector", lambda e, z=z, gt=gt, tmp=tmp, nb=nb, n=n: e.tensor_tensor(out=tmp[:, 0:n], in0=z[:, 0:n], in1=gt[:, nb, 0:n], op=ALU.mult), [z.r, gt.r], [tmp.r])
                    if nb < 3:
                        p.op("gpsimd", lambda e, tmp=tmp, acc=acc, n=n: e.tensor_tensor(out=acc[:, 0:n], in0=acc[:, 0:n], in1=tmp[:, 0:n], op=ALU.add), [acc.r, tmp.r], [acc.r])
                    else:
                        p.op("gpsimd", lambda e, tmp=tmp, acc=acc, oc=oc, n=n: e.tensor_tensor(out=aT[:, oc, 0:n], in0=acc[:, 0:n], in1=tmp[:, 0:n], op=ALU.add), [acc.r, tmp.r], [aT.r])
        for oc in range(8):
            pq = po[oc % 2]

            def mmo(e, pq=pq, oc=oc, n=n):
                for kc in range(8):
                    last = e.matmul(pq[:, 0:n], lhsT=wob[:, kc, oc * 128:(oc + 1) * 128], rhs=aT[:, kc, 0:n], start=(kc == 0), stop=(kc == 7))
                return last
            p.op("tensor", mmo, wo_r + [aT.r], [pq.r])

            def res(e, pq=pq, oc=oc, t0=t0, n=n):
                for (s, m, j) in tok_ranges(t0, n, 128):
                    last = e.scalar_tensor_tensor(out=xt[:, oc, s - t0:s - t0 + m], in0=pq[:, s - t0:s - t0 + m], scalar=mod[:, oc, j:j + 1],
                                                  in1=xt[:, oc, s - t0:s - t0 + m], op0=ALU.mult, op1=ALU.add)
                return last
            p.op("vector", res, [pq.r, mod.r, xt.r], [xt.r])
        o = Reg("o%d" % c)
        outs.append(o)
        p.dma("gpsimd", xm_d[:, t0:t0 + n].rearrange("(kc p) t -> p kc t", p=128), xt[:, :, 0:n], reads=[xt.r], writes=[o])


def emit_b2(kb, xT_d, cT, adaw, adab, n2, wr_d, br_d, sel_d, ident_d, w1_d, w3_d, w2_d, out_d, in_regs, outs, TOK=HT, NCX=128, NE=16):
    p = kb.p
    banks = kb.banks
    nch = (TOK + 511) // 512
    chunks = [(c * 512, min(512, TOK - c * 512)) for c in range(nch)]
    idf = ld(kb, "idf", [128, 128], ident_d)
    ones = kb.sb("ones", [128, 128])
    p.op("vector", lambda e: e.memset(ones[:], 1.0), [], [ones.r])
    sel = ld(kb, "sel", [16, 16, 128], sel_d)
    wr = kb.sb("wr", [128, 8, 20])
    p.dma("sync", wr[:], wr_d.rearrange("(kc p) c -> p kc c", p=128), writes=[wr.r])
    br = ld(kb, "br", [128, 20], br_d)
    n2t = ld(kb, "n2t", [128, 8], n2)
    arena = kb.sb("b2arena", [128, 18432])
    KEL = 256

    class PV:
        def __init__(s, bank, ap):
            s.t, s.r = ap, bank.r

        def __getitem__(s, k):
            return s.t[k]
    sq = kb.sub(arena, 0, [128, 8, 512], F32, "sq")
    h2f = kb.sub(arena, 16 * KEL, [128, 8, 512], F32, "h2f")
    mod = emit_mods(kb, adaw, adab, 24, cT, sq, banks[0])
    asc = kb.sb("asc", [128, 8, 2])
    p.op("vector", lambda e: e.tensor_scalar(out=asc[:], in0=mod[:, 8:16, :], scalar1=1.0, scalar2=None, op0=ALU.add), [mod.r], [asc.r])
    p.op("vector", lambda e: e.tensor_tensor(out=asc[:], in0=asc[:], in1=n2t[:].unsqueeze(2).to_broadcast([128, 8, 2]), op=ALU.mult), [asc.r, n2t.r], [asc.r])
    xT = kb.sb("xT", [128, 8, TOK])
    xr = [[Reg("x%d_%d" % (oc, c)) for c in range(nch)] for oc in range(8)]
    for oc in range(8):
        p.dma("sync", xT[:, oc, :], xT_d[oc * 128:(oc + 1) * 128, :], reads=in_regs, writes=xr[oc])
    h2T = kb.sb("h2T", [128, 8, TOK], BF16)
    h2r = [Reg("h2_%d" % c) for c in range(nch)]
    wT = kb.sb("wT", [16, TOK])
    wTr = [Reg("wT%d" % c) for c in range(nch)]
    pss = banks[7]
    rstd = kb.sb("rstd", [128, 512])
    plg = [PV(banks[5], banks[5].t[:, 0:20]), PV(banks[6], banks[6].t[:, 0:20])]
    pwt = PV(banks[4], banks[4].t[0:16, 0:128])
    R = lambda n, s: kb.sb(n, s)
    L = R("rL", [128, 20]); mg = R("rmg", [128, 1]); nmg = R("rnmg", [128, 1]); eg = R("reg", [128, 4]); sg = R("rsg", [128, 1])
    gsel = R("rgsel", [128, 4]); tmp = R("rtmp", [128, 4, 4]); lsel = R("rlsel", [128, 4]); me = R("rme", [128, 1]); nme = R("rnme", [128, 1])
    ee = R("ree", [128, 4]); m1 = R("rm1", [128, 1]); msk = R("rmsk", [128, 4]); e2 = R("re2", [128, 4]); m2 = R("rm2", [128, 1])
    tv = R("rtv", [128, 4]); sv = R("rsv", [128, 1]); wg = R("rwg", [128, 4]); gp = R("rgp", [128, 4]); wf = R("rwf", [128, 4, 4])
    for c, (t0, n) in enumerate(chunks):
        def sqf(e, t0=t0, n=n):
            for kc in range(8):
                last = e.activation(out=sq[:, kc, 0:n], in_=xT[:, kc, t0:t0 + n], func=AF.Square)
            return last
        p.op("scalar", sqf, [xr[oc][c] for oc in range(8)], [sq.r])

        def ssum(e, n=n):
            for kc in range(8):
                last = e.matmul(pss[:, 0:n], lhsT=ones[:], rhs=sq[:, kc, 0:n], start=(kc == 0), stop=(kc == 7))
            return last
        p.op("tensor", ssum, [sq.r, ones.r], [pss.r])
        p.op("vector", lambda e, n=n: e.tensor_scalar(out=rstd[:, 0:n], in0=pss[:, 0:n], scalar1=1.0 / 1024, scalar2=EPS, op0=ALU.mult, op1=ALU.add), [pss.r], [rstd.r])
        p.op("vector", lambda e, n=n: e.reciprocal(out=rstd[:, 0:n], in_=rstd[:, 0:n]), [rstd.r], [rstd.r])
        p.op("scalar", lambda e, n=n: e.activation(out=rstd[:, 0:n], in_=rstd[:, 0:n], func=AF.Sqrt), [rstd.r], [rstd.r])

        def nrm(e, t0=t0, n=n):
            for kc in range(8):
                last = e.tensor_tensor(out=h2f[:, kc, 0:n], in0=xT[:, kc, t0:t0 + n], in1=rstd[:, 0:n], op=ALU.mult)
            return last
        p.op("vector", nrm, [xr[oc][c] for oc in range(8)] + [rstd.r], [h2f.r])

        def modf(e, t0=t0, n=n):
            for (s, m, j) in tok_ranges(t0, n, NCX):
                for kc in range(8):
                    last = e.activation(out=h2f[:, kc, s - t0:s - t0 + m], in_=h2f[:, kc, s - t0:s - t0 + m], func=AF.Identity, scale=asc[:, kc, j:j + 1], bias=mod[:, kc, j:j + 1])
            return last
        p.op("scalar", modf, [h2f.r, asc.r, mod.r], [h2f.r])
        p.op("gpsimd", lambda e, t0=t0, n=n: e.tensor_copy(out=h2T[:, :, t0:t0 + n], in_=h2f[:, :, 0:n]), [h2f.r], [h2r[c]])
        for tt in range(n // 128):
            pl = plg[tt % 2]

            def rmm(e, tt=tt, pl=pl):
                for kc in range(8):
                    last = e.matmul(pl[:], lhsT=h2f[:, kc, tt * 128:(tt + 1) * 128], rhs=wr[:, kc, :], start=(kc == 0), stop=(kc == 7))
                return last
            p.op("tensor", rmm, [h2f.r, wr.r], [pl.r])
            V = lambda fn, rd, wrt: p.op("vector", fn, rd, wrt)
            V(lambda e, pl=pl: e.tensor_tensor(out=L[:], in0=pl[:], in1=br[:], op=ALU.add), [pl.r, br.r], [L.r])
            V(lambda e: e.reduce_max(out=mg[:], in_=L[:, 0:4], axis=AX.X), [L.r], [mg.r])
            V(lambda e: e.tensor_scalar(out=nmg[:], in0=mg[:], scalar1=-1.0, scalar2=None, op0=ALU.mult), [mg.r], [nmg.r])
            p.op("scalar", lambda e: e.activation(out=eg[:], in_=L[:, 0:4], func=AF.Exp, bias=nmg[:, 0:1], scale=1.0, accum_out=sg[:]), [L.r, nmg.r], [eg.r, sg.r])
            V(lambda e: e.reciprocal(out=sg[:], in_=sg[:]), [sg.r], [sg.r])
            V(lambda e: e.tensor_scalar(out=gsel[:], in0=L[:, 0:4], scalar1=mg[:, 0:1], scalar2=None, op0=ALU.is_ge), [L.r, mg.r], [gsel.r])
            V(lambda e: e.tensor_tensor(out=tmp[:], in0=L[:, 4:20].rearrange("p (g e) -> p g e", g=4), in1=gsel[:].unsqueeze(2).to_broadcast([128, 4, 4]), op=ALU.mult), [L.r, gsel.r], [tmp.r])
            V(lambda e: e.tensor_reduce(out=lsel[:], in_=tmp[:].rearrange("p g e -> p e g"), axis=AX.X, op=ALU.add), [tmp.r], [lsel.r])
            V(lambda e: e.reduce_max(out=me[:], in_=lsel[:], axis=AX.X), [lsel.r], [me.r])
            V(lambda e: e.tensor_scalar(out=nme[:], in0=me[:], scalar1=-1.0, scalar2=None, op0=ALU.mult), [me.r], [nme.r])
            p.op("scalar", lambda e: e.activation(out=ee[:], in_=lsel[:], func=AF.Exp, bias=nme[:, 0:1], scale=1.0), [lsel.r, nme.r], [ee.r])
            V(lambda e: e.reduce_max(out=m1[:], in_=ee[:], axis=AX.X), [ee.r], [m1.r])
            V(lambda e: e.tensor_scalar(out=msk[:], in0=ee[:], scalar1=m1[:, 0:1], scalar2=-1e9, op0=ALU.is_ge, op1=ALU.mult), [ee.r, m1.r], [msk.r])
            V(lambda e: e.tensor_tensor(out=e2[:], in0=ee[:], in1=msk[:], op=ALU.add), [ee.r, msk.r], [e2.r])
            V(lambda e: e.reduce_max(out=m2[:], in_=e2[:], axis=AX.X), [e2.r], [m2.r])
            V(lambda e: e.tensor_scalar(out=tv[:], in0=ee[:], scalar1=m2[:, 0:1], scalar2=None, op0=ALU.is_ge), [ee.r, m2.r], [tv.r])
            V(lambda e: e.tensor_tensor(out=tv[:], in0=tv[:], in1=ee[:], op=ALU.mult), [tv.r, ee.r], [tv.r])
            V(lambda e: e.reduce_sum(out=sv[:], in_=tv[:], axis=AX.X), [tv.r], [sv.r])
            V(lambda e: e.reciprocal(out=sv[:], in_=sv[:]), [sv.r], [sv.r])
            V(lambda e: e.tensor_scalar(out=wg[:], in0=tv[:], scalar1=sv[:, 0:1], scalar2=None, op0=ALU.mult), [tv.r, sv.r], [wg.r])
            V(lambda e: e.tensor_scalar(out=gp[:], in0=gsel[:], scalar1=sg[:, 0:1], scalar2=None, op0=ALU.mult), [gsel.r, sg.r], [gp.r])
            V(lambda e: e.tensor_tensor(out=wf[:], in0=gp[:].unsqueeze(2).to_broadcast([128, 4, 4]), in1=wg[:].unsqueeze(1).to_broadcast([128, 4, 4]), op=ALU.mult), [gp.r, wg.r], [wf.r])
            p.op("tensor", lambda e: e.transpose(out=pwt[:], in_=wf[:].rearrange("p g e -> p (g e)"), identity=idf[:]), [wf.r, idf.r], [pwt.r])
            V(lambda e, a=t0 + tt * 128: e.tensor_copy(out=wT[:, a:a + 128], in_=pwt[:]), [pwt.r], [wTr[c]])
    p.fence()
    w13 = [kb.sub(arena, i * 16 * KEL, [128, 2, 8, 512], BF16, "w13_%d" % i) for i in range(2)]
    w2b = [kb.sub(arena, (32 + 8 * i) * KEL, [128, 4, 1024], BF16, "w2b_%d" % i) for i in range(2)]
    stA = [kb.sub(arena, (48 + 4 * i) * KEL, [128, 1024], F32, "stA%d" % i) for i in range(3)]
    s1 = [kb.sub(arena, (60 + 2 * i) * KEL, [128, 512], F32, "s1_%d" % i) for i in range(2)]
    aT = [kb.sub(arena, (64 + 4 * i) * KEL, [128, 4, 512], BF16, "aT%d" % i) for i in range(2)]
    ph = banks[0:4]
    pw = banks[4]
    po = banks[5:8]
    war_b = [[Reg("wa%d_%d" % (i, k)) for k in range(8)] for i in range(2)]
    wbr_b = [[Reg("wb%d_%d" % (i, k)) for k in range(4)] for i in range(2)]
    units = [(ex, c) for ex in range(NE) for c in range(len(chunks))]
    cnt = {"si": 0, "hc": 0, "oc": 0}

    def load_w(ex):
        wa, wb = w13[ex % 2], w2b[ex % 2]
        war, wbr = war_b[ex % 2], wbr_b[ex % 2]
        for kc in range(8):
            sA = stA[cnt["si"] % 3]; cnt["si"] += 1
            p.dma("sync", sA[:, 0:512], w1_d[ex, kc * 128:(kc + 1) * 128, :], writes=[sA.r])
            p.dma("sync", sA[:, 512:1024], w3_d[ex, kc * 128:(kc + 1) * 128, :], writes=[sA.r])
            p.op("gpsimd", lambda e, sA=sA, wa=wa, kc=kc: e.tensor_copy(out=wa[:, :, kc, :], in_=sA[:].rearrange("p (a c) -> p a c", a=2)), [sA.r], [war[kc]])
        for fc in range(4):
            sA = stA[cnt["si"] % 3]; cnt["si"] += 1
            p.dma("sync", sA[:], w2_d[ex, fc * 128:(fc + 1) * 128, :], writes=[sA.r])
            p.op("gpsimd", lambda e, sA=sA, wb=wb, fc=fc: e.tensor_copy(out=wb[:, fc, :], in_=sA[:]), [sA.r], [wbr[fc]])

    def stageA(ui):
        ex, c = units[ui]
        t0, n = chunks[c]
        wa = w13[ex % 2]
        war = war_b[ex % 2]
        a = aT[ui % 2]
        ops = []
        for fc in range(4):
            def f(fc=fc):
                if fc == 0:
                    if c == 0:
                        load_w(ex)
                    p.op("tensor", lambda e: e.matmul(pw[:, 0:n], lhsT=sel[:, ex, :], rhs=wT[:, t0:t0 + n], start=True, stop=True), [sel.r, wTr[c]], [pw.r])
                p1, p3 = ph[cnt["hc"] % 4], ph[(cnt["hc"] + 1) % 4]; cnt["hc"] += 2
                ss = s1[fc % 2]

                def mm13(e):
                    for kc in range(8):
                        e.matmul(p1[:, 0:n], lhsT=wa[:, 0, kc, fc * 128:(fc + 1) * 128], rhs=h2T[:, kc, t0:t0 + n], start=(kc == 0), stop=(kc == 7))
                    for kc in range(8):
                        last = e.matmul(p3[:, 0:n], lhsT=wa[:, 1, kc, fc * 128:(fc + 1) * 128], rhs=h2T[:, kc, t0:t0 + n], start=(kc == 0), stop=(kc == 7))
                    return last
                p.op("tensor", mm13, war + [h2r[c]], [p1.r, p3.r])
                p.op("scalar", lambda e: e.activation(out=ss[:, 0:n], in_=p1[:, 0:n], func=AF.Silu), [p1.r], [ss.r])
                p.op("vector", lambda e: e.tensor_tensor(out=ss[:, 0:n], in0=ss[:, 0:n], in1=p3[:, 0:n], op=ALU.mult), [ss.r, p3.r], [ss.r])
                p.op("vector", lambda e: e.tensor_tensor(out=a[:, fc, 0:n], in0=ss[:, 0:n], in1=pw[:, 0:n], op=ALU.mult), [ss.r, pw.r], [a.r])
            ops.append(f)
        return ops

    def stageB(ui):
        ex, c = units[ui]
        t0, n = chunks[c]
        wb = w2b[ex % 2]
        wbr = wbr_b[ex % 2]
        a = aT[ui % 2]
        ops = []
        for oc in range(8):
            def f(oc=oc):
                pq = po[cnt["oc"] % len(po)]; cnt["oc"] += 1

                def mm2(e):
                    for fc in range(4):
                        last = e.matmul(pq[:, 0:n], lhsT=wb[:, fc, oc * 128:(oc + 1) * 128], rhs=a[:, fc, 0:n], start=(fc == 0), stop=(fc == 3))
                    return last
                p.op("tensor", mm2, wbr + [a.r], [pq.r])

                def acc(e):
                    for (s, m, j) in tok_ranges(t0, n, NCX):
                        last = e.scalar_tensor_tensor(out=xT[:, oc, s:s + m], in0=pq[:, s - t0:s - t0 + m], scalar=mod[:, 16 + oc, j:j + 1], in1=xT[:, oc, s:s + m], op0=ALU.mult, op1=ALU.add)
                    return last
                p.op("vector", acc, [pq.r, mod.r, xr[oc][c]], [xr[oc][c]])
            ops.append(f)
        return ops

    for f in stageA(0):
        f()
    for ui in range(len(units)):
        A = stageA(ui + 1) if ui + 1 < len(units) else []
        B = stageB(ui)
        for k in range(4):
            if A:
                A[k]()
            B[2 * k]()
            B[2 * k + 1]()
    for oc in range(8):
        o = Reg("o%d" % oc)
        outs.append(o)
        p.dma("gpsimd", out_d[oc * 128:(oc + 1) * 128, :], xT[:, oc, :], reads=xr[oc], writes=[o])


IN_SIZES_ = (512, 1024, 16, 512, 512, 512, 32, 512, 512, 512, 512, 256, 256, 4096)
OFFS_ = np.cumsum([0] + list(IN_SIZES_))
GROUPS = [[0, 1], [2, 3], [4, 5], [6, 7]]


def s1_cols(hf):
    o = OFFS_
    pieces = [("z", o[0] + hf * 256, 256), ("x", o[1] + hf * 256, 256), ("B", o[1] + 512 + hf * 128, 128), ("C", o[1] + 768 + hf * 128, 128),
              ("gq", o[3] + hf * 256, 256), ("gk", o[4] + hf * 256, 256), ("gv", o[5] + hf * 256, 256), ("gr", o[7] + hf * 256, 256),
              ("lx", o[8] + hf * 256, 256), ("lg", o[9] + hf * 256, 256),
              ("aq", o[10] + hf * 256, 256), ("ak", o[11] + hf * 128, 128), ("av", o[12] + hf * 128, 128)]
    cols, rows, r = [], {}, 0
    for nm, st, n in pieces:
        cols.append(np.arange(st, st + n))
        rows[nm] = slice(r, r + n)
        r += n
    cols.append(np.concatenate([o[2] + d * 8 + hf * 4 + np.arange(4) for d in range(2)]))
    rows["dt"] = slice(r, r + 8)
    r += 8
    cols.append(np.arange(o[6], o[6] + 32))
    rows["g1"] = slice(r, r + 32)
    r += 32
    pad = NCC_MIX * 128 - r
    cols.append(np.full(pad, -1))
    r += pad
    cols.append(np.arange(o[13] + hf * 2048, o[13] + (hf + 1) * 2048))
    rows["gate"] = slice(r, r + 2048)
    return np.concatenate(cols), rows


LAYER_SPECS = [("adaw", [1024, 6144]), ("adab", [128, 48]), ("n1", [128, 8]), ("n2", [128, 8]), ("win", [1024, NCC_S1 * 128]),
               ("gq", [128, 1]), ("gk", [128, 1]),
               ("lcw", [128, 2, 4]), ("lcb", [128, 2]), ("lwbd", [128, 8, 128]), ("lbias", [128, 8]), ("llam", [128, 4]),
               ("g2", [16, 2, 256]), ("gb", [128, 4]), ("gn", [128, 2]),
               ("scw", [128, 4, 4]), ("scb", [128, 4]), ("dtb", [4, 2]), ("alog", [4, 2]), ("Dp", [128, 2]),
               ("wbr", [2048, 1024]), ("wo", [1024, 1024]), ("ssdn", [128, 4]),
               ("wr", [1024, 20]), ("br", [128, 20]), ("w1", [16, 1024, 512]), ("w3", [16, 1024, 512]), ("w2", [16, 512, 1024])]
GLOBAL_SPECS = [("xT0", [1024, T]), ("xTm0", [1024, HT]), ("cT", [128, 8, 2]), ("ident", [128, 128]), ("cos", [128, LAT]), ("sin", [128, LAT]),
                ("rm", [128, 128]), ("cm", [128, 512]), ("m2", [128, 128]), ("hm", [128, 2]), ("U", [128, 128]), ("sel", [16, 16, 128]), ("bm", [128, 2])]


def build_fused(NL=2, debug=False, stages=None):
    kb = KB()
    p = kb.p
    G = {n: kb.din(n, s) for n, s in GLOBAL_SPECS}
    W = [{n: kb.din("%s_%d" % (n, l), s) for n, s in LAYER_SPECS} for l in range(NL)]
    pT = kb.scratch("pT", [NCC_MIX * 128, T], debug=debug)
    xs = kb.scratch("xs", [2 * 3072, HT], debug=debug)
    xg = kb.scratch("xg", [2 * 24 * 2 * 128, HT], debug=debug)
    xm = kb.scratch("xm", [1024, HT], debug=debug)
    xnew = kb.scratch("xnew", [1024, HT], debug=debug)
    xall = kb.scratch("xall", [2048, HT], debug=debug)
    oT = kb.dout("oT", [1024, HT])
    outs = []
    _, rows = s1_cols(0)
    for l in range(NL):
        w = W[l]
        if l == 0:
            xsrc = lambda kc, t0, n: [(0, n, G["xT0"][kc * 128:(kc + 1) * 128, t0:t0 + n])]
            xres = G["xTm0"]
        else:
            xsrc = lambda kc, t0, n: [(a - t0, m, h, loc) for (a, m, h, loc) in nat_pieces(t0, n)]
            xres = xnew
        def gather_chunks(chs):
            for h in range(2):
                for i in range((3072 + PIECE - 1) // PIECE):
                    Ri = min(PIECE, 3072 - PIECE * i)
                    src = xs[h * 3072 + PIECE * i:h * 3072 + PIECE * i + Ri, :]
                    dst = xg[h * 6144 + 2 * PIECE * i:h * 6144 + 2 * PIECE * i + 2 * Ri, :]
                    p.cc(lambda e, src=src, dst=dst: e.collective_compute("AllGather", ALU.bypass, replica_groups=GROUPS, ins=[src], outs=[dst]), blocking=True)
        emit_s1(kb, xsrc, G["cT"], w["adaw"][:, 0:2048], w["adab"][:, 0:16], w["n1"], w["win"], pT, xs, outs, [Reg("pT%d" % i) for i in range(NCC_MIX)], xall_ap=xall)
        kb.reset()
        emit_ssd(kb, pT[256:768, :], pT[rows["z"], :], pT[rows["dt"], :], w["scw"], w["scb"], w["dtb"], w["alog"], w["Dp"], G["U"], G["ident"], xs, 0, [], outs)
        kb.reset()
        emit_gla(kb, pT[rows["gq"], :], pT[rows["gk"], :], pT[rows["gv"], :], pT[rows["g1"], :], pT[rows["gr"], :], w["g2"], w["gb"], w["gn"],
                 G["cm"], G["m2"], G["hm"], G["ident"], xs, 256, [], outs)
        kb.reset()
        emit_lru(kb, pT[rows["lx"], :], pT[rows["lg"], :], w["lcw"], w["lcb"], w["lwbd"], w["lbias"], w["llam"], xs, 512, [], outs)
        kb.reset()
        emit_att(kb, pT[rows["aq"], :], pT[rows["ak"], :], pT[rows["av"], :], w["gq"], w["gk"], G["cos"], G["sin"], G["rm"], G["ident"], xs, 768, [], outs)
        kb.reset()
        gather_chunks(range(24))
        kb.reset()
        emit_b1(kb, xg, xres, G["cT"], w["adaw"][:, 2048:3072], w["adab"][:, 16:24], w["wbr"], w["wo"], w["ssdn"], G["bm"], xm, [], outs)
        kb.reset()
        emit_b2(kb, xm, G["cT"], w["adaw"][:, 3072:6144], w["adab"][:, 24:48], w["n2"], w["wr"], w["br"], G["sel"], G["ident"], w["w1"], w["w3"], w["w2"],
                oT if l == NL - 1 else xnew, [], outs)
        kb.reset()
        if l < NL - 1:
            for i in range((1024 + PIECE - 1) // PIECE):
                Ri = min(PIECE, 1024 - PIECE * i)
                src = xnew[PIECE * i:PIECE * i + Ri, :]
                dst = xall[2 * PIECE * i:2 * PIECE * i + 2 * Ri, :]
                p.cc(lambda e, src=src, dst=dst: e.collective_compute("AllGather", ALU.bypass, replica_groups=GROUPS, ins=[src], outs=[dst]))
            kb.reset()
    return kb.finish(outs)


def core_inputs(inp, b, hf, NL=2):
    c = np.ascontiguousarray
    f32 = np.float32
    d = {}
    x_all = np.concatenate([inp["ctx"][b], inp["x"][b]], 0)
    d["xT0"] = c(x_all.T)
    d["xTm0"] = c(np.concatenate([inp["ctx"][b][hf * 128:(hf + 1) * 128], inp["x"][b][hf * 2048:(hf + 1) * 2048]], 0).T)
    c2 = np.stack([inp["c"][b], inp["c_ctx"]], -1)
    d["cT"] = c(c2.reshape(8, 128, 2).transpose(1, 0, 2))
    d["ident"] = np.eye(128, dtype=f32)
    cos, sin, Rm = rope_tables()
    d["cos"], d["sin"], d["rm"] = cos, sin, Rm
    t = np.arange(512)
    d["cm"] = np.broadcast_to((t % 64 != 0).astype(f32)[None], (128, 512)).copy()
    j = np.arange(128)[:, None]
    i = np.arange(128)[None, :]
    d["m2"] = ((j // 64 == i // 64) & (j <= i)).astype(f32)
    d["hm"] = np.stack([(np.arange(128) < 64), (np.arange(128) >= 64)], 1).astype(f32)
    d["U"] = (j <= i).astype(f32)
    sel = np.zeros((16, 16, 128), f32)
    for e in range(16):
        sel[e, e, :] = 1.0
    d["sel"] = sel
    bm = np.zeros((128, 2), f32)
    bm[:, hf] = 1.0
    d["bm"] = bm
    cols, rows = s1_cols(hf)
    ch = slice(hf * 256, (hf + 1) * 256)
    hs = slice(hf * 4, hf * 4 + 4)
    for l in range(NL):
        L = {}
        L["adaw"] = c(inp["ada_w"][l])
        L["adab"] = fm(inp["ada_b"][l], 48)
        L["n1"] = fm(inp["norm1"][l], 8)
        L["n2"] = fm(inp["norm2"][l], 8)
        win = np.zeros((1024, NCC_S1 * 128), f32)
        ok = cols >= 0
        win[:, np.nonzero(ok)[0]] = inp["w_in"][l][:, cols[ok]]
        L["win"] = win
        L["gq"] = c(inp["att_qnorm"][l][:, None])
        L["gk"] = c(inp["att_knorm"][l][:, None])
        L["lcw"] = c(inp["lru_conv_w"][l][:, ch].reshape(4, 2, 128).transpose(2, 1, 0))
        L["lcb"] = c(inp["lru_conv_b"][l][ch].reshape(2, 128).T)
        wbd = np.zeros((128, 8, 128), f32)
        bias = np.zeros((128, 8), f32)
        lam = np.zeros((128, 4), f32)
        for gi, (wk, bk) in enumerate((("lru_wa", "lru_ba"), ("lru_wx", "lru_bx"))):
            for dd in range(2):
                for cc in range(2):
                    idx = gi * 4 + dd * 2 + cc
                    for jj in range(2):
                        blk = hf * 4 + cc * 2 + jj
                        wbd[jj * 64:(jj + 1) * 64, idx, jj * 64:(jj + 1) * 64] = inp[wk][l][dd, blk]
                    bias[:, idx] = inp[bk][l][dd, ch][cc * 128:(cc + 1) * 128]
        for dd in range(2):
            for cc in range(2):
                lam[:, dd * 2 + cc] = inp["lru_lambda"][l][dd, ch][cc * 128:(cc + 1) * 128]
        L["lwbd"], L["lbias"], L["llam"] = wbd, bias, lam
        L["g2"] = c(inp["gla_g2"][l][:, :, ch].transpose(1, 0, 2))
        L["gb"] = c(np.stack([inp["gla_gb"][l][dd, ch][h * 128:(h + 1) * 128] for h in range(2) for dd in range(2)], 1))
        L["gn"] = c(inp["gla_norm"][l][ch].reshape(2, 128).T)
        chans = np.concatenate([np.arange(hf * 256, hf * 256 + 256), 512 + hf * 128 + np.arange(128), 768 + hf * 128 + np.arange(128)])
        L["scw"] = c(inp["ssd_conv_w"][l][:, chans].reshape(4, 4, 128).transpose(2, 1, 0))
        L["scb"] = c(inp["ssd_conv_b"][l][chans].reshape(4, 128).T)
        L["dtb"] = c(inp["ssd_dt_bias"][l][:, hs].T)
        L["alog"] = c(inp["ssd_a_log"][l][:, hs].T)
        L["Dp"] = c(np.repeat(inp["ssd_d"][l][hs], 64).reshape(2, 128).T)
        L["wbr"] = c(inp["w_branch"][l].reshape(2048, 1024))
        L["wo"] = c(inp["w_out"][l])
        L["ssdn"] = fm(inp["ssd_norm"][l], 4)
        L["wr"] = c(np.concatenate([inp["router_wg"][l], inp["router_we"][l]], 1))
        L["br"] = c(np.broadcast_to(np.concatenate([inp["router_bg"][l], inp["router_be"][l]])[None], (128, 20)))
        L["w1"], L["w3"], L["w2"] = c(inp["exp_w1"][l]), c(inp["exp_w3"][l]), c(inp["exp_w2"][l])
        for k, v in L.items():
            d["%s_%d" % (k, l)] = np.ascontiguousarray(v, dtype=f32)
    return {k: np.ascontiguousarray(v, dtype=f32) for k, v in d.items()}


_PROG = {}


def kernel(**inp):
    inp = {k: np.asarray(v) for k, v in inp.items()}
    NB = inp["x"].shape[0]
    if "fused" not in _PROG:
        _PROG["fused"] = build_fused()
    cores = [(b, hf) for b in range(NB) for hf in range(2)]
    ims = [core_inputs(inp, b, hf) for (b, hf) in cores]
    res = run_bass_kernel_spmd(_PROG["fused"], ims, core_ids=list(range(8))).results
    out = np.stack([np.concatenate([res[2 * b]["oT"][:, 128:].T, res[2 * b + 1]["oT"][:, 128:].T], 0) for b in range(NB)])
    return np.ascontiguousarray(out.astype(np.float32))
```

```python
import numpy as np
import concourse.bass as bass
import concourse.mybir as mybir
from concourse.bass_utils import run_bass_kernel_spmd
from contextlib import ExitStack

F32 = mybir.dt.float32
BF16 = mybir.dt.bfloat16
AF = mybir.ActivationFunctionType
ALU = mybir.AluOpType
AX = mybir.AxisListType


class Reg:
    __slots__ = ("name", "w", "r")

    def __init__(self, name=""):
        self.name = name
        self.w = None
        self.r = []


class Ins:
    __slots__ = ("eng", "fn", "deps", "sig", "idx", "isdma", "slot", "target", "n")

    def __init__(self):
        self.slot = None


class Prog:
    ENG = ["tensor", "vector", "scalar", "gpsimd", "sync"]
    RING = 12

    def __init__(self, nc):
        self.nc = nc
        self.q = {e: [] for e in self.ENG}
        self.n = 0

    def op(self, eng, fn, reads=(), writes=(), dma=False):
        I = Ins()
        I.eng, I.fn, I.isdma, I.sig, I.idx = eng, fn, dma, False, None
        I.n = self.n
        self.n += 1
        deps = {}
        for r in reads:
            if r.w is not None:
                deps.setdefault(id(r.w), [r.w, set()])[1].add("raw")
        for w in writes:
            if w.w is not None:
                deps.setdefault(id(w.w), [w.w, set()])[1].add("waw")
            for x in w.r:
                deps.setdefault(id(x), [x, set()])[1].add("war")
        final = []
        for J, kinds in deps.values():
            if J is I:
                continue
            if J.eng == eng and not J.isdma and not dma:
                if "raw" not in kinds or eng == "tensor":
                    continue
            final.append(J)
            J.sig = True
        I.deps = final
        for r in reads:
            r.r.append(I)
        for w in writes:
            w.w = I
            w.r = []
        self.q[eng].append(I)
        return I

    def fence(self):
        self.nf = getattr(self, "nf", 0) + 1
        for e in self.ENG:
            last = None
            for I in reversed(self.q[e]):
                if I.fn == "FENCE":
                    break
                if not I.isdma and I.fn is not None:
                    last = I
                    break
            if last is not None:
                last.sig = True
            I = Ins()
            I.eng, I.fn, I.isdma, I.sig, I.idx, I.deps = e, "FENCE", False, False, None, [last] if last is not None else []
            I.n = self.nf
            self.q[e].append(I)

    def dma(self, eng, out, in_, reads=(), writes=(), **kw):
        return self.op(eng, lambda e: e.dma_start(out=out, in_=in_, **kw), reads, writes, dma=True)

    def cc(self, fn, reads=(), writes=(), blocking=True):
        I = self.op("gpsimd", fn, reads, writes, dma=True)
        I.slot = "cc" if blocking else "ccnb"
        self.ccs = getattr(self, "ccs", []) + [I]
        return I

    def cc_wait_all(self):
        I = Ins()
        I.eng, I.fn, I.isdma, I.sig, I.idx, I.deps = "gpsimd", "CCWAIT", False, False, None, []
        I.n = len(getattr(self, "ccs", []))
        self.q["gpsimd"].append(I)

    def emit(self, final_regs=()):
        nc = self.nc
        self.op("sync", None, reads=list(final_regs))
        sems = {}
        stack = []
        for e in self.ENG:
            cm = nc.semaphore("S_" + e)
            sems[e] = cm.__enter__()
            stack.append(cm)
        cmf = nc.semaphore("S_fence")
        fsem = cmf.__enter__()
        stack.append(cmf)
        rings = {}
        for e in ("sync", "gpsimd", "scalar"):
            rings[e] = []
            for k in range(self.RING):
                cm = nc.semaphore("D_%s_%d" % (e, k))
                rings[e].append(cm.__enter__())
                stack.append(cm)
        ccsem = {}
        cctgt = {}
        if getattr(self, "ccs", []):
            cm = nc.semaphore("C_all")
            csem = cm.__enter__()
            stack.append(cm)
            for k, I in enumerate(self.ccs):
                ccsem[id(I)] = csem
                cctgt[id(I)] = k + 1
        for e in self.ENG:
            cnt = 0
            dcnt = 0
            for I in self.q[e]:
                if I.isdma and getattr(I, "slot", None) in ("cc", "ccnb"):
                    I.target = 1
                elif I.isdma:
                    I.slot = dcnt % self.RING
                    I.target = 16 * (dcnt // self.RING + 1)
                    dcnt += 1
                elif I.sig:
                    cnt += 1
                    I.idx = cnt
        self.counts = {e: len(self.q[e]) for e in self.ENG}

        def run(e, eng):
            seen = {}
            prev = {}

            def wait(key, sem, val):
                if seen.get(key, 0) < val:
                    eng.wait_ge(sem, val)
                    seen[key] = val

            for I in self.q[e]:
                mx = {}
                for J in I.deps:
                    if J.isdma and J.slot in ("cc", "ccnb"):
                        wait(("cc",), ccsem[id(J)], cctgt[id(J)])
                    elif J.isdma:
                        wait((J.eng, J.slot), rings[J.eng][J.slot], J.target)
                    else:
                        mx[J.eng] = max(mx.get(J.eng, 0), J.idx)
                for f, v in mx.items():
                    wait(f, sems[f], v)
                if I.isdma and I.slot in ("cc", "ccnb"):
                    inst = I.fn(eng)
                    inst.then_inc(ccsem[id(I)], 1)
                    if I.slot == "cc":
                        wait(("cc",), ccsem[id(I)], cctgt[id(I)])
                    continue
                if I.fn == "CCWAIT":
                    if I.n > 0:
                        wait(("cc",), csem, I.n)
                    continue
                if I.isdma:
                    p = prev.get(I.slot)
                    if p is not None:
                        wait((e, I.slot), rings[e][I.slot], p.target)
                    prev[I.slot] = I
                if I.fn is None:
                    continue
                if I.fn == "FENCE":
                    for s, pq in prev.items():
                        wait((e, s), rings[e][s], pq.target)
                    eng.sem_inc(fsem, 1)
                    eng.wait_ge(fsem, len(self.ENG) * I.n)
                    continue
                inst = I.fn(eng)
                if I.isdma:
                    inst.then_inc(rings[e][I.slot], 16)
                elif I.sig:
                    inst.then_inc(sems[e], 1)
            if e in rings:
                for s, p in prev.items():
                    wait((e, s), rings[e][s], p.target)

        with nc.Block() as block:
            @block.sync
            def _(eng):
                run("sync", eng)

            @block.tensor
            def _(eng):
                run("tensor", eng)

            @block.vector
            def _(eng):
                run("vector", eng)

            @block.scalar
            def _(eng):
                run("scalar", eng)

            @block.gpsimd
            def _(eng):
                run("gpsimd", eng)
        for cm in reversed(stack):
            cm.__exit__(None, None, None)


EPS = 1e-6
THETA = 10000.0
NCTX, LAT = 256, 4096
T = NCTX + LAT
HT = T // 2
NCC_MIX = 23
NCC_S1 = 39
ARENA_F32 = 50176


class Tile:
    __slots__ = ("t", "r")

    def __init__(self, t, name):
        self.t = t
        self.r = Reg(name)

    def __getitem__(self, k):
        return self.t[k]


class KB:
    def __init__(self):
        self.nc = bass.Bass("TRN2", target_bir_lowering=False)
        self.es = ExitStack()
        self.p = Prog(self.nc)
        self.es.enter_context(self.nc.allow_low_precision("bf16 matmul operands, fp32 accumulation"))
        self.arena = self.es.enter_context(self.nc.sbuf_tensor("arena", [128, ARENA_F32], F32))
        self.banks = [Tile(self.es.enter_context(self.nc.psum_tensor("bank%d" % i, [128, 512], F32)), "bank%d" % i) for i in range(8)]
        self.off = 0
        self.nscr = 0

    def din(self, name, shape, dt=F32):
        return self.nc.dram_tensor(name, list(shape), dt, kind="ExternalInput").ap()

    def dout(self, name, shape, dt=F32):
        return self.nc.dram_tensor(name, list(shape), dt, kind="ExternalOutput").ap()

    def scratch(self, name, shape, dt=F32, debug=False):
        if debug:
            return self.dout(name, shape, dt)
        return self.nc.dram_tensor(name, list(shape), dt).ap()

    def sb(self, name, shape, dt=F32):
        esize = 4 if dt == F32 else 2
        nel = 1
        for d in shape[1:]:
            nel *= d
        nbytes = (nel * esize + 31) // 32 * 32
        assert self.off + nbytes <= ARENA_F32 * 4, ("SBUF arena overflow", name, self.off, nbytes)
        ap = self.arena[0:shape[0], self.off // 4:(self.off + nbytes) // 4]
        if dt != F32:
            ap = ap.bitcast(dt)
        ap = ap[:, 0:nel]
        if len(shape) > 2:
            names = ["d%d" % k for k in range(len(shape) - 1)]
            ap = ap.rearrange("p (%s) -> p %s" % (" ".join(names), " ".join(names)), **{n: shape[k + 1] for k, n in enumerate(names[:-1])})
        self.off += nbytes
        return Tile(ap, name)

    def sub(self, tile, off_el, shape, dt=F32, name="v"):
        esize = 4 if dt == F32 else 2
        nel = 1
        for d in shape[1:]:
            nel *= d
        ap = tile.t[0:shape[0], off_el:off_el + nel * esize // 4]
        if dt != F32:
            ap = ap.bitcast(dt)
        if len(shape) > 2:
            names = ["d%d" % k for k in range(len(shape) - 1)]
            ap = ap.rearrange("p (%s) -> p %s" % (" ".join(names), " ".join(names)), **{n: shape[k + 1] for k, n in enumerate(names[:-1])})
        return Tile(ap, name)

    def psbf(self, bank):
        t = Tile(bank.t[:, 0:512].bitcast(BF16), "bf")
        t.r = bank.r
        return t

    def reset(self):
        self.p.fence()
        self.off = 0

    def finish(self, final_regs):
        self.p.emit(final_regs)
        self.es.close()
        return self.nc


def fm(v, n):
    return np.ascontiguousarray(np.asarray(v).reshape(n, 128).T)


def tok_ranges(t0, n, nctx):
    out = []
    if t0 < nctx:
        m = min(n, nctx - t0)
        out.append((t0, m, 1))
        if n > m:
            out.append((t0 + m, n - m, 0))
    else:
        out.append((t0, n, 0))
    return out


def ld(kb, name, shape, src, dt=F32):
    t = kb.sb(name, shape, dt)
    kb.p.dma("sync", t[:], src, writes=[t.r])
    return t


def consts(kb, ident_d, want_bf=True):
    p = kb.p
    idf = ld(kb, "idf", [128, 128], ident_d)
    ones = kb.sb("ones", [128, 128])
    p.op("vector", lambda e: e.memset(ones[:], 1.0), [], [ones.r])
    idb = None
    if want_bf:
        idb = kb.sb("idb", [128, 128], BF16)
        p.op("vector", lambda e: e.tensor_copy(out=idb[:], in_=idf[:]), [idf.r], [idb.r])
    return idf, idb, ones


def emit_mods(kb, adaw, adab, ncc, cT, aw, psm, name="m"):
    p = kb.p
    ct = kb.sb(name + "ct", [128, 8, 2])
    st = kb.sb(name + "st", [128, 8, 2])
    p.dma("sync", ct[:], cT, writes=[ct.r])
    p.op("scalar", lambda e: e.activation(out=st[:], in_=ct[:], func=AF.Silu), [ct.r], [st.r])
    ab = kb.sb(name + "ab", [128, ncc])
    p.dma("sync", ab[:], adab, writes=[ab.r])
    mod = kb.sb(name + "mod", [128, ncc, 2])
    for g in range(ncc // 4):
        awr = [Reg("aw%d" % k) for k in range(8)]
        for kc in range(8):
            p.dma("sync", aw[:, kc, :], adaw[kc * 128:(kc + 1) * 128, g * 512:(g + 1) * 512], reads=[], writes=[awr[kc], aw.r])

        def mm_mod(e, g=g):
            for cc in range(4):
                for kc in range(8):
                    last = e.matmul(psm[:, 2 * (g * 4 + cc):2 * (g * 4 + cc) + 2], lhsT=aw[:, kc, cc * 128:(cc + 1) * 128], rhs=st[:, kc, :],
                                    start=(kc == 0), stop=(kc == 7))
            return last
        p.op("tensor", mm_mod, [st.r, aw.r] + awr, [psm.r])
    p.op("vector", lambda e: e.tensor_tensor(out=mod[:], in0=psm[:, 0:2 * ncc].rearrange("p (c j) -> p c j", j=2),
                                             in1=ab[:].unsqueeze(2).to_broadcast([128, ncc, 2]), op=ALU.add), [psm.r, ab.r], [mod.r])
    return mod


def nat_pieces(t0, n):
    out = []
    bounds = [(0, 128, 0, 0), (128, 256, 1, 0), (256, 256 + 2048, 0, 128), (256 + 2048, T, 1, 128)]
    for (a, b, h, loc) in bounds:
        lo, hi = max(a, t0), min(b, t0 + n)
        if lo < hi:
            out.append((lo, hi - lo, h, loc + lo - a))
    return out


PIECE = 240
PIECE_B = 480


def gath_parts(total, f0, r, base0=0, n=128, PIECE=240):
    parts = []
    f = f0
    while f < f0 + n:
        i = f // PIECE
        Ri = min(PIECE, total - PIECE * i)
        end = min(f0 + n, PIECE * (i + 1))
        parts.append((base0 + 2 * PIECE * i + r * Ri + (f - PIECE * i), end - f, f - f0))
        f = end
    return parts


def xs_write(p, xs, row0, tile, reads, outs, eng="gpsimd", btile=None):
    if btile is not None:
        p.op("gpsimd", lambda e, src_=tile, dst_=btile: e.tensor_copy(out=dst_[:], in_=src_[:]), list(reads), [btile.r])
        tile, reads = btile, [btile.r]
    for (a, n, h, loc) in nat_pieces(0, T):
        o = Reg("o")
        outs.append(o)
        p.dma(eng, xs[h * 3072 + row0:h * 3072 + row0 + 128, loc:loc + n], tile[:, a:a + n], reads=reads, writes=[o])


def emit_s1(kb, xsrc, cT, adaw, adab, n1, win, pT, xs, xs_regs, pT_regs, xall_ap=None):
    p = kb.p
    banks = kb.banks
    aw = kb.sb("aw", [128, 8, 512])
    mod = emit_mods(kb, adaw, adab, 16, cT, aw, banks[0])
    n1t = ld(kb, "n1t", [128, 8], n1)
    asc = kb.sb("asc", [128, 8, 2])
    p.op("vector", lambda e: e.tensor_scalar(out=asc[:], in0=mod[:, 8:16, :], scalar1=1.0, scalar2=None, op0=ALU.add), [mod.r], [asc.r])
    p.op("vector", lambda e: e.tensor_tensor(out=asc[:], in0=asc[:], in1=n1t[:].unsqueeze(2).to_broadcast([128, 8, 2]), op=ALU.mult), [asc.r, n1t.r], [asc.r])
    ones = kb.sb("ones", [128, 128])
    p.op("vector", lambda e: e.memset(ones[:], 1.0), [], [ones.r])
    nch = (T + 511) // 512
    chunks = [(c * 512, min(512, T - c * 512)) for c in range(nch)]
    hT = kb.sb("hT", [128, 8, T], BF16)
    hr = [Reg("h%d" % c) for c in range(nch)]
    xq = [kb.sb("xq%d" % i, [128, 8, 512]) for i in range(2)]
    sq = [kb.sb("sq%d" % i, [128, 512]) for i in range(2)]
    rstd = kb.sb("rstd", [128, 512])
    tmp = [kb.sb("tmp%d" % i, [128, 512]) for i in range(2)]
    pss = banks[1]
    for c, (t0, n) in enumerate(chunks):
        xc = xq[c % 2]
        for kc in range(8):
            for piece in xsrc(kc, t0, n):
                if len(piece) == 3:
                    off, ln, src = piece
                    p.dma("sync", xc[:, kc, off:off + ln], src, writes=[xc.r])
                else:
                    off, ln, h_, loc = piece
                    for (row, cnt, po_) in gath_parts(1024, kc * 128, h_):
                        p.dma("sync", xc.t[po_:po_ + cnt, kc, off:off + ln], xall_ap[row:row + cnt, loc:loc + ln], writes=[xc.r])
        for kc in range(8):
            s_ = sq[kc % 2]
            p.op("scalar", lambda e, s_=s_, xc=xc, kc=kc, n=n: e.activation(out=s_[:, 0:n], in_=xc[:, kc, 0:n], func=AF.Square), [xc.r], [s_.r])
            p.op("tensor", lambda e, s_=s_, kc=kc, n=n: e.matmul(pss[:, 0:n], lhsT=ones[:], rhs=s_[:, 0:n], start=(kc == 0), stop=(kc == 7)), [ones.r, s_.r], [pss.r])
        p.op("vector", lambda e, n=n: e.tensor_scalar(out=rstd[:, 0:n], in0=pss[:, 0:n], scalar1=1.0 / 1024, scalar2=EPS, op0=ALU.mult, op1=ALU.add), [pss.r], [rstd.r])
        p.op("vector", lambda e, n=n: e.reciprocal(out=rstd[:, 0:n], in_=rstd[:, 0:n]), [rstd.r], [rstd.r])
        p.op("scalar", lambda e, n=n: e.activation(out=rstd[:, 0:n], in_=rstd[:, 0:n], func=AF.Sqrt), [rstd.r], [rstd.r])
        for kc in range(8):
            tm = tmp[kc % 2]
            p.op("vector", lambda e, tm=tm, xc=xc, kc=kc, n=n: e.tensor_tensor(out=tm[:, 0:n], in0=xc[:, kc, 0:n], in1=rstd[:, 0:n], op=ALU.mult), [xc.r, rstd.r], [tm.r])

            def modf(e, tm=tm, kc=kc, t0=t0, n=n):
                for (s, m, j) in tok_ranges(t0, n, NCTX):
                    last = e.activation(out=hT[:, kc, s:s + m], in_=tm[:, s - t0:s - t0 + m], func=AF.Identity, scale=asc[:, kc, j:j + 1], bias=mod[:, kc, j:j + 1])
                return last
            p.op("scalar", modf, [tm.r, asc.r, mod.r], [hr[c]])
    wfs = [kb.sb("wf%d" % i, [128, 8, 128]) for i in range(2)]
    wbs = [kb.sb("wb%d" % i, [128, 8, 128], BF16) for i in range(2)]
    stg = [kb.sb("stg%d" % i, [128, T]) for i in range(2)]
    gbt = kb.sb("gbt", [128, T], BF16)
    sgr = [[Reg("sg%d_%d" % (i, c)) for c in range(nch)] for i in range(2)]
    pps = banks[2:6]
    cnt = 0
    for cc in range(NCC_S1):
        wf, wb, sg = wfs[cc % 2], wbs[cc % 2], stg[cc % 2]
        p.dma("sync", wf[:], win[:, cc * 128:(cc + 1) * 128].rearrange("(kc p) c -> p kc c", p=128), writes=[wf.r])
        p.op("gpsimd", lambda e, wf=wf, wb=wb: e.tensor_copy(out=wb[:], in_=wf[:]), [wf.r], [wb.r])
        for tcn, (t0, n) in enumerate(chunks):
            pp = pps[cnt % 4]

            def mm(e, pp=pp, wb=wb, t0=t0, n=n):
                for kc in range(8):
                    last = e.matmul(pp[:, 0:n], lhsT=wb[:, kc, :], rhs=hT[:, kc, t0:t0 + n], start=(kc == 0), stop=(kc == 7))
                return last
            p.op("tensor", mm, [wb.r, hr[tcn]], [pp.r])
            if cnt % 2 == 0:
                p.op("vector", lambda e, pp=pp, sg=sg, t0=t0, n=n: e.tensor_copy(out=sg[:, t0:t0 + n], in_=pp[:, 0:n]), [pp.r], [sgr[cc % 2][tcn]])
            else:
                p.op("scalar", lambda e, pp=pp, sg=sg, t0=t0, n=n: e.copy(out=sg[:, t0:t0 + n], in_=pp[:, 0:n]), [pp.r], [sgr[cc % 2][tcn]])
            cnt += 1
        if cc < NCC_MIX:
            p.dma("gpsimd", pT[cc * 128:(cc + 1) * 128, :], sg[:], reads=sgr[cc % 2], writes=[pT_regs[cc]])
        else:
            xs_write(p, xs, 1024 + (cc - NCC_MIX) * 128, sg, sgr[cc % 2], xs_regs, btile=gbt)


def rope_tables(L=4096, W=64):
    t = np.arange(L)
    pos = np.stack([t // W, t % W], 0).astype(np.float32)
    d = np.arange(128)
    half = d // 64
    j = d % 32
    inv = (THETA ** (-(j.astype(np.float32)) / 32.0)).astype(np.float32)
    ang = pos[half] * inv[:, None]
    cos = np.cos(ang).astype(np.float32)
    sin = np.sin(ang).astype(np.float32)
    sgn = np.where((d % 64) < 32, -1.0, 1.0).astype(np.float32)
    Rm = np.zeros((128, 128), np.float32)
    partner = np.where((d % 64) < 32, d + 32, d - 32)
    Rm[partner, d] = 1.0
    return cos, (sin * sgn[:, None]).astype(np.float32), Rm


def emit_att(kb, qT_d, kT_d, vT_d, gq_d, gk_d, cos_d, sin_d, rm_d, ident_d, xs, row0, in_regs, outs):
    p = kb.p
    banks = kb.banks
    L = LAT
    NKT = T // 128
    idf, idb, ones = consts(kb, ident_d)
    onesb = kb.sb("onesb", [128, 128], BF16)
    p.op("vector", lambda e: e.tensor_copy(out=onesb[:], in_=ones[:]), [ones.r], [onesb.r])
    rm = ld(kb, "rm", [128, 128], rm_d)
    gq = ld(kb, "gq", [128, 1], gq_d)
    gk = ld(kb, "gk", [128, 1], gk_d)
    cos = ld(kb, "cos", [128, L], cos_d)
    sin = ld(kb, "sin", [128, L], sin_d)
    xst = [kb.sb("xst%d" % i, [128, T]) for i in range(2)]
    vb = kb.sb("vb", [128, NKT, 128], BF16)
    vtb = kb.sb("vtb", [128, T], BF16)
    p.dma("sync", xst[1][:], vT_d, reads=in_regs, writes=[xst[1].r])
    p.op("gpsimd", lambda e: e.tensor_copy(out=vtb[:], in_=xst[1][:]), [xst[1].r], [vtb.r])
    ptr = kb.psbf(banks[1])
    for g in range((NKT + 7) // 8):
        k0, k1 = g * 8, min(NKT, g * 8 + 8)

        def trv(e, k0=k0, k1=k1):
            for kt in range(k0, k1):
                last = e.transpose(out=ptr.t[:, (kt - k0) * 128:(kt - k0 + 1) * 128], in_=vtb[:, kt * 128:(kt + 1) * 128], identity=idb[:])
            return last
        p.op("tensor", trv, [vtb.r, idb.r], [ptr.r])
        p.op("scalar", lambda e, k0=k0, k1=k1: e.copy(out=vb[:, k0:k1, :], in_=ptr.t[:, 0:(k1 - k0) * 128].rearrange("p (a b) -> p a b", b=128)), [ptr.r], [vb.r])

    chunks = [(0, NCTX, False)] + [(NCTX + c * 512, 512, True) for c in range(L // 512)]
    knT = kb.sb("knT", [128, T], BF16)
    qnT = [kb.sb("qnT%d" % i, [128, T], BF16) for i in range(2)]
    sqt = kb.sb("sqt", [128, 512])
    rstd = kb.sb("rstd", [128, 512])
    xg = kb.sb("xg", [128, 512])
    t1 = kb.sb("t1", [128, 512])
    t2 = kb.sb("t2", [128, 512])
    pss, prot = banks[0], banks[1]
    srcs = [(kT_d, gk, knT), (qT_d[0:128, :], gq, qnT[0]), (qT_d[128:256, :], gq, qnT[1])]
    dregs = []
    for si, (src, g, dst) in enumerate(srcs):
        xs_ = xst[si % 2]
        p.dma("sync", xs_[:], src, reads=in_regs, writes=[xs_.r])
        dr = [Reg("d%d_%d" % (si, c)) for c in range(len(chunks))]
        dregs.append(dr)
        for c, (t0, n, lat) in enumerate(chunks):
            p.op("scalar", lambda e, xs_=xs_, t0=t0, n=n: e.activation(out=sqt[:, 0:n], in_=xs_[:, t0:t0 + n], func=AF.Square), [xs_.r], [sqt.r])
            p.op("tensor", lambda e, n=n: e.matmul(pss[:, 0:n], lhsT=ones[:], rhs=sqt[:, 0:n], start=True, stop=True), [ones.r, sqt.r], [pss.r])
            p.op("vector", lambda e, n=n: e.tensor_scalar(out=rstd[:, 0:n], in0=pss[:, 0:n], scalar1=1.0 / 128, scalar2=EPS, op0=ALU.mult, op1=ALU.add), [pss.r], [rstd.r])
            p.op("vector", lambda e, n=n: e.reciprocal(out=rstd[:, 0:n], in_=rstd[:, 0:n]), [rstd.r], [rstd.r])
            p.op("scalar", lambda e, n=n: e.activation(out=rstd[:, 0:n], in_=rstd[:, 0:n], func=AF.Sqrt), [rstd.r], [rstd.r])
            p.op("vector", lambda e, xs_=xs_, g=g, t0=t0, n=n: e.tensor_scalar(out=xg[:, 0:n], in0=xs_[:, t0:t0 + n], scalar1=g[:, 0:1], scalar2=None, op0=ALU.mult), [xs_.r, g.r], [xg.r])
            if lat:
                l0 = t0 - NCTX
                p.op("tensor", lambda e, n=n: e.matmul(prot[:, 0:n], lhsT=rm[:], rhs=xg[:, 0:n], start=True, stop=True), [rm.r, xg.r], [prot.r])
                p.op("gpsimd", lambda e, l0=l0, n=n: e.tensor_tensor(out=t1[:, 0:n], in0=xg[:, 0:n], in1=cos[:, l0:l0 + n], op=ALU.mult), [xg.r, cos.r], [t1.r])
                p.op("vector", lambda e, l0=l0, n=n: e.tensor_tensor(out=t2[:, 0:n], in0=prot[:, 0:n], in1=sin[:, l0:l0 + n], op=ALU.mult), [prot.r, sin.r], [t2.r])
                p.op("gpsimd", lambda e, n=n: e.tensor_tensor(out=t1[:, 0:n], in0=t1[:, 0:n], in1=t2[:, 0:n], op=ALU.add), [t1.r, t2.r], [t1.r])
                p.op("vector", lambda e, dst=dst, t0=t0, n=n: e.tensor_tensor(out=dst[:, t0:t0 + n], in0=t1[:, 0:n], in1=rstd[:, 0:n], op=ALU.mult), [t1.r, rstd.r], [dr[c]])
            else:
                p.op("vector", lambda e, dst=dst, t0=t0, n=n: e.tensor_tensor(out=dst[:, t0:t0 + n], in0=xg[:, 0:n], in1=rstd[:, 0:n], op=ALU.mult), [xg.r, rstd.r], [dr[c]])
    kr, qr = dregs[0], dregs[1:]
    pS = banks[2:5]
    pO = banks[5:7]
    pD = [banks[7], banks[0]]
    pts = [kb.sb("pt%d" % i, [128, 512], BF16) for i in range(3)]
    rden = [kb.sb("rden%d" % i, [128, 512]) for i in range(2)]
    yo = [kb.sb("yo%d" % i, [128, 512], BF16) for i in range(2)]
    scale = 128.0 ** -0.5
    sc = 0
    blk = 0
    for h in range(2):
        for c, (t0, n, lat) in enumerate(chunks):
            nkt = NKT if lat else NCTX // 128
            po, pd = pO[blk % 2], pD[blk % 2]
            def s_exp(kt, sc_):
                ps_, pt = pS[sc_ % 3], pts[sc_ % 3]
                kc = 0 if kt < NCTX // 128 else 1 + (kt * 128 - NCTX) // 512
                p.op("tensor", lambda e, ps_=ps_, kt=kt, h=h, t0=t0, n=n: e.matmul(ps_[:, 0:n], lhsT=knT[:, kt * 128:(kt + 1) * 128], rhs=qnT[h][:, t0:t0 + n], start=True, stop=True),
                     [kr[kc], qr[h][c]], [ps_.r])
                p.op("scalar", lambda e, ps_=ps_, pt=pt, n=n: e.activation(out=pt[:, 0:n], in_=ps_[:, 0:n], func=AF.Exp, scale=scale), [ps_.r], [pt.r])
            s_exp(0, sc)
            for kt in range(nkt):
                pt = pts[sc % 3]
                if kt + 1 < nkt:
                    s_exp(kt + 1, sc + 1)
                sc += 1

                def pv(e, po=po, pd=pd, pt=pt, kt=kt, n=n, nkt=nkt):
                    e.matmul(po[:, 0:n], lhsT=vb[:, kt, :], rhs=pt[:, 0:n], start=(kt == 0), stop=(kt == nkt - 1))
                    return e.matmul(pd[:, 0:n], lhsT=onesb[:], rhs=pt[:, 0:n], start=(kt == 0), stop=(kt == nkt - 1))
                p.op("tensor", pv, [vb.r, onesb.r, pt.r], [po.r, pd.r])
            rd, y = rden[blk % 2], yo[blk % 2]
            p.op("vector", lambda e, rd=rd, pd=pd, n=n: e.reciprocal(out=rd[:, 0:n], in_=pd[:, 0:n]), [pd.r], [rd.r])
            p.op("vector", lambda e, rd=rd, po=po, y=y, n=n: e.tensor_tensor(out=y[:, 0:n], in0=po[:, 0:n], in1=rd[:, 0:n], op=ALU.mult), [po.r, rd.r], [y.r])
            for (a, m, hh, loc) in nat_pieces(t0, n):
                o = Reg("o")
                outs.append(o)
                p.dma("gpsimd", xs[hh * 3072 + row0 + h * 128:hh * 3072 + row0 + (h + 1) * 128, loc:loc + m], y[:, a - t0:a - t0 + m], reads=[y.r], writes=[o])
            blk += 1


def emit_conv(kb, p, x, u, w, b, segs, cc, eng_extra="vector"):
    p.op("scalar", lambda e: e.activation(out=u[:], in_=x[:], func=AF.Identity, scale=w[:, cc, 2:3], bias=b[:, cc:cc + 1]), [x.r, w.r, b.r], [u.r])

    def taps(e):
        for (s0, n) in segs:
            for k, off in ((0, -2), (1, -1), (3, 1)):
                lo = max(0, -off)
                hi = n - max(0, off)
                last = e.scalar_tensor_tensor(out=u[:, s0 + lo:s0 + hi], in0=x[:, s0 + lo + off:s0 + hi + off], scalar=w[:, cc, k:k + 1],
                                              in1=u[:, s0 + lo:s0 + hi], op0=ALU.mult, op1=ALU.add)
        return last
    p.op(eng_extra, taps, [x.r, u.r, w.r], [u.r])


def emit_lru(kb, xT_d, gT_d, cw_d, cb_d, wbd_d, bias_d, lam_d, xs, row0, in_regs, outs):
    p = kb.p
    banks = kb.banks
    L = LAT
    cw = ld(kb, "cw", [128, 2, 4], cw_d)
    cb = ld(kb, "cb", [128, 2], cb_d)
    wbd = ld(kb, "wbd", [128, 8, 128], wbd_d)
    bias = ld(kb, "bias", [128, 8], bias_d)
    lam = ld(kb, "lam", [128, 4], lam_d)
    cl = kb.sb("cl", [128, 4])
    p.op("scalar", lambda e: e.activation(out=cl[:], in_=lam[:], func=AF.Exp, scale=-1.0), [lam.r], [cl.r])
    p.op("scalar", lambda e: e.activation(out=cl[:], in_=cl[:], func=AF.Ln, bias=1.0, scale=1.0), [cl.r], [cl.r])
    p.op("vector", lambda e: e.tensor_scalar(out=cl[:], in0=cl[:], scalar1=-8.0, scalar2=None, op0=ALU.mult), [cl.r], [cl.r])
    x = kb.sb("x", [128, T]); u = kb.sb("u", [128, T]); g = kb.sb("g", [128, T])
    av = [[kb.sb("a%d" % d, [128, T]), kb.sb("v%d" % d, [128, T])] for d in range(2)]
    hh = [kb.sb("h%d" % d, [128, T]) for d in range(2)]
    rt = [kb.sb("rt%d" % i, [128, 512]) for i in range(2)]
    it_ = [kb.sb("it%d" % i, [128, 512]) for i in range(2)]
    s2 = [kb.sb("s2%d" % i, [128, 512]) for i in range(2)]
    ybt = kb.sb("ybt", [128, T], BF16)
    segs = [(0, NCTX), (NCTX, L)]
    chunks = [(0, NCTX)] + [(NCTX + c * 512, 512) for c in range(L // 512)]
    bc = 0
    for cc in range(2):
        p.dma("sync", x[:], xT_d[cc * 128:(cc + 1) * 128, :], reads=in_regs, writes=[x.r])
        p.dma("sync", g[:], gT_d[cc * 128:(cc + 1) * 128, :], reads=in_regs, writes=[g.r])
        emit_conv(kb, p, x, u, cw, cb, segs, cc)
        for d in range(2):
            a, v = av[d]
            for c, (t0, n) in enumerate(chunks):
                pr, pi = banks[bc % 8], banks[(bc + 1) % 8]
                bc += 2
                r_, i_, s_ = rt[c % 2], it_[c % 2], s2[c % 2]
                ia, ix = 0 * 4 + d * 2 + cc, 1 * 4 + d * 2 + cc
                p.op("tensor", lambda e, pr=pr, ia=ia, t0=t0, n=n: e.matmul(pr[:, 0:n], lhsT=wbd[:, ia, :], rhs=u[:, t0:t0 + n], start=True, stop=True), [wbd.r, u.r], [pr.r])
                p.op("tensor", lambda e, pi=pi, ix=ix, t0=t0, n=n: e.matmul(pi[:, 0:n], lhsT=wbd[:, ix, :], rhs=u[:, t0:t0 + n], start=True, stop=True), [wbd.r, u.r], [pi.r])
                p.op("scalar", lambda e, pr=pr, r_=r_, ia=ia, n=n: e.activation(out=r_[:, 0:n], in_=pr[:, 0:n], func=AF.Sigmoid, bias=bias[:, ia:ia + 1], scale=1.0), [pr.r, bias.r], [r_.r])
                p.op("scalar", lambda e, pi=pi, i_=i_, ix=ix, n=n: e.activation(out=i_[:, 0:n], in_=pi[:, 0:n], func=AF.Sigmoid, bias=bias[:, ix:ix + 1], scale=1.0), [pi.r, bias.r], [i_.r])
                p.op("scalar", lambda e, a=a, r_=r_, d=d, cc=cc, t0=t0, n=n: e.activation(out=a[:, t0:t0 + n], in_=r_[:, 0:n], func=AF.Exp, scale=cl[:, d * 2 + cc:d * 2 + cc + 1]), [r_.r, cl.r], [a.r])
                p.op("gpsimd", lambda e, a=a, s_=s_, t0=t0, n=n: e.tensor_tensor(out=s_[:, 0:n], in0=a[:, t0:t0 + n], in1=a[:, t0:t0 + n], op=ALU.mult), [a.r], [s_.r])
                p.op("scalar", lambda e, s_=s_, n=n: e.activation(out=s_[:, 0:n], in_=s_[:, 0:n], func=AF.Sqrt, scale=-1.0, bias=1.0), [s_.r], [s_.r])
                p.op("vector", lambda e, i_=i_, t0=t0, n=n: e.tensor_tensor(out=i_[:, 0:n], in0=i_[:, 0:n], in1=u[:, t0:t0 + n], op=ALU.mult), [i_.r, u.r], [i_.r])
                p.op("vector", lambda e, v=v, i_=i_, s_=s_, t0=t0, n=n: e.tensor_tensor(out=v[:, t0:t0 + n], in0=i_[:, 0:n], in1=s_[:, 0:n], op=ALU.mult), [i_.r, s_.r], [v.r])
            h = hh[d]
            if d == 0:
                p.op("vector", lambda e, a=a, v=v, h=h: e.tensor_tensor_scan(out=h[:, 0:NCTX], data0=a[:, 0:NCTX], data1=v[:, 0:NCTX], initial=0.0, op0=ALU.mult, op1=ALU.add), [a.r, v.r], [h.r])
                p.op("vector", lambda e, a=a, v=v, h=h: e.tensor_tensor_scan(out=h[:, NCTX:T], data0=a[:, NCTX:T], data1=v[:, NCTX:T], initial=h[:, NCTX - 1:NCTX], op0=ALU.mult, op1=ALU.add), [a.r, v.r, h.r], [h.r])
            else:
                p.op("vector", lambda e, a=a, v=v, h=h: e.tensor_tensor_scan(out=h[:, 0:NCTX][:, ::-1], data0=a[:, 0:NCTX][:, ::-1], data1=v[:, 0:NCTX][:, ::-1], initial=0.0, op0=ALU.mult, op1=ALU.add), [a.r, v.r], [h.r])
                p.op("vector", lambda e, a=a, v=v, h=h: e.tensor_tensor_scan(out=h[:, NCTX:T][:, ::-1], data0=a[:, NCTX:T][:, ::-1], data1=v[:, NCTX:T][:, ::-1], initial=h[:, 0:1], op0=ALU.mult, op1=ALU.add), [a.r, v.r, h.r], [h.r])
        z = av[0][0]
        p.op("gpsimd", lambda e: e.tensor_tensor(out=z[:], in0=g[:], in1=g[:], op=ALU.mult), [g.r], [z.r])
        p.op("vector", lambda e: e.tensor_scalar(out=z[:], in0=z[:], scalar1=0.044715, scalar2=1.0, op0=ALU.mult, op1=ALU.add), [z.r], [z.r])
        p.op("gpsimd", lambda e: e.tensor_tensor(out=z[:], in0=z[:], in1=g[:], op=ALU.mult), [z.r, g.r], [z.r])
        p.op("scalar", lambda e: e.activation(out=z[:], in_=z[:], func=AF.Sigmoid, scale=1.5957691216057308), [z.r], [z.r])
        p.op("gpsimd", lambda e: e.tensor_tensor(out=z[:], in0=z[:], in1=g[:], op=ALU.mult), [z.r, g.r], [z.r])
        p.op("vector", lambda e: e.tensor_tensor(out=hh[0][:], in0=hh[0][:], in1=hh[1][:], op=ALU.add), [hh[0].r, hh[1].r], [hh[0].r])
        p.op("vector", lambda e: e.tensor_tensor(out=hh[0][:], in0=hh[0][:], in1=z[:], op=ALU.mult), [hh[0].r, z.r], [hh[0].r])
        xs_write(p, xs, row0 + cc * 128, hh[0], [hh[0].r], outs, btile=ybt)


def rev_segments(e, out_t, in_t, segs):
    for (s0, n) in segs:
        last = e.tensor_copy(out=out_t[:, s0:s0 + n], in_=in_t[:, s0:s0 + n][:, ::-1])
    return last


def scan_nat_lo(t0, n):
    if t0 < NCTX:
        return NCTX - (t0 + n)
    return NCTX + LAT - (t0 - NCTX + n)


def emit_gla(kb, qT_d, kT_d, vT_d, g1_d, rT_d, g2_d, gb_d, gn_d, cm_d, m2_d, hm_d, ident_d, xs, row0, in_regs, outs):
    p = kb.p
    banks = kb.banks
    L = LAT
    NP = T // 128
    ptr = kb.psbf(banks[7])
    plog, patt, pO, pkv = banks[0], banks[1:3], banks[3:5], banks[5:7]
    segs = [(0, NCTX), (NCTX, L)]
    g2 = ld(kb, "g2", [16, 2, 256], g2_d)
    ngb = ld(kb, "ngb", [128, 4], gb_d)
    p.op("vector", lambda e: e.tensor_scalar(out=ngb[:], in0=ngb[:], scalar1=-1.0, scalar2=None, op0=ALU.mult), [ngb.r], [ngb.r])
    gn = ld(kb, "gn", [128, 2], gn_d)
    cm = ld(kb, "cm", [128, 512], cm_d)
    m2 = ld(kb, "m2", [128, 128], m2_d)
    hm = ld(kb, "hm", [128, 2], hm_d)
    idf, idb, ones = consts(kb, ident_d)
    g1b = kb.sb("g1b", [16, 512])
    g1c = kb.sb("g1c", [16, 512])
    osb = [kb.sb("osb%d" % i, [128, T]) for i in range(4)]
    qT = kb.sb("qT", [128, T])
    kT = kb.sb("kT", [128, T])
    tmpT = kb.sb("tmpT", [128, T])
    vtb = kb.sb("vtb", [128, T], BF16)
    vb = kb.sb("vb", [128, NP, 128], BF16)
    F = lambda n: kb.sb(n, [128, 512])
    sp, gcs, dref, dlast, Aex, Bex, Dex, Eex = F("sp"), F("gcs"), F("dref"), F("dlast"), F("Aex"), F("Bex"), F("Dex"), F("Eex")
    dec = kb.sb("dec", [128, 8])
    Bq = lambda n: kb.sb(n, [128, 512], BF16)
    qt, kt, kd, qe = Bq("qt"), Bq("kt"), Bq("kd"), Bq("qe")
    kdT = [kb.sb("kdT%d" % i, [128, 2, 128], BF16) for i in range(2)]
    attm = [kb.sb("attm%d" % i, [128, 128], BF16) for i in range(2)]
    S = kb.sb("S", [128, 128])
    NSB = 4
    Sb = [kb.sb("Sb%d" % i, [128, 128], BF16) for i in range(NSB)]
    scale = 128.0 ** -0.5
    blocks = [(0, NCTX)] + [(NCTX + c * 512, 512) for c in range(L // 512)]
    pc = 0
    for hd in range(4):
        h, d = hd // 2, hd % 2
        for (dst, src) in ((qT, qT_d), (kT, kT_d)):
            if d == 0:
                p.dma("sync", dst[:], src[h * 128:(h + 1) * 128, :], reads=in_regs, writes=[dst.r])
            else:
                p.dma("sync", tmpT[:], src[h * 128:(h + 1) * 128, :], reads=in_regs, writes=[tmpT.r])
                p.op("gpsimd", lambda e, dst=dst: rev_segments(e, dst, tmpT, segs), [tmpT.r], [dst.r])
        p.dma("sync", tmpT[:], vT_d[h * 128:(h + 1) * 128, :], reads=in_regs, writes=[tmpT.r])
        if d == 0:
            p.op("gpsimd", lambda e: e.tensor_copy(out=vtb[:], in_=tmpT[:]), [tmpT.r], [vtb.r])
        else:
            p.op("gpsimd", lambda e: rev_segments(e, vtb, tmpT, segs), [tmpT.r], [vtb.r])
        for g in range((NP + 7) // 8):
            k0, k1 = g * 8, min(NP, g * 8 + 8)

            def trv(e, k0=k0, k1=k1):
                for kt_ in range(k0, k1):
                    last = e.transpose(out=ptr.t[:, (kt_ - k0) * 128:(kt_ - k0 + 1) * 128], in_=vtb[:, kt_ * 128:(kt_ + 1) * 128], identity=idb[:])
                return last
            p.op("tensor", trv, [vtb.r, idb.r], [ptr.r])
            p.op("scalar", lambda e, k0=k0, k1=k1: e.copy(out=vb[:, k0:k1, :], in_=ptr.t[:, 0:(k1 - k0) * 128].rearrange("p (a b) -> p a b", b=128)), [ptr.r], [vb.r])
        p.op("vector", lambda e: e.memset(S[:], 0.0), [], [S.r])
        sbi = 0
        p.op("gpsimd", lambda e, sb_=Sb[0]: e.memset(sb_[:], 0.0), [], [Sb[0].r])
        for (t0, n) in blocks:
            nc_ = n // 64
            v3 = lambda t, n=n: t[:, 0:n].rearrange("p (c l) -> p c l", l=64)
            if d == 0:
                p.dma("sync", g1b[:, 0:n], g1_d[0:16, t0:t0 + n], reads=in_regs, writes=[g1b.r])
                gsrc = g1b
            else:
                lo = scan_nat_lo(t0, n)
                p.dma("sync", g1c[:, 0:n], g1_d[16:32, lo:lo + n], reads=in_regs, writes=[g1c.r])
                p.op("vector", lambda e, n=n: e.tensor_copy(out=g1b[:, 0:n], in_=g1c[:, 0:n][:, ::-1]), [g1c.r], [g1b.r])
                gsrc = g1b
            p.op("tensor", lambda e, d=d, h=h, n=n: e.matmul(plog[:, 0:n], lhsT=g2[:, d, h * 128:(h + 1) * 128], rhs=g1b[:, 0:n], start=True, stop=True), [g2.r, g1b.r], [plog.r])
            p.op("scalar", lambda e, hd=hd, n=n: e.activation(out=sp[:, 0:n], in_=plog[:, 0:n], func=AF.Exp, scale=-1.0, bias=ngb[:, hd:hd + 1]), [plog.r, ngb.r], [sp.r])
            p.op("scalar", lambda e, n=n: e.activation(out=sp[:, 0:n], in_=sp[:, 0:n], func=AF.Ln, scale=1.0, bias=1.0), [sp.r], [sp.r])
            p.op("vector", lambda e, n=n: e.tensor_tensor_scan(out=gcs[:, 0:n], data0=cm[:, 0:n], data1=sp[:, 0:n], initial=0.0, op0=ALU.mult, op1=ALU.add), [cm.r, sp.r], [gcs.r])
            p.op("vector", lambda e, n=n, nc_=nc_, v3=v3: e.tensor_tensor(out=v3(dref), in0=v3(gcs), in1=v3(gcs)[:, :, 32:33].to_broadcast([128, nc_, 64]), op=ALU.subtract), [gcs.r], [dref.r])
            p.op("gpsimd", lambda e, n=n, nc_=nc_, v3=v3: e.tensor_tensor(out=v3(dlast), in0=v3(gcs), in1=v3(gcs)[:, :, 63:64].to_broadcast([128, nc_, 64]), op=ALU.subtract), [gcs.r], [dlast.r])
            A = lambda fn, rd, wr: p.op("scalar", fn, rd, wr)
            A(lambda e, n=n: e.activation(out=Aex[:, 0:n], in_=dref[:, 0:n], func=AF.Exp, scale=-1.0 / 16), [dref.r], [Aex.r])
            A(lambda e, n=n: e.activation(out=Bex[:, 0:n], in_=dref[:, 0:n], func=AF.Exp, scale=1.0 / 16), [dref.r], [Bex.r])
            A(lambda e, n=n: e.activation(out=Dex[:, 0:n], in_=dlast[:, 0:n], func=AF.Exp, scale=1.0 / 16), [dlast.r], [Dex.r])
            A(lambda e, n=n: e.activation(out=Eex[:, 0:n], in_=gcs[:, 0:n], func=AF.Exp, scale=-1.0 / 16), [gcs.r], [Eex.r])
            A(lambda e, nc_=nc_, v3=v3: e.activation(out=dec[:, 0:nc_], in_=v3(gcs)[:, :, 63], func=AF.Exp, scale=-1.0 / 16), [gcs.r], [dec.r])
            p.op("vector", lambda e, t0=t0, n=n: e.scalar_tensor_tensor(out=qt[:, 0:n], in0=qT[:, t0:t0 + n], scalar=scale, in1=Aex[:, 0:n], op0=ALU.mult, op1=ALU.mult), [qT.r, Aex.r], [qt.r])
            p.op("gpsimd", lambda e, t0=t0, n=n: e.tensor_tensor(out=kt[:, 0:n], in0=kT[:, t0:t0 + n], in1=Bex[:, 0:n], op=ALU.mult), [kT.r, Bex.r], [kt.r])
            p.op("vector", lambda e, t0=t0, n=n: e.tensor_tensor(out=kd[:, 0:n], in0=kT[:, t0:t0 + n], in1=Dex[:, 0:n], op=ALU.mult), [kT.r, Dex.r], [kd.r])
            p.op("vector", lambda e, t0=t0, n=n: e.scalar_tensor_tensor(out=qe[:, 0:n], in0=qT[:, t0:t0 + n], scalar=scale, in1=Eex[:, 0:n], op0=ALU.mult, op1=ALU.mult), [qT.r, Eex.r], [qe.r])
            for pp in range(n // 128):
                s = pp * 128
                gp = (t0 + s) // 128
                kT_, am, pa, po, pk = kdT[pc % 2], attm[pc % 2], patt[pc % 2], pO[pc % 2], pkv[pc % 2]
                tr = ptr.t[:, (pc % 2) * 128:(pc % 2) * 128 + 128]
                pc += 1
                p.op("tensor", lambda e, tr=tr, s=s: e.transpose(out=tr, in_=kd[:, s:s + 128], identity=idb[:]), [kd.r, idb.r], [ptr.r])

                def cpk(e, tr=tr, kT_=kT_):
                    e.activation(out=kT_[:, 0, :], in_=tr, func=AF.Identity, scale=hm[:, 0:1])
                    return e.activation(out=kT_[:, 1, :], in_=tr, func=AF.Identity, scale=hm[:, 1:2])
                p.op("scalar", cpk, [ptr.r, hm.r], [kT_.r])
                p.op("tensor", lambda e, pa=pa, s=s: e.matmul(pa[:, 0:128], lhsT=kt[:, s:s + 128], rhs=qt[:, s:s + 128], start=True, stop=True), [kt.r, qt.r], [pa.r])
                p.op("vector", lambda e, pa=pa, am=am: e.tensor_tensor(out=am[:], in0=pa[:, 0:128], in1=m2[:], op=ALU.mult), [pa.r, m2.r], [am.r])

                def mkv(e, pk=pk, kT_=kT_, gp=gp):
                    e.matmul(pk[:, 0:128], lhsT=kT_[:, 0, :], rhs=vb[:, gp, :], start=True, stop=True)
                    return e.matmul(pk[:, 128:256], lhsT=kT_[:, 1, :], rhs=vb[:, gp, :], start=True, stop=True)
                p.op("tensor", mkv, [kT_.r, vb.r], [pk.r])
                sb0 = Sb[sbi % NSB]
                sb1 = Sb[(sbi + 1) % NSB]
                sb2 = Sb[(sbi + 2) % NSB]
                sbi += 2
                c0 = s // 64
                p.op("vector", lambda e, pk=pk, c0=c0: e.scalar_tensor_tensor(out=S[:], in0=S[:], scalar=dec[:, c0:c0 + 1], in1=pk[:, 0:128], op0=ALU.mult, op1=ALU.add), [S.r, dec.r, pk.r], [S.r])
                p.op("scalar", lambda e, sb1=sb1: e.copy(out=sb1[:], in_=S[:]), [S.r], [sb1.r])
                p.op("vector", lambda e, pk=pk, c0=c0: e.scalar_tensor_tensor(out=S[:], in0=S[:], scalar=dec[:, c0 + 1:c0 + 2], in1=pk[:, 128:256], op0=ALU.mult, op1=ALU.add), [S.r, dec.r, pk.r], [S.r])
                p.op("scalar", lambda e, sb2=sb2: e.copy(out=sb2[:], in_=S[:]), [S.r], [sb2.r])

                def mo(e, po=po, am=am, gp=gp, sb0=sb0, sb1=sb1, s=s):
                    e.matmul(po[:, 0:128], lhsT=vb[:, gp, :], rhs=am[:], start=True, stop=False)
                    e.matmul(po[:, 0:64], lhsT=sb0[:], rhs=qe[:, s:s + 64], start=False, stop=False)
                    return e.matmul(po[:, 64:128], lhsT=sb1[:], rhs=qe[:, s + 64:s + 128], start=False, stop=True)
                p.op("tensor", mo, [vb.r, am.r, sb0.r, sb1.r, qe.r], [po.r])
                p.op("scalar", lambda e, po=po, hd=hd, a=t0 + s: e.copy(out=osb[hd][:, a:a + 128], in_=po[:, 0:128]), [po.r], [osb[hd].r])
    rt = kb.sb("rt", [128, 512])
    yy = [kb.sb("yy%d" % i, [128, 512]) for i in range(2)]
    yyb = [kb.sb("yyb%d" % i, [128, 512], BF16) for i in range(2)]
    pss = banks[0]
    bi = 0
    for h in range(2):
        of, ob = osb[2 * h], osb[2 * h + 1]

        def comb(e, of=of, ob=ob):
            for (s0, n) in segs:
                last = e.tensor_tensor(out=of[:, s0:s0 + n], in0=of[:, s0:s0 + n], in1=ob[:, s0:s0 + n][:, ::-1], op=ALU.add)
            return last
        p.op("vector", comb, [of.r, ob.r], [of.r])
        for (t0, n) in blocks:
            y_ = yy[bi % 2]
            bi += 1
            p.op("scalar", lambda e, of=of, t0=t0, n=n: e.activation(out=sp[:, 0:n], in_=of[:, t0:t0 + n], func=AF.Square), [of.r], [sp.r])
            p.op("tensor", lambda e, n=n: e.matmul(pss[:, 0:n], lhsT=ones[:], rhs=sp[:, 0:n], start=True, stop=True), [ones.r, sp.r], [pss.r])
            p.op("vector", lambda e, n=n: e.tensor_scalar(out=gcs[:, 0:n], in0=pss[:, 0:n], scalar1=1.0 / 128, scalar2=EPS, op0=ALU.mult, op1=ALU.add), [pss.r], [gcs.r])
            p.op("vector", lambda e, n=n: e.reciprocal(out=gcs[:, 0:n], in_=gcs[:, 0:n]), [gcs.r], [gcs.r])
            p.op("scalar", lambda e, n=n: e.activation(out=gcs[:, 0:n], in_=gcs[:, 0:n], func=AF.Sqrt), [gcs.r], [gcs.r])
            p.dma("sync", rt[:, 0:n], rT_d[h * 128:(h + 1) * 128, t0:t0 + n], reads=in_regs, writes=[rt.r])
            p.op("scalar", lambda e, n=n: e.activation(out=rt[:, 0:n], in_=rt[:, 0:n], func=AF.Silu), [rt.r], [rt.r])
            p.op("vector", lambda e, of=of, y_=y_, t0=t0, n=n: e.tensor_tensor(out=y_[:, 0:n], in0=of[:, t0:t0 + n], in1=gcs[:, 0:n], op=ALU.mult), [of.r, gcs.r], [y_.r])
            yb_ = yyb[bi % 2]
            p.op("vector", lambda e, h=h, y_=y_, yb_=yb_, n=n: e.scalar_tensor_tensor(out=yb_[:, 0:n], in0=y_[:, 0:n], scalar=gn[:, h:h + 1], in1=rt[:, 0:n], op0=ALU.mult, op1=ALU.mult), [y_.r, gn.r, rt.r], [yb_.r])
            y_ = yb_
            for (a, m, hh_, loc) in nat_pieces(t0, n):
                o = Reg("o")
                outs.append(o)
                p.dma("gpsimd", xs[hh_ * 3072 + row0 + h * 128:hh_ * 3072 + row0 + (h + 1) * 128, loc:loc + m], y_[:, a - t0:a - t0 + m], reads=[y_.r], writes=[o])


def emit_ssd(kb, xbc_d, z_d, dt_d, cw_d, cb_d, dtb_d, alog_d, Dp_d, U_d, ident_d, xs, row0, in_regs, outs):
    p = kb.p
    banks = kb.banks
    L = LAT
    NCH = T // 128
    ptr = kb.psbf(banks[7])
    psm, pCB, pbc, pdiag, poff, pst = banks[0], banks[1], banks[2:4], banks[4], banks[5], banks[6]
    cw = ld(kb, "cw", [128, 4, 4], cw_d)
    cb = ld(kb, "cb", [128, 4], cb_d)
    dtb = ld(kb, "dtb", [4, 2], dtb_d)
    aneg = ld(kb, "aneg", [4, 2], alog_d)
    p.op("scalar", lambda e: e.activation(out=aneg[:], in_=aneg[:], func=AF.Exp), [aneg.r], [aneg.r])
    p.op("vector", lambda e: e.tensor_scalar(out=aneg[:], in0=aneg[:], scalar1=-1.0, scalar2=None, op0=ALU.mult), [aneg.r], [aneg.r])
    Dp = ld(kb, "Dp", [128, 2], Dp_d)
    U = ld(kb, "U", [128, 128], U_d)
    idf, idb, ones = consts(kb, ident_d)
    segs = [(0, NCTX), (NCTX, L)]
    arr = [[kb.sb("arr%d_%d" % (d, k), [128, T], BF16) for k in range(4)] for d in range(2)]
    xin = [kb.sb("xin%d" % i, [128, T]) for i in range(2)]
    u = kb.sb("u", [128, T])
    for k in range(4):
        xi = xin[k % 2]
        p.dma("sync", xi[:], xbc_d[k * 128:(k + 1) * 128, :], reads=in_regs, writes=[xi.r])
        emit_conv(kb, p, xi, u, cw, cb, segs, k)
        p.op("scalar", lambda e, k=k: e.activation(out=arr[0][k][:], in_=u[:], func=AF.Silu), [u.r], [arr[0][k].r])
        p.op("gpsimd", lambda e, k=k: rev_segments(e, arr[1][k], arr[0][k], segs), [arr[0][k].r], [arr[1][k].r])
    yacc = [xin[0], xin[1]]
    dtp = kb.sb("dtp", [4, 2, T])
    F = lambda n, s, dt=F32: kb.sb(n, s, dt)
    xBt = [F("xBt%d" % i, [128, 384], BF16) for i in range(2)]
    dtk = [F("dtk%d" % i, [128, 8]) for i in range(2)]
    acl = [F("acl%d" % i, [128, 8]) for i in range(2)]
    nac = [F("nac%d" % i, [128, 4]) for i in range(2)]
    eac = [F("eac%d" % i, [128, 4]) for i in range(2)]
    wdt = [F("wdt%d" % i, [128, 4]) for i in range(2)]
    dcy = [F("dcy%d" % i, [128, 4]) for i in range(2)]
    dtx = [F("dtx%d" % i, [128, 256], BF16) for i in range(2)]
    xw = [F("xw%d" % i, [128, 256], BF16) for i in range(2)]
    rU = [F("rU%d" % i, [128, 128]) for i in range(2)]
    sg = [F("sg%d" % i, [128, 128]) for i in range(2)]
    Lt = [F("Lt%d" % i, [128, 128]) for i in range(2)]
    Mb = [F("Mb%d" % i, [128, 128], BF16) for i in range(8)]
    CBs = [F("CBs%d" % i, [128, 128]) for i in range(2)]
    yt1 = [F("yt1_%d" % i, [128, 256]) for i in range(2)]
    yt2 = [F("yt2_%d" % i, [128, 256]) for i in range(2)]
    H = F("H", [128, 256])
    Hb = [F("Hb%d" % i, [128, 256], BF16) for i in range(2)]
    nonlocal_it = [0]
    nonlocal_hc = [0]
    for d in range(2):
        slot = 0 if d == 0 else 1
        p.dma("sync", dtp[:, slot, :], dt_d[d * 4:(d + 1) * 4, :], reads=in_regs, writes=[dtp.r])
        p.op("scalar", lambda e, slot=slot, d=d: e.activation(out=dtp[:, slot, :], in_=dtp[:, slot, :], func=AF.Exp, bias=dtb[:, d:d + 1], scale=1.0), [dtp.r, dtb.r], [dtp.r])
        p.op("scalar", lambda e, slot=slot: e.activation(out=dtp[:, slot, :], in_=dtp[:, slot, :], func=AF.Ln, bias=1.0, scale=1.0), [dtp.r], [dtp.r])
        if d == 1:
            def revdt(e):
                for (s0, n) in segs:
                    last = e.tensor_copy(out=dtp[:, 0, s0:s0 + n], in_=dtp[:, 1, s0:s0 + n][:, ::-1])
                return last
            p.op("vector", revdt, [dtp.r], [dtp.r])
        p.op("vector", lambda e, d=d: e.tensor_scalar(out=dtp[:, 1, :], in0=dtp[:, 0, :], scalar1=aneg[:, d:d + 1], scalar2=None, op0=ALU.mult), [dtp.r, aneg.r], [dtp.r])
        p.op("vector", lambda e: e.memset(H[:], 0.0), [], [H.r])
        p.op("gpsimd", lambda e, hb=Hb[nonlocal_hc[0] % 2]: e.memset(hb[:], 0.0), [], [Hb[nonlocal_hc[0] % 2].r])
        A = arr[d]
        def prep(c):
                nonlocal_it[0] += 1
                s = c * 128
                i2 = (nonlocal_it[0] - 1) % 2
                xb_, dk, ac, na, ea, wd, dc, dx, xw_ = xBt[i2], dtk[i2], acl[i2], nac[i2], eac[i2], wdt[i2], dcy[i2], dtx[i2], xw[i2]
                y1, y2, cbs = yt1[i2], yt2[i2], CBs[i2]
                hb_cur = Hb[nonlocal_hc[0] % 2]
                hb_nxt = Hb[(nonlocal_hc[0] + 1) % 2]
                nonlocal_hc[0] += 1
                return dict(s=s, i2=i2, xb_=xb_, dk=dk, ac=ac, na=na, ea=ea, wd=wd, dc=dc, dx=dx, xw_=xw_, y1=y1, y2=y2, cbs=cbs, hb_cur=hb_cur, hb_nxt=hb_nxt)

        def stage1(c, v):
            s, i2, xb_, dk, ac, na, ea, wd, dc, dx, xw_, cbs = v['s'], v['i2'], v['xb_'], v['dk'], v['ac'], v['na'], v['ea'], v['wd'], v['dc'], v['dx'], v['xw_'], v['cbs']

            def trx(e, s=s, A=A):
                e.transpose(out=ptr.t[:, 0:128], in_=A[0][:, s:s + 128], identity=idb[:])
                e.transpose(out=ptr.t[:, 128:256], in_=A[1][:, s:s + 128], identity=idb[:])
                return e.transpose(out=ptr.t[:, 256:384], in_=A[2][:, s:s + 128], identity=idb[:])
            p.op("tensor", trx, [A[0].r, A[1].r, A[2].r, idb.r], [ptr.r])
            p.op("scalar", lambda e, xb_=xb_: e.copy(out=xb_[:], in_=ptr.t[:, 0:384]), [ptr.r], [xb_.r])

            def trd(e, s=s):
                e.transpose(out=psm[:, 0:4], in_=dtp[:, 0, s:s + 128], identity=idf[0:4, 0:4])
                return e.transpose(out=psm[:, 4:8], in_=dtp[:, 1, s:s + 128], identity=idf[0:4, 0:4])
            p.op("tensor", trd, [dtp.r, idf.r], [psm.r])
            p.op("vector", lambda e, dk=dk: e.tensor_copy(out=dk[:], in_=psm[:, 0:8]), [psm.r], [dk.r])

            def mac(e, dk=dk):
                e.matmul(psm[:, 8:12], lhsT=U[:], rhs=dk[:, 4:8], start=True, stop=True)
                return e.matmul(psm[:, 12:16], lhsT=ones[:], rhs=dk[:, 4:8], start=True, stop=True)
            p.op("tensor", mac, [U.r, ones.r, dk.r], [psm.r])
            p.op("vector", lambda e, ac=ac: e.tensor_copy(out=ac[:], in_=psm[:, 8:16]), [psm.r], [ac.r])
            p.op("vector", lambda e, ac=ac, na=na: e.tensor_scalar(out=na[:], in0=ac[:, 0:4], scalar1=-1.0, scalar2=None, op0=ALU.mult), [ac.r], [na.r])
            p.op("scalar", lambda e, ac=ac, ea=ea: e.activation(out=ea[:], in_=ac[:, 0:4], func=AF.Exp), [ac.r], [ea.r])
            p.op("scalar", lambda e, ac=ac, dc=dc: e.activation(out=dc[:], in_=ac[:, 4:8], func=AF.Exp), [ac.r], [dc.r])
            p.op("vector", lambda e, ac=ac, wd=wd: e.tensor_tensor(out=wd[:], in0=ac[:, 4:8], in1=ac[:, 0:4], op=ALU.subtract), [ac.r], [wd.r])
            p.op("scalar", lambda e, wd=wd: e.activation(out=wd[:], in_=wd[:], func=AF.Exp), [wd.r], [wd.r])
            p.op("vector", lambda e, wd=wd, dk=dk: e.tensor_tensor(out=wd[:], in0=wd[:], in1=dk[:, 0:4], op=ALU.mult), [wd.r, dk.r], [wd.r])
            x3 = lambda t: t[:, 0:256].rearrange("p (h q) -> p h q", q=64)
            p.op("vector", lambda e, dx=dx, xb_=xb_, dk=dk, x3=x3: e.tensor_tensor(out=x3(dx), in0=x3(xb_), in1=dk[:, 0:4].unsqueeze(2).to_broadcast([128, 4, 64]), op=ALU.mult), [xb_.r, dk.r], [dx.r])
            p.op("vector", lambda e, xw_=xw_, xb_=xb_, wd=wd, x3=x3: e.tensor_tensor(out=x3(xw_), in0=x3(xb_), in1=wd[:].unsqueeze(2).to_broadcast([128, 4, 64]), op=ALU.mult), [xb_.r, wd.r], [xw_.r])
            p.op("tensor", lambda e, s=s, A=A: e.matmul(pCB[:, 0:128], lhsT=A[2][:, s:s + 128], rhs=A[3][:, s:s + 128], start=True, stop=True), [A[2].r, A[3].r], [pCB.r])
            p.op("scalar", lambda e, cbs=cbs: e.copy(out=cbs[:], in_=pCB[:, 0:128]), [pCB.r], [cbs.r])
            for h in range(4):
                j2 = h % 2
                ru, sg_, lt, pb = rU[j2], sg[j2], Lt[j2], pbc[j2]
                mb = Mb[i2 * 4 + h]
                p.op("vector", lambda e, ru=ru, dk=dk, h=h: e.tensor_scalar(out=ru[:], in0=U[:], scalar1=dk[:, 4 + h:5 + h], scalar2=None, op0=ALU.mult), [U.r, dk.r], [ru.r])
                p.op("tensor", lambda e, pb=pb, ru=ru: e.matmul(pb[:, 0:128], lhsT=ones[:], rhs=ru[:], start=True, stop=True), [ones.r, ru.r], [pb.r])
                p.op("vector", lambda e, sg_=sg_, pb=pb, na=na, h=h: e.tensor_scalar(out=sg_[:], in0=pb[:, 0:128], scalar1=na[:, h:h + 1], scalar2=0.0, op0=ALU.add, op1=ALU.min), [pb.r, na.r], [sg_.r])
                p.op("scalar", lambda e, lt=lt, sg_=sg_: e.activation(out=lt[:], in_=sg_[:], func=AF.Exp), [sg_.r], [lt.r])
                p.op("vector", lambda e, lt=lt: e.tensor_tensor(out=lt[:], in0=lt[:], in1=U[:], op=ALU.mult), [lt.r, U.r], [lt.r])
                p.op("vector", lambda e, lt=lt, cbs=cbs, mb=mb: e.tensor_tensor(out=mb[:], in0=lt[:], in1=cbs[:], op=ALU.mult), [lt.r, cbs.r], [mb.r])

        def stage2(c, v):
            s, xb_, xw_, dc, ea, y1, y2, hb_cur, hb_nxt = v['s'], v['xb_'], v['xw_'], v['dc'], v['ea'], v['y1'], v['y2'], v['hb_cur'], v['hb_nxt']
            x3 = lambda t: t[:, 0:256].rearrange("p (h q) -> p h q", q=64)
            dx, i2 = v['dx'], v['i2']
            for h in range(4):
                mb = Mb[i2 * 4 + h]
                p.op("tensor", lambda e, mb=mb, dx=dx, h=h: e.matmul(pdiag[:, h * 64:(h + 1) * 64], lhsT=mb[:], rhs=dx[:, h * 64:(h + 1) * 64], start=True, stop=True), [mb.r, dx.r], [pdiag.r])
            p.op("tensor", lambda e, s=s, A=A, hb_cur=hb_cur: e.matmul(poff[:, 0:256], lhsT=A[3][:, s:s + 128], rhs=hb_cur[:], start=True, stop=True), [A[3].r, hb_cur.r], [poff.r])
            p.op("tensor", lambda e, xb_=xb_, xw_=xw_: e.matmul(pst[:, 0:256], lhsT=xb_[:, 256:384], rhs=xw_[:], start=True, stop=True), [xb_.r, xw_.r], [pst.r])
            H3 = H[:].rearrange("p (h q) -> p h q", q=64)
            p.op("vector", lambda e, dc=dc, H3=H3: e.tensor_tensor(out=H3, in0=H3, in1=dc[:].unsqueeze(2).to_broadcast([128, 4, 64]), op=ALU.mult), [H.r, dc.r], [H.r])
            p.op("vector", lambda e: e.tensor_tensor(out=H[:], in0=H[:], in1=pst[:, 0:256], op=ALU.add), [H.r, pst.r], [H.r])
            p.op("scalar", lambda e, hb_nxt=hb_nxt: e.copy(out=hb_nxt[:], in_=H[:]), [H.r], [hb_nxt.r])
            p.op("scalar", lambda e, y1=y1: e.copy(out=y1[:], in_=pdiag[:, 0:256]), [pdiag.r], [y1.r])
            p.op("vector", lambda e, y2=y2, ea=ea, x3=x3: e.tensor_tensor(out=x3(y2), in0=poff[:, 0:256].rearrange("p (h q) -> p h q", q=64),
                                                                        in1=ea[:].unsqueeze(2).to_broadcast([128, 4, 64]), op=ALU.mult), [poff.r, ea.r], [y2.r])
            p.op("vector", lambda e, y1=y1, y2=y2: e.tensor_tensor(out=y2[:], in0=y2[:], in1=y1[:], op=ALU.add), [y1.r, y2.r], [y2.r])
            for k in range(2):
                pb = pbc[k]
                p.op("tensor", lambda e, pb=pb, y2=y2, k=k: e.transpose(out=pb[:, 128:256], in_=y2[:, k * 128:(k + 1) * 128], identity=idf[:]), [y2.r, idf.r], [pb.r])
                if d == 0:
                    p.op("scalar", lambda e, pb=pb, k=k, s=s: e.copy(out=yacc[k][:, s:s + 128], in_=pb[:, 128:256]), [pb.r], [yacc[k].r])
                else:
                    lo = scan_nat_lo(s, 128)
                    p.op("vector", lambda e, pb=pb, k=k, lo=lo: e.tensor_tensor(out=yacc[k][:, lo:lo + 128][:, ::-1], in0=yacc[k][:, lo:lo + 128][:, ::-1], in1=pb[:, 128:256], op=ALU.add),
                         [pb.r, yacc[k].r], [yacc[k].r])

        vs_ = [prep(c) for c in range(NCH)]
        stage1(0, vs_[0])
        for c in range(NCH):
            if c + 1 < NCH:
                stage1(c + 1, vs_[c + 1])
            stage2(c, vs_[c])
    zt = u
    for k in range(2):
        p.op("vector", lambda e, k=k: e.scalar_tensor_tensor(out=yacc[k][:], in0=arr[0][k][:], scalar=Dp[:, k:k + 1], in1=yacc[k][:], op0=ALU.mult, op1=ALU.add),
             [arr[0][k].r, Dp.r, yacc[k].r], [yacc[k].r])
        p.dma("sync", zt[:], z_d[k * 128:(k + 1) * 128, :], reads=in_regs, writes=[zt.r])
        p.op("scalar", lambda e: e.activation(out=zt[:], in_=zt[:], func=AF.Silu), [zt.r], [zt.r])
        p.op("vector", lambda e, k=k: e.tensor_tensor(out=yacc[k][:], in0=yacc[k][:], in1=zt[:], op=ALU.mult), [yacc[k].r, zt.r], [yacc[k].r])
        xs_write(p, xs, row0 + k * 128, yacc[k], [yacc[k].r], outs, btile=arr[1][k])


def emit_b1(kb, xg, xT_d, cT, adaw, adab, wbr_d, wo_d, sn_d, bm_d, xm_d, in_regs, outs, TOK=HT):
    p = kb.p
    banks = kb.banks
    XR = 3072
    nch = (TOK + 511) // 512
    chunks = [(c * 512, min(512, TOK - c * 512)) for c in range(nch)]
    ya = kb.sb("ystg_a", [128, 8192])
    ystg = kb.sub(ya, 0, [128, 16, 512], F32, "ystg")
    mod = emit_mods(kb, adaw, adab, 8, cT, kb.sub(ya, 0, [128, 8, 512], F32, "awstg"), banks[0])
    bm = ld(kb, "bm", [128, 2], bm_d)
    ssdn = ld(kb, "ssdn", [128, 4], sn_d)
    ones = kb.sb("ones", [128, 128])
    p.op("vector", lambda e: e.memset(ones[:], 1.0), [], [ones.r])
    wbb = kb.sb("wbb", [128, 16, 1024], BF16)
    wob = kb.sb("wob", [128, 8, 1024], BF16)
    wst = [kb.sb("wst%d" % i, [128, 1024]) for i in range(2)]
    wbr_r = [Reg("wbr%d" % k) for k in range(16)]
    wo_r = [Reg("wo%d" % k) for k in range(8)]
    for k in range(24):
        st = wst[k % 2]
        src = wbr_d[k * 128:(k + 1) * 128, :] if k < 16 else wo_d[(k - 16) * 128:(k - 15) * 128, :]
        p.dma("sync", st[:], src, writes=[st.r])
        if k < 16:
            p.op("gpsimd", lambda e, st=st, k=k: e.tensor_copy(out=wbb[:, k, :], in_=st[:]), [st.r], [wbr_r[k]])
        else:
            p.op("gpsimd", lambda e, st=st, k=k: e.tensor_copy(out=wob[:, k - 16, :], in_=st[:]), [st.r], [wo_r[k - 16]])
    y2 = [kb.sb("y2_%d" % i, [128, 4, 512], BF16) for i in range(2)]
    y3 = [kb.sb("y3_%d" % i, [128, 4, 512], BF16) for i in range(2)]
    yb = kb.sb("yb", [128, 16, 512], BF16)
    xt = kb.sb("xt", [128, 8, 512])
    gts = [kb.sb("gt%d" % i, [128, 4, 512]) for i in range(2)]
    gt2 = [kb.sb("gu%d" % i, [128, 4, 512], BF16) for i in range(2)]
    gt3 = [kb.sb("gv%d" % i, [128, 4, 512], BF16) for i in range(2)]
    accs = [kb.sb("acc%d" % i, [128, 512]) for i in range(2)]
    tmps = [kb.sb("tmp%d" % i, [128, 512]) for i in range(2)]
    aT = kb.sb("aT", [128, 8, 512], BF16)
    pz = banks[0:4]
    po = banks[4:6]
    pss = banks[6]
    zc = 0
    gc = 0
    yc = 0

    def yparts(kc, h):
        n_, r_, j_ = kc // 4, (kc // 2) % 2, kc % 2
        return gath_parts(3072, n_ * 256 + j_ * 128, r_, h * 6144, PIECE=PIECE_B)

    def gparts(nb, oc, h):
        return gath_parts(3072, 1024 + (nb % 2) * 1024 + oc * 128, nb // 2, h * 6144, PIECE=PIECE_B)

    for c, (t0, n) in enumerate(chunks):
        for g4 in range(4):
            yy = y2[yc % 2]
            yz = y3[yc % 2]
            yc += 1
            for j in range(4):
                for (row, cnt, po_) in yparts(g4 * 4 + j, 0):
                    p.dma("sync", yz.t[po_:po_ + cnt, j, 0:n], xg[row:row + cnt, t0:t0 + n], reads=in_regs, writes=[yz.r])
                for (row, cnt, po_) in yparts(g4 * 4 + j, 1):
                    p.dma("sync", yy.t[po_:po_ + cnt, j, 0:n], xg[row:row + cnt, t0:t0 + n], reads=in_regs, writes=[yy.r])
            p.op("vector", lambda e, g4=g4, yz=yz, n=n: e.tensor_scalar(out=ystg[:, g4 * 4:g4 * 4 + 4, 0:n], in0=yz[:, :, 0:n], scalar1=bm[:, 0:1], scalar2=None, op0=ALU.mult),
                 [yz.r, bm.r], [ystg.r])
            p.op("vector", lambda e, g4=g4, yy=yy, n=n: e.scalar_tensor_tensor(out=ystg[:, g4 * 4:g4 * 4 + 4, 0:n], in0=yy[:, :, 0:n], scalar=bm[:, 1:2], in1=ystg[:, g4 * 4:g4 * 4 + 4, 0:n],
                                                                             op0=ALU.mult, op1=ALU.add), [ystg.r, yy.r, bm.r], [ystg.r])
        for kc in range(4):
            sq = tmps[kc % 2]
            p.op("scalar", lambda e, sq=sq, kc=kc, n=n: e.activation(out=sq[:, 0:n], in_=ystg[:, kc, 0:n], func=AF.Square), [ystg.r], [sq.r])
            p.op("tensor", lambda e, sq=sq, kc=kc, n=n: e.matmul(pss[:, 0:n], lhsT=ones[:], rhs=sq[:, 0:n], start=(kc == 0), stop=(kc == 3)), [ones.r, sq.r], [pss.r])
        rs = accs[0]
        p.op("vector", lambda e, rs=rs, n=n: e.tensor_scalar(out=rs[:, 0:n], in0=pss[:, 0:n], scalar1=1.0 / 512, scalar2=EPS, op0=ALU.mult, op1=ALU.add), [pss.r], [rs.r])
        p.op("vector", lambda e, rs=rs, n=n: e.reciprocal(out=rs[:, 0:n], in_=rs[:, 0:n]), [rs.r], [rs.r])
        p.op("scalar", lambda e, rs=rs, n=n: e.activation(out=rs[:, 0:n], in_=rs[:, 0:n], func=AF.Sqrt), [rs.r], [rs.r])

        def nrm(e, rs=rs, n=n):
            for kc in range(4):
                last = e.scalar_tensor_tensor(out=ystg[:, kc, 0:n], in0=ystg[:, kc, 0:n], scalar=ssdn[:, kc:kc + 1], in1=rs[:, 0:n], op0=ALU.mult, op1=ALU.mult)
            return last
        p.op("vector", nrm, [ystg.r, ssdn.r, rs.r], [ystg.r])
        p.op("gpsimd", lambda e, n=n: e.tensor_copy(out=yb[:, :, 0:n], in_=ystg[:, :, 0:n]), [ystg.r], [yb.r])
        p.dma("sync", xt[:, :, 0:n], xT_d[:, t0:t0 + n].rearrange("(kc p) t -> p kc t", p=128), reads=in_regs, writes=[xt.r])
        for oc in range(8):
            gt, gu = gts[gc % 2], gt2[gc % 2]
            acc = accs[gc % 2]
            gc += 1
            gw = gt3[(gc - 1) % 2]
            for nb in range(4):
                for (row, cnt, po_) in gparts(nb, oc, 0):
                    p.dma("sync", gw.t[po_:po_ + cnt, nb, 0:n], xg[row:row + cnt, t0:t0 + n], reads=in_regs, writes=[gw.r])
                for (row, cnt, po_) in gparts(nb, oc, 1):
                    p.dma("sync", gu.t[po_:po_ + cnt, nb, 0:n], xg[row:row + cnt, t0:t0 + n], reads=in_regs, writes=[gu.r])
            p.op("gpsimd", lambda e, gt=gt, gw=gw, n=n: e.tensor_scalar(out=gt[:, :, 0:n], in0=gw[:, :, 0:n], scalar1=bm[:, 0:1], scalar2=None, op0=ALU.mult), [gw.r, bm.r], [gt.r])
            p.op("vector", lambda e, gt=gt, gu=gu, n=n: e.scalar_tensor_tensor(out=gt[:, :, 0:n], in0=gu[:, :, 0:n], scalar=bm[:, 1:2], in1=gt[:, :, 0:n], op0=ALU.mult, op1=ALU.add),
                 [gt.r, gu.r, bm.r], [gt.r])
            p.op("scalar", lambda e, gt=gt, n=n: e.activation(out=gt[:, :, 0:n], in_=gt[:, :, 0:n], func=AF.Sigmoid), [gt.r], [gt.r])
            for nb in range(4):
                z = pz[zc % 4]
                zc += 1

                def mmz(e, z=z, nb=nb, oc=oc, n=n):
                    for kc in range(4):
                        last = e.matmul(z[:, 0:n], lhsT=wbb[:, nb * 4 + kc, oc * 128:(oc + 1) * 128], rhs=yb[:, nb * 4 + kc, 0:n], start=(kc == 0), stop=(kc == 3))
                    return last
                p.op("tensor", mmz, wbr_r[nb * 4:nb * 4 + 4] + [yb.r], [z.r])
                if nb == 0:
                    p.op("vector", lambda e, z=z, gt=gt, acc=acc, n=n: e.tensor_tensor(out=acc[:, 0:n], in0=z[:, 0:n], in1=gt[:, 0, 0:n], op=ALU.mult), [z.r, gt.r], [acc.r])
                else:
                    tmp = tmps[nb % 2]
                    p.op("vector", lambda e, z=z, gt=gt, tmp=tmp, nb=nb, n=n: e.tensor_tensor(out=tmp[:, 0:n], in0=z[:, 0:n], in1=gt[:, nb, 0:n], op=ALU.mult), [z.r, gt.r], [tmp.r])
                    if nb < 3:
                        p.op("gpsimd", lambda e, tmp=tmp, acc=acc, n=n: e.tensor_tensor(out=acc[:, 0:n], in0=acc[:, 0:n], in1=tmp[:, 0:n], op=ALU.add), [acc.r, tmp.r], [acc.r])
                    else:
                        p.op("gpsimd", lambda e, tmp=tmp, acc=acc, oc=oc, n=n: e.tensor_tensor(out=aT[:, oc, 0:n], in0=acc[:, 0:n], in1=tmp[:, 0:n], op=ALU.add), [acc.r, tmp.r], [aT.r])
        for oc in range(8):
            pq = po[oc % 2]

            def mmo(e, pq=pq, oc=oc, n=n):
                for kc in range(8):
                    last = e.matmul(pq[:, 0:n], lhsT=wob[:, kc, oc * 128:(oc + 1) * 128], rhs=aT[:, kc, 0:n], start=(kc == 0), stop=(kc == 7))
                return last
            p.op("tensor", mmo, wo_r + [aT.r], [pq.r])

            def res(e, pq=pq, oc=oc, t0=t0, n=n):
                for (s, m, j) in tok_ranges(t0, n, 128):
                    last = e.scalar_tensor_tensor(out=xt[:, oc, s - t0:s - t0 + m], in0=pq[:, s - t0:s - t0 + m], scalar=mod[:, oc, j:j + 1],
                                                  in1=xt[:, oc, s - t0:s - t0 + m], op0=ALU.mult, op1=ALU.add)
                return last
            p.op("vector", res, [pq.r, mod.r, xt.r], [xt.r])
        o = Reg("o%d" % c)
        outs.append(o)
        p.dma("gpsimd", xm_d[:, t0:t0 + n].rearrange("(kc p) t -> p kc t", p=128), xt[:, :, 0:n], reads=[xt.r], writes=[o])


def emit_b2(kb, xT_d, cT, adaw, adab, n2, wr_d, br_d, sel_d, ident_d, w1_d, w3_d, w2_d, out_d, in_regs, outs, TOK=HT, NCX=128, NE=16):
    p = kb.p
    banks = kb.banks
    nch = (TOK + 511) // 512
    chunks = [(c * 512, min(512, TOK - c * 512)) for c in range(nch)]
    idf = ld(kb, "idf", [128, 128], ident_d)
    ones = kb.sb("ones", [128, 128])
    p.op("vector", lambda e: e.memset(ones[:], 1.0), [], [ones.r])
    sel = ld(kb, "sel", [16, 16, 128], sel_d)
    wr = kb.sb("wr", [128, 8, 20])
    p.dma("sync", wr[:], wr_d.rearrange("(kc p) c -> p kc c", p=128), writes=[wr.r])
    br = ld(kb, "br", [128, 20], br_d)
    n2t = ld(kb, "n2t", [128, 8], n2)
    arena = kb.sb("b2arena", [128, 18432])
    KEL = 256

    class PV:
        def __init__(s, bank, ap):
            s.t, s.r = ap, bank.r

        def __getitem__(s, k):
            return s.t[k]
    sq = kb.sub(arena, 0, [128, 8, 512], F32, "sq")
    h2f = kb.sub(arena, 16 * KEL, [128, 8, 512], F32, "h2f")
    mod = emit_mods(kb, adaw, adab, 24, cT, sq, banks[0])
    asc = kb.sb("asc", [128, 8, 2])
    p.op("vector", lambda e: e.tensor_scalar(out=asc[:], in0=mod[:, 8:16, :], scalar1=1.0, scalar2=None, op0=ALU.add), [mod.r], [asc.r])
    p.op("vector", lambda e: e.tensor_tensor(out=asc[:], in0=asc[:], in1=n2t[:].unsqueeze(2).to_broadcast([128, 8, 2]), op=ALU.mult), [asc.r, n2t.r], [asc.r])
    xT = kb.sb("xT", [128, 8, TOK])
    xr = [[Reg("x%d_%d" % (oc, c)) for c in range(nch)] for oc in range(8)]
    for oc in range(8):
        p.dma("sync", xT[:, oc, :], xT_d[oc * 128:(oc + 1) * 128, :], reads=in_regs, writes=xr[oc])
    h2T = kb.sb("h2T", [128, 8, TOK], BF16)
    h2r = [Reg("h2_%d" % c) for c in range(nch)]
    wT = kb.sb("wT", [16, TOK])
    wTr = [Reg("wT%d" % c) for c in range(nch)]
    pss = banks[7]
    rstd = kb.sb("rstd", [128, 512])
    plg = [PV(banks[5], banks[5].t[:, 0:20]), PV(banks[6], banks[6].t[:, 0:20])]
    pwt = PV(banks[4], banks[4].t[0:16, 0:128])
    R = lambda n, s: kb.sb(n, s)
    L = R("rL", [128, 20]); mg = R("rmg", [128, 1]); nmg = R("rnmg", [128, 1]); eg = R("reg", [128, 4]); sg = R("rsg", [128, 1])
    gsel = R("rgsel", [128, 4]); tmp = R("rtmp", [128, 4, 4]); lsel = R("rlsel", [128, 4]); me = R("rme", [128, 1]); nme = R("rnme", [128, 1])
    ee = R("ree", [128, 4]); m1 = R("rm1", [128, 1]); msk = R("rmsk", [128, 4]); e2 = R("re2", [128, 4]); m2 = R("rm2", [128, 1])
    tv = R("rtv", [128, 4]); sv = R("rsv", [128, 1]); wg = R("rwg", [128, 4]); gp = R("rgp", [128, 4]); wf = R("rwf", [128, 4, 4])
    for c, (t0, n) in enumerate(chunks):
        def sqf(e, t0=t0, n=n):
            for kc in range(8):
                last = e.activation(out=sq[:, kc, 0:n], in_=xT[:, kc, t0:t0 + n], func=AF.Square)
            return last
        p.op("scalar", sqf, [xr[oc][c] for oc in range(8)], [sq.r])

        def ssum(e, n=n):
            for kc in range(8):
                last = e.matmul(pss[:, 0:n], lhsT=ones[:], rhs=sq[:, kc, 0:n], start=(kc == 0), stop=(kc == 7))
            return last
        p.op("tensor", ssum, [sq.r, ones.r], [pss.r])
        p.op("vector", lambda e, n=n: e.tensor_scalar(out=rstd[:, 0:n], in0=pss[:, 0:n], scalar1=1.0 / 1024, scalar2=EPS, op0=ALU.mult, op1=ALU.add), [pss.r], [rstd.r])
        p.op("vector", lambda e, n=n: e.reciprocal(out=rstd[:, 0:n], in_=rstd[:, 0:n]), [rstd.r], [rstd.r])
        p.op("scalar", lambda e, n=n: e.activation(out=rstd[:, 0:n], in_=rstd[:, 0:n], func=AF.Sqrt), [rstd.r], [rstd.r])

        def nrm(e, t0=t0, n=n):
            for kc in range(8):
                last = e.tensor_tensor(out=h2f[:, kc, 0:n], in0=xT[:, kc, t0:t0 + n], in1=rstd[:, 0:n], op=ALU.mult)
            return last
        p.op("vector", nrm, [xr[oc][c] for oc in range(8)] + [rstd.r], [h2f.r])

        def modf(e, t0=t0, n=n):
            for (s, m, j) in tok_ranges(t0, n, NCX):
                for kc in range(8):
                    last = e.activation(out=h2f[:, kc, s - t0:s - t0 + m], in_=h2f[:, kc, s - t0:s - t0 + m], func=AF.Identity, scale=asc[:, kc, j:j + 1], bias=mod[:, kc, j:j + 1])
            return last
        p.op("scalar", modf, [h2f.r, asc.r, mod.r], [h2f.r])
        p.op("gpsimd", lambda e, t0=t0, n=n: e.tensor_copy(out=h2T[:, :, t0:t0 + n], in_=h2f[:, :, 0:n]), [h2f.r], [h2r[c]])
        for tt in range(n // 128):
            pl = plg[tt % 2]

            def rmm(e, tt=tt, pl=pl):
                for kc in range(8):
                    last = e.matmul(pl[:], lhsT=h2f[:, kc, tt * 128:(tt + 1) * 128], rhs=wr[:, kc, :], start=(kc == 0), stop=(kc == 7))
                return last
            p.op("tensor", rmm, [h2f.r, wr.r], [pl.r])
            V = lambda fn, rd, wrt: p.op("vector", fn, rd, wrt)
            V(lambda e, pl=pl: e.tensor_tensor(out=L[:], in0=pl[:], in1=br[:], op=ALU.add), [pl.r, br.r], [L.r])
            V(lambda e: e.reduce_max(out=mg[:], in_=L[:, 0:4], axis=AX.X), [L.r], [mg.r])
            V(lambda e: e.tensor_scalar(out=nmg[:], in0=mg[:], scalar1=-1.0, scalar2=None, op0=ALU.mult), [mg.r], [nmg.r])
            p.op("scalar", lambda e: e.activation(out=eg[:], in_=L[:, 0:4], func=AF.Exp, bias=nmg[:, 0:1], scale=1.0, accum_out=sg[:]), [L.r, nmg.r], [eg.r, sg.r])
            V(lambda e: e.reciprocal(out=sg[:], in_=sg[:]), [sg.r], [sg.r])
            V(lambda e: e.tensor_scalar(out=gsel[:], in0=L[:, 0:4], scalar1=mg[:, 0:1], scalar2=None, op0=ALU.is_ge), [L.r, mg.r], [gsel.r])
            V(lambda e: e.tensor_tensor(out=tmp[:], in0=L[:, 4:20].rearrange("p (g e) -> p g e", g=4), in1=gsel[:].unsqueeze(2).to_broadcast([128, 4, 4]), op=ALU.mult), [L.r, gsel.r], [tmp.r])
            V(lambda e: e.tensor_reduce(out=lsel[:], in_=tmp[:].rearrange("p g e -> p e g"), axis=AX.X, op=ALU.add), [tmp.r], [lsel.r])
            V(lambda e: e.reduce_max(out=me[:], in_=lsel[:], axis=AX.X), [lsel.r], [me.r])
            V(lambda e: e.tensor_scalar(out=nme[:], in0=me[:], scalar1=-1.0, scalar2=None, op0=ALU.mult), [me.r], [nme.r])
            p.op("scalar", lambda e: e.activation(out=ee[:], in_=lsel[:], func=AF.Exp, bias=nme[:, 0:1], scale=1.0), [lsel.r, nme.r], [ee.r])
            V(lambda e: e.reduce_max(out=m1[:], in_=ee[:], axis=AX.X), [ee.r], [m1.r])
            V(lambda e: e.tensor_scalar(out=msk[:], in0=ee[:], scalar1=m1[:, 0:1], scalar2=-1e9, op0=ALU.is_ge, op1=ALU.mult), [ee.r, m1.r], [msk.r])
            V(lambda e: e.tensor_tensor(out=e2[:], in0=ee[:], in1=msk[:], op=ALU.add), [ee.r, msk.r], [e2.r])
            V(lambda e: e.reduce_max(out=m2[:], in_=e2[:], axis=AX.X), [e2.r], [m2.r])
            V(lambda e: e.tensor_scalar(out=tv[:], in0=ee[:], scalar1=m2[:, 0:1], scalar2=None, op0=ALU.is_ge), [ee.r, m2.r], [tv.r])
            V(lambda e: e.tensor_tensor(out=tv[:], in0=tv[:], in1=ee[:], op=ALU.mult), [tv.r, ee.r], [tv.r])
            V(lambda e: e.reduce_sum(out=sv[:], in_=tv[:], axis=AX.X), [tv.r], [sv.r])
            V(lambda e: e.reciprocal(out=sv[:], in_=sv[:]), [sv.r], [sv.r])
            V(lambda e: e.tensor_scalar(out=wg[:], in0=tv[:], scalar1=sv[:, 0:1], scalar2=None, op0=ALU.mult), [tv.r, sv.r], [wg.r])
            V(lambda e: e.tensor_scalar(out=gp[:], in0=gsel[:], scalar1=sg[:, 0:1], scalar2=None, op0=ALU.mult), [gsel.r, sg.r], [gp.r])
            V(lambda e: e.tensor_tensor(out=wf[:], in0=gp[:].unsqueeze(2).to_broadcast([128, 4, 4]), in1=wg[:].unsqueeze(1).to_broadcast([128, 4, 4]), op=ALU.mult), [gp.r, wg.r], [wf.r])
            p.op("tensor", lambda e: e.transpose(out=pwt[:], in_=wf[:].rearrange("p g e -> p (g e)"), identity=idf[:]), [wf.r, idf.r], [pwt.r])
            V(lambda e, a=t0 + tt * 128: e.tensor_copy(out=wT[:, a:a + 128], in_=pwt[:]), [pwt.r], [wTr[c]])
    p.fence()
    w13 = [kb.sub(arena, i * 16 * KEL, [128, 2, 8, 512], BF16, "w13_%d" % i) for i in range(2)]
    w2b = [kb.sub(arena, (32 + 8 * i) * KEL, [128, 4, 1024], BF16, "w2b_%d" % i) for i in range(2)]
    stA = [kb.sub(arena, (48 + 4 * i) * KEL, [128, 1024], F32, "stA%d" % i) for i in range(3)]
    s1 = [kb.sub(arena, (60 + 2 * i) * KEL, [128, 512], F32, "s1_%d" % i) for i in range(2)]
    aT = [kb.sub(arena, (64 + 4 * i) * KEL, [128, 4, 512], BF16, "aT%d" % i) for i in range(2)]
    ph = banks[0:4]
    pw = banks[4]
    po = banks[5:8]
    war_b = [[Reg("wa%d_%d" % (i, k)) for k in range(8)] for i in range(2)]
    wbr_b = [[Reg("wb%d_%d" % (i, k)) for k in range(4)] for i in range(2)]
    units = [(ex, c) for ex in range(NE) for c in range(len(chunks))]
    cnt = {"si": 0, "hc": 0, "oc": 0}

    def load_w(ex):
        wa, wb = w13[ex % 2], w2b[ex % 2]
        war, wbr = war_b[ex % 2], wbr_b[ex % 2]
        for kc in range(8):
            sA = stA[cnt["si"] % 3]; cnt["si"] += 1
            p.dma("sync", sA[:, 0:512], w1_d[ex, kc * 128:(kc + 1) * 128, :], writes=[sA.r])
            p.dma("sync", sA[:, 512:1024], w3_d[ex, kc * 128:(kc + 1) * 128, :], writes=[sA.r])
            p.op("gpsimd", lambda e, sA=sA, wa=wa, kc=kc: e.tensor_copy(out=wa[:, :, kc, :], in_=sA[:].rearrange("p (a c) -> p a c", a=2)), [sA.r], [war[kc]])
        for fc in range(4):
            sA = stA[cnt["si"] % 3]; cnt["si"] += 1
            p.dma("sync", sA[:], w2_d[ex, fc * 128:(fc + 1) * 128, :], writes=[sA.r])
            p.op("gpsimd", lambda e, sA=sA, wb=wb, fc=fc: e.tensor_copy(out=wb[:, fc, :], in_=sA[:]), [sA.r], [wbr[fc]])

    def stageA(ui):
        ex, c = units[ui]
        t0, n = chunks[c]
        wa = w13[ex % 2]
        war = war_b[ex % 2]
        a = aT[ui % 2]
        ops = []
        for fc in range(4):
            def f(fc=fc):
                if fc == 0:
                    if c == 0:
                        load_w(ex)
                    p.op("tensor", lambda e: e.matmul(pw[:, 0:n], lhsT=sel[:, ex, :], rhs=wT[:, t0:t0 + n], start=True, stop=True), [sel.r, wTr[c]], [pw.r])
                p1, p3 = ph[cnt["hc"] % 4], ph[(cnt["hc"] + 1) % 4]; cnt["hc"] += 2
                ss = s1[fc % 2]

                def mm13(e):
                    for kc in range(8):
                        e.matmul(p1[:, 0:n], lhsT=wa[:, 0, kc, fc * 128:(fc + 1) * 128], rhs=h2T[:, kc, t0:t0 + n], start=(kc == 0), stop=(kc == 7))
                    for kc in range(8):
                        last = e.matmul(p3[:, 0:n], lhsT=wa[:, 1, kc, fc * 128:(fc + 1) * 128], rhs=h2T[:, kc, t0:t0 + n], start=(kc == 0), stop=(kc == 7))
                    return last
                p.op("tensor", mm13, war + [h2r[c]], [p1.r, p3.r])
                p.op("scalar", lambda e: e.activation(out=ss[:, 0:n], in_=p1[:, 0:n], func=AF.Silu), [p1.r], [ss.r])
                p.op("vector", lambda e: e.tensor_tensor(out=ss[:, 0:n], in0=ss[:, 0:n], in1=p3[:, 0:n], op=ALU.mult), [ss.r, p3.r], [ss.r])
                p.op("vector", lambda e: e.tensor_tensor(out=a[:, fc, 0:n], in0=ss[:, 0:n], in1=pw[:, 0:n], op=ALU.mult), [ss.r, pw.r], [a.r])
            ops.append(f)
        return ops

    def stageB(ui):
        ex, c = units[ui]
        t0, n = chunks[c]
        wb = w2b[ex % 2]
        wbr = wbr_b[ex % 2]
        a = aT[ui % 2]
        ops = []
        for oc in range(8):
            def f(oc=oc):
                pq = po[cnt["oc"] % len(po)]; cnt["oc"] += 1

                def mm2(e):
                    for fc in range(4):
                        last = e.matmul(pq[:, 0:n], lhsT=wb[:, fc, oc * 128:(oc + 1) * 128], rhs=a[:, fc, 0:n], start=(fc == 0), stop=(fc == 3))
                    return last
                p.op("tensor", mm2, wbr + [a.r], [pq.r])

                def acc(e):
                    for (s, m, j) in tok_ranges(t0, n, NCX):
                        last = e.scalar_tensor_tensor(out=xT[:, oc, s:s + m], in0=pq[:, s - t0:s - t0 + m], scalar=mod[:, 16 + oc, j:j + 1], in1=xT[:, oc, s:s + m], op0=ALU.mult, op1=ALU.add)
                    return last
                p.op("vector", acc, [pq.r, mod.r, xr[oc][c]], [xr[oc][c]])
            ops.append(f)
        return ops

    for f in stageA(0):
        f()
    for ui in range(len(units)):
        A = stageA(ui + 1) if ui + 1 < len(units) else []
        B = stageB(ui)
        for k in range(4):
            if A:
                A[k]()
            B[2 * k]()
            B[2 * k + 1]()
    for oc in range(8):
        o = Reg("o%d" % oc)
        outs.append(o)
        p.dma("gpsimd", out_d[oc * 128:(oc + 1) * 128, :], xT[:, oc, :], reads=xr[oc], writes=[o])


IN_SIZES_ = (512, 1024, 16, 512, 512, 512, 32, 512, 512, 512, 512, 256, 256, 4096)
OFFS_ = np.cumsum([0] + list(IN_SIZES_))
GROUPS = [[0, 1], [2, 3], [4, 5], [6, 7]]


def s1_cols(hf):
    o = OFFS_
    pieces = [("z", o[0] + hf * 256, 256), ("x", o[1] + hf * 256, 256), ("B", o[1] + 512 + hf * 128, 128), ("C", o[1] + 768 + hf * 128, 128),
              ("gq", o[3] + hf * 256, 256), ("gk", o[4] + hf * 256, 256), ("gv", o[5] + hf * 256, 256), ("gr", o[7] + hf * 256, 256),
              ("lx", o[8] + hf * 256, 256), ("lg", o[9] + hf * 256, 256),
              ("aq", o[10] + hf * 256, 256), ("ak", o[11] + hf * 128, 128), ("av", o[12] + hf * 128, 128)]
    cols, rows, r = [], {}, 0
    for nm, st, n in pieces:
        cols.append(np.arange(st, st + n))
        rows[nm] = slice(r, r + n)
        r += n
    cols.append(np.concatenate([o[2] + d * 8 + hf * 4 + np.arange(4) for d in range(2)]))
    rows["dt"] = slice(r, r + 8)
    r += 8
    cols.append(np.arange(o[6], o[6] + 32))
    rows["g1"] = slice(r, r + 32)
    r += 32
    pad = NCC_MIX * 128 - r
    cols.append(np.full(pad, -1))
    r += pad
    cols.append(np.arange(o[13] + hf * 2048, o[13] + (hf + 1) * 2048))
    rows["gate"] = slice(r, r + 2048)
    return np.concatenate(cols), rows


LAYER_SPECS = [("adaw", [1024, 6144]), ("adab", [128, 48]), ("n1", [128, 8]), ("n2", [128, 8]), ("win", [1024, NCC_S1 * 128]),
               ("gq", [128, 1]), ("gk", [128, 1]),
               ("lcw", [128, 2, 4]), ("lcb", [128, 2]), ("lwbd", [128, 8, 128]), ("lbias", [128, 8]), ("llam", [128, 4]),
               ("g2", [16, 2, 256]), ("gb", [128, 4]), ("gn", [128, 2]),
               ("scw", [128, 4, 4]), ("scb", [128, 4]), ("dtb", [4, 2]), ("alog", [4, 2]), ("Dp", [128, 2]),
               ("wbr", [2048, 1024]), ("wo", [1024, 1024]), ("ssdn", [128, 4]),
               ("wr", [1024, 20]), ("br", [128, 20]), ("w1", [16, 1024, 512]), ("w3", [16, 1024, 512]), ("w2", [16, 512, 1024])]
GLOBAL_SPECS = [("xT0", [1024, T]), ("xTm0", [1024, HT]), ("cT", [128, 8, 2]), ("ident", [128, 128]), ("cos", [128, LAT]), ("sin", [128, LAT]),
                ("rm", [128, 128]), ("cm", [128, 512]), ("m2", [128, 128]), ("hm", [128, 2]), ("U", [128, 128]), ("sel", [16, 16, 128]), ("bm", [128, 2])]


def build_fused(NL=2, debug=False, stages=None):
    kb = KB()
    p = kb.p
    G = {n: kb.din(n, s) for n, s in GLOBAL_SPECS}
    W = [{n: kb.din("%s_%d" % (n, l), s) for n, s in LAYER_SPECS} for l in range(NL)]
    pT = kb.scratch("pT", [NCC_MIX * 128, T], debug=debug)
    xs32 = kb.scratch("xs", [2 * 3072, HT // 2], debug=debug)
    xg32 = kb.scratch("xg", [2 * 24 * 2 * 128, HT // 2], debug=debug)
    xs = xs32.bitcast(BF16)
    xg = xg32.bitcast(BF16)
    xm = kb.scratch("xm", [1024, HT], debug=debug)
    xnew = kb.scratch("xnew", [1024, HT], debug=debug)
    xall = kb.scratch("xall", [2048, HT], debug=debug)
    oT = kb.dout("oT", [1024, HT])
    outs = []
    _, rows = s1_cols(0)
    for l in range(NL):
        w = W[l]
        if l == 0:
            xsrc = lambda kc, t0, n: [(0, n, G["xT0"][kc * 128:(kc + 1) * 128, t0:t0 + n])]
            xres = G["xTm0"]
        else:
            xsrc = lambda kc, t0, n: [(a - t0, m, h, loc) for (a, m, h, loc) in nat_pieces(t0, n)]
            xres = xnew
        def gather_chunks(chs):
            for h in range(2):
                for i in range((3072 + PIECE_B - 1) // PIECE_B):
                    Ri = min(PIECE_B, 3072 - PIECE_B * i)
                    src = xs32[h * 3072 + PIECE_B * i:h * 3072 + PIECE_B * i + Ri, :]
                    dst = xg32[h * 6144 + 2 * PIECE_B * i:h * 6144 + 2 * PIECE_B * i + 2 * Ri, :]
                    p.cc(lambda e, src=src, dst=dst: e.collective_compute("AllGather", ALU.bypass, replica_groups=GROUPS, ins=[src], outs=[dst]), blocking=True)
        emit_s1(kb, xsrc, G["cT"], w["adaw"][:, 0:2048], w["adab"][:, 0:16], w["n1"], w["win"], pT, xs, outs, [Reg("pT%d" % i) for i in range(NCC_MIX)], xall_ap=xall)
        kb.reset()
        emit_ssd(kb, pT[256:768, :], pT[rows["z"], :], pT[rows["dt"], :], w["scw"], w["scb"], w["dtb"], w["alog"], w["Dp"], G["U"], G["ident"], xs, 0, [], outs)
        kb.reset()
        emit_gla(kb, pT[rows["gq"], :], pT[rows["gk"], :], pT[rows["gv"], :], pT[rows["g1"], :], pT[rows["gr"], :], w["g2"], w["gb"], w["gn"],
                 G["cm"], G["m2"], G["hm"], G["ident"], xs, 256, [], outs)
        kb.reset()
        emit_lru(kb, pT[rows["lx"], :], pT[rows["lg"], :], w["lcw"], w["lcb"], w["lwbd"], w["lbias"], w["llam"], xs, 512, [], outs)
        kb.reset()
        emit_att(kb, pT[rows["aq"], :], pT[rows["ak"], :], pT[rows["av"], :], w["gq"], w["gk"], G["cos"], G["sin"], G["rm"], G["ident"], xs, 768, [], outs)
        kb.reset()
        gather_chunks(range(24))
        kb.reset()
        emit_b1(kb, xg, xres, G["cT"], w["adaw"][:, 2048:3072], w["adab"][:, 16:24], w["wbr"], w["wo"], w["ssdn"], G["bm"], xm, [], outs)
        kb.reset()
        emit_b2(kb, xm, G["cT"], w["adaw"][:, 3072:6144], w["adab"][:, 24:48], w["n2"], w["wr"], w["br"], G["sel"], G["ident"], w["w1"], w["w3"], w["w2"],
                oT if l == NL - 1 else xnew, [], outs)
        kb.reset()
        if l < NL - 1:
            for i in range((1024 + PIECE - 1) // PIECE):
                Ri = min(PIECE, 1024 - PIECE * i)
                src = xnew[PIECE * i:PIECE * i + Ri, :]
                dst = xall[2 * PIECE * i:2 * PIECE * i + 2 * Ri, :]
                p.cc(lambda e, src=src, dst=dst: e.collective_compute("AllGather", ALU.bypass, replica_groups=GROUPS, ins=[src], outs=[dst]))
            kb.reset()
    return kb.finish(outs)


def core_inputs(inp, b, hf, NL=2):
    c = np.ascontiguousarray
    f32 = np.float32
    d = {}
    x_all = np.concatenate([inp["ctx"][b], inp["x"][b]], 0)
    d["xT0"] = c(x_all.T)
    d["xTm0"] = c(np.concatenate([inp["ctx"][b][hf * 128:(hf + 1) * 128], inp["x"][b][hf * 2048:(hf + 1) * 2048]], 0).T)
    c2 = np.stack([inp["c"][b], inp["c_ctx"]], -1)
    d["cT"] = c(c2.reshape(8, 128, 2).transpose(1, 0, 2))
    d["ident"] = np.eye(128, dtype=f32)
    cos, sin, Rm = rope_tables()
    d["cos"], d["sin"], d["rm"] = cos, sin, Rm
    t = np.arange(512)
    d["cm"] = np.broadcast_to((t % 64 != 0).astype(f32)[None], (128, 512)).copy()
    j = np.arange(128)[:, None]
    i = np.arange(128)[None, :]
    d["m2"] = ((j // 64 == i // 64) & (j <= i)).astype(f32)
    d["hm"] = np.stack([(np.arange(128) < 64), (np.arange(128) >= 64)], 1).astype(f32)
    d["U"] = (j <= i).astype(f32)
    sel = np.zeros((16, 16, 128), f32)
    for e in range(16):
        sel[e, e, :] = 1.0
    d["sel"] = sel
    bm = np.zeros((128, 2), f32)
    bm[:, hf] = 1.0
    d["bm"] = bm
    cols, rows = s1_cols(hf)
    ch = slice(hf * 256, (hf + 1) * 256)
    hs = slice(hf * 4, hf * 4 + 4)
    for l in range(NL):
        L = {}
        L["adaw"] = c(inp["ada_w"][l])
        L["adab"] = fm(inp["ada_b"][l], 48)
        L["n1"] = fm(inp["norm1"][l], 8)
        L["n2"] = fm(inp["norm2"][l], 8)
        win = np.zeros((1024, NCC_S1 * 128), f32)
        ok = cols >= 0
        win[:, np.nonzero(ok)[0]] = inp["w_in"][l][:, cols[ok]]
        L["win"] = win
        L["gq"] = c(inp["att_qnorm"][l][:, None])
        L["gk"] = c(inp["att_knorm"][l][:, None])
        L["lcw"] = c(inp["lru_conv_w"][l][:, ch].reshape(4, 2, 128).transpose(2, 1, 0))
        L["lcb"] = c(inp["lru_conv_b"][l][ch].reshape(2, 128).T)
        wbd = np.zeros((128, 8, 128), f32)
        bias = np.zeros((128, 8), f32)
        lam = np.zeros((128, 4), f32)
        for gi, (wk, bk) in enumerate((("lru_wa", "lru_ba"), ("lru_wx", "lru_bx"))):
            for dd in range(2):
                for cc in range(2):
                    idx = gi * 4 + dd * 2 + cc
                    for jj in range(2):
                        blk = hf * 4 + cc * 2 + jj
                        wbd[jj * 64:(jj + 1) * 64, idx, jj * 64:(jj + 1) * 64] = inp[wk][l][dd, blk]
                    bias[:, idx] = inp[bk][l][dd, ch][cc * 128:(cc + 1) * 128]
        for dd in range(2):
            for cc in range(2):
                lam[:, dd * 2 + cc] = inp["lru_lambda"][l][dd, ch][cc * 128:(cc + 1) * 128]
        L["lwbd"], L["lbias"], L["llam"] = wbd, bias, lam
        L["g2"] = c(inp["gla_g2"][l][:, :, ch].transpose(1, 0, 2))
        L["gb"] = c(np.stack([inp["gla_gb"][l][dd, ch][h * 128:(h + 1) * 128] for h in range(2) for dd in range(2)], 1))
        L["gn"] = c(inp["gla_norm"][l][ch].reshape(2, 128).T)
        chans = np.concatenate([np.arange(hf * 256, hf * 256 + 256), 512 + hf * 128 + np.arange(128), 768 + hf * 128 + np.arange(128)])
        L["scw"] = c(inp["ssd_conv_w"][l][:, chans].reshape(4, 4, 128).transpose(2, 1, 0))
        L["scb"] = c(inp["ssd_conv_b"][l][chans].reshape(4, 128).T)
        L["dtb"] = c(inp["ssd_dt_bias"][l][:, hs].T)
        L["alog"] = c(inp["ssd_a_log"][l][:, hs].T)
        L["Dp"] = c(np.repeat(inp["ssd_d"][l][hs], 64).reshape(2, 128).T)
        L["wbr"] = c(inp["w_branch"][l].reshape(2048, 1024))
        L["wo"] = c(inp["w_out"][l])
        L["ssdn"] = fm(inp["ssd_norm"][l], 4)
        L["wr"] = c(np.concatenate([inp["router_wg"][l], inp["router_we"][l]], 1))
        L["br"] = c(np.broadcast_to(np.concatenate([inp["router_bg"][l], inp["router_be"][l]])[None], (128, 20)))
        L["w1"], L["w3"], L["w2"] = c(inp["exp_w1"][l]), c(inp["exp_w3"][l]), c(inp["exp_w2"][l])
        for k, v in L.items():
            d["%s_%d" % (k, l)] = np.ascontiguousarray(v, dtype=f32)
    return {k: np.ascontiguousarray(v, dtype=f32) for k, v in d.items()}


_PROG = {}


def kernel(**inp):
    inp = {k: np.asarray(v) for k, v in inp.items()}
    NB = inp["x"].shape[0]
    if "fused" not in _PROG:
        _PROG["fused"] = build_fused()
    cores = [(b, hf) for b in range(NB) for hf in range(2)]
    ims = [core_inputs(inp, b, hf) for (b, hf) in cores]
    res = run_bass_kernel_spmd(_PROG["fused"], ims, core_ids=list(range(8))).results
    out = np.stack([np.concatenate([res[2 * b]["oT"][:, 128:].T, res[2 * b + 1]["oT"][:, 128:].T], 0) for b in range(NB)])
    return np.ascontiguousarray(out.astype(np.float32))
```
